# Optimizing a Trainium2 kernel written in Bass

```python
import math
import jax, jax.numpy as jnp
from jax import lax
import numpy as np

D_MODEL = 1024
BATCH = 8
SEQ = 4096
DEPTH = 4

CHUNK = 64
Q_BLOCK = 128
TOKEN_BLOCK = 128
EPS = 1e-6
H_A = 4
DH_A = 64
DV_A = 2 * DH_A
H_B = 4
DK_B = 64
DV_B = 128
GATE_RANK = 16
GATE_TAU = 16.0
H_C = 16
DH_C = 64
N_BUCKETS = 32
MAX_DISTANCE = 128
N_GROUPS = 4
EXPERTS_PER_GROUP = 8
N_EXPERTS = N_GROUPS * EXPERTS_PER_GROUP
TOP_K = 2
D_EXPERT = 512

N_EVEN = (DEPTH + 1) // 2
N_ODD = DEPTH // 2
EVEN_SIZES = (H_A * 2 * DH_A, H_A * 2 * DH_A, H_A * DV_A,
              H_B * DK_B, H_B * DK_B, H_B * DV_B, H_B * DV_B, GATE_RANK)
EVEN_COLS = sum(EVEN_SIZES)
MIX_EVEN = H_A * DV_A + H_B * DV_B
MIX_ODD = H_C * DH_C

kernel_name = "hybrid_diff_gla_stickbreak_hmoe"


def rmsnorm(x, g):
    xf = x.astype(jnp.float32)
    y = xf * lax.rsqrt(jnp.mean(xf * xf, axis=-1, keepdims=True) + EPS)
    return (y * g.astype(jnp.float32)).astype(x.dtype)


def t5_bucket(rel):
    nb = N_BUCKETS // 2
    max_exact = nb // 2
    ret = jnp.where(rel > 0, nb, 0)
    n = jnp.abs(rel)
    nf = jnp.maximum(n, 1).astype(jnp.float32)
    large = max_exact + (jnp.log(nf / max_exact) / math.log(MAX_DISTANCE / max_exact)
                         * (nb - max_exact)).astype(jnp.int32)
    large = jnp.minimum(large, nb - 1)
    return ret + jnp.where(n < max_exact, n, large)


def diff_attention(q, k, v, lam, rel_bias):
    B, S = q.shape[0], q.shape[1]
    nq = S // Q_BLOCK
    kpos = jnp.arange(S)
    q_blocks = jnp.moveaxis(q.reshape(B, nq, Q_BLOCK, H_A, 2, DH_A), 1, 0)
    starts = jnp.arange(nq) * Q_BLOCK

    def one(args):
        qb, start = args
        qpos = start + jnp.arange(Q_BLOCK)
        logits = jnp.einsum('bqhmd,bkhmd->bhmqk', qb, k).astype(jnp.float32) * (DH_A ** -0.5)
        bias = jnp.transpose(rel_bias[t5_bucket(kpos[None, :] - qpos[:, None])], (2, 0, 1))
        logits = logits + bias[None, :, None].astype(jnp.float32)
        mask = (kpos[None, :] // CHUNK) <= (qpos[:, None] // CHUNK)
        p = jax.nn.softmax(jnp.where(mask, logits, -jnp.inf), axis=-1)
        w = p[:, :, 0] - lam * p[:, :, 1]
        return jnp.einsum('bhqk,bkhe->bqhe', w.astype(v.dtype), v)

    o = lax.map(one, (q_blocks, starts))
    return jnp.moveaxis(o, 0, 1).reshape(B, S, H_A, DV_A)


def gla(q, k, v, log_alpha):
    B, S = q.shape[0], q.shape[1]
    nc = S // CHUNK
    f32 = jnp.float32
    q = q.astype(f32).reshape(B, nc, CHUNK, H_B, DK_B) * (DK_B ** -0.5)
    k = k.astype(f32).reshape(B, nc, CHUNK, H_B, DK_B)
    v = v.astype(f32).reshape(B, nc, CHUNK, H_B, DV_B)
    b = jnp.cumsum(log_alpha.reshape(B, nc, CHUNK, H_B, DK_B), axis=2)
    b_last = b[:, :, -1]
    q_dec = q * jnp.exp(b)
    k_inv = k * jnp.exp(-b)
    k_dec = k * jnp.exp(b_last[:, :, None] - b)
    causal = jnp.tril(jnp.ones((CHUNK, CHUNK), dtype=bool))
    scores = jnp.einsum('bnthd,bnshd->bnhts', q_dec, k_inv)
    intra = jnp.einsum('bnhts,bnshe->bnthe', jnp.where(causal, scores, 0.0), v)

    def step(state, xs):
        q_c, k_c, v_c, bl = xs
        o = jnp.einsum('bthd,bhde->bthe', q_c, state)
        state = jnp.exp(bl)[..., None] * state + jnp.einsum('bshd,bshe->bhde', k_c, v_c)
        return state, o

    xs = (jnp.moveaxis(q_dec, 1, 0), jnp.moveaxis(k_dec, 1, 0), jnp.moveaxis(v, 1, 0),
          jnp.moveaxis(b_last, 1, 0))
    _, inter = lax.scan(step, jnp.zeros((B, H_B, DK_B, DV_B), f32), xs)
    return (intra + jnp.moveaxis(inter, 0, 1)).reshape(B, S, H_B, DV_B)


def stick_breaking(q, k, v):
    B, S = q.shape[0], q.shape[1]
    nq = S // Q_BLOCK
    kpos = jnp.arange(S)
    q_blocks = jnp.moveaxis(q.reshape(B, nq, Q_BLOCK, H_C, DH_C), 1, 0)
    starts = jnp.arange(nq) * Q_BLOCK

    def one(args):
        qb, start = args
        qpos = start + jnp.arange(Q_BLOCK)
        z = jnp.einsum('bqhd,bkhd->bhqk', qb, k).astype(jnp.float32) * (DH_C ** -0.5)
        mask = kpos[None, :] < qpos[:, None]
        log_1m = jnp.where(mask, jax.nn.log_sigmoid(-z), 0.0)
        suffix = lax.cumsum(log_1m, axis=3, reverse=True) - log_1m
        a = jnp.where(mask, jnp.exp(jax.nn.log_sigmoid(z) + suffix), 0.0)
        return jnp.einsum('bhqk,bkhd->bqhd', a.astype(v.dtype), v)

    o = lax.map(one, (q_blocks, starts))
    return jnp.moveaxis(o, 0, 1).reshape(B, S, H_C, DH_C)


def even_mixer(h, w_in, lam_p, subln_g, w_gk2, b_gk, gla_g, w_out, rel_bias, layer_idx):
    B, S, _ = h.shape
    idx = np.cumsum(EVEN_SIZES)[:-1].tolist()
    aq, ak, av, bq, bk, bv, br, bg = jnp.split(h @ w_in, idx, axis=-1)
    lambda_init = 0.8 - 0.6 * math.exp(-0.3 * layer_idx)
    lp = lam_p.astype(jnp.float32)
    lam = jnp.exp(jnp.sum(lp[0] * lp[1])) - jnp.exp(jnp.sum(lp[2] * lp[3])) + lambda_init
    oa = diff_attention(aq.reshape(B, S, H_A, 2, DH_A), ak.reshape(B, S, H_A, 2, DH_A),
                        av.reshape(B, S, H_A, DV_A), lam, rel_bias)
    oa = rmsnorm(oa, subln_g) * (1.0 - lambda_init)
    log_alpha = jax.nn.log_sigmoid((bg @ w_gk2 + b_gk).astype(jnp.float32)) / GATE_TAU
    ob = gla(bq.reshape(B, S, H_B, DK_B), bk.reshape(B, S, H_B, DK_B),
             bv.reshape(B, S, H_B, DV_B), log_alpha.reshape(B, S, H_B, DK_B)).astype(h.dtype)
    ob = rmsnorm(ob, gla_g) * jax.nn.silu(br.reshape(B, S, H_B, DV_B))
    o = jnp.concatenate([oa.reshape(B, S, H_A * DV_A), ob.reshape(B, S, H_B * DV_B)], axis=-1)
    return o @ w_out


def odd_mixer(h, w_in, w_out):
    B, S, _ = h.shape
    q, k, v = jnp.split(h @ w_in, 3, axis=-1)
    o = stick_breaking(q.reshape(B, S, H_C, DH_C), k.reshape(B, S, H_C, DH_C),
                       v.reshape(B, S, H_C, DH_C))
    return o.reshape(B, S, MIX_ODD) @ w_out


def hier_moe(h, w_rg, b_rg, w_re, b_re, w_gate, w_up, w_down):
    B, S, D = h.shape
    hf = h.reshape(B * S, D)
    n_tok = B * S
    p_g = jax.nn.softmax((hf @ w_rg + b_rg).astype(jnp.float32), axis=-1)
    g_val, g_idx = lax.top_k(p_g, 1)
    g_sel = g_idx[:, 0]
    e_logits = (hf @ w_re + b_re).astype(jnp.float32).reshape(n_tok, N_GROUPS, EXPERTS_PER_GROUP)
    e_in_group = jnp.take_along_axis(e_logits, g_sel[:, None, None], axis=1)[:, 0]
    top_v, top_i = lax.top_k(jax.nn.softmax(e_in_group, axis=-1), TOP_K)
    top_v = top_v / jnp.sum(top_v, axis=-1, keepdims=True)
    weights = g_val * top_v
    expert_idx = g_sel[:, None] * EXPERTS_PER_GROUP + top_i
    combine = jnp.sum(jax.nn.one_hot(expert_idx, N_EXPERTS, dtype=jnp.float32)
                      * weights[..., None], axis=1).astype(h.dtype)
    nb = n_tok // TOKEN_BLOCK

    def run(args):
        xb, cb = args
        g = jnp.einsum('td,edf->tef', xb, w_gate)
        u = jnp.einsum('td,edf->tef', xb, w_up)
        act = jax.nn.silu(g) * u * cb[:, :, None]
        return jnp.einsum('tef,efd->td', act, w_down)

    y = lax.map(run, (hf.reshape(nb, TOKEN_BLOCK, D), combine.reshape(nb, TOKEN_BLOCK, N_EXPERTS)))
    return y.reshape(B, S, D)


def setup_inputs(seed: int = 0) -> dict:
    key = jax.random.key(seed)
    ks = jax.random.split(key, 26)
    f32 = jnp.float32

    def nrm(k, shape, scale):
        return jax.random.normal(k, shape, f32) * scale

    D = D_MODEL
    return {
        "x": nrm(ks[0], (BATCH, SEQ, D), 1.0),
        "c": nrm(ks[1], (BATCH, D), 1.0),
        "w_ada": nrm(ks[2], (DEPTH, D, 6 * D), 0.5 * D ** -0.5),
        "b_ada": nrm(ks[3], (DEPTH, 6 * D), 0.02),
        "norm_mix": 1.0 + nrm(ks[4], (DEPTH, D), 0.05),
        "norm_ffn": 1.0 + nrm(ks[5], (DEPTH, D), 0.05),
        "norm_final": 1.0 + nrm(ks[6], (D,), 0.05),
        "rel_bias": nrm(ks[7], (N_BUCKETS, H_A), 0.5),
        "even_w_in": nrm(ks[8], (N_EVEN, D, EVEN_COLS), D ** -0.5),
        "even_lambda": nrm(ks[9], (N_EVEN, 4, DH_A), 0.1),
        "even_subln": 1.0 + nrm(ks[10], (N_EVEN, DV_A), 0.05),
        "even_w_gk2": nrm(ks[11], (N_EVEN, GATE_RANK, H_B * DK_B), GATE_RANK ** -0.5),
        "even_b_gk": nrm(ks[12], (N_EVEN, H_B * DK_B), 0.1),
        "even_gla_norm": 1.0 + nrm(ks[13], (N_EVEN, DV_B), 0.05),
        "even_w_out": nrm(ks[14], (N_EVEN, MIX_EVEN, D), MIX_EVEN ** -0.5),
        "odd_w_in": nrm(ks[15], (N_ODD, D, 3 * MIX_ODD), D ** -0.5),
        "odd_w_out": nrm(ks[16], (N_ODD, MIX_ODD, D), MIX_ODD ** -0.5),
        "router_group_w": nrm(ks[17], (DEPTH, D, N_GROUPS), D ** -0.5),
        "router_group_b": nrm(ks[18], (DEPTH, N_GROUPS), 0.01),
        "router_expert_w": nrm(ks[19], (DEPTH, D, N_EXPERTS), D ** -0.5),
        "router_expert_b": nrm(ks[20], (DEPTH, N_EXPERTS), 0.01),
        "expert_w_gate": nrm(ks[21], (DEPTH, N_EXPERTS, D, D_EXPERT), D ** -0.5),
        "expert_w_up": nrm(ks[22], (DEPTH, N_EXPERTS, D, D_EXPERT), D ** -0.5),
        "expert_w_down": nrm(ks[23], (DEPTH, N_EXPERTS, D_EXPERT, D), D_EXPERT ** -0.5),
    }


def reference(x, c, w_ada, b_ada, norm_mix, norm_ffn, norm_final, rel_bias,
              even_w_in, even_lambda, even_subln, even_w_gk2, even_b_gk, even_gla_norm,
              even_w_out, odd_w_in, odd_w_out, router_group_w, router_group_b,
              router_expert_w, router_expert_b, expert_w_gate, expert_w_up, expert_w_down):
    mod_all = jnp.einsum('bd,lde->lbe', jax.nn.silu(c), w_ada) + b_ada[:, None, :]
    for l in range(DEPTH):
        shift1, scale1, gate1, shift2, scale2, gate2 = jnp.split(mod_all[l][:, None, :], 6, axis=-1)
        h = rmsnorm(x, norm_mix[l]) * (1.0 + scale1) + shift1
        i = l // 2
        if l % 2 == 0:
            m = even_mixer(h, even_w_in[i], even_lambda[i], even_subln[i], even_w_gk2[i],
                           even_b_gk[i], even_gla_norm[i], even_w_out[i], rel_bias, l)
        else:
            m = odd_mixer(h, odd_w_in[i], odd_w_out[i])
        x = x + gate1 * m
        h = rmsnorm(x, norm_ffn[l]) * (1.0 + scale2) + shift2
        x = x + gate2 * hier_moe(h, router_group_w[l], router_group_b[l], router_expert_w[l],
                                 router_expert_b[l], expert_w_gate[l], expert_w_up[l],
                                 expert_w_down[l])
    return rmsnorm(x, norm_final)
```

```python
import math
from contextlib import ExitStack
import numpy as np
import concourse.bass as bass
import concourse.mybir as mybir
from concourse.bass_utils import run_bass_kernel_spmd

F32 = mybir.dt.float32
BF16 = mybir.dt.bfloat16
AF = mybir.ActivationFunctionType
ALU = mybir.AluOpType
AX = mybir.AxisListType

S = 4096
D = 1024
NT = 32
DEPTH = 4
EPS = 1e-6
EVEN_COLS = 3088
NEXP = 32
DEXP = 512
EPOCH = 12000


class Res:
    __slots__ = ("w", "r", "name")

    def __init__(self, name=""):
        self.w = None
        self.r = []
        self.name = name


class Tile:
    def __init__(self, ap, name=""):
        self.t = ap
        self.res = Res(name)

    def __getitem__(self, idx):
        return self.t[idx]


class Eng:
    def __init__(self, k, name, h, is_pe=False):
        self.k = k
        self.name = name
        self.h = h
        self.is_pe = is_pe
        self.sem = k.newsem(name + "_s0")
        self.count = 0
        self.nep = 0
        self.seen = {}
        self.hist = [(self.sem, 0)]

    def tick(self, inst):
        if self.count >= EPOCH:
            self.nep += 1
            self.sem = self.k.newsem("%s_s%d" % (self.name, self.nep))
            self.count = 0
        self.count += 1
        inst.then_inc(self.sem, 1)
        return (self.sem, self.count, self.name)

    def cur(self):
        return (self.sem, self.count, self.name)

    def wait(self, ev):
        sem, val, _ = ev
        if val <= 0:
            return
        key = id(sem)
        if self.seen.get(key, 0) >= val:
            return
        self.h.wait_ge(sem, val)
        self.seen[key] = val


class DmaQ:
    def __init__(self, k, name, eng, nslots=8):
        self.k = k
        self.eng = eng
        self.name = name
        self.sems = [k.newsem("%s_d%d" % (name, i)) for i in range(nslots)]
        self.vals = [0] * nslots
        self.i = 0

    def issue(self, emit, deps):
        s = self.i % len(self.sems)
        self.i += 1
        sem = self.sems[s]
        if self.vals[s] > 0:
            self.eng.wait((sem, self.vals[s], "dma"))
        for ev in deps:
            self.eng.wait(ev)
        if self.vals[s] >= 16 * 1500:
            sem = self.k.newsem("%s_d%d_%d" % (self.name, s, self.i))
            self.sems[s] = sem
            self.vals[s] = 0
        inst = emit()
        self.vals[s] += 16
        inst.then_inc(sem, 16)
        return (sem, self.vals[s], "dma")


class K:
    def __init__(self, nc, stack):
        self.nc = nc
        self.stack = stack
        self.nsem = 0
        self.pe = Eng(self, "pe", nc.tensor, is_pe=True)
        self.act = Eng(self, "act", nc.scalar)
        self.dve = Eng(self, "dve", nc.vector)
        self.pool = Eng(self, "pool", nc.gpsimd)
        self.sp = Eng(self, "sp", nc.sync)
        self.engs = [self.pe, self.act, self.dve, self.pool, self.sp]
        self.q_sp = DmaQ(self, "qsp", self.sp, 12)
        self.q_pool = DmaQ(self, "qpool", self.pool, 8)
        self.qs = [self.q_sp, self.q_pool]
        self.n_inst = 0

    def newsem(self, name):
        self.nsem += 1
        return self.stack.enter_context(self.nc.semaphore(name))

    def sb(self, st, name, shape, dtype):
        self.uid = getattr(self, "uid", 0) + 1
        name = "%s_u%d" % (name, self.uid)
        t = st.enter_context(self.nc.sbuf_tensor(name, list(shape), dtype))
        return Tile(t, name)

    def ps(self, st, name, shape, dtype=F32):
        self.uid = getattr(self, "uid", 0) + 1
        name = "%s_u%d" % (name, self.uid)
        t = st.enter_context(self.nc.psum_tensor(name, list(shape), dtype))
        return Tile(t, name)

    @staticmethod
    def _resof(x):
        return x.res if isinstance(x, Tile) else x

    def _deps(self, reads, writes):
        deps = []
        for r in reads:
            r = self._resof(r)
            if r.w is not None:
                deps.append(r.w)
        for w in writes:
            w = self._resof(w)
            if w.w is not None:
                deps.append(w.w)
            deps.extend(w.r)
        return deps

    def _commit(self, ev, reads, writes):
        for r in reads:
            r = self._resof(r)
            r.r = [e for e in r.r if e[0] is not ev[0]]
            r.r.append(ev)
        for w in writes:
            w = self._resof(w)
            w.w = ev
            w.r = []

    def op(self, eng, emit, reads=(), writes=()):
        for ev in self._deps(reads, writes):
            if eng.is_pe and ev[2] == "pe":
                continue
            eng.wait(ev)
        inst = emit()
        ev = eng.tick(inst)
        self._commit(ev, reads, writes)
        self.n_inst += 1
        return ev

    def dma(self, q, out, in_, reads=(), writes=(), **kw):
        deps = self._deps(reads, writes)
        ev = q.issue(lambda: q.eng.h.dma_start(out=out, in_=in_, **kw), deps)
        self._commit(ev, reads, writes)
        self.n_inst += 1
        return ev

    def barrier(self):
        evs = [e.cur() for e in self.engs]
        for q in self.qs:
            for s, v in zip(q.sems, q.vals):
                evs.append((s, v, "dma"))
        for e in self.engs:
            for ev in evs:
                if ev[0] is e.sem:
                    continue
                e.wait(ev)


class Ring:
    def __init__(self, tiles):
        self.tiles = tiles
        self.i = 0

    def next(self):
        t = self.tiles[self.i % len(self.tiles)]
        self.i += 1
        return t


def _t5_bucket_np(rel):
    nb = 16
    max_exact = 8
    ret = np.where(rel > 0, nb, 0)
    n = np.abs(rel)
    nf = np.maximum(n, 1).astype(np.float32)
    large = max_exact + (np.log(nf / max_exact) / np.float32(math.log(128 / max_exact))
                         * (nb - max_exact)).astype(np.int32)
    large = np.minimum(large, nb - 1)
    return ret + np.where(n < max_exact, n, large)


DELTAS = [-128, 0, 128, 256, 384]
R0 = 511
NF = 1280


def make_consts():
    c = {}
    c["c_ident"] = np.eye(128, dtype=np.float32)
    c["c_exch"] = np.ascontiguousarray(np.eye(128, dtype=np.float32)[::-1])
    i = np.arange(128)[:, None]
    j = np.arange(128)[None, :]
    c["c_mlt"] = (i < j).astype(np.float32)
    c["c_uneg"] = -(i >= j).astype(np.float32)
    same = (i // 64) == (j // 64)
    c["c_gtri"] = (-(1.0 / 16.0) * (same & (i <= j))).astype(np.float32)
    c["c_gm2"] = (-(1.0 / 16.0) * (same & (i > j))).astype(np.float32)
    c["c_gmask"] = (same & (i <= j)).astype(np.float32)
    n = np.arange(NF)
    bk = _t5_bucket_np(R0 - n)
    oh = np.zeros((32, NF), np.float32)
    oh[bk, n] = 1.0
    oh[:, 1151:] = 0.0
    c["c_ohb"] = oh
    nm = np.zeros((5, 128, 512), np.float32)
    for di, dl in enumerate(DELTAS):
        kk = dl + np.arange(128)[:, None]
        qq = np.arange(512)[None, :]
        allowed = (kk // 64) <= (qq // 64)
        nm[di] = np.where(allowed, 0.0, -30000.0)
    c["c_negmask"] = nm
    return c


WEIGHT_NAMES = ["w_ada", "b_ada", "norm_mix", "norm_ffn", "norm_final", "rel_bias",
                "even_w_in", "even_lambda", "even_subln", "even_w_gk2", "even_b_gk", "even_gla_norm",
                "even_w_out", "odd_w_in", "odd_w_out", "router_group_w", "router_group_b",
                "router_expert_w", "router_expert_b", "expert_w_gate", "expert_w_up", "expert_w_down"]
WEIGHT_SHAPES = {
    "w_ada": [4, 1024, 6144], "b_ada": [4, 6144], "norm_mix": [4, 1024], "norm_ffn": [4, 1024],
    "norm_final": [1, 1024], "rel_bias": [32, 4], "even_w_in": [2, 1024, 3088], "even_lambda": [2, 256],
    "even_subln": [2, 128], "even_w_gk2": [2, 16, 256], "even_b_gk": [2, 256], "even_gla_norm": [2, 128],
    "even_w_out": [2, 1024, 1024], "odd_w_in": [2, 1024, 3072], "odd_w_out": [2, 1024, 1024],
    "router_group_w": [4, 1024, 4], "router_group_b": [4, 4], "router_expert_w": [4, 1024, 32],
    "router_expert_b": [4, 32], "expert_w_gate": [4, 32, 1024, 512], "expert_w_up": [4, 32, 1024, 512],
    "expert_w_down": [4, 32, 512, 1024],
}


def build(nlayers=DEPTH, stop=None, flags=()):
    nc = bass.Bass("TRN2", target_bir_lowering=False)
    I = {}
    I["x"] = nc.dram_tensor("x", [S, D], F32, kind="ExternalInput").ap()
    I["c"] = nc.dram_tensor("c", [1, D], F32, kind="ExternalInput").ap()
    for nm in WEIGHT_NAMES:
        I[nm] = nc.dram_tensor(nm, WEIGHT_SHAPES[nm], F32, kind="ExternalInput").ap()
    consts = make_consts()
    for nm, v in consts.items():
        I[nm] = nc.dram_tensor(nm, list(v.shape), F32, kind="ExternalInput").ap()
    OUT = nc.dram_tensor("out", [S, D], F32, kind="ExternalOutput").ap()
    XS = nc.dram_tensor("xs_scr", [S, D], F32, kind="Internal").ap()
    FB = nc.dram_tensor("fb_scr", [25, 128, S], BF16, kind="Internal").ap()
    TB = nc.dram_tensor("tb_scr", [S, 1280], BF16, kind="Internal").ap()
    FS_T = nc.dram_tensor("fs_scr", [4, NF], F32, kind="Internal")
    FS = FS_T.ap()

    with ExitStack() as gst:
        k = K(nc, gst)
        pe, act, dve, pool, sp = k.pe, k.act, k.dve, k.pool, k.sp

        def mm(out, lhsT, rhs, start, stop, reads, writes):
            k.op(pe, lambda: nc.tensor.matmul(out, lhsT=lhsT, rhs=rhs, start=start, stop=stop), reads, writes)

        def tr(out, in_, ident, reads, writes):
            k.op(pe, lambda: nc.tensor.transpose(out=out, in_=in_, identity=ident), reads, writes)

        def actf(out, in_, func, reads, writes, **kw):
            k.op(act, lambda: nc.scalar.activation(out=out, in_=in_, func=func, **kw), reads, writes)

        def tt(eng, out, in0, in1, op, reads, writes):
            k.op(eng, lambda: eng.h.tensor_tensor(out=out, in0=in0, in1=in1, op=op), reads, writes)

        def ts(eng, out, in0, s1, s2, op0, op1, reads, writes):
            if op1 is None:
                k.op(eng, lambda: eng.h.tensor_scalar(out=out, in0=in0, scalar1=s1, scalar2=None, op0=op0),
                     reads, writes)
            else:
                k.op(eng, lambda: eng.h.tensor_scalar(out=out, in0=in0, scalar1=s1, scalar2=s2, op0=op0, op1=op1),
                     reads, writes)

        def stt(out, in0, scalar, in1, op0, op1, reads, writes):
            k.op(dve, lambda: nc.vector.scalar_tensor_tensor(out=out, in0=in0, scalar=scalar, in1=in1,
                                                             op0=op0, op1=op1), reads, writes)

        def cp(eng, out, in_, reads, writes):
            k.op(eng, lambda: eng.h.tensor_copy(out=out, in_=in_), reads, writes)

        def recip(out, in_, reads, writes):
            k.op(dve, lambda: nc.vector.reciprocal(out=out, in_=in_), reads, writes)

        def memset(eng, t, val, writes):
            k.op(eng, lambda: eng.h.memset(t, val), (), writes)

        def rmax(out, in_, reads, writes):
            k.op(dve, lambda: nc.vector.tensor_reduce(out=out, in_=in_, axis=AX.X, op=ALU.max), reads, writes)

        xres = [Res("x%d" % i) for i in range(NT)]
        fbres = [Res("fb%d" % i) for i in range(25)]
        tbres = Res("tb")
        fsres = Res("fs")

        ident32 = k.sb(gst, "ident32", [128, 128], F32)
        ones32 = k.sb(gst, "ones32", [128, 128], F32)
        onesb = k.sb(gst, "onesb", [128, 128], BF16)
        negonesb = k.sb(gst, "negonesb", [128, 128], BF16)
        zerob = k.sb(gst, "zerob", [128, 128], BF16)
        identb = k.sb(gst, "identb", [128, 128], BF16)
        k.dma(k.q_sp, ident32[:, :], I["c_ident"][:, :], writes=[ident32])
        memset(dve, ones32[:, :], 1.0, [ones32])
        memset(dve, onesb[:, :], 1.0, [onesb])
        memset(dve, negonesb[:, :], -1.0, [negonesb])
        memset(dve, zerob[:, :], 0.0, [zerob])
        cp(dve, identb[:, :], ident32[:, :], [ident32], [identb])

        modT = k.sb(gst, "modT", [128, 64], F32)
        gmod1 = k.sb(gst, "gmod1", [128, 8], F32)
        gmod2 = k.sb(gst, "gmod2", [128, 8], F32)
        gbc1 = k.sb(gst, "gbc1", [128, D], F32)
        gbc2 = k.sb(gst, "gbc2", [128, D], F32)
        scT = k.sb(gst, "scT", [128, 8], F32)
        cmb = k.sb(gst, "cmb", [128, NT, NEXP], F32)
        hT = k.sb(gst, "hT", [128, 8, S], BF16)

        def emit_mod(l):
            with ExitStack() as st:
                crow = k.sb(st, "crow", [1, D], F32)
                sc_bc = k.sb(st, "sc_bc", [128, 8, 128], F32)
                wseg = Ring([k.sb(st, "wseg%d" % i, [128, 8, 512], F32) for i in range(2)])
                brow = Ring([k.sb(st, "brow%d" % i, [1, 512], F32) for i in range(2)])
                nrow = k.sb(st, "nrow", [1, 2 * D], F32)
                pm = k.ps(st, "pm", [128, 512], F32)
                pg = Ring([k.ps(st, "pg%d" % i, [128, 512], F32) for i in range(2)])
                k.dma(k.q_sp, crow[:, :], I["c"][:, :], writes=[crow])
                for kc in range(8):
                    mm(pm[:, kc:kc + 1], crow[0:1, kc * 128:(kc + 1) * 128], ones32[0:1, 0:1], True, True,
                       [crow, ones32], [pm])
                actf(scT[:, :], pm[:, 0:8], AF.Silu, [pm], [scT])
                for kc in range(8):
                    actf(sc_bc[:, kc, :], ones32[:, :], AF.Copy, [ones32, scT], [sc_bc], scale=scT[:, kc:kc + 1])
                wv = I["w_ada"][l].rearrange("(kc p) n -> p kc n", p=128)
                for seg in range(6):
                    for half in range(2):
                        n0 = seg * 1024 + half * 512
                        w = wseg.next()
                        b = brow.next()
                        k.dma(k.q_sp, w[:, :, :], wv[:, :, n0:n0 + 512], writes=[w])
                        k.dma(k.q_sp, b[:, :], I["b_ada"][l:l + 1, n0:n0 + 512], writes=[b])
                        if seg in (2, 5):
                            p = pg.next()
                            for kc in range(8):
                                mm(p[:, :], sc_bc[:, kc, :], w[:, kc, :], kc == 0, False, [sc_bc, w], [p])
                            mm(p[:, :], ones32[0:1, :], b[0:1, :], False, True, [ones32, b], [p])
                            g = gbc1 if seg == 2 else gbc2
                            cp(dve, g[:, half * 512:(half + 1) * 512], p[:, :], [p], [g])
                        else:
                            for j in range(4):
                                col = seg * 8 + half * 4 + j
                                for kc in range(8):
                                    mm(pm[:, col:col + 1], w[:, kc, j * 128:(j + 1) * 128], scT[:, kc:kc + 1],
                                       kc == 0, False, [w, scT], [pm])
                                mm(pm[:, col:col + 1], b[0:1, j * 128:(j + 1) * 128], ones32[0:1, 0:1], False, True,
                                   [b, ones32], [pm])
                k.dma(k.q_sp, nrow[:, 0:D], I["norm_mix"][l:l + 1, :], writes=[nrow])
                k.dma(k.q_sp, nrow[:, D:2 * D], I["norm_ffn"][l:l + 1, :], writes=[nrow])
                for j in range(16):
                    mm(pm[:, 48 + j:49 + j], nrow[0:1, j * 128:(j + 1) * 128], ones32[0:1, 0:1], True, True,
                       [nrow, ones32], [pm])
                cp(dve, modT[:, :], pm[:, 0:64], [pm], [modT])
                stt(gmod1[:, :], modT[:, 8:16], 1.0, modT[:, 48:56], ALU.add, ALU.mult, [modT], [gmod1])
                stt(gmod2[:, :], modT[:, 32:40], 1.0, modT[:, 56:64], ALU.add, ALU.mult, [modT], [gmod2])
                k.barrier()

        def emit_norm(xsrc, gmod, shiftc, layer, router):
            with ExitStack() as st:
                xt_r = Ring([k.sb(st, "n_xt%d" % i, [128, D], F32) for i in range(3)])
                xs_r = Ring([k.sb(st, "n_xs%d" % i, [128, D], F32) for i in range(2)])
                junk = k.sb(st, "n_junk", [128, D], BF16)
                sm_r = Ring([k.sb(st, "n_sm%d" % i, [128, 4], F32) for i in range(4)])
                ps_r = Ring([k.ps(st, "n_ps%d" % i, [128, D], F32) for i in range(2)])
                if router:
                    h32_r = Ring([k.sb(st, "n_h32%d" % i, [128, 8, 128], F32) for i in range(2)])
                    wr32 = k.sb(st, "n_wr32", [128, 8, 36], F32)
                    brr = k.sb(st, "n_brr", [1, 36], F32)
                    plg_r = Ring([k.ps(st, "n_plg%d" % i, [128, 512], F32) for i in range(2)])
                    rt_r = Ring([k.sb(st, "n_rt%d" % i, [128, 128], F32) for i in range(2)])
                    k.dma(k.q_sp, wr32[:, :, 0:4],
                          I["router_group_w"][layer].rearrange("(kc p) n -> p kc n", p=128), writes=[wr32])
                    k.dma(k.q_sp, wr32[:, :, 4:36],
                          I["router_expert_w"][layer].rearrange("(kc p) n -> p kc n", p=128), writes=[wr32])
                    k.dma(k.q_sp, brr[:, 0:4], I["router_group_b"][layer:layer + 1, :], writes=[brr])
                    k.dma(k.q_sp, brr[:, 4:36], I["router_expert_b"][layer:layer + 1, :], writes=[brr])
                for t in range(2 if (router and 'nt2' in flags) else NT):
                    xt = xt_r.next()
                    k.dma(k.q_sp, xt[:, :], xsrc[t * 128:(t + 1) * 128, :], reads=[xres[t]], writes=[xt])
                    sm = sm_r.next()
                    actf(junk[:, :], xt[:, :], AF.Square, [xt], [junk, sm], accum_out=sm[:, 0:1])
                    ts(dve, sm[:, 1:2], sm[:, 0:1], 1.0 / D, EPS, ALU.mult, ALU.add, [sm], [sm])
                    actf(sm[:, 2:3], sm[:, 1:2], AF.Sqrt, [sm], [sm])
                    recip(sm[:, 3:4], sm[:, 2:3], [sm], [sm])
                    xs = xs_r.next()
                    ts(dve, xs[:, :], xt[:, :], sm[:, 3:4], None, ALU.mult, None, [xt, sm], [xs])
                    ps = ps_r.next()
                    for kc in range(8):
                        tr(ps[:, kc * 128:(kc + 1) * 128], xs[:, kc * 128:(kc + 1) * 128], ident32[:, :],
                           [xs, ident32], [ps])
                    for kc in range(8):
                        actf(hT[:, kc, t * 128:(t + 1) * 128], ps[:, kc * 128:(kc + 1) * 128], AF.Identity,
                             [ps, gmod, modT], [hT], scale=gmod[:, kc:kc + 1], bias=shiftc[:, kc:kc + 1])
                    if router and 'r0' not in flags:
                        h32 = h32_r.next()
                        for kc in range(8):
                            actf(h32[:, kc, :], ps[:, kc * 128:(kc + 1) * 128], AF.Identity,
                                 [ps, gmod, modT], [h32], scale=gmod[:, kc:kc + 1], bias=shiftc[:, kc:kc + 1])
                        if 'r1' in flags:
                            continue
                        plg = plg_r.next()
                        for kc in range(8):
                            mm(plg[:, 0:36], h32[:, kc, :], wr32[:, kc, :], kc == 0, False, [h32, wr32], [plg])
                        mm(plg[:, 0:36], ones32[0:1, :], brr[0:1, :], False, True, [ones32, brr], [plg])
                        if 'rA' in flags:
                            continue
                        rt = rt_r.next()
                        R = [rt]
                        lg = rt[:, 0:36]
                        cp(dve, lg, plg[:, 0:36], [plg], R)
                        gm = rt[:, 36:37]
                        rmax(gm, rt[:, 0:4], R, R)
                        ohg = rt[:, 40:44]
                        ts(dve, ohg, rt[:, 0:4], gm, None, ALU.is_equal, None, R, R)
                        ngm = rt[:, 37:38]
                        ts(dve, ngm, gm, -1.0, None, ALU.mult, None, R, R)
                        gs = rt[:, 38:39]
                        actf(rt[:, 44:48], rt[:, 0:4], AF.Exp, R, R, bias=ngm, accum_out=gs)
                        gval = rt[:, 39:40]
                        recip(gval, gs, R, R)
                        if 'rB' in flags:
                            continue
                        el = rt[:, 48:56]
                        ts(dve, el, rt[:, 4:12], rt[:, 40:41], None, ALU.mult, None, R, R)
                        for g in range(1, 4):
                            stt(el, rt[:, 4 + 8 * g:12 + 8 * g], rt[:, 40 + g:41 + g], el, ALU.mult, ALU.add, R, R)
                        l1 = rt[:, 56:57]
                        rmax(l1, el, R, R)
                        oh1 = rt[:, 64:72]
                        ts(dve, oh1, el, l1, None, ALU.is_equal, None, R, R)
                        elm = rt[:, 72:80]
                        stt(elm, oh1, -1e30, el, ALU.mult, ALU.add, R, R)
                        l2 = rt[:, 57:58]
                        rmax(l2, elm, R, R)
                        oh2 = rt[:, 80:88]
                        ts(dve, oh2, elm, l2, None, ALU.is_equal, None, R, R)
                        if 'rC' in flags:
                            continue
                        dd = rt[:, 58:59]
                        tt(dve, dd, l2, l1, ALU.subtract, R, R)
                        ee = rt[:, 59:60]
                        actf(ee, dd, AF.Exp, R, R)
                        den = rt[:, 60:61]
                        ts(dve, den, ee, 1.0, None, ALU.add, None, R, R)
                        w1 = rt[:, 61:62]
                        recip(w1, den, R, R)
                        W1 = rt[:, 62:63]
                        tt(dve, W1, w1, gval, ALU.mult, R, R)
                        W2 = rt[:, 63:64]
                        tt(dve, W2, W1, ee, ALU.mult, R, R)
                        cg = rt[:, 88:96]
                        ts(dve, cg, oh1, W1, None, ALU.mult, None, R, R)
                        stt(cg, oh2, W2, cg, ALU.mult, ALU.add, R, R)
                        for g in range(4):
                            ts(dve, cmb[:, t, 8 * g:8 * g + 8], cg, rt[:, 40 + g:41 + g], None, ALU.mult, None,
                               R, [cmb])
                k.barrier()

        def emit_inproj(wsrc, ncols, fjobs, tjobs):
            with ExitStack() as st:
                W = k.sb(st, "ip_W", [128, 8, ncols], BF16)
                wv = wsrc.rearrange("(kc p) n -> p kc n", p=128)
                c0 = 0
                while c0 < ncols:
                    w_ = min(512, ncols - c0)
                    k.dma(k.q_pool, W[:, :, c0:c0 + w_], wv[:, :, c0:c0 + w_], writes=[W])
                    c0 += w_
                stg_r = Ring([k.sb(st, "ip_stg%d" % i, [128, S], BF16) for i in range(2)])
                stt_r = Ring([k.sb(st, "ip_stt%d" % i, [128, 4, 512], BF16) for i in range(2)])
                ps_r = Ring([k.ps(st, "ip_ps%d" % i, [128, 512], F32) for i in range(4)])
                n = 0
                for (col0, nr, fb, scale) in fjobs:
                    stg = stg_r.next()
                    for tg in range(8):
                        ps = ps_r.next()
                        for kc in range(8):
                            mm(ps[0:nr, :], W[:, kc, col0:col0 + nr], hT[:, kc, tg * 512:(tg + 1) * 512],
                               kc == 0, kc == 7, [W, hT], [ps])
                        if n % 2 == 0:
                            actf(stg[0:nr, tg * 512:(tg + 1) * 512], ps[0:nr, :], AF.Copy, [ps], [stg], scale=scale)
                        else:
                            ts(dve, stg[0:nr, tg * 512:(tg + 1) * 512], ps[0:nr, :], scale, None, ALU.mult, None,
                               [ps], [stg])
                        n += 1
                    k.dma(k.q_sp, FB[fb, 0:nr, :], stg[0:nr, :], reads=[stg], writes=[fbres[fb]])
                for (col0, wd, tcol0) in tjobs:
                    for t4 in range(8):
                        stg = stt_r.next()
                        for j in range(4):
                            t = t4 * 4 + j
                            ps = ps_r.next()
                            for kc in range(8):
                                mm(ps[:, 0:wd], hT[:, kc, t * 128:(t + 1) * 128], W[:, kc, col0:col0 + wd],
                                   kc == 0, kc == 7, [W, hT], [ps])
                            if n % 2 == 0:
                                actf(stg[:, j, 0:wd], ps[:, 0:wd], AF.Copy, [ps], [stg])
                            else:
                                cp(dve, stg[:, j, 0:wd], ps[:, 0:wd], [ps], [stg])
                            n += 1
                        k.dma(k.q_sp,
                              TB[t4 * 512:(t4 + 1) * 512, tcol0:tcol0 + wd].rearrange("(j p) c -> p j c", p=128),
                              stg[:, :, 0:wd], reads=[stg], writes=[tbres])
                k.barrier()

        def emit_outproj(wsrc, gbc, xsrc):
            with ExitStack() as st:
                W = k.sb(st, "op_W", [128, 8, D], BF16)
                wv = wsrc.rearrange("(kc p) n -> p kc n", p=128)
                for h in range(2):
                    k.dma(k.q_pool, W[:, :, h * 512:(h + 1) * 512], wv[:, :, h * 512:(h + 1) * 512], writes=[W])
                for kc in range(8):
                    tt(dve, W[:, kc, :], W[:, kc, :], gbc[:, :], ALU.mult, [W, gbc], [W])
                xt_r = Ring([k.sb(st, "op_xt%d" % i, [128, D], F32) for i in range(3)])
                xn_r = Ring([k.sb(st, "op_xn%d" % i, [128, D], F32) for i in range(2)])
                ps_r = Ring([k.ps(st, "op_ps%d" % i, [128, D], F32) for i in range(3)])
                for t in range(NT):
                    xt = xt_r.next()
                    k.dma(k.q_sp, xt[:, :], xsrc[t * 128:(t + 1) * 128, :], reads=[xres[t]], writes=[xt])
                    ps = ps_r.next()
                    for h in range(2):
                        for kc in range(8):
                            mm(ps[:, h * 512:(h + 1) * 512], hT[:, kc, t * 128:(t + 1) * 128],
                               W[:, kc, h * 512:(h + 1) * 512], kc == 0, kc == 7, [hT, W], [ps])
                    xn = xn_r.next()
                    tt(dve, xn[:, :], xt[:, :], ps[:, :], ALU.add, [xt, ps], [xn])
                    k.dma(k.q_sp, XS[t * 128:(t + 1) * 128, :], xn[:, :], reads=[xn], writes=[xres[t]])
                k.barrier()

        def emit_odd_mixer():
            with ExitStack() as st:
                mltb = k.sb(st, "sb_mlt", [128, 128], BF16)
                unegb = k.sb(st, "sb_uneg", [128, 128], BF16)
                k.dma(k.q_pool, mltb[:, :], I["c_mlt"][:, :], writes=[mltb])
                k.dma(k.q_pool, unegb[:, :], I["c_uneg"][:, :], writes=[unegb])
                qT_r = Ring([k.sb(st, "sb_qT%d" % i, [128, S], BF16) for i in range(2)])
                kT_r = Ring([k.sb(st, "sb_kT%d" % i, [128, S], BF16) for i in range(2)])
                vt_r = Ring([k.sb(st, "sb_vt%d" % i, [128, NT, 128], BF16) for i in range(2)])
                e32_r = Ring([k.sb(st, "sb_e%d" % i, [128, 512], F32) for i in range(6)])
                sp_r = Ring([k.sb(st, "sb_sp%d" % i, [128, 512], BF16) for i in range(10)])
                rb_r = Ring([k.sb(st, "sb_rb%d" % i, [128, 512], BF16) for i in range(10)])
                ab_r = Ring([k.sb(st, "sb_ab%d" % i, [128, 512], BF16) for i in range(6)])
                R32s = [k.sb(st, "sb_R32_%d" % i, [128, 512], F32) for i in range(2)]
                pz_r = Ring([k.ps(st, "sb_pz%d" % i, [128, 512], F32) for i in range(3)])
                pw_r = Ring([k.ps(st, "sb_pw%d" % i, [128, 512], F32) for i in range(3)])
                pos = [k.ps(st, "sb_po%d" % i, [128, 512], F32) for i in range(2)]
                for hp in range(8):
                    qT = qT_r.next()
                    kT = kT_r.next()
                    vt = vt_r.next()
                    k.dma(k.q_sp, qT[:, :], FB[hp, :, :], reads=[fbres[hp]], writes=[qT])
                    k.dma(k.q_sp, kT[:, :], FB[8 + hp, :, :], reads=[fbres[8 + hp]], writes=[kT])
                    k.dma(k.q_sp, vt[:, :, :],
                          TB[:, hp * 128:(hp + 1) * 128].rearrange("(t p) c -> p t c", p=128),
                          reads=[tbres], writes=[vt])
                    for qg in range(8):
                        kbs = list(range(4 * qg + 3, -1, -1))
                        nb = len(kbs)
                        items = [[None] * nb, [None] * nb]
                        pend = {}
                        pendB = []
                        for j in range(2):
                            mm(pos[j][:, :], zerob[:, :], qT[:, qg * 512:(qg + 1) * 512], True, False,
                               [zerob, qT], [pos[j]])

                        def stageA(j, i):
                            b0 = 64 * j
                            R32 = R32s[j]
                            kb = kbs[i]
                            j0 = max(0, kb - 4 * qg)
                            c0 = 128 * j0
                            wq = 512 - c0
                            q0 = qg * 512 + c0
                            diag = kb >= 4 * qg
                            pz = pz_r.next()
                            mm(pz[:, 0:wq], kT[b0:b0 + 64, kb * 128:(kb + 1) * 128], qT[b0:b0 + 64, q0:q0 + wq],
                               True, True, [kT, qT], [pz])
                            e32 = e32_r.next()
                            actf(e32[:, 0:wq], pz[:, 0:wq], AF.Exp, [pz], [e32])
                            pend[(j, i)] = (R32, kb, c0, wq, q0, diag, e32)

                        def stageA2(j, i):
                            R32, kb, c0, wq, q0, diag, e32 = pend.pop((j, i))
                            spb = sp_r.next()
                            if 'sbx_noln' not in flags:
                                actf(spb[:, 0:wq], e32[:, 0:wq], AF.Ln, [e32], [spb], bias=1.0)
                            if diag:
                                tt(pool, spb[:, 0:128], spb[:, 0:128], mltb[:, :], ALU.mult, [spb, mltb], [spb])
                            rb = None
                            if i > 0 and 'sbx_nor' not in flags:
                                rb = rb_r.next()
                                cp(dve, rb[:, :], R32[:, :], [R32], [rb])
                            if i < nb - 1 and 'sbx_nor' not in flags:
                                if i == 0:
                                    if c0 > 0:
                                        memset(dve, R32[:, 0:c0], 0.0, [R32])
                                    cp(dve, R32[:, c0:512], spb[:, 0:wq], [spb], [R32])
                                else:
                                    tt(dve, R32[:, c0:512], R32[:, c0:512], spb[:, 0:wq], ALU.add, [R32, spb],
                                       [R32])
                            items[j][i] = (kb, c0, wq, q0, diag, spb, rb)

                        def stageB(j, i):
                            if 'sbx_nob' in flags:
                                return
                            b0 = 64 * j
                            po = pos[j]
                            kb, c0, wq, q0, diag, spb, rb = items[j][i]
                            pw = pw_r.next()
                            mm(pw[:, 0:wq], kT[b0:b0 + 64, kb * 128:(kb + 1) * 128], qT[b0:b0 + 64, q0:q0 + wq],
                               True, False, [kT, qT], [pw])
                            mm(pw[:, 0:wq], unegb[:, :], spb[:, 0:wq], False, rb is None, [unegb, spb], [pw])
                            if rb is not None:
                                mm(pw[:, 0:wq], negonesb[:, :], rb[:, c0:512], False, True, [negonesb, rb], [pw])
                            ab = ab_r.next()
                            actf(ab[:, 0:wq], pw[:, 0:wq], AF.Exp, [pw], [ab])
                            if diag:
                                tt(dve, ab[:, 0:128], ab[:, 0:128], mltb[:, :], ALU.mult, [ab, mltb], [ab])
                            pendB.append((po, c0, wq, kb, ab, i))
                            while len(pendB) > 2:
                                stageB2()

                        def stageB2():
                            po, c0, wq, kb, ab, i = pendB.pop(0)
                            mm(po[:, c0:512], vt[:, kb, :], ab[:, 0:wq], False, i == nb - 1, [vt, ab], [po])

                        GW = 2
                        nw = nb // GW
                        for w in range(nw + 1):
                            if w < nw:
                                for i in range(w * GW, (w + 1) * GW):
                                    for j in range(2):
                                        stageA(j, i)
                                for i in range(w * GW, (w + 1) * GW):
                                    for j in range(2):
                                        stageA2(j, i)
                            if w > 0:
                                for i in range((w - 1) * GW, w * GW):
                                    for j in range(2):
                                        stageB(j, i)
                        while pendB:
                            stageB2()
                        for j in range(2):
                            b0 = 64 * j
                            if j == 0:
                                actf(hT[b0:b0 + 64, hp, qg * 512:(qg + 1) * 512], pos[j][b0:b0 + 64, :], AF.Copy,
                                     [pos[j]], [hT])
                            else:
                                cp(dve, hT[b0:b0 + 64, hp, qg * 512:(qg + 1) * 512], pos[j][b0:b0 + 64, :],
                                   [pos[j]], [hT])
                k.barrier()

        def emit_even_mixer(i_even, layer):
            lam_init = 0.8 - 0.6 * math.exp(-0.3 * layer)
            with ExitStack() as st:
                BM = [[k.sb(st, "BM%d_%d" % (h, di), [128, 512], BF16) for di in range(5)] for h in range(4)]
                b15 = k.sb(st, "b15", [128, 4], F32)
                neglam = k.sb(st, "neglam", [128, 1], F32)
                gsub = k.sb(st, "gsub", [128, 1], F32)
                with ExitStack() as st2:
                    rb = k.sb(st2, "rb", [32, 4], F32)
                    ohb = k.sb(st2, "ohb", [32, NF], F32)
                    fsb = k.sb(st2, "fsb", [4, NF], F32)
                    exch = k.sb(st2, "exch", [128, 128], F32)
                    nmk = [k.sb(st2, "nmk%d" % di, [128, 512], F32) for di in range(5)]
                    tl_r = Ring([k.sb(st2, "tl%d" % i, [128, 512], F32) for i in range(2)])
                    lrow = k.sb(st2, "lrow", [1, 264], F32)
                    grow = k.sb(st2, "grow", [1, 128], F32)
                    pf = Ring([k.ps(st2, "pf%d" % i, [128, 512], F32) for i in range(2)])
                    k.dma(k.q_sp, rb[:, :], I["rel_bias"][:, :], writes=[rb])
                    k.dma(k.q_sp, ohb[:, :], I["c_ohb"][:, :], writes=[ohb])
                    k.dma(k.q_sp, exch[:, :], I["c_exch"][:, :], writes=[exch])
                    k.dma(k.q_sp, b15[:, :], I["rel_bias"][15].partition_broadcast(128), writes=[b15])
                    for di in range(5):
                        k.dma(k.q_sp, nmk[di][:, :], I["c_negmask"][di], writes=[nmk[di]])
                    for c3 in range(3):
                        c0 = c3 * 512
                        w_ = min(512, NF - c0)
                        p = pf.next()
                        mm(p[0:4, 0:w_], rb[0:32, 0:4], ohb[0:32, c0:c0 + w_], True, True, [rb, ohb], [p])
                        cp(dve, fsb[0:4, c0:c0 + w_], p[0:4, 0:w_], [p], [fsb])
                    k.dma(k.q_sp, FS[:, :], fsb[:, :], reads=[fsb], writes=[fsres])
                    for h in range(4):
                        for di, dl in enumerate(DELTAS):
                            tl = tl_r.next()
                            src = bass.AP(tensor=FS_T, offset=h * NF + (384 - dl), ap=[[1, 128], [1, 512]])
                            k.dma(k.q_sp, tl[:, :], src, reads=[fsres], writes=[tl])
                            p = pf.next()
                            mm(p[:, :], exch[:, :], tl[:, :], True, True, [exch, tl], [p])
                            tt(dve, BM[h][di][:, :], p[:, :], nmk[di][:, :], ALU.add, [p, nmk[di]], [BM[h][di]])
                    k.dma(k.q_sp, lrow[:, 0:256], I["even_lambda"][i_even:i_even + 1, :], writes=[lrow])
                    LR = [lrow]
                    tt(dve, lrow[:, 0:64], lrow[:, 0:64], lrow[:, 64:128], ALU.mult, LR, LR)
                    tt(dve, lrow[:, 128:192], lrow[:, 128:192], lrow[:, 192:256], ALU.mult, LR, LR)
                    k.op(dve, lambda: nc.vector.tensor_reduce(out=lrow[:, 256:257], in_=lrow[:, 0:64], axis=AX.X,
                                                              op=ALU.add), LR, LR)
                    k.op(dve, lambda: nc.vector.tensor_reduce(out=lrow[:, 257:258], in_=lrow[:, 128:192], axis=AX.X,
                                                              op=ALU.add), LR, LR)
                    actf(lrow[:, 258:260], lrow[:, 256:258], AF.Exp, LR, LR)
                    tt(dve, lrow[:, 260:261], lrow[:, 259:260], lrow[:, 258:259], ALU.subtract, LR, LR)
                    ts(dve, lrow[:, 261:262], lrow[:, 260:261], -lam_init, None, ALU.add, None, LR, LR)
                    p = pf.next()
                    mm(p[:, 0:1], ones32[0:1, :], lrow[0:1, 261:262], True, True, [ones32, lrow], [p])
                    cp(dve, neglam[:, :], p[:, 0:1], [p], [neglam])
                    k.dma(k.q_sp, grow[:, :], I["even_subln"][i_even:i_even + 1, :], writes=[grow])
                    p = pf.next()
                    mm(p[:, 0:1], grow[0:1, :], ones32[0:1, 0:1], True, True, [grow, ones32], [p])
                    ts(dve, gsub[:, :], p[:, 0:1], 1.0 - lam_init, None, ALU.mult, None, [p], [gsub])
                    k.barrier()
                qT_r = Ring([k.sb(st, "da_qT%d" % i, [128, S], BF16) for i in range(2)])
                kT_r = Ring([k.sb(st, "da_kT%d" % i, [128, S], BF16) for i in range(2)])
                vt_r = Ring([k.sb(st, "da_vt%d" % i, [128, NT, 128], BF16) for i in range(2)])
                E_r = Ring([k.sb(st, "da_E%d" % i, [128, 512], BF16) for i in range(5)])
                f_r = Ring([k.sb(st, "da_f%d" % i, [128, 512], F32) for i in range(4)])
                o32_r = Ring([k.sb(st, "da_o%d" % i, [128, 512], F32) for i in range(2)])
                sq_r = Ring([k.sb(st, "da_sq%d" % i, [128, 512], BF16) for i in range(2)])
                ps_r = Ring([k.ps(st, "da_ps%d" % i, [128, 512], F32) for i in range(3)])
                pu = [k.ps(st, "da_pu%d" % i, [128, 512], F32) for i in range(2)]
                pd = [k.ps(st, "da_pd%d" % i, [128, 512], F32) for i in range(2)]
                pss = k.ps(st, "da_pss", [128, 512], F32)
                for h in range(0 if 'ev_noA' in flags else 4):
                    qT = qT_r.next()
                    kT = kT_r.next()
                    vt = vt_r.next()
                    k.dma(k.q_sp, qT[:, :], FB[h, :, :], reads=[fbres[h]], writes=[qT])
                    k.dma(k.q_sp, kT[:, :], FB[4 + h, :, :], reads=[fbres[4 + h]], writes=[kT])
                    k.dma(k.q_sp, vt[:, :, :],
                          TB[:, h * 128:(h + 1) * 128].rearrange("(t p) c -> p t c", p=128),
                          reads=[tbres], writes=[vt])
                    for qg in range(8):
                        nkb = 4 * qg + 4
                        its = [(kb, m) for kb in range(nkb) for m in range(2)]
                        held = {}

                        def st1(idx):
                            kb, m = its[idx]
                            j0 = max(0, kb - 4 * qg)
                            c0 = 128 * j0
                            wq = 512 - c0
                            q0 = qg * 512 + c0
                            dl = kb * 128 - qg * 512
                            b0 = 64 * m
                            ps = ps_r.next()
                            near = dl >= -128
                            mm(ps[:, 0:wq], kT[b0:b0 + 64, kb * 128:(kb + 1) * 128], qT[b0:b0 + 64, q0:q0 + wq],
                               True, not near, [kT, qT], [ps])
                            E = E_r.next()
                            if near:
                                bm = BM[h][DELTAS.index(dl)]
                                mm(ps[:, 0:wq], identb[:, :], bm[:, c0:512], False, True, [identb, bm], [ps])
                                actf(E[:, 0:wq], ps[:, 0:wq], AF.Exp, [ps], [E])
                            else:
                                actf(E[:, 0:wq], ps[:, 0:wq], AF.Exp, [ps, b15], [E], bias=b15[:, h:h + 1])
                            held[idx] = (E, c0, wq)

                        def st2(idx):
                            kb, m = its[idx]
                            E, c0, wq = held.pop(idx)
                            mm(pu[m][:, c0:512], vt[:, kb, :], E[:, 0:wq], kb == 0, kb == nkb - 1, [vt, E], [pu[m]])
                            mm(pd[m][:, c0:512], onesb[:, :], E[:, 0:wq], kb == 0, kb == nkb - 1, [onesb, E],
                               [pd[m]])

                        SK = 2
                        for idx in range(len(its) + SK):
                            if idx < len(its):
                                st1(idx)
                            if idx >= SK:
                                st2(idx - SK)
                        r0 = f_r.next()
                        recip(r0[:, :], pd[0][:, :], [pd[0]], [r0])
                        t0 = f_r.next()
                        tt(dve, t0[:, :], pu[0][:, :], r0[:, :], ALU.mult, [pu[0], r0], [t0])
                        r1 = f_r.next()
                        recip(r1[:, :], pd[1][:, :], [pd[1]], [r1])
                        t1 = f_r.next()
                        tt(dve, t1[:, :], pu[1][:, :], r1[:, :], ALU.mult, [pu[1], r1], [t1])
                        o32 = o32_r.next()
                        stt(o32[:, :], t1[:, :], neglam[:, 0:1], t0[:, :], ALU.mult, ALU.add, [t1, t0, neglam], [o32])
                        sq = sq_r.next()
                        actf(sq[:, :], o32[:, :], AF.Square, [o32], [sq])
                        mm(pss[:, :], onesb[:, :], sq[:, :], True, True, [onesb, sq], [pss])
                        rs = f_r.next()
                        actf(rs[:, :], pss[:, :], AF.Sqrt, [pss], [rs], scale=1.0 / 128.0, bias=EPS)
                        rs2 = f_r.next()
                        recip(rs2[:, :], rs[:, :], [rs], [rs2])
                        stt(hT[:, h, qg * 512:(qg + 1) * 512], o32[:, :], gsub[:, 0:1], rs2[:, :], ALU.mult, ALU.mult,
                            [o32, gsub, rs2], [hT])
                k.barrier()
            with ExitStack() as st:
                gtri = k.sb(st, "g_tri", [128, 128], F32)
                gm2 = k.sb(st, "g_m2", [128, 128], F32)
                gmaskb = k.sb(st, "g_mask", [128, 128], BF16)
                wgk = k.sb(st, "g_wgk", [16, 256], BF16)
                bgk = k.sb(st, "g_bgk", [1, 256], BF16)
                glag = k.sb(st, "g_lag", [128, 1], F32)
                grow = k.sb(st, "g_grow", [1, 128], F32)
                k.dma(k.q_sp, gtri[:, :], I["c_gtri"][:, :], writes=[gtri])
                k.dma(k.q_sp, gm2[:, :], I["c_gm2"][:, :], writes=[gm2])
                k.dma(k.q_pool, gmaskb[:, :], I["c_gmask"][:, :], writes=[gmaskb])
                k.dma(k.q_pool, wgk[:, :], I["even_w_gk2"][i_even], writes=[wgk])
                k.dma(k.q_pool, bgk[:, :], I["even_b_gk"][i_even:i_even + 1, :], writes=[bgk])
                k.dma(k.q_sp, grow[:, :], I["even_gla_norm"][i_even:i_even + 1, :], writes=[grow])
                pA = Ring([k.ps(st, "g_pA%d" % i, [128, 512], F32) for i in range(3)])
                pS = Ring([k.ps(st, "g_pS%d" % i, [128, 512], F32) for i in range(2)])
                pO = Ring([k.ps(st, "g_pO%d" % i, [128, 512], F32) for i in range(2)])
                p = pA.next()
                mm(p[:, 0:1], grow[0:1, :], ones32[0:1, 0:1], True, True, [grow, ones32], [p])
                cp(dve, glag[:, :], p[:, 0:1], [p], [glag])
                qg_r = Ring([k.sb(st, "g_q%d" % i, [128, 2, 512], BF16) for i in range(2)])
                kg_r = Ring([k.sb(st, "g_k%d" % i, [128, 2, 512], BF16) for i in range(2)])
                kt_r = Ring([k.sb(st, "g_kt%d" % i, [128, 4, 256], BF16) for i in range(2)])
                vg_r = Ring([k.sb(st, "g_v%d" % i, [128, 4, 512], BF16) for i in range(2)])
                bg_r = Ring([k.sb(st, "g_bg%d" % i, [16, 512], BF16) for i in range(2)])
                br_r = Ring([k.sb(st, "g_br%d" % i, [128, 4, 512], BF16) for i in range(2)])
                og_r = Ring([k.sb(st, "g_og%d" % i, [128, 4, 512], F32) for i in range(2)])
                e32_r = Ring([k.sb(st, "g_e%d" % i, [128, 256], F32) for i in range(2)])
                sp_r = Ring([k.sb(st, "g_sp%d" % i, [128, 256], F32) for i in range(2)])
                kf_r = Ring([k.sb(st, "g_kf%d" % i, [128, 256], F32) for i in range(2)])
                kd_r = Ring([k.sb(st, "g_kd%d" % i, [128, 256], BF16) for i in range(2)])
                eb_r = Ring([k.sb(st, "g_eb%d" % i, [128, 256], F32) for i in range(2)])
                en_r = Ring([k.sb(st, "g_en%d" % i, [128, 256], F32) for i in range(2)])
                qd_r = Ring([k.sb(st, "g_qd%d" % i, [128, 2, 128], BF16) for i in range(2)])
                ki_r = Ring([k.sb(st, "g_ki%d" % i, [128, 2, 128], BF16) for i in range(2)])
                at_r = Ring([k.sb(st, "g_at%d" % i, [128, 128], BF16) for i in range(3)])
                S32_r = [Ring([k.sb(st, "g_S32_%d_%d" % (h, i), [128, 128], F32) for i in range(2)]) for h in range(4)]
                Sb_r = [Ring([k.sb(st, "g_Sb_%d_%d" % (h, i), [128, 128], BF16) for i in range(3)]) for h in range(4)]
                sq_r = Ring([k.sb(st, "g_sq%d" % i, [128, 512], BF16) for i in range(2)])
                f_r = Ring([k.sb(st, "g_f%d" % i, [128, 512], F32) for i in range(4)])
                S32 = []
                Sb = []
                for h in range(4):
                    s = S32_r[h].next()
                    memset(dve, s[:, :], 0.0, [s])
                    S32.append(s)
                    b = Sb_r[h].next()
                    memset(pool, b[:, :], 0.0, [b])
                    Sb.append(b)
                for g8 in range(0 if 'ev_noB' in flags else 8):
                    t0g = g8 * 512
                    qgt = qg_r.next()
                    kgt = kg_r.next()
                    ktt = kt_r.next()
                    vgt = vg_r.next()
                    bgt = bg_r.next()
                    brt = br_r.next()
                    og = og_r.next()
                    for hh in range(2):
                        k.dma(k.q_sp, qgt[:, hh, :], FB[12 + hh, :, t0g:t0g + 512], reads=[fbres[12 + hh]], writes=[qgt])
                        k.dma(k.q_sp, kgt[:, hh, :], FB[14 + hh, :, t0g:t0g + 512], reads=[fbres[14 + hh]], writes=[kgt])
                    k.dma(k.q_sp, ktt[:, :, :],
                          TB[t0g:t0g + 512, 512:768].rearrange("(j p) c -> p j c", p=128), reads=[tbres], writes=[ktt])
                    k.dma(k.q_sp, vgt[:, :, :],
                          TB[t0g:t0g + 512, 768:1280].rearrange("(j p) c -> p j c", p=128), reads=[tbres], writes=[vgt])
                    k.dma(k.q_sp, bgt[:, :], FB[24, 0:16, t0g:t0g + 512], reads=[fbres[24]], writes=[bgt])
                    for h in range(4):
                        k.dma(k.q_sp, brt[:, h, :], FB[20 + h, :, t0g:t0g + 512], reads=[fbres[20 + h]], writes=[brt])
                    for j4 in range(4):
                        tc0 = j4 * 128
                        ppre = pA.next()
                        mm(ppre[:, 0:256], bgt[0:16, tc0:tc0 + 128], wgk[0:16, :], True, False, [bgt, wgk], [ppre])
                        mm(ppre[:, 0:256], onesb[0:1, :], bgk[0:1, :], False, True, [onesb, bgk], [ppre])
                        e32 = e32_r.next()
                        actf(e32[:, :], ppre[:, 0:256], AF.Exp, [ppre], [e32], scale=-1.0)
                        sp32 = sp_r.next()
                        actf(sp32[:, :], e32[:, :], AF.Ln, [e32], [sp32], bias=1.0)
                        pb = pA.next()
                        for hh in range(2):
                            mm(pb[:, hh * 128:(hh + 1) * 128], sp32[:, hh * 128:(hh + 1) * 128], gtri[:, :], True, True,
                               [sp32, gtri], [pb])
                        pk = pA.next()
                        mm(pk[:, 0:256], gm2[:, :], sp32[:, :], True, True, [gm2, sp32], [pk])
                        kf = kf_r.next()
                        actf(kf[:, :], pk[:, 0:256], AF.Exp, [pk], [kf])
                        kd = kd_r.next()
                        tt(dve, kd[:, :], ktt[:, j4, :], kf[:, :], ALU.mult, [ktt, kf], [kd])
                        eb = eb_r.next()
                        actf(eb[:, :], pb[:, 0:256], AF.Exp, [pb], [eb])
                        en = en_r.next()
                        actf(en[:, :], pb[:, 0:256], AF.Exp, [pb], [en], scale=-1.0)
                        qd = qd_r.next()
                        ki = ki_r.next()
                        for hh in range(2):
                            tt(dve, qd[:, hh, :], qgt[:, hh, tc0:tc0 + 128], eb[:, hh * 128:(hh + 1) * 128], ALU.mult,
                               [qgt, eb], [qd])
                            tt(dve, ki[:, hh, :], kgt[:, hh, tc0:tc0 + 128], en[:, hh * 128:(hh + 1) * 128], ALU.mult,
                               [kgt, en], [ki])
                        for h in range(4):
                            hh, jj = h // 2, h % 2
                            b0 = 64 * jj
                            psc = pS.next()
                            mm(psc[:, 0:128], ki[b0:b0 + 64, hh, :], qd[b0:b0 + 64, hh, :], True, True, [ki, qd], [psc])
                            at = at_r.next()
                            tt(dve, at[:, :], psc[:, 0:128], gmaskb[:, :], ALU.mult, [psc, gmaskb], [at])
                            po = pO.next()
                            mm(po[:, 0:128], vgt[:, j4, h * 128:(h + 1) * 128], at[:, :], True, False, [vgt, at], [po])
                            mm(po[:, 0:64], Sb[h][b0:b0 + 64, :], qd[b0:b0 + 64, hh, 0:64], False, False,
                               [Sb[h], qd], [po])
                            pst = pS.next()
                            mm(pst[:, 0:128], kd[0:64, hh * 128:(hh + 1) * 128], vgt[0:64, j4, h * 128:(h + 1) * 128],
                               True, True, [kd, vgt], [pst])
                            s1 = S32_r[h].next()
                            stt(s1[b0:b0 + 64, :], S32[h][b0:b0 + 64, :], eb[b0:b0 + 64, hh * 128 + 63:hh * 128 + 64],
                                pst[b0:b0 + 64, 0:128], ALU.mult, ALU.add, [S32[h], eb, pst], [s1])
                            sb1 = Sb_r[h].next()
                            actf(sb1[b0:b0 + 64, :], s1[b0:b0 + 64, :], AF.Copy, [s1], [sb1])
                            mm(po[:, 64:128], sb1[b0:b0 + 64, :], qd[b0:b0 + 64, hh, 64:128], False, True,
                               [sb1, qd], [po])
                            pst2 = pS.next()
                            mm(pst2[:, 0:128], kd[64:128, hh * 128:(hh + 1) * 128],
                               vgt[64:128, j4, h * 128:(h + 1) * 128], True, True, [kd, vgt], [pst2])
                            s2 = S32_r[h].next()
                            stt(s2[b0:b0 + 64, :], s1[b0:b0 + 64, :], eb[b0:b0 + 64, hh * 128 + 127:hh * 128 + 128],
                                pst2[b0:b0 + 64, 0:128], ALU.mult, ALU.add, [s1, eb, pst2], [s2])
                            sb2 = Sb_r[h].next()
                            actf(sb2[b0:b0 + 64, :], s2[b0:b0 + 64, :], AF.Copy, [s2], [sb2])
                            S32[h] = s2
                            Sb[h] = sb2
                            actf(og[:, h, tc0:tc0 + 128], po[:, 0:128], AF.Copy, [po], [og])
                    for h in range(4):
                        sq = sq_r.next()
                        actf(sq[:, :], og[:, h, :], AF.Square, [og], [sq])
                        pn = pA.next()
                        mm(pn[:, :], onesb[:, :], sq[:, :], True, True, [onesb, sq], [pn])
                        rs = f_r.next()
                        actf(rs[:, :], pn[:, :], AF.Sqrt, [pn], [rs], scale=1.0 / 128.0, bias=EPS)
                        rs2 = f_r.next()
                        recip(rs2[:, :], rs[:, :], [rs], [rs2])
                        sl = f_r.next()
                        actf(sl[:, :], brt[:, h, :], AF.Silu, [brt], [sl])
                        t1 = f_r.next()
                        stt(t1[:, :], og[:, h, :], glag[:, 0:1], rs2[:, :], ALU.mult, ALU.mult, [og, glag, rs2], [t1])
                        tt(dve, hT[:, 4 + h, t0g:t0g + 512], t1[:, :], sl[:, :], ALU.mult, [t1, sl], [hT])
                k.barrier()

        def emit_moe(layer):
            with ExitStack() as st:
                wg_r = Ring([k.sb(st, "me_wg%d" % i, [128, 8, DEXP], BF16) for i in range(2)])
                wu_r = Ring([k.sb(st, "me_wu%d" % i, [128, 8, DEXP], BF16) for i in range(2)])
                wd_r = Ring([k.sb(st, "me_wd%d" % i, [128, 4, D], BF16) for i in range(2)])
                yacc = k.sb(st, "me_yacc", [128, 8, D], F32)
                aT_r = Ring([k.sb(st, "me_aT%d" % i, [128, 4, 512], BF16) for i in range(2)])
                sg_r = Ring([k.sb(st, "me_sg%d" % i, [128, 512], BF16) for i in range(3)])
                xt_r = Ring([k.sb(st, "me_xt%d" % i, [128, D], F32) for i in range(2)])
                pg_r = Ring([k.ps(st, "me_pg%d" % i, [128, 512], F32) for i in range(2)])
                pu_r = Ring([k.ps(st, "me_pu%d" % i, [128, 512], F32) for i in range(2)])
                py_r = Ring([k.ps(st, "me_py%d" % i, [128, D], F32) for i in range(2)])
                for qtr in range(4):
                    memset(dve, yacc[:, :, :], 0.0, [yacc])
                    jobs = [(e, tg) for e in range(NEXP) for tg in range(2)]
                    wts = {}

                    def load_w(e):
                        wg = wg_r.next()
                        wu = wu_r.next()
                        wd = wd_r.next()
                        k.dma(k.q_pool, wg[:, :, :],
                              I["expert_w_gate"][layer, e].rearrange("(kc p) f -> p kc f", p=128), writes=[wg])
                        k.dma(k.q_pool, wu[:, :, :],
                              I["expert_w_up"][layer, e].rearrange("(kc p) f -> p kc f", p=128), writes=[wu])
                        k.dma(k.q_pool, wd[:, :, :],
                              I["expert_w_down"][layer, e].rearrange("(fc p) n -> p fc n", p=128), writes=[wd])
                        wts[e] = (wg, wu, wd)

                    acts = {}

                    def emit_gu(e, tg):
                        wg, wu, wd = wts[e]
                        G = qtr * 2 + tg
                        aT = aT_r.next()
                        for fc in range(4):
                            pg = pg_r.next()
                            pu = pu_r.next()
                            for kc in range(8):
                                mm(pg[:, :], wg[:, kc, fc * 128:(fc + 1) * 128], hT[:, kc, G * 512:(G + 1) * 512],
                                   kc == 0, kc == 7, [wg, hT], [pg])
                            for kc in range(8):
                                mm(pu[:, :], wu[:, kc, fc * 128:(fc + 1) * 128], hT[:, kc, G * 512:(G + 1) * 512],
                                   kc == 0, kc == 7, [wu, hT], [pu])
                            sg = sg_r.next()
                            actf(sg[:, :], pg[:, :], AF.Silu, [pg], [sg])
                            tt(dve, aT[:, fc, :], sg[:, :], pu[:, :], ALU.mult, [sg, pu], [aT])
                        acts[(e, tg)] = aT

                    def emit_down(e, tg):
                        wg, wu, wd = wts[e]
                        aT = acts.pop((e, tg))
                        for t4 in range(4):
                            ti = tg * 4 + t4
                            T = qtr * 8 + ti
                            py = py_r.next()
                            for nh in range(2):
                                for fc in range(4):
                                    mm(py[:, nh * 512:(nh + 1) * 512], aT[:, fc, t4 * 128:(t4 + 1) * 128],
                                       wd[:, fc, nh * 512:(nh + 1) * 512], fc == 0, fc == 3, [aT, wd], [py])
                            stt(yacc[:, ti, :], py[:, :], cmb[:, T, e:e + 1], yacc[:, ti, :], ALU.mult, ALU.add,
                                [py, cmb, yacc], [yacc])

                    load_w(0)
                    for idx, (e, tg) in enumerate(jobs):
                        emit_gu(e, tg)
                        if idx > 0:
                            emit_down(*jobs[idx - 1])
                        if tg == 0 and e + 1 < NEXP:
                            load_w(e + 1)
                    emit_down(*jobs[-1])
                    for ti in range(8):
                        T = qtr * 8 + ti
                        xt = xt_r.next()
                        k.dma(k.q_sp, xt[:, :], XS[T * 128:(T + 1) * 128, :], reads=[xres[T]], writes=[xt])
                        tt(dve, yacc[:, ti, :], yacc[:, ti, :], gbc2[:, :], ALU.mult, [yacc, gbc2], [yacc])
                        tt(dve, xt[:, :], xt[:, :], yacc[:, ti, :], ALU.add, [xt, yacc], [xt])
                        k.dma(k.q_sp, XS[T * 128:(T + 1) * 128, :], xt[:, :], reads=[xt], writes=[xres[T]])
                k.barrier()

        def emit_final(do_norm):
            with ExitStack() as st:
                xt_r = Ring([k.sb(st, "f_xt%d" % i, [128, D], F32) for i in range(3)])
                xo_r = Ring([k.sb(st, "f_xo%d" % i, [128, D], F32) for i in range(2)])
                junk = k.sb(st, "f_junk", [128, D], BF16)
                sm_r = Ring([k.sb(st, "f_sm%d" % i, [128, 4], F32) for i in range(4)])
                gf = k.sb(st, "f_gf", [128, D], F32)
                k.dma(k.q_sp, gf[:, :], I["norm_final"][0].partition_broadcast(128), writes=[gf])
                evs = []
                for t in range(NT):
                    xt = xt_r.next()
                    k.dma(k.q_sp, xt[:, :], XS[t * 128:(t + 1) * 128, :], reads=[xres[t]], writes=[xt])
                    if do_norm:
                        sm = sm_r.next()
                        actf(junk[:, :], xt[:, :], AF.Square, [xt], [junk, sm], accum_out=sm[:, 0:1])
                        ts(dve, sm[:, 1:2], sm[:, 0:1], 1.0 / D, EPS, ALU.mult, ALU.add, [sm], [sm])
                        actf(sm[:, 2:3], sm[:, 1:2], AF.Sqrt, [sm], [sm])
                        recip(sm[:, 3:4], sm[:, 2:3], [sm], [sm])
                        xo = xo_r.next()
                        stt(xo[:, :], xt[:, :], sm[:, 3:4], gf[:, :], ALU.mult, ALU.mult, [xt, sm, gf], [xo])
                        src = xo
                    else:
                        src = xt
                    evs.append(k.dma(k.q_sp, OUT[t * 128:(t + 1) * 128, :], src[:, :], reads=[src]))
                for ev in evs:
                    sp.wait(ev)

        xsrc = I["x"]
        done = False
        if stop == ("evenonly",):
            emit_even_mixer(0, 0)
            emit_final(False)
            done = True
            nlayers = 0
        if stop == ("oddonly",):
            emit_odd_mixer()
            emit_final(False)
            done = True
            nlayers = 0
        for l in range(nlayers):
            emit_mod(l)
            emit_norm(xsrc, gmod1, modT[:, 0:8], l, router=False)
            ie = l // 2
            if 'skipmix' in flags:
                pass
            elif l % 2 == 0:
                fj = [(c * 128, 128, c, 0.125) for c in range(4)]
                fj += [(512 + c * 128, 128, 4 + c, 1.0) for c in range(4)]
                fj += [(1536 + c * 128, 128, 12 + c, 0.125) for c in range(2)]
                fj += [(1792 + c * 128, 128, 14 + c, 1.0) for c in range(2)]
                fj += [(2560 + c * 128, 128, 20 + c, 1.0) for c in range(4)]
                fj += [(3072, 16, 24, 1.0)]
                tj = [(1024, 512, 0), (1792, 256, 512), (2048, 512, 768)]
                emit_inproj(I["even_w_in"][ie], EVEN_COLS, fj, tj)
                emit_even_mixer(ie, l)
                emit_outproj(I["even_w_out"][ie], gbc1, xsrc)
            else:
                fj = [(c * 128, 128, c, 0.125) for c in range(8)]
                fj += [(1024 + c * 128, 128, 8 + c, 1.0) for c in range(8)]
                tj = [(2048, 512, 0), (2560, 512, 512)]
                emit_inproj(I["odd_w_in"][ie], 3072, fj, tj)
                emit_odd_mixer()
                emit_outproj(I["odd_w_out"][ie], gbc1, xsrc)
            if 'skipmix' not in flags:
                xsrc = XS
            if stop == ("mix", l):
                emit_final(False)
                done = True
                break
            emit_norm(xsrc, gmod2, modT[:, 24:32], l, router=True)
            if 'nomoe' not in flags:
                emit_moe(l)
            if stop == ("ffn", l):
                emit_final(False)
                done = True
                break
        if not done:
            emit_final(True)
        build.stats = (k.n_inst, k.nsem)
    return nc


_CACHE = {}


def _get_nc(nlayers=DEPTH, stop=None):
    key = (nlayers, stop)
    if key not in _CACHE:
        _CACHE[key] = build(nlayers, stop)
    return _CACHE[key]


def make_in_maps(inputs, cores):
    consts = make_consts()
    shared = {}
    for nm in WEIGHT_NAMES:
        a = np.ascontiguousarray(np.asarray(inputs[nm], dtype=np.float32))
        shared[nm] = a.reshape(WEIGHT_SHAPES[nm])
    shared.update(consts)
    x = np.asarray(inputs["x"], dtype=np.float32)
    c = np.asarray(inputs["c"], dtype=np.float32)
    maps = []
    for b in cores:
        m = dict(shared)
        m["x"] = np.ascontiguousarray(x[b])
        m["c"] = np.ascontiguousarray(c[b:b + 1])
        maps.append(m)
    return maps


def kernel(**inputs):
    nc = _get_nc()
    in_maps = make_in_maps(inputs, list(range(8)))
    res = run_bass_kernel_spmd(nc, in_maps, core_ids=list(range(8)))
    return np.stack([np.asarray(r["out"]) for r in res.results], axis=0).astype(np.float32)
```

```python
import math
from contextlib import ExitStack
import numpy as np
import concourse.bass as bass
import concourse.mybir as mybir
from concourse.bass_utils import run_bass_kernel_spmd

F32 = mybir.dt.float32
BF16 = mybir.dt.bfloat16
AF = mybir.ActivationFunctionType
ALU = mybir.AluOpType
AX = mybir.AxisListType

S = 4096
D = 1024
NT = 32
DEPTH = 4
EPS = 1e-6
EVEN_COLS = 3088
NEXP = 32
DEXP = 512
EPOCH = 12000


class Res:
    __slots__ = ("w", "r", "name")

    def __init__(self, name=""):
        self.w = None
        self.r = []
        self.name = name


class Tile:
    def __init__(self, ap, name=""):
        self.t = ap
        self.res = Res(name)

    def __getitem__(self, idx):
        return self.t[idx]


class Eng:
    def __init__(self, k, name, h, is_pe=False):
        self.k = k
        self.name = name
        self.h = h
        self.is_pe = is_pe
        self.sem = k.newsem(name + "_s0")
        self.count = 0
        self.nep = 0
        self.seen = {}
        self.hist = [(self.sem, 0)]

    def tick(self, inst):
        if self.count >= EPOCH:
            self.nep += 1
            self.sem = self.k.newsem("%s_s%d" % (self.name, self.nep))
            self.count = 0
        self.count += 1
        inst.then_inc(self.sem, 1)
        return (self.sem, self.count, self.name)

    def cur(self):
        return (self.sem, self.count, self.name)

    def wait(self, ev):
        sem, val, _ = ev
        if val <= 0:
            return
        key = id(sem)
        if self.seen.get(key, 0) >= val:
            return
        self.h.wait_ge(sem, val)
        self.seen[key] = val


class DmaQ:
    def __init__(self, k, name, eng, nslots=8):
        self.k = k
        self.eng = eng
        self.name = name
        self.sems = [k.newsem("%s_d%d" % (name, i)) for i in range(nslots)]
        self.vals = [0] * nslots
        self.i = 0

    def issue(self, emit, deps):
        s = self.i % len(self.sems)
        self.i += 1
        sem = self.sems[s]
        if self.vals[s] > 0:
            self.eng.wait((sem, self.vals[s], "dma"))
        for ev in deps:
            self.eng.wait(ev)
        if self.vals[s] >= 16 * 1500:
            sem = self.k.newsem("%s_d%d_%d" % (self.name, s, self.i))
            self.sems[s] = sem
            self.vals[s] = 0
        inst = emit()
        self.vals[s] += 16
        inst.then_inc(sem, 16)
        return (sem, self.vals[s], "dma")


class K:
    def __init__(self, nc, stack):
        self.nc = nc
        self.stack = stack
        self.nsem = 0
        self.pe = Eng(self, "pe", nc.tensor, is_pe=True)
        self.act = Eng(self, "act", nc.scalar)
        self.dve = Eng(self, "dve", nc.vector)
        self.pool = Eng(self, "pool", nc.gpsimd)
        self.sp = Eng(self, "sp", nc.sync)
        self.engs = [self.pe, self.act, self.dve, self.pool, self.sp]
        self.q_sp = DmaQ(self, "qsp", self.sp, 12)
        self.q_pool = DmaQ(self, "qpool", self.pool, 8)
        self.qs = [self.q_sp, self.q_pool]
        self.n_inst = 0

    def newsem(self, name):
        self.nsem += 1
        return self.stack.enter_context(self.nc.semaphore(name))

    def sb(self, st, name, shape, dtype):
        self.uid = getattr(self, "uid", 0) + 1
        name = "%s_u%d" % (name, self.uid)
        t = st.enter_context(self.nc.sbuf_tensor(name, list(shape), dtype))
        return Tile(t, name)

    def ps(self, st, name, shape, dtype=F32):
        self.uid = getattr(self, "uid", 0) + 1
        name = "%s_u%d" % (name, self.uid)
        t = st.enter_context(self.nc.psum_tensor(name, list(shape), dtype))
        return Tile(t, name)

    @staticmethod
    def _resof(x):
        return x.res if isinstance(x, Tile) else x

    def _deps(self, reads, writes):
        deps = []
        for r in reads:
            r = self._resof(r)
            if r.w is not None:
                deps.append(r.w)
        for w in writes:
            w = self._resof(w)
            if w.w is not None:
                deps.append(w.w)
            deps.extend(w.r)
        return deps

    def _commit(self, ev, reads, writes):
        for r in reads:
            r = self._resof(r)
            r.r = [e for e in r.r if e[0] is not ev[0]]
            r.r.append(ev)
        for w in writes:
            w = self._resof(w)
            w.w = ev
            w.r = []

    def op(self, eng, emit, reads=(), writes=()):
        for ev in self._deps(reads, writes):
            if eng.is_pe and ev[2] == "pe":
                continue
            eng.wait(ev)
        inst = emit()
        ev = eng.tick(inst)
        self._commit(ev, reads, writes)
        self.n_inst += 1
        return ev

    def dma(self, q, out, in_, reads=(), writes=(), **kw):
        deps = self._deps(reads, writes)
        ev = q.issue(lambda: q.eng.h.dma_start(out=out, in_=in_, **kw), deps)
        self._commit(ev, reads, writes)
        self.n_inst += 1
        return ev

    def barrier(self):
        evs = [e.cur() for e in self.engs]
        for q in self.qs:
            for s, v in zip(q.sems, q.vals):
                evs.append((s, v, "dma"))
        for e in self.engs:
            for ev in evs:
                if ev[0] is e.sem:
                    continue
                e.wait(ev)


class Ring:
    def __init__(self, tiles):
        self.tiles = tiles
        self.i = 0

    def next(self):
        t = self.tiles[self.i % len(self.tiles)]
        self.i += 1
        return t


def _t5_bucket_np(rel):
    nb = 16
    max_exact = 8
    ret = np.where(rel > 0, nb, 0)
    n = np.abs(rel)
    nf = np.maximum(n, 1).astype(np.float32)
    large = max_exact + (np.log(nf / max_exact) / np.float32(math.log(128 / max_exact))
                         * (nb - max_exact)).astype(np.int32)
    large = np.minimum(large, nb - 1)
    return ret + np.where(n < max_exact, n, large)


DELTAS = [-128, 0, 128, 256, 384]
NBLK = 48
BSZ = 512
NSLOT = NBLK * BSZ
R0 = 511
NF = 1280


def make_consts():
    c = {}
    c["c_ident"] = np.eye(128, dtype=np.float32)
    c["c_exch"] = np.ascontiguousarray(np.eye(128, dtype=np.float32)[::-1])
    i = np.arange(128)[:, None]
    j = np.arange(128)[None, :]
    c["c_mlt"] = (i < j).astype(np.float32)
    c["c_uneg"] = -(i >= j).astype(np.float32)
    same = (i // 64) == (j // 64)
    c["c_gtri"] = (-(1.0 / 16.0) * (same & (i <= j))).astype(np.float32)
    c["c_gm2"] = (-(1.0 / 16.0) * (same & (i > j))).astype(np.float32)
    c["c_gmask"] = (same & (i <= j)).astype(np.float32)
    n = np.arange(NF)
    bk = _t5_bucket_np(R0 - n)
    oh = np.zeros((32, NF), np.float32)
    oh[bk, n] = 1.0
    oh[:, 1151:] = 0.0
    c["c_ohb"] = oh
    nm = np.zeros((5, 128, 512), np.float32)
    for di, dl in enumerate(DELTAS):
        kk = dl + np.arange(128)[:, None]
        qq = np.arange(512)[None, :]
        allowed = (kk // 64) <= (qq // 64)
        nm[di] = np.where(allowed, 0.0, -30000.0)
    c["c_negmask"] = nm
    p = np.arange(128, dtype=np.float32)[:, None]
    c["c_iog"] = (p + 128.0 * np.arange(8, dtype=np.float32)[None, :]).astype(np.float32)
    c["c_iob"] = np.tile(np.arange(NBLK, dtype=np.float32)[None, :], (128, 1))
    return c


WEIGHT_NAMES = ["w_ada", "b_ada", "norm_mix", "norm_ffn", "norm_final", "rel_bias",
                "even_w_in", "even_lambda", "even_subln", "even_w_gk2", "even_b_gk", "even_gla_norm",
                "even_w_out", "odd_w_in", "odd_w_out", "router_group_w", "router_group_b",
                "router_expert_w", "router_expert_b", "expert_w_gate", "expert_w_up", "expert_w_down"]
WEIGHT_SHAPES = {
    "w_ada": [4, 1024, 6144], "b_ada": [4, 6144], "norm_mix": [4, 1024], "norm_ffn": [4, 1024],
    "norm_final": [1, 1024], "rel_bias": [32, 4], "even_w_in": [2, 1024, 3088], "even_lambda": [2, 256],
    "even_subln": [2, 128], "even_w_gk2": [2, 16, 256], "even_b_gk": [2, 256], "even_gla_norm": [2, 128],
    "even_w_out": [2, 1024, 1024], "odd_w_in": [2, 1024, 3072], "odd_w_out": [2, 1024, 1024],
    "router_group_w": [4, 1024, 4], "router_group_b": [4, 4], "router_expert_w": [4, 1024, 32],
    "router_expert_b": [4, 32], "expert_w_gate": [4, 32, 1024, 512], "expert_w_up": [4, 32, 1024, 512],
    "expert_w_down": [4, 32, 512, 1024],
}


def build(nlayers=DEPTH, stop=None, flags=()):
    SPARSE = 'dense' not in flags
    nc = bass.Bass("TRN2", target_bir_lowering=False)
    I = {}
    I["x"] = nc.dram_tensor("x", [S, D], F32, kind="ExternalInput").ap()
    I["c"] = nc.dram_tensor("c", [1, D], F32, kind="ExternalInput").ap()
    for nm in WEIGHT_NAMES:
        I[nm] = nc.dram_tensor(nm, WEIGHT_SHAPES[nm], F32, kind="ExternalInput").ap()
    consts = make_consts()
    for nm, v in consts.items():
        I[nm] = nc.dram_tensor(nm, list(v.shape), F32, kind="ExternalInput").ap()
    OUT = nc.dram_tensor("out", [S, D], F32, kind="ExternalOutput").ap()
    XS = nc.dram_tensor("xs_scr", [S, D], F32, kind="Internal").ap()
    FB = nc.dram_tensor("fb_scr", [25, 128, S], BF16, kind="Internal").ap()
    TB = nc.dram_tensor("tb_scr", [S, 1280], BF16, kind="Internal").ap()
    FS_T = nc.dram_tensor("fs_scr", [4, NF], F32, kind="Internal")
    FS = FS_T.ap()
    XN = nc.dram_tensor("xn_scr", [S, D], F32, kind="Internal").ap()
    XG = nc.dram_tensor("xg_scr", [NSLOT, D], F32, kind="Internal").ap()
    YS = nc.dram_tensor("ys_scr", [NSLOT, D], F32, kind="Internal").ap()

    with ExitStack() as gst:
        k = K(nc, gst)
        pe, act, dve, pool, sp = k.pe, k.act, k.dve, k.pool, k.sp

        def mm(out, lhsT, rhs, start, stop, reads, writes):
            k.op(pe, lambda: nc.tensor.matmul(out, lhsT=lhsT, rhs=rhs, start=start, stop=stop), reads, writes)

        def tr(out, in_, ident, reads, writes):
            k.op(pe, lambda: nc.tensor.transpose(out=out, in_=in_, identity=ident), reads, writes)

        def actf(out, in_, func, reads, writes, **kw):
            k.op(act, lambda: nc.scalar.activation(out=out, in_=in_, func=func, **kw), reads, writes)

        def tt(eng, out, in0, in1, op, reads, writes):
            k.op(eng, lambda: eng.h.tensor_tensor(out=out, in0=in0, in1=in1, op=op), reads, writes)

        def ts(eng, out, in0, s1, s2, op0, op1, reads, writes):
            if op1 is None:
                k.op(eng, lambda: eng.h.tensor_scalar(out=out, in0=in0, scalar1=s1, scalar2=None, op0=op0),
                     reads, writes)
            else:
                k.op(eng, lambda: eng.h.tensor_scalar(out=out, in0=in0, scalar1=s1, scalar2=s2, op0=op0, op1=op1),
                     reads, writes)

        def stt(out, in0, scalar, in1, op0, op1, reads, writes):
            k.op(dve, lambda: nc.vector.scalar_tensor_tensor(out=out, in0=in0, scalar=scalar, in1=in1,
                                                             op0=op0, op1=op1), reads, writes)

        def cp(eng, out, in_, reads, writes):
            k.op(eng, lambda: eng.h.tensor_copy(out=out, in_=in_), reads, writes)

        def recip(out, in_, reads, writes):
            k.op(dve, lambda: nc.vector.reciprocal(out=out, in_=in_), reads, writes)

        def memset(eng, t, val, writes):
            k.op(eng, lambda: eng.h.memset(t, val), (), writes)

        def rmax(out, in_, reads, writes):
            k.op(dve, lambda: nc.vector.tensor_reduce(out=out, in_=in_, axis=AX.X, op=ALU.max), reads, writes)

        xres = [Res("x%d" % i) for i in range(NT)]
        fbres = [Res("fb%d" % i) for i in range(25)]
        tbres = Res("tb")
        fsres = Res("fs")

        ident32 = k.sb(gst, "ident32", [128, 128], F32)
        ones32 = k.sb(gst, "ones32", [128, 128], F32)
        onesb = k.sb(gst, "onesb", [128, 128], BF16)
        negonesb = k.sb(gst, "negonesb", [128, 128], BF16)
        zerob = k.sb(gst, "zerob", [128, 128], BF16)
        identb = k.sb(gst, "identb", [128, 128], BF16)
        k.dma(k.q_sp, ident32[:, :], I["c_ident"][:, :], writes=[ident32])
        memset(dve, ones32[:, :], 1.0, [ones32])
        memset(dve, onesb[:, :], 1.0, [onesb])
        memset(dve, negonesb[:, :], -1.0, [negonesb])
        memset(dve, zerob[:, :], 0.0, [zerob])
        cp(dve, identb[:, :], ident32[:, :], [ident32], [identb])

        modT = k.sb(gst, "modT", [128, 64], F32)
        gmod1 = k.sb(gst, "gmod1", [128, 8], F32)
        gmod2 = k.sb(gst, "gmod2", [128, 8], F32)
        gbc1 = k.sb(gst, "gbc1", [128, D], F32)
        gbc2 = k.sb(gst, "gbc2", [128, D], F32)
        scT = k.sb(gst, "scT", [128, 8], F32)
        cmb = k.sb(gst, "cmb", [128, NT, NEXP], F32)
        hT = k.sb(gst, "hT", [128, 8, S], BF16)
        r_ohg = k.sb(gst, "r_ohg", [128, NT, 4], F32)
        r_oh1 = k.sb(gst, "r_oh1", [128, NT, 8], F32)
        r_oh2 = k.sb(gst, "r_oh2", [128, NT, 8], F32)
        r_v = k.sb(gst, "r_v", [128, 12, NT], F32)
        s12u = k.sb(gst, "s12u", [128, 2, NT], mybir.dt.uint32)
        idxg = k.sb(gst, "idxg", [128, NBLK, 8], mybir.dt.uint32)
        idxd = k.sb(gst, "idxd", [128, NBLK, 4], mybir.dt.uint32)
        xnres = Res("xn")
        xgres = Res("xg")
        ysres = [Res("ys%d" % i) for i in range(NBLK)]

        def idma(out, out_off, in_, in_off, reads, writes):
            deps = k._deps(reads, writes)
            ev = k.q_pool.issue(lambda: nc.gpsimd.indirect_dma_start(out=out, out_offset=out_off, in_=in_,
                                                                     in_offset=in_off), deps)
            k._commit(ev, reads, writes)
            k.n_inst += 1
            return ev

        def emit_mod(l):
            with ExitStack() as st:
                crow = k.sb(st, "crow", [1, D], F32)
                sc_bc = k.sb(st, "sc_bc", [128, 8, 128], F32)
                wseg = Ring([k.sb(st, "wseg%d" % i, [128, 8, 512], F32) for i in range(2)])
                brow = Ring([k.sb(st, "brow%d" % i, [1, 512], F32) for i in range(2)])
                nrow = k.sb(st, "nrow", [1, 2 * D], F32)
                pm = k.ps(st, "pm", [128, 512], F32)
                pg = Ring([k.ps(st, "pg%d" % i, [128, 512], F32) for i in range(2)])
                k.dma(k.q_sp, crow[:, :], I["c"][:, :], writes=[crow])
                for kc in range(8):
                    mm(pm[:, kc:kc + 1], crow[0:1, kc * 128:(kc + 1) * 128], ones32[0:1, 0:1], True, True,
                       [crow, ones32], [pm])
                actf(scT[:, :], pm[:, 0:8], AF.Silu, [pm], [scT])
                for kc in range(8):
                    actf(sc_bc[:, kc, :], ones32[:, :], AF.Copy, [ones32, scT], [sc_bc], scale=scT[:, kc:kc + 1])
                wv = I["w_ada"][l].rearrange("(kc p) n -> p kc n", p=128)
                for seg in range(6):
                    for half in range(2):
                        n0 = seg * 1024 + half * 512
                        w = wseg.next()
                        b = brow.next()
                        k.dma(k.q_sp, w[:, :, :], wv[:, :, n0:n0 + 512], writes=[w])
                        k.dma(k.q_sp, b[:, :], I["b_ada"][l:l + 1, n0:n0 + 512], writes=[b])
                        if seg in (2, 5):
                            p = pg.next()
                            for kc in range(8):
                                mm(p[:, :], sc_bc[:, kc, :], w[:, kc, :], kc == 0, False, [sc_bc, w], [p])
                            mm(p[:, :], ones32[0:1, :], b[0:1, :], False, True, [ones32, b], [p])
                            g = gbc1 if seg == 2 else gbc2
                            cp(dve, g[:, half * 512:(half + 1) * 512], p[:, :], [p], [g])
                        else:
                            for j in range(4):
                                col = seg * 8 + half * 4 + j
                                for kc in range(8):
                                    mm(pm[:, col:col + 1], w[:, kc, j * 128:(j + 1) * 128], scT[:, kc:kc + 1],
                                       kc == 0, False, [w, scT], [pm])
                                mm(pm[:, col:col + 1], b[0:1, j * 128:(j + 1) * 128], ones32[0:1, 0:1], False, True,
                                   [b, ones32], [pm])
                k.dma(k.q_sp, nrow[:, 0:D], I["norm_mix"][l:l + 1, :], writes=[nrow])
                k.dma(k.q_sp, nrow[:, D:2 * D], I["norm_ffn"][l:l + 1, :], writes=[nrow])
                for j in range(16):
                    mm(pm[:, 48 + j:49 + j], nrow[0:1, j * 128:(j + 1) * 128], ones32[0:1, 0:1], True, True,
                       [nrow, ones32], [pm])
                cp(dve, modT[:, :], pm[:, 0:64], [pm], [modT])
                stt(gmod1[:, :], modT[:, 8:16], 1.0, modT[:, 48:56], ALU.add, ALU.mult, [modT], [gmod1])
                stt(gmod2[:, :], modT[:, 32:40], 1.0, modT[:, 56:64], ALU.add, ALU.mult, [modT], [gmod2])
                k.barrier()

        def emit_norm(xsrc, gmod, shiftc, layer, router):
            with ExitStack() as st:
                xt_r = Ring([k.sb(st, "n_xt%d" % i, [128, D], F32) for i in range(4)])
                xs_r = Ring([k.sb(st, "n_xs%d" % i, [128, D], F32) for i in range(3)])
                junk = k.sb(st, "n_junk", [128, D], BF16)
                sm_r = Ring([k.sb(st, "n_sm%d" % i, [128, 4], F32) for i in range(6)])
                ps_r = Ring([k.ps(st, "n_ps%d" % i, [128, D], F32) for i in range(2)])
                if router:
                    h32_r = Ring([k.sb(st, "n_h32%d" % i, [128, 8, 128], F32) for i in range(2)])
                    wr32 = k.sb(st, "n_wr32", [128, 8, 36], F32)
                    brr = k.sb(st, "n_brr", [1, 36], F32)
                    plg_r = Ring([k.ps(st, "n_plg%d" % i, [128, 512], F32) for i in range(2)])
                    lgall = k.sb(st, "n_lgall", [128, NT, 36], F32)
                    k.dma(k.q_sp, wr32[:, :, 0:4],
                          I["router_group_w"][layer].rearrange("(kc p) n -> p kc n", p=128), writes=[wr32])
                    k.dma(k.q_sp, wr32[:, :, 4:36],
                          I["router_expert_w"][layer].rearrange("(kc p) n -> p kc n", p=128), writes=[wr32])
                    k.dma(k.q_sp, brr[:, 0:4], I["router_group_b"][layer:layer + 1, :], writes=[brr])
                    k.dma(k.q_sp, brr[:, 4:36], I["router_expert_b"][layer:layer + 1, :], writes=[brr])
                state = {}

                def s1(t):
                    xt = xt_r.next()
                    k.dma(k.q_sp, xt[:, :], xsrc[t * 128:(t + 1) * 128, :], reads=[xres[t]], writes=[xt])
                    sm = sm_r.next()
                    actf(junk[:, :], xt[:, :], AF.Square, [xt], [junk, sm], accum_out=sm[:, 0:1])
                    state[t] = (xt, sm)

                def s2(t):
                    xt, sm = state[t]
                    ts(dve, sm[:, 1:2], sm[:, 0:1], 1.0 / D, EPS, ALU.mult, ALU.add, [sm], [sm])
                    actf(sm[:, 2:3], sm[:, 1:2], AF.Sqrt, [sm], [sm])
                    recip(sm[:, 3:4], sm[:, 2:3], [sm], [sm])
                    xs = xs_r.next()
                    ts(dve, xs[:, :], xt[:, :], sm[:, 3:4], None, ALU.mult, None, [xt, sm], [xs])
                    if router and SPARSE:
                        k.dma(k.q_sp, XN[t * 128:(t + 1) * 128, :], xs[:, :], reads=[xs], writes=[xnres])
                    state[t] = xs

                def s3(t):
                    xs = state.pop(t)
                    ps = ps_r.next()
                    for kc in range(8):
                        tr(ps[:, kc * 128:(kc + 1) * 128], xs[:, kc * 128:(kc + 1) * 128], ident32[:, :],
                           [xs, ident32], [ps])
                    for kc in range(8):
                        actf(hT[:, kc, t * 128:(t + 1) * 128], ps[:, kc * 128:(kc + 1) * 128], AF.Identity,
                             [ps, gmod, modT], [hT], scale=gmod[:, kc:kc + 1], bias=shiftc[:, kc:kc + 1])
                    if router:
                        h32 = h32_r.next()
                        for kc in range(8):
                            actf(h32[:, kc, :], ps[:, kc * 128:(kc + 1) * 128], AF.Identity,
                                 [ps, gmod, modT], [h32], scale=gmod[:, kc:kc + 1], bias=shiftc[:, kc:kc + 1])
                        plg = plg_r.next()
                        for kc in range(8):
                            mm(plg[:, 0:36], h32[:, kc, :], wr32[:, kc, :], kc == 0, False, [h32, wr32], [plg])
                        mm(plg[:, 0:36], ones32[0:1, :], brr[0:1, :], False, True, [ones32, brr], [plg])
                        cp(dve, lgall[:, t, :], plg[:, 0:36], [plg], [lgall])

                for i in range(NT + 2):
                    if i < NT:
                        s1(i)
                    if 1 <= i <= NT:
                        s2(i - 1)
                    if i >= 2:
                        s3(i - 2)
                if router:
                    def bc(ap2, n):
                        return ap2.unsqueeze(2).to_broadcast([128, NT, n])
                    f4 = k.sb(st, "r_f4", [128, NT, 4], F32)
                    ohg = r_ohg
                    v = r_v
                    el = k.sb(st, "r_el", [128, NT, 8], F32)
                    t8 = k.sb(st, "r_t8", [128, NT, 8], F32)
                    oh1 = r_oh1
                    oh2 = r_oh2
                    elm = k.sb(st, "r_elm", [128, NT, 8], F32)
                    cg = k.sb(st, "r_cg", [128, NT, 8], F32)
                    RR = [f4, ohg, v, el, t8, oh1, oh2, elm, cg, lgall]
                    gm, gs, gval, l1, l2, dd, ee, den, w1, W1, W2 = [v[:, i, :] for i in range(11)]

                    def red(out, in_, op):
                        k.op(dve, lambda: nc.vector.tensor_reduce(out=out, in_=in_, axis=AX.X, op=op), RR, RR)

                    lgg = lgall[:, :, 0:4]
                    red(gm, lgg, ALU.max)
                    tt(dve, ohg[:, :, :], lgg, bc(gm, 4), ALU.is_equal, RR, RR)
                    tt(dve, f4[:, :, :], lgg, bc(gm, 4), ALU.subtract, RR, RR)
                    actf(f4[:, :, :], f4[:, :, :], AF.Exp, RR, RR)
                    red(gs, f4[:, :, :], ALU.add)
                    recip(gval, gs, RR, RR)
                    for g in range(4):
                        src = lgall[:, :, 4 + 8 * g:12 + 8 * g]
                        if g == 0:
                            tt(dve, el[:, :, :], src, bc(ohg[:, :, g], 8), ALU.mult, RR, RR)
                        else:
                            tt(dve, t8[:, :, :], src, bc(ohg[:, :, g], 8), ALU.mult, RR, RR)
                            tt(dve, el[:, :, :], el[:, :, :], t8[:, :, :], ALU.add, RR, RR)
                    red(l1, el[:, :, :], ALU.max)
                    tt(dve, oh1[:, :, :], el[:, :, :], bc(l1, 8), ALU.is_equal, RR, RR)
                    stt(elm[:, :, :], oh1[:, :, :], -1e30, el[:, :, :], ALU.mult, ALU.add, RR, RR)
                    red(l2, elm[:, :, :], ALU.max)
                    tt(dve, oh2[:, :, :], elm[:, :, :], bc(l2, 8), ALU.is_equal, RR, RR)
                    tt(dve, dd, l2, l1, ALU.subtract, RR, RR)
                    actf(ee, dd, AF.Exp, RR, RR)
                    ts(dve, den, ee, 1.0, None, ALU.add, None, RR, RR)
                    recip(w1, den, RR, RR)
                    tt(dve, W1, w1, gval, ALU.mult, RR, RR)
                    tt(dve, W2, W1, ee, ALU.mult, RR, RR)
                    tt(dve, cg[:, :, :], oh1[:, :, :], bc(W1, 8), ALU.mult, RR, RR)
                    tt(dve, t8[:, :, :], oh2[:, :, :], bc(W2, 8), ALU.mult, RR, RR)
                    tt(dve, cg[:, :, :], cg[:, :, :], t8[:, :, :], ALU.add, RR, RR)
                    for g in range(4):
                        tt(dve, cmb[:, :, 8 * g:8 * g + 8], cg[:, :, :], bc(ohg[:, :, g], 8), ALU.mult, RR, [cmb])
                k.barrier()

        def emit_inproj(wsrc, ncols, fjobs, tjobs):
            with ExitStack() as st:
                W = k.sb(st, "ip_W", [128, 8, ncols], BF16)
                wv = wsrc.rearrange("(kc p) n -> p kc n", p=128)
                c0 = 0
                while c0 < ncols:
                    w_ = min(512, ncols - c0)
                    k.dma(k.q_pool, W[:, :, c0:c0 + w_], wv[:, :, c0:c0 + w_], writes=[W])
                    c0 += w_
                stg_r = Ring([k.sb(st, "ip_stg%d" % i, [128, S], BF16) for i in range(2)])
                stt_r = Ring([k.sb(st, "ip_stt%d" % i, [128, 4, 512], BF16) for i in range(2)])
                ps_r = Ring([k.ps(st, "ip_ps%d" % i, [128, 512], F32) for i in range(4)])
                n = 0
                for (col0, nr, fb, scale) in fjobs:
                    stg = stg_r.next()
                    for tg in range(8):
                        ps = ps_r.next()
                        for kc in range(8):
                            mm(ps[0:nr, :], W[:, kc, col0:col0 + nr], hT[:, kc, tg * 512:(tg + 1) * 512],
                               kc == 0, kc == 7, [W, hT], [ps])
                        if n % 2 == 0:
                            actf(stg[0:nr, tg * 512:(tg + 1) * 512], ps[0:nr, :], AF.Copy, [ps], [stg], scale=scale)
                        else:
                            ts(dve, stg[0:nr, tg * 512:(tg + 1) * 512], ps[0:nr, :], scale, None, ALU.mult, None,
                               [ps], [stg])
                        n += 1
                    k.dma(k.q_sp, FB[fb, 0:nr, :], stg[0:nr, :], reads=[stg], writes=[fbres[fb]])
                for (col0, wd, tcol0) in tjobs:
                    for t4 in range(8):
                        stg = stt_r.next()
                        for j in range(4):
                            t = t4 * 4 + j
                            ps = ps_r.next()
                            for kc in range(8):
                                mm(ps[:, 0:wd], hT[:, kc, t * 128:(t + 1) * 128], W[:, kc, col0:col0 + wd],
                                   kc == 0, kc == 7, [W, hT], [ps])
                            if n % 2 == 0:
                                actf(stg[:, j, 0:wd], ps[:, 0:wd], AF.Copy, [ps], [stg])
                            else:
                                cp(dve, stg[:, j, 0:wd], ps[:, 0:wd], [ps], [stg])
                            n += 1
                        k.dma(k.q_sp,
                              TB[t4 * 512:(t4 + 1) * 512, tcol0:tcol0 + wd].rearrange("(j p) c -> p j c", p=128),
                              stg[:, :, 0:wd], reads=[stg], writes=[tbres])
                k.barrier()

        def emit_outproj(wsrc, gbc, xsrc):
            with ExitStack() as st:
                W = k.sb(st, "op_W", [128, 8, D], BF16)
                wv = wsrc.rearrange("(kc p) n -> p kc n", p=128)
                for h in range(2):
                    k.dma(k.q_pool, W[:, :, h * 512:(h + 1) * 512], wv[:, :, h * 512:(h + 1) * 512], writes=[W])
                for kc in range(8):
                    tt(dve, W[:, kc, :], W[:, kc, :], gbc[:, :], ALU.mult, [W, gbc], [W])
                xt_r = Ring([k.sb(st, "op_xt%d" % i, [128, D], F32) for i in range(3)])
                xn_r = Ring([k.sb(st, "op_xn%d" % i, [128, D], F32) for i in range(2)])
                ps_r = Ring([k.ps(st, "op_ps%d" % i, [128, D], F32) for i in range(3)])
                for t in range(NT):
                    xt = xt_r.next()
                    k.dma(k.q_sp, xt[:, :], xsrc[t * 128:(t + 1) * 128, :], reads=[xres[t]], writes=[xt])
                    ps = ps_r.next()
                    for h in range(2):
                        for kc in range(8):
                            mm(ps[:, h * 512:(h + 1) * 512], hT[:, kc, t * 128:(t + 1) * 128],
                               W[:, kc, h * 512:(h + 1) * 512], kc == 0, kc == 7, [hT, W], [ps])
                    xn = xn_r.next()
                    tt(dve, xn[:, :], xt[:, :], ps[:, :], ALU.add, [xt, ps], [xn])
                    k.dma(k.q_sp, XS[t * 128:(t + 1) * 128, :], xn[:, :], reads=[xn], writes=[xres[t]])
                k.barrier()

        def emit_odd_mixer():
            with ExitStack() as st:
                mltb = k.sb(st, "sb_mlt", [128, 128], BF16)
                unegb = k.sb(st, "sb_uneg", [128, 128], BF16)
                k.dma(k.q_pool, mltb[:, :], I["c_mlt"][:, :], writes=[mltb])
                k.dma(k.q_pool, unegb[:, :], I["c_uneg"][:, :], writes=[unegb])
                qT_r = Ring([k.sb(st, "sb_qT%d" % i, [128, S], BF16) for i in range(2)])
                kT_r = Ring([k.sb(st, "sb_kT%d" % i, [128, S], BF16) for i in range(2)])
                vt_r = Ring([k.sb(st, "sb_vt%d" % i, [128, NT, 128], BF16) for i in range(2)])
                e32_r = Ring([k.sb(st, "sb_e%d" % i, [128, 512], F32) for i in range(6)])
                sp_r = Ring([k.sb(st, "sb_sp%d" % i, [128, 512], BF16) for i in range(10)])
                rb_r = Ring([k.sb(st, "sb_rb%d" % i, [128, 512], BF16) for i in range(10)])
                ab_r = Ring([k.sb(st, "sb_ab%d" % i, [128, 512], BF16) for i in range(6)])
                R32s = [k.sb(st, "sb_R32_%d" % i, [128, 512], F32) for i in range(2)]
                pz_r = Ring([k.ps(st, "sb_pz%d" % i, [128, 512], F32) for i in range(3)])
                pw_r = Ring([k.ps(st, "sb_pw%d" % i, [128, 512], F32) for i in range(3)])
                pos = [k.ps(st, "sb_po%d" % i, [128, 512], F32) for i in range(2)]
                for hp in range(8):
                    qT = qT_r.next()
                    kT = kT_r.next()
                    vt = vt_r.next()
                    k.dma(k.q_sp, qT[:, :], FB[hp, :, :], reads=[fbres[hp]], writes=[qT])
                    k.dma(k.q_sp, kT[:, :], FB[8 + hp, :, :], reads=[fbres[8 + hp]], writes=[kT])
                    k.dma(k.q_sp, vt[:, :, :],
                          TB[:, hp * 128:(hp + 1) * 128].rearrange("(t p) c -> p t c", p=128),
                          reads=[tbres], writes=[vt])
                    for qg in range(8):
                        kbs = list(range(4 * qg + 3, -1, -1))
                        nb = len(kbs)
                        items = [[None] * nb, [None] * nb]
                        pend = {}
                        pendB = []
                        for j in range(2):
                            mm(pos[j][:, :], zerob[:, :], qT[:, qg * 512:(qg + 1) * 512], True, False,
                               [zerob, qT], [pos[j]])

                        def stageA(j, i):
                            b0 = 64 * j
                            R32 = R32s[j]
                            kb = kbs[i]
                            j0 = max(0, kb - 4 * qg)
                            c0 = 128 * j0
                            wq = 512 - c0
                            q0 = qg * 512 + c0
                            diag = kb >= 4 * qg
                            pz = pz_r.next()
                            mm(pz[:, 0:wq], kT[b0:b0 + 64, kb * 128:(kb + 1) * 128], qT[b0:b0 + 64, q0:q0 + wq],
                               True, True, [kT, qT], [pz])
                            e32 = e32_r.next()
                            actf(e32[:, 0:wq], pz[:, 0:wq], AF.Exp, [pz], [e32])
                            pend[(j, i)] = (R32, kb, c0, wq, q0, diag, e32)

                        def stageA2(j, i):
                            R32, kb, c0, wq, q0, diag, e32 = pend.pop((j, i))
                            spb = sp_r.next()
                            if 'sbx_noln' not in flags:
                                actf(spb[:, 0:wq], e32[:, 0:wq], AF.Ln, [e32], [spb], bias=1.0)
                            if diag:
                                tt(pool, spb[:, 0:128], spb[:, 0:128], mltb[:, :], ALU.mult, [spb, mltb], [spb])
                            rb = None
                            if i > 0 and 'sbx_nor' not in flags:
                                rb = rb_r.next()
                                cp(dve, rb[:, :], R32[:, :], [R32], [rb])
                            if i < nb - 1 and 'sbx_nor' not in flags:
                                if i == 0:
                                    if c0 > 0:
                                        memset(dve, R32[:, 0:c0], 0.0, [R32])
                                    cp(dve, R32[:, c0:512], spb[:, 0:wq], [spb], [R32])
                                else:
                                    tt(dve, R32[:, c0:512], R32[:, c0:512], spb[:, 0:wq], ALU.add, [R32, spb],
                                       [R32])
                            items[j][i] = (kb, c0, wq, q0, diag, spb, rb)

                        def stageB(j, i):
                            if 'sbx_nob' in flags:
                                return
                            b0 = 64 * j
                            po = pos[j]
                            kb, c0, wq, q0, diag, spb, rb = items[j][i]
                            pw = pw_r.next()
                            mm(pw[:, 0:wq], kT[b0:b0 + 64, kb * 128:(kb + 1) * 128], qT[b0:b0 + 64, q0:q0 + wq],
                               True, False, [kT, qT], [pw])
                            mm(pw[:, 0:wq], unegb[:, :], spb[:, 0:wq], False, rb is None, [unegb, spb], [pw])
                            if rb is not None:
                                mm(pw[:, 0:wq], negonesb[:, :], rb[:, c0:512], False, True, [negonesb, rb], [pw])
                            ab = ab_r.next()
                            actf(ab[:, 0:wq], pw[:, 0:wq], AF.Exp, [pw], [ab])
                            if diag:
                                tt(dve, ab[:, 0:128], ab[:, 0:128], mltb[:, :], ALU.mult, [ab, mltb], [ab])
                            pendB.append((po, c0, wq, kb, ab, i))
                            while len(pendB) > 2:
                                stageB2()

                        def stageB2():
                            po, c0, wq, kb, ab, i = pendB.pop(0)
                            mm(po[:, c0:512], vt[:, kb, :], ab[:, 0:wq], False, i == nb - 1, [vt, ab], [po])

                        GW = 2
                        nw = nb // GW
                        for w in range(nw + 1):
                            if w < nw:
                                for i in range(w * GW, (w + 1) * GW):
                                    for j in range(2):
                                        stageA(j, i)
                                for i in range(w * GW, (w + 1) * GW):
                                    for j in range(2):
                                        stageA2(j, i)
                            if w > 0:
                                for i in range((w - 1) * GW, w * GW):
                                    for j in range(2):
                                        stageB(j, i)
                        while pendB:
                            stageB2()
                        for j in range(2):
                            b0 = 64 * j
                            if j == 0:
                                actf(hT[b0:b0 + 64, hp, qg * 512:(qg + 1) * 512], pos[j][b0:b0 + 64, :], AF.Copy,
                                     [pos[j]], [hT])
                            else:
                                cp(dve, hT[b0:b0 + 64, hp, qg * 512:(qg + 1) * 512], pos[j][b0:b0 + 64, :],
                                   [pos[j]], [hT])
                k.barrier()

        def emit_even_mixer(i_even, layer):
            lam_init = 0.8 - 0.6 * math.exp(-0.3 * layer)
            with ExitStack() as st:
                BM = [[k.sb(st, "BM%d_%d" % (h, di), [128, 512], BF16) for di in range(5)] for h in range(4)]
                b15 = k.sb(st, "b15", [128, 4], F32)
                neglam = k.sb(st, "neglam", [128, 1], F32)
                gsub = k.sb(st, "gsub", [128, 1], F32)
                with ExitStack() as st2:
                    rb = k.sb(st2, "rb", [32, 4], F32)
                    ohb = k.sb(st2, "ohb", [32, NF], F32)
                    fsb = k.sb(st2, "fsb", [4, NF], F32)
                    exch = k.sb(st2, "exch", [128, 128], F32)
                    nmk = [k.sb(st2, "nmk%d" % di, [128, 512], F32) for di in range(5)]
                    tl_r = Ring([k.sb(st2, "tl%d" % i, [128, 512], F32) for i in range(2)])
                    lrow = k.sb(st2, "lrow", [1, 264], F32)
                    grow = k.sb(st2, "grow", [1, 128], F32)
                    pf = Ring([k.ps(st2, "pf%d" % i, [128, 512], F32) for i in range(2)])
                    k.dma(k.q_sp, rb[:, :], I["rel_bias"][:, :], writes=[rb])
                    k.dma(k.q_sp, ohb[:, :], I["c_ohb"][:, :], writes=[ohb])
                    k.dma(k.q_sp, exch[:, :], I["c_exch"][:, :], writes=[exch])
                    k.dma(k.q_sp, b15[:, :], I["rel_bias"][15].partition_broadcast(128), writes=[b15])
                    for di in range(5):
                        k.dma(k.q_sp, nmk[di][:, :], I["c_negmask"][di], writes=[nmk[di]])
                    for c3 in range(3):
                        c0 = c3 * 512
                        w_ = min(512, NF - c0)
                        p = pf.next()
                        mm(p[0:4, 0:w_], rb[0:32, 0:4], ohb[0:32, c0:c0 + w_], True, True, [rb, ohb], [p])
                        cp(dve, fsb[0:4, c0:c0 + w_], p[0:4, 0:w_], [p], [fsb])
                    k.dma(k.q_sp, FS[:, :], fsb[:, :], reads=[fsb], writes=[fsres])
                    for h in range(4):
                        for di, dl in enumerate(DELTAS):
                            tl = tl_r.next()
                            src = bass.AP(tensor=FS_T, offset=h * NF + (384 - dl), ap=[[1, 128], [1, 512]])
                            k.dma(k.q_sp, tl[:, :], src, reads=[fsres], writes=[tl])
                            p = pf.next()
                            mm(p[:, :], exch[:, :], tl[:, :], True, True, [exch, tl], [p])
                            tt(dve, BM[h][di][:, :], p[:, :], nmk[di][:, :], ALU.add, [p, nmk[di]], [BM[h][di]])
                    k.dma(k.q_sp, lrow[:, 0:256], I["even_lambda"][i_even:i_even + 1, :], writes=[lrow])
                    LR = [lrow]
                    tt(dve, lrow[:, 0:64], lrow[:, 0:64], lrow[:, 64:128], ALU.mult, LR, LR)
                    tt(dve, lrow[:, 128:192], lrow[:, 128:192], lrow[:, 192:256], ALU.mult, LR, LR)
                    k.op(dve, lambda: nc.vector.tensor_reduce(out=lrow[:, 256:257], in_=lrow[:, 0:64], axis=AX.X,
                                                              op=ALU.add), LR, LR)
                    k.op(dve, lambda: nc.vector.tensor_reduce(out=lrow[:, 257:258], in_=lrow[:, 128:192], axis=AX.X,
                                                              op=ALU.add), LR, LR)
                    actf(lrow[:, 258:260], lrow[:, 256:258], AF.Exp, LR, LR)
                    tt(dve, lrow[:, 260:261], lrow[:, 259:260], lrow[:, 258:259], ALU.subtract, LR, LR)
                    ts(dve, lrow[:, 261:262], lrow[:, 260:261], -lam_init, None, ALU.add, None, LR, LR)
                    p = pf.next()
                    mm(p[:, 0:1], ones32[0:1, :], lrow[0:1, 261:262], True, True, [ones32, lrow], [p])
                    cp(dve, neglam[:, :], p[:, 0:1], [p], [neglam])
                    k.dma(k.q_sp, grow[:, :], I["even_subln"][i_even:i_even + 1, :], writes=[grow])
                    p = pf.next()
                    mm(p[:, 0:1], grow[0:1, :], ones32[0:1, 0:1], True, True, [grow, ones32], [p])
                    ts(dve, gsub[:, :], p[:, 0:1], 1.0 - lam_init, None, ALU.mult, None, [p], [gsub])
                    k.barrier()
                qT_r = Ring([k.sb(st, "da_qT%d" % i, [128, S], BF16) for i in range(2)])
                kT_r = Ring([k.sb(st, "da_kT%d" % i, [128, S], BF16) for i in range(2)])
                vt_r = Ring([k.sb(st, "da_vt%d" % i, [128, NT, 128], BF16) for i in range(2)])
                E_r = Ring([k.sb(st, "da_E%d" % i, [128, 512], BF16) for i in range(5)])
                f_r = Ring([k.sb(st, "da_f%d" % i, [128, 512], F32) for i in range(4)])
                o32_r = Ring([k.sb(st, "da_o%d" % i, [128, 512], F32) for i in range(2)])
                sq_r = Ring([k.sb(st, "da_sq%d" % i, [128, 512], BF16) for i in range(2)])
                ps_r = Ring([k.ps(st, "da_ps%d" % i, [128, 512], F32) for i in range(3)])
                pu = [k.ps(st, "da_pu%d" % i, [128, 512], F32) for i in range(2)]
                pd = [k.ps(st, "da_pd%d" % i, [128, 512], F32) for i in range(2)]
                pss = k.ps(st, "da_pss", [128, 512], F32)
                for h in range(0 if 'ev_noA' in flags else 4):
                    qT = qT_r.next()
                    kT = kT_r.next()
                    vt = vt_r.next()
                    k.dma(k.q_sp, qT[:, :], FB[h, :, :], reads=[fbres[h]], writes=[qT])
                    k.dma(k.q_sp, kT[:, :], FB[4 + h, :, :], reads=[fbres[4 + h]], writes=[kT])
                    k.dma(k.q_sp, vt[:, :, :],
                          TB[:, h * 128:(h + 1) * 128].rearrange("(t p) c -> p t c", p=128),
                          reads=[tbres], writes=[vt])
                    for qg in range(8):
                        nkb = 4 * qg + 4
                        its = [(kb, m) for kb in range(nkb) for m in range(2)]
                        held = {}

                        def st1(idx):
                            kb, m = its[idx]
                            j0 = max(0, kb - 4 * qg)
                            c0 = 128 * j0
                            wq = 512 - c0
                            q0 = qg * 512 + c0
                            dl = kb * 128 - qg * 512
                            b0 = 64 * m
                            ps = ps_r.next()
                            near = dl >= -128
                            mm(ps[:, 0:wq], kT[b0:b0 + 64, kb * 128:(kb + 1) * 128], qT[b0:b0 + 64, q0:q0 + wq],
                               True, not near, [kT, qT], [ps])
                            E = E_r.next()
                            if near:
                                bm = BM[h][DELTAS.index(dl)]
                                mm(ps[:, 0:wq], identb[:, :], bm[:, c0:512], False, True, [identb, bm], [ps])
                                actf(E[:, 0:wq], ps[:, 0:wq], AF.Exp, [ps], [E])
                            else:
                                actf(E[:, 0:wq], ps[:, 0:wq], AF.Exp, [ps, b15], [E], bias=b15[:, h:h + 1])
                            held[idx] = (E, c0, wq)

                        def st2(idx):
                            kb, m = its[idx]
                            E, c0, wq = held.pop(idx)
                            mm(pu[m][:, c0:512], vt[:, kb, :], E[:, 0:wq], kb == 0, kb == nkb - 1, [vt, E], [pu[m]])
                            mm(pd[m][:, c0:512], onesb[:, :], E[:, 0:wq], kb == 0, kb == nkb - 1, [onesb, E],
                               [pd[m]])

                        SK = 2
                        for idx in range(len(its) + SK):
                            if idx < len(its):
                                st1(idx)
                            if idx >= SK:
                                st2(idx - SK)
                        r0 = f_r.next()
                        recip(r0[:, :], pd[0][:, :], [pd[0]], [r0])
                        t0 = f_r.next()
                        tt(dve, t0[:, :], pu[0][:, :], r0[:, :], ALU.mult, [pu[0], r0], [t0])
                        r1 = f_r.next()
                        recip(r1[:, :], pd[1][:, :], [pd[1]], [r1])
                        t1 = f_r.next()
                        tt(dve, t1[:, :], pu[1][:, :], r1[:, :], ALU.mult, [pu[1], r1], [t1])
                        o32 = o32_r.next()
                        stt(o32[:, :], t1[:, :], neglam[:, 0:1], t0[:, :], ALU.mult, ALU.add, [t1, t0, neglam], [o32])
                        sq = sq_r.next()
                        actf(sq[:, :], o32[:, :], AF.Square, [o32], [sq])
                        mm(pss[:, :], onesb[:, :], sq[:, :], True, True, [onesb, sq], [pss])
                        rs = f_r.next()
                        actf(rs[:, :], pss[:, :], AF.Sqrt, [pss], [rs], scale=1.0 / 128.0, bias=EPS)
                        rs2 = f_r.next()
                        recip(rs2[:, :], rs[:, :], [rs], [rs2])
                        stt(hT[:, h, qg * 512:(qg + 1) * 512], o32[:, :], gsub[:, 0:1], rs2[:, :], ALU.mult, ALU.mult,
                            [o32, gsub, rs2], [hT])
                k.barrier()
            with ExitStack() as st:
                gtri = k.sb(st, "g_tri", [128, 128], F32)
                gm2 = k.sb(st, "g_m2", [128, 128], F32)
                gmaskb = k.sb(st, "g_mask", [128, 128], BF16)
                wgk = k.sb(st, "g_wgk", [16, 256], BF16)
                bgk = k.sb(st, "g_bgk", [1, 256], BF16)
                glag = k.sb(st, "g_lag", [128, 1], F32)
                grow = k.sb(st, "g_grow", [1, 128], F32)
                k.dma(k.q_sp, gtri[:, :], I["c_gtri"][:, :], writes=[gtri])
                k.dma(k.q_sp, gm2[:, :], I["c_gm2"][:, :], writes=[gm2])
                k.dma(k.q_pool, gmaskb[:, :], I["c_gmask"][:, :], writes=[gmaskb])
                k.dma(k.q_pool, wgk[:, :], I["even_w_gk2"][i_even], writes=[wgk])
                k.dma(k.q_pool, bgk[:, :], I["even_b_gk"][i_even:i_even + 1, :], writes=[bgk])
                k.dma(k.q_sp, grow[:, :], I["even_gla_norm"][i_even:i_even + 1, :], writes=[grow])
                pA = Ring([k.ps(st, "g_pA%d" % i, [128, 512], F32) for i in range(3)])
                pS = Ring([k.ps(st, "g_pS%d" % i, [128, 512], F32) for i in range(2)])
                pO = Ring([k.ps(st, "g_pO%d" % i, [128, 512], F32) for i in range(2)])
                p = pA.next()
                mm(p[:, 0:1], grow[0:1, :], ones32[0:1, 0:1], True, True, [grow, ones32], [p])
                cp(dve, glag[:, :], p[:, 0:1], [p], [glag])
                qg_r = Ring([k.sb(st, "g_q%d" % i, [128, 2, 512], BF16) for i in range(2)])
                kg_r = Ring([k.sb(st, "g_k%d" % i, [128, 2, 512], BF16) for i in range(2)])
                kt_r = Ring([k.sb(st, "g_kt%d" % i, [128, 4, 256], BF16) for i in range(2)])
                vg_r = Ring([k.sb(st, "g_v%d" % i, [128, 4, 512], BF16) for i in range(2)])
                bg_r = Ring([k.sb(st, "g_bg%d" % i, [16, 512], BF16) for i in range(2)])
                br_r = Ring([k.sb(st, "g_br%d" % i, [128, 4, 512], BF16) for i in range(2)])
                og_r = Ring([k.sb(st, "g_og%d" % i, [128, 4, 512], F32) for i in range(2)])
                e32_r = Ring([k.sb(st, "g_e%d" % i, [128, 256], F32) for i in range(2)])
                sp_r = Ring([k.sb(st, "g_sp%d" % i, [128, 256], F32) for i in range(2)])
                kf_r = Ring([k.sb(st, "g_kf%d" % i, [128, 256], F32) for i in range(2)])
                kd_r = Ring([k.sb(st, "g_kd%d" % i, [128, 256], BF16) for i in range(2)])
                eb_r = Ring([k.sb(st, "g_eb%d" % i, [128, 256], F32) for i in range(2)])
                en_r = Ring([k.sb(st, "g_en%d" % i, [128, 256], F32) for i in range(2)])
                qd_r = Ring([k.sb(st, "g_qd%d" % i, [128, 2, 128], BF16) for i in range(2)])
                ki_r = Ring([k.sb(st, "g_ki%d" % i, [128, 2, 128], BF16) for i in range(2)])
                at_r = Ring([k.sb(st, "g_at%d" % i, [128, 128], BF16) for i in range(3)])
                S32_r = [Ring([k.sb(st, "g_S32_%d_%d" % (h, i), [128, 128], F32) for i in range(2)]) for h in range(4)]
                Sb_r = [Ring([k.sb(st, "g_Sb_%d_%d" % (h, i), [128, 128], BF16) for i in range(3)]) for h in range(4)]
                sq_r = Ring([k.sb(st, "g_sq%d" % i, [128, 512], BF16) for i in range(2)])
                f_r = Ring([k.sb(st, "g_f%d" % i, [128, 512], F32) for i in range(4)])
                S32 = []
                Sb = []
                for h in range(4):
                    s = S32_r[h].next()
                    memset(dve, s[:, :], 0.0, [s])
                    S32.append(s)
                    b = Sb_r[h].next()
                    memset(pool, b[:, :], 0.0, [b])
                    Sb.append(b)
                for g8 in range(0 if 'ev_noB' in flags else 8):
                    t0g = g8 * 512
                    qgt = qg_r.next()
                    kgt = kg_r.next()
                    ktt = kt_r.next()
                    vgt = vg_r.next()
                    bgt = bg_r.next()
                    brt = br_r.next()
                    og = og_r.next()
                    for hh in range(2):
                        k.dma(k.q_sp, qgt[:, hh, :], FB[12 + hh, :, t0g:t0g + 512], reads=[fbres[12 + hh]], writes=[qgt])
                        k.dma(k.q_sp, kgt[:, hh, :], FB[14 + hh, :, t0g:t0g + 512], reads=[fbres[14 + hh]], writes=[kgt])
                    k.dma(k.q_sp, ktt[:, :, :],
                          TB[t0g:t0g + 512, 512:768].rearrange("(j p) c -> p j c", p=128), reads=[tbres], writes=[ktt])
                    k.dma(k.q_sp, vgt[:, :, :],
                          TB[t0g:t0g + 512, 768:1280].rearrange("(j p) c -> p j c", p=128), reads=[tbres], writes=[vgt])
                    k.dma(k.q_sp, bgt[:, :], FB[24, 0:16, t0g:t0g + 512], reads=[fbres[24]], writes=[bgt])
                    for h in range(4):
                        k.dma(k.q_sp, brt[:, h, :], FB[20 + h, :, t0g:t0g + 512], reads=[fbres[20 + h]], writes=[brt])
                    for j4 in range(4):
                        tc0 = j4 * 128
                        ppre = pA.next()
                        mm(ppre[:, 0:256], bgt[0:16, tc0:tc0 + 128], wgk[0:16, :], True, False, [bgt, wgk], [ppre])
                        mm(ppre[:, 0:256], onesb[0:1, :], bgk[0:1, :], False, True, [onesb, bgk], [ppre])
                        e32 = e32_r.next()
                        actf(e32[:, :], ppre[:, 0:256], AF.Exp, [ppre], [e32], scale=-1.0)
                        sp32 = sp_r.next()
                        actf(sp32[:, :], e32[:, :], AF.Ln, [e32], [sp32], bias=1.0)
                        pb = pA.next()
                        for hh in range(2):
                            mm(pb[:, hh * 128:(hh + 1) * 128], sp32[:, hh * 128:(hh + 1) * 128], gtri[:, :], True, True,
                               [sp32, gtri], [pb])
                        pk = pA.next()
                        mm(pk[:, 0:256], gm2[:, :], sp32[:, :], True, True, [gm2, sp32], [pk])
                        kf = kf_r.next()
                        actf(kf[:, :], pk[:, 0:256], AF.Exp, [pk], [kf])
                        kd = kd_r.next()
                        tt(dve, kd[:, :], ktt[:, j4, :], kf[:, :], ALU.mult, [ktt, kf], [kd])
                        eb = eb_r.next()
                        actf(eb[:, :], pb[:, 0:256], AF.Exp, [pb], [eb])
                        en = en_r.next()
                        actf(en[:, :], pb[:, 0:256], AF.Exp, [pb], [en], scale=-1.0)
                        qd = qd_r.next()
                        ki = ki_r.next()
                        for hh in range(2):
                            tt(dve, qd[:, hh, :], qgt[:, hh, tc0:tc0 + 128], eb[:, hh * 128:(hh + 1) * 128], ALU.mult,
                               [qgt, eb], [qd])
                            tt(dve, ki[:, hh, :], kgt[:, hh, tc0:tc0 + 128], en[:, hh * 128:(hh + 1) * 128], ALU.mult,
                               [kgt, en], [ki])
                        for h in range(4):
                            hh, jj = h // 2, h % 2
                            b0 = 64 * jj
                            psc = pS.next()
                            mm(psc[:, 0:128], ki[b0:b0 + 64, hh, :], qd[b0:b0 + 64, hh, :], True, True, [ki, qd], [psc])
                            at = at_r.next()
                            tt(dve, at[:, :], psc[:, 0:128], gmaskb[:, :], ALU.mult, [psc, gmaskb], [at])
                            po = pO.next()
                            mm(po[:, 0:128], vgt[:, j4, h * 128:(h + 1) * 128], at[:, :], True, False, [vgt, at], [po])
                            mm(po[:, 0:64], Sb[h][b0:b0 + 64, :], qd[b0:b0 + 64, hh, 0:64], False, False,
                               [Sb[h], qd], [po])
                            pst = pS.next()
                            mm(pst[:, 0:128], kd[0:64, hh * 128:(hh + 1) * 128], vgt[0:64, j4, h * 128:(h + 1) * 128],
                               True, True, [kd, vgt], [pst])
                            s1 = S32_r[h].next()
                            stt(s1[b0:b0 + 64, :], S32[h][b0:b0 + 64, :], eb[b0:b0 + 64, hh * 128 + 63:hh * 128 + 64],
                                pst[b0:b0 + 64, 0:128], ALU.mult, ALU.add, [S32[h], eb, pst], [s1])
                            sb1 = Sb_r[h].next()
                            actf(sb1[b0:b0 + 64, :], s1[b0:b0 + 64, :], AF.Copy, [s1], [sb1])
                            mm(po[:, 64:128], sb1[b0:b0 + 64, :], qd[b0:b0 + 64, hh, 64:128], False, True,
                               [sb1, qd], [po])
                            pst2 = pS.next()
                            mm(pst2[:, 0:128], kd[64:128, hh * 128:(hh + 1) * 128],
                               vgt[64:128, j4, h * 128:(h + 1) * 128], True, True, [kd, vgt], [pst2])
                            s2 = S32_r[h].next()
                            stt(s2[b0:b0 + 64, :], s1[b0:b0 + 64, :], eb[b0:b0 + 64, hh * 128 + 127:hh * 128 + 128],
                                pst2[b0:b0 + 64, 0:128], ALU.mult, ALU.add, [s1, eb, pst2], [s2])
                            sb2 = Sb_r[h].next()
                            actf(sb2[b0:b0 + 64, :], s2[b0:b0 + 64, :], AF.Copy, [s2], [sb2])
                            S32[h] = s2
                            Sb[h] = sb2
                            actf(og[:, h, tc0:tc0 + 128], po[:, 0:128], AF.Copy, [po], [og])
                    for h in range(4):
                        sq = sq_r.next()
                        actf(sq[:, :], og[:, h, :], AF.Square, [og], [sq])
                        pn = pA.next()
                        mm(pn[:, :], onesb[:, :], sq[:, :], True, True, [onesb, sq], [pn])
                        rs = f_r.next()
                        actf(rs[:, :], pn[:, :], AF.Sqrt, [pn], [rs], scale=1.0 / 128.0, bias=EPS)
                        rs2 = f_r.next()
                        recip(rs2[:, :], rs[:, :], [rs], [rs2])
                        sl = f_r.next()
                        actf(sl[:, :], brt[:, h, :], AF.Silu, [brt], [sl])
                        t1 = f_r.next()
                        stt(t1[:, :], og[:, h, :], glag[:, 0:1], rs2[:, :], ALU.mult, ALU.mult, [og, glag, rs2], [t1])
                        tt(dve, hT[:, 4 + h, t0g:t0g + 512], t1[:, :], sl[:, :], ALU.mult, [t1, sl], [hT])
                k.barrier()

        def emit_moe(layer):
            with ExitStack() as st:
                wg_r = Ring([k.sb(st, "me_wg%d" % i, [128, 8, DEXP], BF16) for i in range(2)])
                wu_r = Ring([k.sb(st, "me_wu%d" % i, [128, 8, DEXP], BF16) for i in range(2)])
                wd_r = Ring([k.sb(st, "me_wd%d" % i, [128, 4, D], BF16) for i in range(2)])
                yacc = k.sb(st, "me_yacc", [128, 8, D], F32)
                aT_r = Ring([k.sb(st, "me_aT%d" % i, [128, 4, 512], BF16) for i in range(2)])
                sg_r = Ring([k.sb(st, "me_sg%d" % i, [128, 512], BF16) for i in range(3)])
                xt_r = Ring([k.sb(st, "me_xt%d" % i, [128, D], F32) for i in range(2)])
                pg_r = Ring([k.ps(st, "me_pg%d" % i, [128, 512], F32) for i in range(2)])
                pu_r = Ring([k.ps(st, "me_pu%d" % i, [128, 512], F32) for i in range(2)])
                py_r = Ring([k.ps(st, "me_py%d" % i, [128, D], F32) for i in range(2)])
                for qtr in range(4):
                    memset(dve, yacc[:, :, :], 0.0, [yacc])
                    jobs = [(e, tg) for e in range(NEXP) for tg in range(2)]
                    wts = {}

                    def load_w(e):
                        wg = wg_r.next()
                        wu = wu_r.next()
                        wd = wd_r.next()
                        k.dma(k.q_pool, wg[:, :, :],
                              I["expert_w_gate"][layer, e].rearrange("(kc p) f -> p kc f", p=128), writes=[wg])
                        k.dma(k.q_pool, wu[:, :, :],
                              I["expert_w_up"][layer, e].rearrange("(kc p) f -> p kc f", p=128), writes=[wu])
                        k.dma(k.q_pool, wd[:, :, :],
                              I["expert_w_down"][layer, e].rearrange("(fc p) n -> p fc n", p=128), writes=[wd])
                        wts[e] = (wg, wu, wd)

                    acts = {}

                    def emit_gu(e, tg):
                        wg, wu, wd = wts[e]
                        G = qtr * 2 + tg
                        aT = aT_r.next()
                        for fc in range(4):
                            pg = pg_r.next()
                            pu = pu_r.next()
                            for kc in range(8):
                                mm(pg[:, :], wg[:, kc, fc * 128:(fc + 1) * 128], hT[:, kc, G * 512:(G + 1) * 512],
                                   kc == 0, kc == 7, [wg, hT], [pg])
                            for kc in range(8):
                                mm(pu[:, :], wu[:, kc, fc * 128:(fc + 1) * 128], hT[:, kc, G * 512:(G + 1) * 512],
                                   kc == 0, kc == 7, [wu, hT], [pu])
                            sg = sg_r.next()
                            actf(sg[:, :], pg[:, :], AF.Silu, [pg], [sg])
                            tt(dve, aT[:, fc, :], sg[:, :], pu[:, :], ALU.mult, [sg, pu], [aT])
                        acts[(e, tg)] = aT

                    def emit_down(e, tg):
                        wg, wu, wd = wts[e]
                        aT = acts.pop((e, tg))
                        for t4 in range(4):
                            ti = tg * 4 + t4
                            T = qtr * 8 + ti
                            py = py_r.next()
                            for nh in range(2):
                                for fc in range(4):
                                    mm(py[:, nh * 512:(nh + 1) * 512], aT[:, fc, t4 * 128:(t4 + 1) * 128],
                                       wd[:, fc, nh * 512:(nh + 1) * 512], fc == 0, fc == 3, [aT, wd], [py])
                            stt(yacc[:, ti, :], py[:, :], cmb[:, T, e:e + 1], yacc[:, ti, :], ALU.mult, ALU.add,
                                [py, cmb, yacc], [yacc])

                    load_w(0)
                    for idx, (e, tg) in enumerate(jobs):
                        emit_gu(e, tg)
                        if idx > 0:
                            emit_down(*jobs[idx - 1])
                        if tg == 0 and e + 1 < NEXP:
                            load_w(e + 1)
                    emit_down(*jobs[-1])
                    for ti in range(8):
                        T = qtr * 8 + ti
                        xt = xt_r.next()
                        k.dma(k.q_sp, xt[:, :], XS[T * 128:(T + 1) * 128, :], reads=[xres[T]], writes=[xt])
                        tt(dve, yacc[:, ti, :], yacc[:, ti, :], gbc2[:, :], ALU.mult, [yacc, gbc2], [yacc])
                        tt(dve, xt[:, :], xt[:, :], yacc[:, ti, :], ALU.add, [xt, yacc], [xt])
                        k.dma(k.q_sp, XS[T * 128:(T + 1) * 128, :], xt[:, :], reads=[xt], writes=[xres[T]])
                k.barrier()

        def emit_slots(layer):
            with ExitStack() as st:
                Aall = k.sb(st, "sl_A", [128, NT, 32], BF16)
                ag = k.sb(st, "sl_ag", [128, NT, 8], F32)
                mltb = k.sb(st, "sl_mlt", [128, 128], BF16)
                pos = k.sb(st, "sl_pos", [128, NT, 32], F32)
                cnt = k.sb(st, "sl_cnt", [128, 32], F32)
                nb_ = k.sb(st, "sl_nb", [128, 32], F32)
                pa = k.sb(st, "sl_pa", [128, 32], F32)
                pb = k.sb(st, "sl_pb", [128, 32], F32)
                smg = k.sb(st, "sl_smg", [128, NT, 8], F32)
                t8 = k.sb(st, "sl_t8", [128, NT, 8], F32)
                sf = k.sb(st, "sl_sf", [128, 2, NT], F32)
                iog = k.sb(st, "sl_iog", [128, 8], F32)
                iob = k.sb(st, "sl_iob", [128, NBLK], F32)
                cmpt = k.sb(st, "sl_cmp", [128, NBLK, 32], F32)
                eb = k.sb(st, "sl_eb", [128, NBLK], F32)
                fg = k.sb(st, "sl_fg", [128, NBLK, 8], F32)
                ppos = k.ps(st, "sl_ppos", [128, NT * 32], F32)
                pcnt = k.ps(st, "sl_pcnt", [128, 512], F32)
                k.dma(k.q_pool, mltb[:, :], I["c_mlt"][:, :], writes=[mltb])
                k.dma(k.q_sp, iog[:, :], I["c_iog"][:, :], writes=[iog])
                k.dma(k.q_sp, iob[:, :], I["c_iob"][:, :], writes=[iob])
                RR = [Aall, ag, pos, cnt, nb_, pa, pb, smg, t8, sf, cmpt, eb, fg, r_ohg, r_oh1, r_oh2, r_v]

                def bc(ap2, n):
                    return ap2.unsqueeze(2).to_broadcast([128, NT, n])
                tt(dve, ag[:, :, :], r_oh1[:, :, :], r_oh2[:, :, :], ALU.add, RR, RR)
                for g in range(4):
                    tt(dve, Aall[:, :, 8 * g:8 * g + 8], ag[:, :, :], bc(r_ohg[:, :, g], 8), ALU.mult, RR, RR)
                for T in range(NT):
                    for T2 in range(T):
                        mm(ppos[:, T * 32:(T + 1) * 32], onesb[:, :], Aall[:, T2, :], T2 == 0, False,
                           [onesb, Aall], [ppos])
                    mm(ppos[:, T * 32:(T + 1) * 32], mltb[:, :], Aall[:, T, :], T == 0, True, [mltb, Aall], [ppos])
                for T in range(NT):
                    mm(pcnt[:, 0:32], onesb[:, :], Aall[:, T, :], T == 0, T == NT - 1, [onesb, Aall], [pcnt])
                cp(dve, pos[:, 0:16, :], ppos[:, 0:512].rearrange("p (t e) -> p t e", e=32), [ppos], RR)
                cp(dve, pos[:, 16:32, :], ppos[:, 512:1024].rearrange("p (t e) -> p t e", e=32), [ppos], RR)
                cp(dve, cnt[:, :], pcnt[:, 0:32], [pcnt], RR)
                ts(dve, nb_[:, :], cnt[:, :], 0.0, None, ALU.is_gt, None, RR, RR)
                for m in range(1, 8):
                    stt(nb_[:, :], cnt[:, :], float(BSZ * m), nb_[:, :], ALU.is_gt, ALU.add, RR, RR)
                cp(dve, pa[:, :], nb_[:, :], RR, RR)
                a_, b_ = pa, pb
                for sh in (1, 2, 4, 8, 16):
                    cp(dve, b_[:, 0:sh], a_[:, 0:sh], RR, RR)
                    tt(dve, b_[:, sh:32], a_[:, sh:32], a_[:, 0:32 - sh], ALU.add, RR, RR)
                    a_, b_ = b_, a_
                incl = a_
                excl = b_
                tt(dve, excl[:, :], incl[:, :], nb_[:, :], ALU.subtract, RR, RR)
                ts(dve, excl[:, :], excl[:, :], float(BSZ), None, ALU.mult, None, RR, RR)
                tt(dve, pos[:, :, :], pos[:, :, :], excl[:, :].unsqueeze(1).to_broadcast([128, NT, 32]), ALU.add,
                   RR, RR)
                for g in range(4):
                    if g == 0:
                        tt(dve, smg[:, :, :], pos[:, :, 0:8], bc(r_ohg[:, :, 0], 8), ALU.mult, RR, RR)
                    else:
                        tt(dve, t8[:, :, :], pos[:, :, 8 * g:8 * g + 8], bc(r_ohg[:, :, g], 8), ALU.mult, RR, RR)
                        tt(dve, smg[:, :, :], smg[:, :, :], t8[:, :, :], ALU.add, RR, RR)
                tt(dve, t8[:, :, :], smg[:, :, :], r_oh1[:, :, :], ALU.mult, RR, RR)
                k.op(dve, lambda: nc.vector.tensor_reduce(out=sf[:, 0, :], in_=t8[:, :, :], axis=AX.X, op=ALU.add),
                     RR, RR)
                tt(dve, t8[:, :, :], smg[:, :, :], r_oh2[:, :, :], ALU.mult, RR, RR)
                k.op(dve, lambda: nc.vector.tensor_reduce(out=sf[:, 1, :], in_=t8[:, :, :], axis=AX.X, op=ALU.add),
                     RR, RR)
                cp(dve, s12u[:, :, :], sf[:, :, :], RR, [s12u])
                tt(dve, cmpt[:, :, :], incl[:, :].unsqueeze(1).to_broadcast([128, NBLK, 32]),
                   iob[:, :].unsqueeze(2).to_broadcast([128, NBLK, 32]), ALU.is_le, RR + [iob], RR)
                k.op(dve, lambda: nc.vector.tensor_reduce(out=eb[:, :], in_=cmpt[:, :, :], axis=AX.X, op=ALU.add),
                     RR, RR)
                ts(dve, eb[:, :], eb[:, :], 31.0, None, ALU.min, None, RR, RR)
                ts(dve, eb[:, :], eb[:, :], 512.0, float(layer * NEXP * DEXP), ALU.mult, ALU.add, RR, RR)
                tt(dve, fg[:, :, 0:4], eb[:, :].unsqueeze(2).to_broadcast([128, NBLK, 4]),
                   iog[:, 0:4].unsqueeze(1).to_broadcast([128, NBLK, 4]), ALU.add, RR + [iog], RR)
                cp(dve, idxd[:, :, :], fg[:, :, 0:4], RR, [idxd])
                ts(dve, eb[:, :], eb[:, :], 2.0, None, ALU.mult, None, RR, RR)
                tt(dve, fg[:, :, :], eb[:, :].unsqueeze(2).to_broadcast([128, NBLK, 8]),
                   iog[:, :].unsqueeze(1).to_broadcast([128, NBLK, 8]), ALU.add, RR + [iog], RR)
                cp(dve, idxg[:, :, :], fg[:, :, :], RR, [idxg])
                xr_r = Ring([k.sb(st, "sl_xr%d" % i, [128, D], F32) for i in range(3)])
                for T in range(NT):
                    xr = xr_r.next()
                    k.dma(k.q_sp, xr[:, :], XN[T * 128:(T + 1) * 128, :], reads=[xnres], writes=[xr])
                    for kk in range(2):
                        idma(XG[:, :], bass.IndirectOffsetOnAxis(ap=s12u[:, kk, T:T + 1], axis=0), xr[:, :], None,
                             [xr, s12u], [xgres])
                k.barrier()

        def emit_moe_sparse(layer):
            with ExitStack() as st:
                wg_r = Ring([k.sb(st, "ms_wg%d" % i, [128, 8, DEXP], BF16) for i in range(2)])
                wu_r = Ring([k.sb(st, "ms_wu%d" % i, [128, 8, DEXP], BF16) for i in range(2)])
                wd_r = Ring([k.sb(st, "ms_wd%d" % i, [128, 4, D], BF16) for i in range(2)])
                xg_r = Ring([k.sb(st, "ms_xg%d" % i, [128, D], F32) for i in range(3)])
                hs_r = Ring([k.sb(st, "ms_hs%d" % i, [128, 8, 512], BF16) for i in range(2)])
                aT_r = Ring([k.sb(st, "ms_aT%d" % i, [128, 4, 512], BF16) for i in range(2)])
                sg_r = Ring([k.sb(st, "ms_sg%d" % i, [128, 512], BF16) for i in range(3)])
                yo_r = Ring([k.sb(st, "ms_yo%d" % i, [128, D], F32) for i in range(3)])
                ptr_r = Ring([k.ps(st, "ms_ptr%d" % i, [128, D], F32) for i in range(1)])
                pg_r = Ring([k.ps(st, "ms_pg%d" % i, [128, 512], F32) for i in range(2)])
                pu_r = Ring([k.ps(st, "ms_pu%d" % i, [128, 512], F32) for i in range(2)])
                py_r = Ring([k.ps(st, "ms_py%d" % i, [128, D], F32) for i in range(1)])
                wgv = I["expert_w_gate"].rearrange("l e d f -> (l e d) f")
                wuv = I["expert_w_up"].rearrange("l e d f -> (l e d) f")
                wdv = I["expert_w_down"].rearrange("l e f n -> (l e f) n")
                wts = {}
                hss = {}
                acts = {}

                def load_blk(b):
                    wg = wg_r.next()
                    wu = wu_r.next()
                    wd = wd_r.next()
                    for kc in range(8):
                        idma(wg[:, kc, :], None, wgv, bass.IndirectOffsetOnAxis(ap=idxg[:, b, kc:kc + 1], axis=0),
                             [idxg], [wg])
                        idma(wu[:, kc, :], None, wuv, bass.IndirectOffsetOnAxis(ap=idxg[:, b, kc:kc + 1], axis=0),
                             [idxg], [wu])
                    for fc in range(4):
                        idma(wd[:, fc, :], None, wdv, bass.IndirectOffsetOnAxis(ap=idxd[:, b, fc:fc + 1], axis=0),
                             [idxd], [wd])
                    wts[b] = (wg, wu, wd)

                def prep_h(b):
                    hs = hs_r.next()
                    for j in range(4):
                        xg = xg_r.next()
                        r0 = b * BSZ + j * 128
                        k.dma(k.q_sp, xg[:, :], XG[r0:r0 + 128, :], reads=[xgres], writes=[xg])
                        ptr = ptr_r.next()
                        for kc in range(8):
                            tr(ptr[:, kc * 128:(kc + 1) * 128], xg[:, kc * 128:(kc + 1) * 128], ident32[:, :],
                               [xg, ident32], [ptr])
                        for kc in range(8):
                            actf(hs[:, kc, j * 128:(j + 1) * 128], ptr[:, kc * 128:(kc + 1) * 128], AF.Identity,
                                 [ptr, gmod2, modT], [hs], scale=gmod2[:, kc:kc + 1], bias=modT[:, 24 + kc:25 + kc])
                    hss[b] = hs

                def emit_gu(b):
                    wg, wu, wd = wts[b]
                    hs = hss.pop(b)
                    aT = aT_r.next()
                    for fc in range(4):
                        pg = pg_r.next()
                        pu = pu_r.next()
                        for kc in range(8):
                            mm(pg[:, :], wg[:, kc, fc * 128:(fc + 1) * 128], hs[:, kc, :], kc == 0, kc == 7,
                               [wg, hs], [pg])
                        for kc in range(8):
                            mm(pu[:, :], wu[:, kc, fc * 128:(fc + 1) * 128], hs[:, kc, :], kc == 0, kc == 7,
                               [wu, hs], [pu])
                        sg = sg_r.next()
                        actf(sg[:, :], pg[:, :], AF.Silu, [pg], [sg])
                        tt(dve, aT[:, fc, :], sg[:, :], pu[:, :], ALU.mult, [sg, pu], [aT])
                    acts[b] = aT

                def emit_down(b):
                    wg, wu, wd = wts.pop(b)
                    aT = acts.pop(b)
                    for j in range(4):
                        py = py_r.next()
                        for nh in range(2):
                            for fc in range(4):
                                mm(py[:, nh * 512:(nh + 1) * 512], aT[:, fc, j * 128:(j + 1) * 128],
                                   wd[:, fc, nh * 512:(nh + 1) * 512], fc == 0, fc == 3, [aT, wd], [py])
                        yo = yo_r.next()
                        cp(dve, yo[:, :], py[:, :], [py], [yo])
                        r0 = b * BSZ + j * 128
                        k.dma(k.q_sp, YS[r0:r0 + 128, :], yo[:, :], reads=[yo], writes=[ysres[b]])

                load_blk(0)
                prep_h(0)
                for b in range(NBLK):
                    emit_gu(b)
                    if b > 0:
                        emit_down(b - 1)
                    if b + 1 < NBLK:
                        load_blk(b + 1)
                        prep_h(b + 1)
                emit_down(NBLK - 1)
                k.barrier()
            with ExitStack() as st:
                r1_r = Ring([k.sb(st, "ms_r1%d" % i, [128, D], F32) for i in range(2)])
                r2_r = Ring([k.sb(st, "ms_r2%d" % i, [128, D], F32) for i in range(2)])
                xt_r = Ring([k.sb(st, "ms_xt%d" % i, [128, D], F32) for i in range(2)])
                for T in range(NT):
                    r1 = r1_r.next()
                    r2 = r2_r.next()
                    idma(r1[:, :], None, YS[:, :], bass.IndirectOffsetOnAxis(ap=s12u[:, 0, T:T + 1], axis=0),
                         ysres + [s12u], [r1])
                    idma(r2[:, :], None, YS[:, :], bass.IndirectOffsetOnAxis(ap=s12u[:, 1, T:T + 1], axis=0),
                         ysres + [s12u], [r2])
                    xt = xt_r.next()
                    k.dma(k.q_sp, xt[:, :], XS[T * 128:(T + 1) * 128, :], reads=[xres[T]], writes=[xt])
                    ts(dve, r1[:, :], r1[:, :], r_v[:, 9, T:T + 1], None, ALU.mult, None, [r1, r_v], [r1])
                    stt(r1[:, :], r2[:, :], r_v[:, 10, T:T + 1], r1[:, :], ALU.mult, ALU.add, [r2, r_v, r1], [r1])
                    tt(dve, r1[:, :], r1[:, :], gbc2[:, :], ALU.mult, [r1, gbc2], [r1])
                    tt(dve, xt[:, :], xt[:, :], r1[:, :], ALU.add, [xt, r1], [xt])
                    k.dma(k.q_sp, XS[T * 128:(T + 1) * 128, :], xt[:, :], reads=[xt], writes=[xres[T]])
                k.barrier()

        def emit_final(do_norm):
            with ExitStack() as st:
                xt_r = Ring([k.sb(st, "f_xt%d" % i, [128, D], F32) for i in range(3)])
                xo_r = Ring([k.sb(st, "f_xo%d" % i, [128, D], F32) for i in range(2)])
                junk = k.sb(st, "f_junk", [128, D], BF16)
                sm_r = Ring([k.sb(st, "f_sm%d" % i, [128, 4], F32) for i in range(4)])
                gf = k.sb(st, "f_gf", [128, D], F32)
                k.dma(k.q_sp, gf[:, :], I["norm_final"][0].partition_broadcast(128), writes=[gf])
                evs = []
                for t in range(NT):
                    xt = xt_r.next()
                    k.dma(k.q_sp, xt[:, :], XS[t * 128:(t + 1) * 128, :], reads=[xres[t]], writes=[xt])
                    if do_norm:
                        sm = sm_r.next()
                        actf(junk[:, :], xt[:, :], AF.Square, [xt], [junk, sm], accum_out=sm[:, 0:1])
                        ts(dve, sm[:, 1:2], sm[:, 0:1], 1.0 / D, EPS, ALU.mult, ALU.add, [sm], [sm])
                        actf(sm[:, 2:3], sm[:, 1:2], AF.Sqrt, [sm], [sm])
                        recip(sm[:, 3:4], sm[:, 2:3], [sm], [sm])
                        xo = xo_r.next()
                        stt(xo[:, :], xt[:, :], sm[:, 3:4], gf[:, :], ALU.mult, ALU.mult, [xt, sm, gf], [xo])
                        src = xo
                    else:
                        src = xt
                    evs.append(k.dma(k.q_sp, OUT[t * 128:(t + 1) * 128, :], src[:, :], reads=[src]))
                for ev in evs:
                    sp.wait(ev)

        xsrc = I["x"]
        done = False
        if stop == ("evenonly",):
            emit_even_mixer(0, 0)
            emit_final(False)
            done = True
            nlayers = 0
        if stop == ("oddonly",):
            emit_odd_mixer()
            emit_final(False)
            done = True
            nlayers = 0
        for l in range(nlayers):
            emit_mod(l)
            emit_norm(xsrc, gmod1, modT[:, 0:8], l, router=False)
            ie = l // 2
            if 'skipmix' in flags:
                pass
            elif l % 2 == 0:
                fj = [(c * 128, 128, c, 0.125) for c in range(4)]
                fj += [(512 + c * 128, 128, 4 + c, 1.0) for c in range(4)]
                fj += [(1536 + c * 128, 128, 12 + c, 0.125) for c in range(2)]
                fj += [(1792 + c * 128, 128, 14 + c, 1.0) for c in range(2)]
                fj += [(2560 + c * 128, 128, 20 + c, 1.0) for c in range(4)]
                fj += [(3072, 16, 24, 1.0)]
                tj = [(1024, 512, 0), (1792, 256, 512), (2048, 512, 768)]
                emit_inproj(I["even_w_in"][ie], EVEN_COLS, fj, tj)
                emit_even_mixer(ie, l)
                emit_outproj(I["even_w_out"][ie], gbc1, xsrc)
            else:
                fj = [(c * 128, 128, c, 0.125) for c in range(8)]
                fj += [(1024 + c * 128, 128, 8 + c, 1.0) for c in range(8)]
                tj = [(2048, 512, 0), (2560, 512, 512)]
                emit_inproj(I["odd_w_in"][ie], 3072, fj, tj)
                emit_odd_mixer()
                emit_outproj(I["odd_w_out"][ie], gbc1, xsrc)
            if 'skipmix' not in flags:
                xsrc = XS
            if stop == ("mix", l):
                emit_final(False)
                done = True
                break
            emit_norm(xsrc, gmod2, modT[:, 24:32], l, router=True)
            if 'nomoe' not in flags:
                if SPARSE:
                    emit_slots(layer=l)
                    emit_moe_sparse(l)
                else:
                    emit_moe(l)
            if stop == ("ffn", l):
                emit_final(False)
                done = True
                break
        if not done:
            emit_final(True)
        build.stats = (k.n_inst, k.nsem)
    return nc


_CACHE = {}


def _get_nc(nlayers=DEPTH, stop=None):
    key = (nlayers, stop)
    if key not in _CACHE:
        _CACHE[key] = build(nlayers, stop)
    return _CACHE[key]


def make_in_maps(inputs, cores):
    consts = make_consts()
    shared = {}
    for nm in WEIGHT_NAMES:
        a = np.ascontiguousarray(np.asarray(inputs[nm], dtype=np.float32))
        shared[nm] = a.reshape(WEIGHT_SHAPES[nm])
    shared.update(consts)
    x = np.asarray(inputs["x"], dtype=np.float32)
    c = np.asarray(inputs["c"], dtype=np.float32)
    maps = []
    for b in cores:
        m = dict(shared)
        m["x"] = np.ascontiguousarray(x[b])
        m["c"] = np.ascontiguousarray(c[b:b + 1])
        maps.append(m)
    return maps


def kernel(**inputs):
    nc = _get_nc()
    in_maps = make_in_maps(inputs, list(range(8)))
    res = run_bass_kernel_spmd(nc, in_maps, core_ids=list(range(8)))
    return np.stack([np.asarray(r["out"]) for r in res.results], axis=0).astype(np.float32)
```

```python
import math
from contextlib import ExitStack
import numpy as np
import concourse.bass as bass
import concourse.mybir as mybir
from concourse.bass_utils import run_bass_kernel_spmd

F32 = mybir.dt.float32
BF16 = mybir.dt.bfloat16
AF = mybir.ActivationFunctionType
ALU = mybir.AluOpType
AX = mybir.AxisListType

S = 4096
D = 1024
NT = 32
DEPTH = 4
EPS = 1e-6
EVEN_COLS = 3088
NEXP = 32
DEXP = 512
EPOCH = 12000


class Res:
    __slots__ = ("w", "r", "name")

    def __init__(self, name=""):
        self.w = None
        self.r = []
        self.name = name


class Tile:
    def __init__(self, ap, name=""):
        self.t = ap
        self.res = Res(name)

    def __getitem__(self, idx):
        return self.t[idx]


class Eng:
    def __init__(self, k, name, h, is_pe=False):
        self.k = k
        self.name = name
        self.h = h
        self.is_pe = is_pe
        self.sem = k.newsem(name + "_s0")
        self.count = 0
        self.nep = 0
        self.seen = {}
        self.hist = [(self.sem, 0)]

    def tick(self, inst):
        if self.count >= EPOCH:
            self.nep += 1
            self.sem = self.k.newsem("%s_s%d" % (self.name, self.nep))
            self.count = 0
        self.count += 1
        inst.then_inc(self.sem, 1)
        return (self.sem, self.count, self.name)

    def cur(self):
        return (self.sem, self.count, self.name)

    def wait(self, ev):
        sem, val, _ = ev
        if val <= 0:
            return
        key = id(sem)
        if self.seen.get(key, 0) >= val:
            return
        self.h.wait_ge(sem, val)
        self.seen[key] = val


class DmaQ:
    def __init__(self, k, name, eng, nslots=8):
        self.k = k
        self.eng = eng
        self.name = name
        self.sems = [k.newsem("%s_d%d" % (name, i)) for i in range(nslots)]
        self.vals = [0] * nslots
        self.i = 0

    def issue(self, emit, deps):
        s = self.i % len(self.sems)
        self.i += 1
        sem = self.sems[s]
        if self.vals[s] > 0:
            self.eng.wait((sem, self.vals[s], "dma"))
        for ev in deps:
            self.eng.wait(ev)
        if self.vals[s] >= 16 * 1500:
            sem = self.k.newsem("%s_d%d_%d" % (self.name, s, self.i))
            self.sems[s] = sem
            self.vals[s] = 0
        inst = emit()
        self.vals[s] += 16
        inst.then_inc(sem, 16)
        return (sem, self.vals[s], "dma")


class K:
    def __init__(self, nc, stack):
        self.nc = nc
        self.stack = stack
        self.nsem = 0
        self.pe = Eng(self, "pe", nc.tensor, is_pe=True)
        self.act = Eng(self, "act", nc.scalar)
        self.dve = Eng(self, "dve", nc.vector)
        self.pool = Eng(self, "pool", nc.gpsimd)
        self.sp = Eng(self, "sp", nc.sync)
        self.engs = [self.pe, self.act, self.dve, self.pool, self.sp]
        self.q_sp = DmaQ(self, "qsp", self.sp, 12)
        self.q_pool = DmaQ(self, "qpool", self.pool, 8)
        self.qs = [self.q_sp, self.q_pool]
        self.n_inst = 0

    def newsem(self, name):
        self.nsem += 1
        return self.stack.enter_context(self.nc.semaphore(name))

    def sb(self, st, name, shape, dtype):
        self.uid = getattr(self, "uid", 0) + 1
        name = "%s_u%d" % (name, self.uid)
        t = st.enter_context(self.nc.sbuf_tensor(name, list(shape), dtype))
        return Tile(t, name)

    def ps(self, st, name, shape, dtype=F32):
        self.uid = getattr(self, "uid", 0) + 1
        name = "%s_u%d" % (name, self.uid)
        t = st.enter_context(self.nc.psum_tensor(name, list(shape), dtype))
        return Tile(t, name)

    @staticmethod
    def _resof(x):
        return x.res if isinstance(x, Tile) else x

    def _deps(self, reads, writes):
        deps = []
        for r in reads:
            r = self._resof(r)
            if r.w is not None:
                deps.append(r.w)
        for w in writes:
            w = self._resof(w)
            if w.w is not None:
                deps.append(w.w)
            deps.extend(w.r)
        return deps

    def _commit(self, ev, reads, writes):
        for r in reads:
            r = self._resof(r)
            r.r = [e for e in r.r if e[0] is not ev[0]]
            r.r.append(ev)
        for w in writes:
            w = self._resof(w)
            w.w = ev
            w.r = []

    def op(self, eng, emit, reads=(), writes=()):
        for ev in self._deps(reads, writes):
            if eng.is_pe and ev[2] == "pe":
                continue
            eng.wait(ev)
        inst = emit()
        ev = eng.tick(inst)
        self._commit(ev, reads, writes)
        self.n_inst += 1
        return ev

    def dma(self, q, out, in_, reads=(), writes=(), **kw):
        deps = self._deps(reads, writes)
        ev = q.issue(lambda: q.eng.h.dma_start(out=out, in_=in_, **kw), deps)
        self._commit(ev, reads, writes)
        self.n_inst += 1
        return ev

    def barrier(self):
        evs = [e.cur() for e in self.engs]
        for q in self.qs:
            for s, v in zip(q.sems, q.vals):
                evs.append((s, v, "dma"))
        for e in self.engs:
            for ev in evs:
                if ev[0] is e.sem:
                    continue
                e.wait(ev)


class Ring:
    def __init__(self, tiles):
        self.tiles = tiles
        self.i = 0

    def next(self):
        t = self.tiles[self.i % len(self.tiles)]
        self.i += 1
        return t


def _t5_bucket_np(rel):
    nb = 16
    max_exact = 8
    ret = np.where(rel > 0, nb, 0)
    n = np.abs(rel)
    nf = np.maximum(n, 1).astype(np.float32)
    large = max_exact + (np.log(nf / max_exact) / np.float32(math.log(128 / max_exact))
                         * (nb - max_exact)).astype(np.int32)
    large = np.minimum(large, nb - 1)
    return ret + np.where(n < max_exact, n, large)


DELTAS = [-128, 0, 128, 256, 384]
NBLK = 48
BSZ = 512
NSLOT = NBLK * BSZ
R0 = 511
NF = 1280


def make_consts():
    c = {}
    c["c_ident"] = np.eye(128, dtype=np.float32)
    c["c_exch"] = np.ascontiguousarray(np.eye(128, dtype=np.float32)[::-1])
    i = np.arange(128)[:, None]
    j = np.arange(128)[None, :]
    c["c_mlt"] = (i < j).astype(np.float32)
    c["c_uneg"] = -(i >= j).astype(np.float32)
    same = (i // 64) == (j // 64)
    c["c_gtri"] = (-(1.0 / 16.0) * (same & (i <= j))).astype(np.float32)
    c["c_gm2"] = (-(1.0 / 16.0) * (same & (i > j))).astype(np.float32)
    c["c_gmask"] = (same & (i <= j)).astype(np.float32)
    n = np.arange(NF)
    bk = _t5_bucket_np(R0 - n)
    oh = np.zeros((32, NF), np.float32)
    oh[bk, n] = 1.0
    oh[:, 1151:] = 0.0
    c["c_ohb"] = oh
    nm = np.zeros((5, 128, 512), np.float32)
    for di, dl in enumerate(DELTAS):
        kk = dl + np.arange(128)[:, None]
        qq = np.arange(512)[None, :]
        allowed = (kk // 64) <= (qq // 64)
        nm[di] = np.where(allowed, 0.0, -30000.0)
    c["c_negmask"] = nm
    p = np.arange(128, dtype=np.float32)[:, None]
    c["c_iog"] = (p + 128.0 * np.arange(8, dtype=np.float32)[None, :]).astype(np.float32)
    c["c_io2"] = (2.0 * p + np.arange(2, dtype=np.float32)[None, :]).astype(np.float32)
    c["c_iob"] = np.tile(np.arange(NBLK, dtype=np.float32)[None, :], (128, 1))
    return c


WEIGHT_NAMES = ["w_ada", "b_ada", "norm_mix", "norm_ffn", "norm_final", "rel_bias",
                "even_w_in", "even_lambda", "even_subln", "even_w_gk2", "even_b_gk", "even_gla_norm",
                "even_w_out", "odd_w_in", "odd_w_out", "router_group_w", "router_group_b",
                "router_expert_w", "router_expert_b", "expert_w_gate", "expert_w_up", "expert_w_down"]
WEIGHT_SHAPES = {
    "w_ada": [4, 1024, 6144], "b_ada": [4, 6144], "norm_mix": [4, 1024], "norm_ffn": [4, 1024],
    "norm_final": [1, 1024], "rel_bias": [32, 4], "even_w_in": [2, 1024, 3088], "even_lambda": [2, 256],
    "even_subln": [2, 128], "even_w_gk2": [2, 16, 256], "even_b_gk": [2, 256], "even_gla_norm": [2, 128],
    "even_w_out": [2, 1024, 1024], "odd_w_in": [2, 1024, 3072], "odd_w_out": [2, 1024, 1024],
    "router_group_w": [4, 1024, 4], "router_group_b": [4, 4], "router_expert_w": [4, 1024, 32],
    "router_expert_b": [4, 32], "expert_w_gate": [4, 32, 1024, 512], "expert_w_up": [4, 32, 1024, 512],
    "expert_w_down": [4, 32, 512, 1024],
}


def build(nlayers=DEPTH, stop=None, flags=()):
    SPARSE = 'dense' not in flags
    nc = bass.Bass("TRN2", target_bir_lowering=False)
    I = {}
    I["x"] = nc.dram_tensor("x", [S, D], F32, kind="ExternalInput").ap()
    I["c"] = nc.dram_tensor("c", [1, D], F32, kind="ExternalInput").ap()
    for nm in WEIGHT_NAMES:
        I[nm] = nc.dram_tensor(nm, WEIGHT_SHAPES[nm], F32, kind="ExternalInput").ap()
    consts = make_consts()
    for nm, v in consts.items():
        I[nm] = nc.dram_tensor(nm, list(v.shape), F32, kind="ExternalInput").ap()
    OUT = nc.dram_tensor("out", [S, D], F32, kind="ExternalOutput").ap()
    XS = nc.dram_tensor("xs_scr", [S, D], F32, kind="Internal").ap()
    FB = nc.dram_tensor("fb_scr", [25, 128, S], BF16, kind="Internal").ap()
    TB = nc.dram_tensor("tb_scr", [S, 1280], BF16, kind="Internal").ap()
    FS_T = nc.dram_tensor("fs_scr", [4, NF], F32, kind="Internal")
    FS = FS_T.ap()
    XN = nc.dram_tensor("xn_scr", [S, D], F32, kind="Internal").ap()
    MV = nc.dram_tensor("mv_scr", [2, D], F32, kind="Internal").ap()
    XG = nc.dram_tensor("xg_scr", [NSLOT, D], F32, kind="Internal").ap()
    YS = nc.dram_tensor("ys_scr", [NSLOT, D], F32, kind="Internal").ap()

    with ExitStack() as gst:
        k = K(nc, gst)
        pe, act, dve, pool, sp = k.pe, k.act, k.dve, k.pool, k.sp

        def mm(out, lhsT, rhs, start, stop, reads, writes):
            k.op(pe, lambda: nc.tensor.matmul(out, lhsT=lhsT, rhs=rhs, start=start, stop=stop), reads, writes)

        def tr(out, in_, ident, reads, writes):
            k.op(pe, lambda: nc.tensor.transpose(out=out, in_=in_, identity=ident), reads, writes)

        def actf(out, in_, func, reads, writes, **kw):
            k.op(act, lambda: nc.scalar.activation(out=out, in_=in_, func=func, **kw), reads, writes)

        def tt(eng, out, in0, in1, op, reads, writes):
            k.op(eng, lambda: eng.h.tensor_tensor(out=out, in0=in0, in1=in1, op=op), reads, writes)

        def ts(eng, out, in0, s1, s2, op0, op1, reads, writes):
            if op1 is None:
                k.op(eng, lambda: eng.h.tensor_scalar(out=out, in0=in0, scalar1=s1, scalar2=None, op0=op0),
                     reads, writes)
            else:
                k.op(eng, lambda: eng.h.tensor_scalar(out=out, in0=in0, scalar1=s1, scalar2=s2, op0=op0, op1=op1),
                     reads, writes)

        def stt(out, in0, scalar, in1, op0, op1, reads, writes):
            k.op(dve, lambda: nc.vector.scalar_tensor_tensor(out=out, in0=in0, scalar=scalar, in1=in1,
                                                             op0=op0, op1=op1), reads, writes)

        def cp(eng, out, in_, reads, writes):
            k.op(eng, lambda: eng.h.tensor_copy(out=out, in_=in_), reads, writes)

        def recip(out, in_, reads, writes):
            k.op(dve, lambda: nc.vector.reciprocal(out=out, in_=in_), reads, writes)

        def memset(eng, t, val, writes):
            k.op(eng, lambda: eng.h.memset(t, val), (), writes)

        def rmax(out, in_, reads, writes):
            k.op(dve, lambda: nc.vector.tensor_reduce(out=out, in_=in_, axis=AX.X, op=ALU.max), reads, writes)

        xres = [Res("x%d" % i) for i in range(NT)]
        fbres = [Res("fb%d" % i) for i in range(25)]
        tbres = Res("tb")
        fsres = Res("fs")

        ident32 = k.sb(gst, "ident32", [128, 128], F32)
        ones32 = k.sb(gst, "ones32", [128, 128], F32)
        onesb = k.sb(gst, "onesb", [128, 128], BF16)
        negonesb = k.sb(gst, "negonesb", [128, 128], BF16)
        zerob = k.sb(gst, "zerob", [128, 128], BF16)
        identb = k.sb(gst, "identb", [128, 128], BF16)
        k.dma(k.q_sp, ident32[:, :], I["c_ident"][:, :], writes=[ident32])
        memset(dve, ones32[:, :], 1.0, [ones32])
        memset(dve, onesb[:, :], 1.0, [onesb])
        memset(dve, negonesb[:, :], -1.0, [negonesb])
        memset(dve, zerob[:, :], 0.0, [zerob])
        cp(dve, identb[:, :], ident32[:, :], [ident32], [identb])

        modT = k.sb(gst, "modT", [128, 64], F32)
        gmod1 = k.sb(gst, "gmod1", [128, 8], F32)
        gmod2 = k.sb(gst, "gmod2", [128, 8], F32)
        gbc1 = k.sb(gst, "gbc1", [128, D], F32)
        gbc2 = k.sb(gst, "gbc2", [128, D], F32)
        scT = k.sb(gst, "scT", [128, 8], F32)
        cmb = k.sb(gst, "cmb", [128, NT, NEXP], F32)
        H = {}
        r_ohg = k.sb(gst, "r_ohg", [128, NT, 4], F32)
        r_oh1 = k.sb(gst, "r_oh1", [128, NT, 8], F32)
        r_oh2 = k.sb(gst, "r_oh2", [128, NT, 8], F32)
        r_v = k.sb(gst, "r_v", [128, 12, NT], F32)
        s12u = k.sb(gst, "s12u", [128, 2, NT], mybir.dt.uint32)
        idxg = k.sb(gst, "idxg", [128, NBLK, 8], mybir.dt.uint32)
        idxd = k.sb(gst, "idxd", [128, NBLK, 4], mybir.dt.uint32)
        idx2 = k.sb(gst, "idx2", [128, NBLK, 2], mybir.dt.uint32)
        gsb = k.sb(gst, "gsb", [128, 2, 8], F32)
        mvres = Res("mv")
        xnres = Res("xn")
        xgres = Res("xg")
        ysres = [Res("ys%d" % i) for i in range(NBLK)]

        def idma(out, out_off, in_, in_off, reads, writes):
            deps = k._deps(reads, writes)
            ev = k.q_pool.issue(lambda: nc.gpsimd.indirect_dma_start(out=out, out_offset=out_off, in_=in_,
                                                                     in_offset=in_off), deps)
            k._commit(ev, reads, writes)
            k.n_inst += 1
            return ev

        def emit_mod(l):
            with ExitStack() as st:
                crow = k.sb(st, "crow", [1, D], F32)
                sc_bc = k.sb(st, "sc_bc", [128, 8, 128], F32)
                wseg = Ring([k.sb(st, "wseg%d" % i, [128, 8, 512], F32) for i in range(2)])
                brow = Ring([k.sb(st, "brow%d" % i, [1, 512], F32) for i in range(2)])
                nrow = k.sb(st, "nrow", [1, 2 * D], F32)
                pm = k.ps(st, "pm", [128, 512], F32)
                pg = Ring([k.ps(st, "pg%d" % i, [128, 512], F32) for i in range(2)])
                k.dma(k.q_sp, crow[:, :], I["c"][:, :], writes=[crow])
                for kc in range(8):
                    mm(pm[:, kc:kc + 1], crow[0:1, kc * 128:(kc + 1) * 128], ones32[0:1, 0:1], True, True,
                       [crow, ones32], [pm])
                actf(scT[:, :], pm[:, 0:8], AF.Silu, [pm], [scT])
                for kc in range(8):
                    actf(sc_bc[:, kc, :], ones32[:, :], AF.Copy, [ones32, scT], [sc_bc], scale=scT[:, kc:kc + 1])
                wv = I["w_ada"][l].rearrange("(kc p) n -> p kc n", p=128)
                for seg in range(6):
                    for half in range(2):
                        n0 = seg * 1024 + half * 512
                        w = wseg.next()
                        b = brow.next()
                        k.dma(k.q_sp, w[:, :, :], wv[:, :, n0:n0 + 512], writes=[w])
                        k.dma(k.q_sp, b[:, :], I["b_ada"][l:l + 1, n0:n0 + 512], writes=[b])
                        if seg in (2, 5):
                            p = pg.next()
                            for kc in range(8):
                                mm(p[:, :], sc_bc[:, kc, :], w[:, kc, :], kc == 0, False, [sc_bc, w], [p])
                            mm(p[:, :], ones32[0:1, :], b[0:1, :], False, True, [ones32, b], [p])
                            g = gbc1 if seg == 2 else gbc2
                            cp(dve, g[:, half * 512:(half + 1) * 512], p[:, :], [p], [g])
                        else:
                            for j in range(4):
                                col = seg * 8 + half * 4 + j
                                for kc in range(8):
                                    mm(pm[:, col:col + 1], w[:, kc, j * 128:(j + 1) * 128], scT[:, kc:kc + 1],
                                       kc == 0, False, [w, scT], [pm])
                                mm(pm[:, col:col + 1], b[0:1, j * 128:(j + 1) * 128], ones32[0:1, 0:1], False, True,
                                   [b, ones32], [pm])
                k.dma(k.q_sp, nrow[:, 0:D], I["norm_mix"][l:l + 1, :], writes=[nrow])
                k.dma(k.q_sp, nrow[:, D:2 * D], I["norm_ffn"][l:l + 1, :], writes=[nrow])
                for j in range(16):
                    mm(pm[:, 48 + j:49 + j], nrow[0:1, j * 128:(j + 1) * 128], ones32[0:1, 0:1], True, True,
                       [nrow, ones32], [pm])
                cp(dve, modT[:, :], pm[:, 0:64], [pm], [modT])
                stt(gmod1[:, :], modT[:, 8:16], 1.0, modT[:, 48:56], ALU.add, ALU.mult, [modT], [gmod1])
                stt(gmod2[:, :], modT[:, 32:40], 1.0, modT[:, 56:64], ALU.add, ALU.mult, [modT], [gmod2])
                k.barrier()

        def emit_norm(xsrc, gmod, shiftc, layer, router):
            with ExitStack() as st:
                xt_r = Ring([k.sb(st, "n_xt%d" % i, [128, D], F32) for i in range(4)])
                xs_r = Ring([k.sb(st, "n_xs%d" % i, [128, D], F32) for i in range(3)])
                junk = k.sb(st, "n_junk", [128, D], BF16)
                sm_r = Ring([k.sb(st, "n_sm%d" % i, [128, 4], F32) for i in range(6)])
                ps_r = Ring([k.ps(st, "n_ps%d" % i, [128, D], F32) for i in range(2)])
                if router:
                    h32_r = Ring([k.sb(st, "n_h32%d" % i, [128, 8, 128], F32) for i in range(2)])
                    wr32 = k.sb(st, "n_wr32", [128, 8, 36], F32)
                    brr = k.sb(st, "n_brr", [1, 36], F32)
                    plg_r = Ring([k.ps(st, "n_plg%d" % i, [128, 512], F32) for i in range(2)])
                    lgall = k.sb(st, "n_lgall", [128, NT, 36], F32)
                    k.dma(k.q_sp, wr32[:, :, 0:4],
                          I["router_group_w"][layer].rearrange("(kc p) n -> p kc n", p=128), writes=[wr32])
                    k.dma(k.q_sp, wr32[:, :, 4:36],
                          I["router_expert_w"][layer].rearrange("(kc p) n -> p kc n", p=128), writes=[wr32])
                    k.dma(k.q_sp, brr[:, 0:4], I["router_group_b"][layer:layer + 1, :], writes=[brr])
                    k.dma(k.q_sp, brr[:, 4:36], I["router_expert_b"][layer:layer + 1, :], writes=[brr])
                state = {}

                def s1(t):
                    xt = xt_r.next()
                    k.dma(k.q_sp, xt[:, :], xsrc[t * 128:(t + 1) * 128, :], reads=[xres[t]], writes=[xt])
                    sm = sm_r.next()
                    actf(junk[:, :], xt[:, :], AF.Square, [xt], [junk, sm], accum_out=sm[:, 0:1])
                    state[t] = (xt, sm)

                def s2(t):
                    xt, sm = state[t]
                    ts(dve, sm[:, 1:2], sm[:, 0:1], 1.0 / D, EPS, ALU.mult, ALU.add, [sm], [sm])
                    actf(sm[:, 2:3], sm[:, 1:2], AF.Sqrt, [sm], [sm])
                    recip(sm[:, 3:4], sm[:, 2:3], [sm], [sm])
                    xs = xs_r.next()
                    ts(dve, xs[:, :], xt[:, :], sm[:, 3:4], None, ALU.mult, None, [xt, sm], [xs])
                    if router and SPARSE:
                        k.dma(k.q_sp, XN[t * 128:(t + 1) * 128, :], xs[:, :], reads=[xs], writes=[xnres])
                    state[t] = xs

                def s3(t):
                    xs = state.pop(t)
                    ps = ps_r.next()
                    for kc in range(8):
                        tr(ps[:, kc * 128:(kc + 1) * 128], xs[:, kc * 128:(kc + 1) * 128], ident32[:, :],
                           [xs, ident32], [ps])
                    for kc in range(8):
                        if router and SPARSE:
                            break
                        actf(H["hT"][:, kc, t * 128:(t + 1) * 128], ps[:, kc * 128:(kc + 1) * 128], AF.Identity,
                             [ps, gmod, modT], [H["hT"]], scale=gmod[:, kc:kc + 1], bias=shiftc[:, kc:kc + 1])
                    if router:
                        h32 = h32_r.next()
                        for kc in range(8):
                            actf(h32[:, kc, :], ps[:, kc * 128:(kc + 1) * 128], AF.Identity,
                                 [ps, gmod, modT], [h32], scale=gmod[:, kc:kc + 1], bias=shiftc[:, kc:kc + 1])
                        plg = plg_r.next()
                        for kc in range(8):
                            mm(plg[:, 0:36], h32[:, kc, :], wr32[:, kc, :], kc == 0, False, [h32, wr32], [plg])
                        mm(plg[:, 0:36], ones32[0:1, :], brr[0:1, :], False, True, [ones32, brr], [plg])
                        cp(dve, lgall[:, t, :], plg[:, 0:36], [plg], [lgall])

                for i in range(NT + 2):
                    if i < NT:
                        s1(i)
                    if 1 <= i <= NT:
                        s2(i - 1)
                    if i >= 2:
                        s3(i - 2)
                if router:
                    def bc(ap2, n):
                        return ap2.unsqueeze(2).to_broadcast([128, NT, n])
                    f4 = k.sb(st, "r_f4", [128, NT, 4], F32)
                    ohg = r_ohg
                    v = r_v
                    el = k.sb(st, "r_el", [128, NT, 8], F32)
                    t8 = k.sb(st, "r_t8", [128, NT, 8], F32)
                    oh1 = r_oh1
                    oh2 = r_oh2
                    elm = k.sb(st, "r_elm", [128, NT, 8], F32)
                    cg = k.sb(st, "r_cg", [128, NT, 8], F32)
                    RR = [f4, ohg, v, el, t8, oh1, oh2, elm, cg, lgall]
                    gm, gs, gval, l1, l2, dd, ee, den, w1, W1, W2 = [v[:, i, :] for i in range(11)]

                    def red(out, in_, op):
                        k.op(dve, lambda: nc.vector.tensor_reduce(out=out, in_=in_, axis=AX.X, op=op), RR, RR)

                    lgg = lgall[:, :, 0:4]
                    red(gm, lgg, ALU.max)
                    tt(dve, ohg[:, :, :], lgg, bc(gm, 4), ALU.is_equal, RR, RR)
                    tt(dve, f4[:, :, :], lgg, bc(gm, 4), ALU.subtract, RR, RR)
                    actf(f4[:, :, :], f4[:, :, :], AF.Exp, RR, RR)
                    red(gs, f4[:, :, :], ALU.add)
                    recip(gval, gs, RR, RR)
                    for g in range(4):
                        src = lgall[:, :, 4 + 8 * g:12 + 8 * g]
                        if g == 0:
                            tt(dve, el[:, :, :], src, bc(ohg[:, :, g], 8), ALU.mult, RR, RR)
                        else:
                            tt(dve, t8[:, :, :], src, bc(ohg[:, :, g], 8), ALU.mult, RR, RR)
                            tt(dve, el[:, :, :], el[:, :, :], t8[:, :, :], ALU.add, RR, RR)
                    red(l1, el[:, :, :], ALU.max)
                    tt(dve, oh1[:, :, :], el[:, :, :], bc(l1, 8), ALU.is_equal, RR, RR)
                    stt(elm[:, :, :], oh1[:, :, :], -1e30, el[:, :, :], ALU.mult, ALU.add, RR, RR)
                    red(l2, elm[:, :, :], ALU.max)
                    tt(dve, oh2[:, :, :], elm[:, :, :], bc(l2, 8), ALU.is_equal, RR, RR)
                    tt(dve, dd, l2, l1, ALU.subtract, RR, RR)
                    actf(ee, dd, AF.Exp, RR, RR)
                    ts(dve, den, ee, 1.0, None, ALU.add, None, RR, RR)
                    recip(w1, den, RR, RR)
                    tt(dve, W1, w1, gval, ALU.mult, RR, RR)
                    tt(dve, W2, W1, ee, ALU.mult, RR, RR)
                    tt(dve, cg[:, :, :], oh1[:, :, :], bc(W1, 8), ALU.mult, RR, RR)
                    tt(dve, t8[:, :, :], oh2[:, :, :], bc(W2, 8), ALU.mult, RR, RR)
                    tt(dve, cg[:, :, :], cg[:, :, :], t8[:, :, :], ALU.add, RR, RR)
                    for g in range(4):
                        tt(dve, cmb[:, :, 8 * g:8 * g + 8], cg[:, :, :], bc(ohg[:, :, g], 8), ALU.mult, RR, [cmb])
                k.barrier()

        def emit_inproj(wsrc, ncols, fjobs, tjobs):
            with ExitStack() as st:
                W = k.sb(st, "ip_W", [128, 8, ncols], BF16)
                wv = wsrc.rearrange("(kc p) n -> p kc n", p=128)
                c0 = 0
                while c0 < ncols:
                    w_ = min(512, ncols - c0)
                    k.dma(k.q_pool, W[:, :, c0:c0 + w_], wv[:, :, c0:c0 + w_], writes=[W])
                    c0 += w_
                stg_r = Ring([k.sb(st, "ip_stg%d" % i, [128, S], BF16) for i in range(2)])
                stt_r = Ring([k.sb(st, "ip_stt%d" % i, [128, 4, 512], BF16) for i in range(2)])
                ps_r = Ring([k.ps(st, "ip_ps%d" % i, [128, 512], F32) for i in range(4)])
                n = 0
                for (col0, nr, fb, scale) in fjobs:
                    stg = stg_r.next()
                    for tg in range(8):
                        ps = ps_r.next()
                        for kc in range(8):
                            mm(ps[0:nr, :], W[:, kc, col0:col0 + nr], H["hT"][:, kc, tg * 512:(tg + 1) * 512],
                               kc == 0, kc == 7, [W, H["hT"]], [ps])
                        if n % 2 == 0:
                            actf(stg[0:nr, tg * 512:(tg + 1) * 512], ps[0:nr, :], AF.Copy, [ps], [stg], scale=scale)
                        else:
                            ts(dve, stg[0:nr, tg * 512:(tg + 1) * 512], ps[0:nr, :], scale, None, ALU.mult, None,
                               [ps], [stg])
                        n += 1
                    k.dma(k.q_sp, FB[fb, 0:nr, :], stg[0:nr, :], reads=[stg], writes=[fbres[fb]])
                for (col0, wd, tcol0) in tjobs:
                    for t4 in range(8):
                        stg = stt_r.next()
                        for j in range(4):
                            t = t4 * 4 + j
                            ps = ps_r.next()
                            for kc in range(8):
                                mm(ps[:, 0:wd], H["hT"][:, kc, t * 128:(t + 1) * 128], W[:, kc, col0:col0 + wd],
                                   kc == 0, kc == 7, [W, H["hT"]], [ps])
                            if n % 2 == 0:
                                actf(stg[:, j, 0:wd], ps[:, 0:wd], AF.Copy, [ps], [stg])
                            else:
                                cp(dve, stg[:, j, 0:wd], ps[:, 0:wd], [ps], [stg])
                            n += 1
                        k.dma(k.q_sp,
                              TB[t4 * 512:(t4 + 1) * 512, tcol0:tcol0 + wd].rearrange("(j p) c -> p j c", p=128),
                              stg[:, :, 0:wd], reads=[stg], writes=[tbres])
                k.barrier()

        def emit_outproj(wsrc, gbc, xsrc):
            with ExitStack() as st:
                W = k.sb(st, "op_W", [128, 8, D], BF16)
                wv = wsrc.rearrange("(kc p) n -> p kc n", p=128)
                for h in range(2):
                    k.dma(k.q_pool, W[:, :, h * 512:(h + 1) * 512], wv[:, :, h * 512:(h + 1) * 512], writes=[W])
                for kc in range(8):
                    tt(dve, W[:, kc, :], W[:, kc, :], gbc[:, :], ALU.mult, [W, gbc], [W])
                xt_r = Ring([k.sb(st, "op_xt%d" % i, [128, D], F32) for i in range(3)])
                xn_r = Ring([k.sb(st, "op_xn%d" % i, [128, D], F32) for i in range(2)])
                ps_r = Ring([k.ps(st, "op_ps%d" % i, [128, D], F32) for i in range(3)])
                for t in range(NT):
                    xt = xt_r.next()
                    k.dma(k.q_sp, xt[:, :], xsrc[t * 128:(t + 1) * 128, :], reads=[xres[t]], writes=[xt])
                    ps = ps_r.next()
                    for h in range(2):
                        for kc in range(8):
                            mm(ps[:, h * 512:(h + 1) * 512], H["hT"][:, kc, t * 128:(t + 1) * 128],
                               W[:, kc, h * 512:(h + 1) * 512], kc == 0, kc == 7, [H["hT"], W], [ps])
                    xn = xn_r.next()
                    tt(dve, xn[:, :], xt[:, :], ps[:, :], ALU.add, [xt, ps], [xn])
                    k.dma(k.q_sp, XS[t * 128:(t + 1) * 128, :], xn[:, :], reads=[xn], writes=[xres[t]])
                k.barrier()

        def emit_odd_mixer():
            with ExitStack() as st:
                mltb = k.sb(st, "sb_mlt", [128, 128], BF16)
                unegb = k.sb(st, "sb_uneg", [128, 128], BF16)
                k.dma(k.q_pool, mltb[:, :], I["c_mlt"][:, :], writes=[mltb])
                k.dma(k.q_pool, unegb[:, :], I["c_uneg"][:, :], writes=[unegb])
                qT_r = Ring([k.sb(st, "sb_qT%d" % i, [128, S], BF16) for i in range(2)])
                kT_r = Ring([k.sb(st, "sb_kT%d" % i, [128, S], BF16) for i in range(2)])
                vt_r = Ring([k.sb(st, "sb_vt%d" % i, [128, NT, 128], BF16) for i in range(2)])
                e32_r = Ring([k.sb(st, "sb_e%d" % i, [128, 512], F32) for i in range(6)])
                sp_r = Ring([k.sb(st, "sb_sp%d" % i, [128, 512], BF16) for i in range(10)])
                rb_r = Ring([k.sb(st, "sb_rb%d" % i, [128, 512], BF16) for i in range(10)])
                ab_r = Ring([k.sb(st, "sb_ab%d" % i, [128, 512], BF16) for i in range(6)])
                R32s = [k.sb(st, "sb_R32_%d" % i, [128, 512], F32) for i in range(2)]
                pz_r = Ring([k.ps(st, "sb_pz%d" % i, [128, 512], F32) for i in range(3)])
                pw_r = Ring([k.ps(st, "sb_pw%d" % i, [128, 512], F32) for i in range(3)])
                pos = [k.ps(st, "sb_po%d" % i, [128, 512], F32) for i in range(2)]
                for hp in range(8):
                    qT = qT_r.next()
                    kT = kT_r.next()
                    vt = vt_r.next()
                    k.dma(k.q_sp, qT[:, :], FB[hp, :, :], reads=[fbres[hp]], writes=[qT])
                    k.dma(k.q_sp, kT[:, :], FB[8 + hp, :, :], reads=[fbres[8 + hp]], writes=[kT])
                    k.dma(k.q_sp, vt[:, :, :],
                          TB[:, hp * 128:(hp + 1) * 128].rearrange("(t p) c -> p t c", p=128),
                          reads=[tbres], writes=[vt])
                    for qg in range(8):
                        kbs = list(range(4 * qg + 3, -1, -1))
                        nb = len(kbs)
                        items = [[None] * nb, [None] * nb]
                        pend = {}
                        pendB = []
                        for j in range(2):
                            mm(pos[j][:, :], zerob[:, :], qT[:, qg * 512:(qg + 1) * 512], True, False,
                               [zerob, qT], [pos[j]])

                        def stageA(j, i):
                            b0 = 64 * j
                            R32 = R32s[j]
                            kb = kbs[i]
                            j0 = max(0, kb - 4 * qg)
                            c0 = 128 * j0
                            wq = 512 - c0
                            q0 = qg * 512 + c0
                            diag = kb >= 4 * qg
                            pz = pz_r.next()
                            mm(pz[:, 0:wq], kT[b0:b0 + 64, kb * 128:(kb + 1) * 128], qT[b0:b0 + 64, q0:q0 + wq],
                               True, True, [kT, qT], [pz])
                            e32 = e32_r.next()
                            actf(e32[:, 0:wq], pz[:, 0:wq], AF.Exp, [pz], [e32])
                            pend[(j, i)] = (R32, kb, c0, wq, q0, diag, e32)

                        def stageA2(j, i):
                            R32, kb, c0, wq, q0, diag, e32 = pend.pop((j, i))
                            spb = sp_r.next()
                            if 'sbx_noln' not in flags:
                                actf(spb[:, 0:wq], e32[:, 0:wq], AF.Ln, [e32], [spb], bias=1.0)
                            if diag:
                                tt(pool, spb[:, 0:128], spb[:, 0:128], mltb[:, :], ALU.mult, [spb, mltb], [spb])
                            rb = None
                            if i > 0 and 'sbx_nor' not in flags:
                                rb = rb_r.next()
                                cp(dve, rb[:, :], R32[:, :], [R32], [rb])
                            if i < nb - 1 and 'sbx_nor' not in flags:
                                if i == 0:
                                    if c0 > 0:
                                        memset(dve, R32[:, 0:c0], 0.0, [R32])
                                    cp(dve, R32[:, c0:512], spb[:, 0:wq], [spb], [R32])
                                else:
                                    tt(dve, R32[:, c0:512], R32[:, c0:512], spb[:, 0:wq], ALU.add, [R32, spb],
                                       [R32])
                            items[j][i] = (kb, c0, wq, q0, diag, spb, rb)

                        def stageB(j, i):
                            if 'sbx_nob' in flags:
                                return
                            b0 = 64 * j
                            po = pos[j]
                            kb, c0, wq, q0, diag, spb, rb = items[j][i]
                            pw = pw_r.next()
                            mm(pw[:, 0:wq], kT[b0:b0 + 64, kb * 128:(kb + 1) * 128], qT[b0:b0 + 64, q0:q0 + wq],
                               True, False, [kT, qT], [pw])
                            mm(pw[:, 0:wq], unegb[:, :], spb[:, 0:wq], False, rb is None, [unegb, spb], [pw])
                            if rb is not None:
                                mm(pw[:, 0:wq], negonesb[:, :], rb[:, c0:512], False, True, [negonesb, rb], [pw])
                            ab = ab_r.next()
                            actf(ab[:, 0:wq], pw[:, 0:wq], AF.Exp, [pw], [ab])
                            if diag:
                                tt(dve, ab[:, 0:128], ab[:, 0:128], mltb[:, :], ALU.mult, [ab, mltb], [ab])
                            pendB.append((po, c0, wq, kb, ab, i))
                            while len(pendB) > 2:
                                stageB2()

                        def stageB2():
                            po, c0, wq, kb, ab, i = pendB.pop(0)
                            mm(po[:, c0:512], vt[:, kb, :], ab[:, 0:wq], False, i == nb - 1, [vt, ab], [po])

                        GW = 2
                        nw = nb // GW
                        for w in range(nw + 1):
                            if w < nw:
                                for i in range(w * GW, (w + 1) * GW):
                                    for j in range(2):
                                        stageA(j, i)
                                for i in range(w * GW, (w + 1) * GW):
                                    for j in range(2):
                                        stageA2(j, i)
                            if w > 0:
                                for i in range((w - 1) * GW, w * GW):
                                    for j in range(2):
                                        stageB(j, i)
                        while pendB:
                            stageB2()
                        for j in range(2):
                            b0 = 64 * j
                            if j == 0:
                                actf(H["hT"][b0:b0 + 64, hp, qg * 512:(qg + 1) * 512], pos[j][b0:b0 + 64, :], AF.Copy,
                                     [pos[j]], [H["hT"]])
                            else:
                                cp(dve, H["hT"][b0:b0 + 64, hp, qg * 512:(qg + 1) * 512], pos[j][b0:b0 + 64, :],
                                   [pos[j]], [H["hT"]])
                k.barrier()

        def emit_even_mixer(i_even, layer):
            lam_init = 0.8 - 0.6 * math.exp(-0.3 * layer)
            with ExitStack() as st:
                BM = [[k.sb(st, "BM%d_%d" % (h, di), [128, 512], BF16) for di in range(5)] for h in range(4)]
                b15 = k.sb(st, "b15", [128, 4], F32)
                neglam = k.sb(st, "neglam", [128, 1], F32)
                gsub = k.sb(st, "gsub", [128, 1], F32)
                with ExitStack() as st2:
                    rb = k.sb(st2, "rb", [32, 4], F32)
                    ohb = k.sb(st2, "ohb", [32, NF], F32)
                    fsb = k.sb(st2, "fsb", [4, NF], F32)
                    exch = k.sb(st2, "exch", [128, 128], F32)
                    nmk = [k.sb(st2, "nmk%d" % di, [128, 512], F32) for di in range(5)]
                    tl_r = Ring([k.sb(st2, "tl%d" % i, [128, 512], F32) for i in range(2)])
                    lrow = k.sb(st2, "lrow", [1, 264], F32)
                    grow = k.sb(st2, "grow", [1, 128], F32)
                    pf = Ring([k.ps(st2, "pf%d" % i, [128, 512], F32) for i in range(2)])
                    k.dma(k.q_sp, rb[:, :], I["rel_bias"][:, :], writes=[rb])
                    k.dma(k.q_sp, ohb[:, :], I["c_ohb"][:, :], writes=[ohb])
                    k.dma(k.q_sp, exch[:, :], I["c_exch"][:, :], writes=[exch])
                    k.dma(k.q_sp, b15[:, :], I["rel_bias"][15].partition_broadcast(128), writes=[b15])
                    for di in range(5):
                        k.dma(k.q_sp, nmk[di][:, :], I["c_negmask"][di], writes=[nmk[di]])
                    for c3 in range(3):
                        c0 = c3 * 512
                        w_ = min(512, NF - c0)
                        p = pf.next()
                        mm(p[0:4, 0:w_], rb[0:32, 0:4], ohb[0:32, c0:c0 + w_], True, True, [rb, ohb], [p])
                        cp(dve, fsb[0:4, c0:c0 + w_], p[0:4, 0:w_], [p], [fsb])
                    k.dma(k.q_sp, FS[:, :], fsb[:, :], reads=[fsb], writes=[fsres])
                    for h in range(4):
                        for di, dl in enumerate(DELTAS):
                            tl = tl_r.next()
                            src = bass.AP(tensor=FS_T, offset=h * NF + (384 - dl), ap=[[1, 128], [1, 512]])
                            k.dma(k.q_sp, tl[:, :], src, reads=[fsres], writes=[tl])
                            p = pf.next()
                            mm(p[:, :], exch[:, :], tl[:, :], True, True, [exch, tl], [p])
                            tt(dve, BM[h][di][:, :], p[:, :], nmk[di][:, :], ALU.add, [p, nmk[di]], [BM[h][di]])
                    k.dma(k.q_sp, lrow[:, 0:256], I["even_lambda"][i_even:i_even + 1, :], writes=[lrow])
                    LR = [lrow]
                    tt(dve, lrow[:, 0:64], lrow[:, 0:64], lrow[:, 64:128], ALU.mult, LR, LR)
                    tt(dve, lrow[:, 128:192], lrow[:, 128:192], lrow[:, 192:256], ALU.mult, LR, LR)
                    k.op(dve, lambda: nc.vector.tensor_reduce(out=lrow[:, 256:257], in_=lrow[:, 0:64], axis=AX.X,
                                                              op=ALU.add), LR, LR)
                    k.op(dve, lambda: nc.vector.tensor_reduce(out=lrow[:, 257:258], in_=lrow[:, 128:192], axis=AX.X,
                                                              op=ALU.add), LR, LR)
                    actf(lrow[:, 258:260], lrow[:, 256:258], AF.Exp, LR, LR)
                    tt(dve, lrow[:, 260:261], lrow[:, 259:260], lrow[:, 258:259], ALU.subtract, LR, LR)
                    ts(dve, lrow[:, 261:262], lrow[:, 260:261], -lam_init, None, ALU.add, None, LR, LR)
                    p = pf.next()
                    mm(p[:, 0:1], ones32[0:1, :], lrow[0:1, 261:262], True, True, [ones32, lrow], [p])
                    cp(dve, neglam[:, :], p[:, 0:1], [p], [neglam])
                    k.dma(k.q_sp, grow[:, :], I["even_subln"][i_even:i_even + 1, :], writes=[grow])
                    p = pf.next()
                    mm(p[:, 0:1], grow[0:1, :], ones32[0:1, 0:1], True, True, [grow, ones32], [p])
                    ts(dve, gsub[:, :], p[:, 0:1], 1.0 - lam_init, None, ALU.mult, None, [p], [gsub])
                    k.barrier()
                qT_r = Ring([k.sb(st, "da_qT%d" % i, [128, S], BF16) for i in range(2)])
                kT_r = Ring([k.sb(st, "da_kT%d" % i, [128, S], BF16) for i in range(2)])
                vt_r = Ring([k.sb(st, "da_vt%d" % i, [128, NT, 128], BF16) for i in range(2)])
                E_r = Ring([k.sb(st, "da_E%d" % i, [128, 512], BF16) for i in range(5)])
                f_r = Ring([k.sb(st, "da_f%d" % i, [128, 512], F32) for i in range(4)])
                o32_r = Ring([k.sb(st, "da_o%d" % i, [128, 512], F32) for i in range(2)])
                sq_r = Ring([k.sb(st, "da_sq%d" % i, [128, 512], BF16) for i in range(2)])
                ps_r = Ring([k.ps(st, "da_ps%d" % i, [128, 512], F32) for i in range(3)])
                pu = [k.ps(st, "da_pu%d" % i, [128, 512], F32) for i in range(2)]
                pd = [k.ps(st, "da_pd%d" % i, [128, 512], F32) for i in range(2)]
                pss = k.ps(st, "da_pss", [128, 512], F32)
                for h in range(0 if 'ev_noA' in flags else 4):
                    qT = qT_r.next()
                    kT = kT_r.next()
                    vt = vt_r.next()
                    k.dma(k.q_sp, qT[:, :], FB[h, :, :], reads=[fbres[h]], writes=[qT])
                    k.dma(k.q_sp, kT[:, :], FB[4 + h, :, :], reads=[fbres[4 + h]], writes=[kT])
                    k.dma(k.q_sp, vt[:, :, :],
                          TB[:, h * 128:(h + 1) * 128].rearrange("(t p) c -> p t c", p=128),
                          reads=[tbres], writes=[vt])
                    for qg in range(8):
                        nkb = 4 * qg + 4
                        its = [(kb, m) for kb in range(nkb) for m in range(2)]
                        held = {}

                        def st1(idx):
                            kb, m = its[idx]
                            j0 = max(0, kb - 4 * qg)
                            c0 = 128 * j0
                            wq = 512 - c0
                            q0 = qg * 512 + c0
                            dl = kb * 128 - qg * 512
                            b0 = 64 * m
                            ps = ps_r.next()
                            near = dl >= -128
                            mm(ps[:, 0:wq], kT[b0:b0 + 64, kb * 128:(kb + 1) * 128], qT[b0:b0 + 64, q0:q0 + wq],
                               True, not near, [kT, qT], [ps])
                            E = E_r.next()
                            if near:
                                bm = BM[h][DELTAS.index(dl)]
                                mm(ps[:, 0:wq], identb[:, :], bm[:, c0:512], False, True, [identb, bm], [ps])
                                actf(E[:, 0:wq], ps[:, 0:wq], AF.Exp, [ps], [E])
                            else:
                                actf(E[:, 0:wq], ps[:, 0:wq], AF.Exp, [ps, b15], [E], bias=b15[:, h:h + 1])
                            held[idx] = (E, c0, wq)

                        def st2(idx):
                            kb, m = its[idx]
                            E, c0, wq = held.pop(idx)
                            mm(pu[m][:, c0:512], vt[:, kb, :], E[:, 0:wq], kb == 0, kb == nkb - 1, [vt, E], [pu[m]])
                            mm(pd[m][:, c0:512], onesb[:, :], E[:, 0:wq], kb == 0, kb == nkb - 1, [onesb, E],
                               [pd[m]])

                        SK = 2
                        for idx in range(len(its) + SK):
                            if idx < len(its):
                                st1(idx)
                            if idx >= SK:
                                st2(idx - SK)
                        r0 = f_r.next()
                        recip(r0[:, :], pd[0][:, :], [pd[0]], [r0])
                        t0 = f_r.next()
                        tt(dve, t0[:, :], pu[0][:, :], r0[:, :], ALU.mult, [pu[0], r0], [t0])
                        r1 = f_r.next()
                        recip(r1[:, :], pd[1][:, :], [pd[1]], [r1])
                        t1 = f_r.next()
                        tt(dve, t1[:, :], pu[1][:, :], r1[:, :], ALU.mult, [pu[1], r1], [t1])
                        o32 = o32_r.next()
                        stt(o32[:, :], t1[:, :], neglam[:, 0:1], t0[:, :], ALU.mult, ALU.add, [t1, t0, neglam], [o32])
                        sq = sq_r.next()
                        actf(sq[:, :], o32[:, :], AF.Square, [o32], [sq])
                        mm(pss[:, :], onesb[:, :], sq[:, :], True, True, [onesb, sq], [pss])
                        rs = f_r.next()
                        actf(rs[:, :], pss[:, :], AF.Sqrt, [pss], [rs], scale=1.0 / 128.0, bias=EPS)
                        rs2 = f_r.next()
                        recip(rs2[:, :], rs[:, :], [rs], [rs2])
                        stt(H["hT"][:, h, qg * 512:(qg + 1) * 512], o32[:, :], gsub[:, 0:1], rs2[:, :], ALU.mult, ALU.mult,
                            [o32, gsub, rs2], [H["hT"]])
                k.barrier()
            with ExitStack() as st:
                gtri = k.sb(st, "g_tri", [128, 128], F32)
                gm2 = k.sb(st, "g_m2", [128, 128], F32)
                gmaskb = k.sb(st, "g_mask", [128, 128], BF16)
                wgk = k.sb(st, "g_wgk", [16, 256], BF16)
                bgk = k.sb(st, "g_bgk", [1, 256], BF16)
                glag = k.sb(st, "g_lag", [128, 1], F32)
                grow = k.sb(st, "g_grow", [1, 128], F32)
                k.dma(k.q_sp, gtri[:, :], I["c_gtri"][:, :], writes=[gtri])
                k.dma(k.q_sp, gm2[:, :], I["c_gm2"][:, :], writes=[gm2])
                k.dma(k.q_pool, gmaskb[:, :], I["c_gmask"][:, :], writes=[gmaskb])
                k.dma(k.q_pool, wgk[:, :], I["even_w_gk2"][i_even], writes=[wgk])
                k.dma(k.q_pool, bgk[:, :], I["even_b_gk"][i_even:i_even + 1, :], writes=[bgk])
                k.dma(k.q_sp, grow[:, :], I["even_gla_norm"][i_even:i_even + 1, :], writes=[grow])
                pA = Ring([k.ps(st, "g_pA%d" % i, [128, 512], F32) for i in range(3)])
                pS = Ring([k.ps(st, "g_pS%d" % i, [128, 512], F32) for i in range(2)])
                pO = Ring([k.ps(st, "g_pO%d" % i, [128, 512], F32) for i in range(2)])
                p = pA.next()
                mm(p[:, 0:1], grow[0:1, :], ones32[0:1, 0:1], True, True, [grow, ones32], [p])
                cp(dve, glag[:, :], p[:, 0:1], [p], [glag])
                qg_r = Ring([k.sb(st, "g_q%d" % i, [128, 2, 512], BF16) for i in range(2)])
                kg_r = Ring([k.sb(st, "g_k%d" % i, [128, 2, 512], BF16) for i in range(2)])
                kt_r = Ring([k.sb(st, "g_kt%d" % i, [128, 4, 256], BF16) for i in range(2)])
                vg_r = Ring([k.sb(st, "g_v%d" % i, [128, 4, 512], BF16) for i in range(2)])
                bg_r = Ring([k.sb(st, "g_bg%d" % i, [16, 512], BF16) for i in range(2)])
                br_r = Ring([k.sb(st, "g_br%d" % i, [128, 4, 512], BF16) for i in range(2)])
                og_r = Ring([k.sb(st, "g_og%d" % i, [128, 4, 512], F32) for i in range(2)])
                e32_r = Ring([k.sb(st, "g_e%d" % i, [128, 256], F32) for i in range(2)])
                sp_r = Ring([k.sb(st, "g_sp%d" % i, [128, 256], F32) for i in range(2)])
                kf_r = Ring([k.sb(st, "g_kf%d" % i, [128, 256], F32) for i in range(2)])
                kd_r = Ring([k.sb(st, "g_kd%d" % i, [128, 256], BF16) for i in range(2)])
                eb_r = Ring([k.sb(st, "g_eb%d" % i, [128, 256], F32) for i in range(2)])
                en_r = Ring([k.sb(st, "g_en%d" % i, [128, 256], F32) for i in range(2)])
                qd_r = Ring([k.sb(st, "g_qd%d" % i, [128, 2, 128], BF16) for i in range(2)])
                ki_r = Ring([k.sb(st, "g_ki%d" % i, [128, 2, 128], BF16) for i in range(2)])
                at_r = Ring([k.sb(st, "g_at%d" % i, [128, 128], BF16) for i in range(3)])
                S32_r = [Ring([k.sb(st, "g_S32_%d_%d" % (h, i), [128, 128], F32) for i in range(2)]) for h in range(4)]
                Sb_r = [Ring([k.sb(st, "g_Sb_%d_%d" % (h, i), [128, 128], BF16) for i in range(3)]) for h in range(4)]
                sq_r = Ring([k.sb(st, "g_sq%d" % i, [128, 512], BF16) for i in range(2)])
                f_r = Ring([k.sb(st, "g_f%d" % i, [128, 512], F32) for i in range(4)])
                S32 = []
                Sb = []
                for h in range(4):
                    s = S32_r[h].next()
                    memset(dve, s[:, :], 0.0, [s])
                    S32.append(s)
                    b = Sb_r[h].next()
                    memset(pool, b[:, :], 0.0, [b])
                    Sb.append(b)
                for g8 in range(0 if 'ev_noB' in flags else 8):
                    t0g = g8 * 512
                    qgt = qg_r.next()
                    kgt = kg_r.next()
                    ktt = kt_r.next()
                    vgt = vg_r.next()
                    bgt = bg_r.next()
                    brt = br_r.next()
                    og = og_r.next()
                    for hh in range(2):
                        k.dma(k.q_sp, qgt[:, hh, :], FB[12 + hh, :, t0g:t0g + 512], reads=[fbres[12 + hh]], writes=[qgt])
                        k.dma(k.q_sp, kgt[:, hh, :], FB[14 + hh, :, t0g:t0g + 512], reads=[fbres[14 + hh]], writes=[kgt])
                    k.dma(k.q_sp, ktt[:, :, :],
                          TB[t0g:t0g + 512, 512:768].rearrange("(j p) c -> p j c", p=128), reads=[tbres], writes=[ktt])
                    k.dma(k.q_sp, vgt[:, :, :],
                          TB[t0g:t0g + 512, 768:1280].rearrange("(j p) c -> p j c", p=128), reads=[tbres], writes=[vgt])
                    k.dma(k.q_sp, bgt[:, :], FB[24, 0:16, t0g:t0g + 512], reads=[fbres[24]], writes=[bgt])
                    for h in range(4):
                        k.dma(k.q_sp, brt[:, h, :], FB[20 + h, :, t0g:t0g + 512], reads=[fbres[20 + h]], writes=[brt])
                    for j4 in range(4):
                        tc0 = j4 * 128
                        ppre = pA.next()
                        mm(ppre[:, 0:256], bgt[0:16, tc0:tc0 + 128], wgk[0:16, :], True, False, [bgt, wgk], [ppre])
                        mm(ppre[:, 0:256], onesb[0:1, :], bgk[0:1, :], False, True, [onesb, bgk], [ppre])
                        e32 = e32_r.next()
                        actf(e32[:, :], ppre[:, 0:256], AF.Exp, [ppre], [e32], scale=-1.0)
                        sp32 = sp_r.next()
                        actf(sp32[:, :], e32[:, :], AF.Ln, [e32], [sp32], bias=1.0)
                        pb = pA.next()
                        for hh in range(2):
                            mm(pb[:, hh * 128:(hh + 1) * 128], sp32[:, hh * 128:(hh + 1) * 128], gtri[:, :], True, True,
                               [sp32, gtri], [pb])
                        pk = pA.next()
                        mm(pk[:, 0:256], gm2[:, :], sp32[:, :], True, True, [gm2, sp32], [pk])
                        kf = kf_r.next()
                        actf(kf[:, :], pk[:, 0:256], AF.Exp, [pk], [kf])
                        kd = kd_r.next()
                        tt(dve, kd[:, :], ktt[:, j4, :], kf[:, :], ALU.mult, [ktt, kf], [kd])
                        eb = eb_r.next()
                        actf(eb[:, :], pb[:, 0:256], AF.Exp, [pb], [eb])
                        en = en_r.next()
                        actf(en[:, :], pb[:, 0:256], AF.Exp, [pb], [en], scale=-1.0)
                        qd = qd_r.next()
                        ki = ki_r.next()
                        for hh in range(2):
                            tt(dve, qd[:, hh, :], qgt[:, hh, tc0:tc0 + 128], eb[:, hh * 128:(hh + 1) * 128], ALU.mult,
                               [qgt, eb], [qd])
                            tt(dve, ki[:, hh, :], kgt[:, hh, tc0:tc0 + 128], en[:, hh * 128:(hh + 1) * 128], ALU.mult,
                               [kgt, en], [ki])
                        for h in range(4):
                            hh, jj = h // 2, h % 2
                            b0 = 64 * jj
                            psc = pS.next()
                            mm(psc[:, 0:128], ki[b0:b0 + 64, hh, :], qd[b0:b0 + 64, hh, :], True, True, [ki, qd], [psc])
                            at = at_r.next()
                            tt(dve, at[:, :], psc[:, 0:128], gmaskb[:, :], ALU.mult, [psc, gmaskb], [at])
                            po = pO.next()
                            mm(po[:, 0:128], vgt[:, j4, h * 128:(h + 1) * 128], at[:, :], True, False, [vgt, at], [po])
                            mm(po[:, 0:64], Sb[h][b0:b0 + 64, :], qd[b0:b0 + 64, hh, 0:64], False, False,
                               [Sb[h], qd], [po])
                            pst = pS.next()
                            mm(pst[:, 0:128], kd[0:64, hh * 128:(hh + 1) * 128], vgt[0:64, j4, h * 128:(h + 1) * 128],
                               True, True, [kd, vgt], [pst])
                            s1 = S32_r[h].next()
                            stt(s1[b0:b0 + 64, :], S32[h][b0:b0 + 64, :], eb[b0:b0 + 64, hh * 128 + 63:hh * 128 + 64],
                                pst[b0:b0 + 64, 0:128], ALU.mult, ALU.add, [S32[h], eb, pst], [s1])
                            sb1 = Sb_r[h].next()
                            actf(sb1[b0:b0 + 64, :], s1[b0:b0 + 64, :], AF.Copy, [s1], [sb1])
                            mm(po[:, 64:128], sb1[b0:b0 + 64, :], qd[b0:b0 + 64, hh, 64:128], False, True,
                               [sb1, qd], [po])
                            pst2 = pS.next()
                            mm(pst2[:, 0:128], kd[64:128, hh * 128:(hh + 1) * 128],
                               vgt[64:128, j4, h * 128:(h + 1) * 128], True, True, [kd, vgt], [pst2])
                            s2 = S32_r[h].next()
                            stt(s2[b0:b0 + 64, :], s1[b0:b0 + 64, :], eb[b0:b0 + 64, hh * 128 + 127:hh * 128 + 128],
                                pst2[b0:b0 + 64, 0:128], ALU.mult, ALU.add, [s1, eb, pst2], [s2])
                            sb2 = Sb_r[h].next()
                            actf(sb2[b0:b0 + 64, :], s2[b0:b0 + 64, :], AF.Copy, [s2], [sb2])
                            S32[h] = s2
                            Sb[h] = sb2
                            actf(og[:, h, tc0:tc0 + 128], po[:, 0:128], AF.Copy, [po], [og])
                    for h in range(4):
                        sq = sq_r.next()
                        actf(sq[:, :], og[:, h, :], AF.Square, [og], [sq])
                        pn = pA.next()
                        mm(pn[:, :], onesb[:, :], sq[:, :], True, True, [onesb, sq], [pn])
                        rs = f_r.next()
                        actf(rs[:, :], pn[:, :], AF.Sqrt, [pn], [rs], scale=1.0 / 128.0, bias=EPS)
                        rs2 = f_r.next()
                        recip(rs2[:, :], rs[:, :], [rs], [rs2])
                        sl = f_r.next()
                        actf(sl[:, :], brt[:, h, :], AF.Silu, [brt], [sl])
                        t1 = f_r.next()
                        stt(t1[:, :], og[:, h, :], glag[:, 0:1], rs2[:, :], ALU.mult, ALU.mult, [og, glag, rs2], [t1])
                        tt(dve, H["hT"][:, 4 + h, t0g:t0g + 512], t1[:, :], sl[:, :], ALU.mult, [t1, sl], [H["hT"]])
                k.barrier()

        def emit_moe(layer):
            with ExitStack() as st:
                wg_r = Ring([k.sb(st, "me_wg%d" % i, [128, 8, DEXP], BF16) for i in range(2)])
                wu_r = Ring([k.sb(st, "me_wu%d" % i, [128, 8, DEXP], BF16) for i in range(2)])
                wd_r = Ring([k.sb(st, "me_wd%d" % i, [128, 4, D], BF16) for i in range(2)])
                yacc = k.sb(st, "me_yacc", [128, 8, D], F32)
                aT_r = Ring([k.sb(st, "me_aT%d" % i, [128, 4, 512], BF16) for i in range(2)])
                sg_r = Ring([k.sb(st, "me_sg%d" % i, [128, 512], BF16) for i in range(3)])
                xt_r = Ring([k.sb(st, "me_xt%d" % i, [128, D], F32) for i in range(2)])
                pg_r = Ring([k.ps(st, "me_pg%d" % i, [128, 512], F32) for i in range(2)])
                pu_r = Ring([k.ps(st, "me_pu%d" % i, [128, 512], F32) for i in range(2)])
                py_r = Ring([k.ps(st, "me_py%d" % i, [128, D], F32) for i in range(2)])
                for qtr in range(4):
                    memset(dve, yacc[:, :, :], 0.0, [yacc])
                    jobs = [(e, tg) for e in range(NEXP) for tg in range(2)]
                    wts = {}

                    def load_w(e):
                        wg = wg_r.next()
                        wu = wu_r.next()
                        wd = wd_r.next()
                        k.dma(k.q_pool, wg[:, :, :],
                              I["expert_w_gate"][layer, e].rearrange("(kc p) f -> p kc f", p=128), writes=[wg])
                        k.dma(k.q_pool, wu[:, :, :],
                              I["expert_w_up"][layer, e].rearrange("(kc p) f -> p kc f", p=128), writes=[wu])
                        k.dma(k.q_pool, wd[:, :, :],
                              I["expert_w_down"][layer, e].rearrange("(fc p) n -> p fc n", p=128), writes=[wd])
                        wts[e] = (wg, wu, wd)

                    acts = {}

                    def emit_gu(e, tg):
                        wg, wu, wd = wts[e]
                        G = qtr * 2 + tg
                        aT = aT_r.next()
                        for fc in range(4):
                            pg = pg_r.next()
                            pu = pu_r.next()
                            for kc in range(8):
                                mm(pg[:, :], wg[:, kc, fc * 128:(fc + 1) * 128], H["hT"][:, kc, G * 512:(G + 1) * 512],
                                   kc == 0, kc == 7, [wg, H["hT"]], [pg])
                            for kc in range(8):
                                mm(pu[:, :], wu[:, kc, fc * 128:(fc + 1) * 128], H["hT"][:, kc, G * 512:(G + 1) * 512],
                                   kc == 0, kc == 7, [wu, H["hT"]], [pu])
                            sg = sg_r.next()
                            actf(sg[:, :], pg[:, :], AF.Silu, [pg], [sg])
                            tt(dve, aT[:, fc, :], sg[:, :], pu[:, :], ALU.mult, [sg, pu], [aT])
                        acts[(e, tg)] = aT

                    def emit_down(e, tg):
                        wg, wu, wd = wts[e]
                        aT = acts.pop((e, tg))
                        for t4 in range(4):
                            ti = tg * 4 + t4
                            T = qtr * 8 + ti
                            py = py_r.next()
                            for nh in range(2):
                                for fc in range(4):
                                    mm(py[:, nh * 512:(nh + 1) * 512], aT[:, fc, t4 * 128:(t4 + 1) * 128],
                                       wd[:, fc, nh * 512:(nh + 1) * 512], fc == 0, fc == 3, [aT, wd], [py])
                            stt(yacc[:, ti, :], py[:, :], cmb[:, T, e:e + 1], yacc[:, ti, :], ALU.mult, ALU.add,
                                [py, cmb, yacc], [yacc])

                    load_w(0)
                    for idx, (e, tg) in enumerate(jobs):
                        emit_gu(e, tg)
                        if idx > 0:
                            emit_down(*jobs[idx - 1])
                        if tg == 0 and e + 1 < NEXP:
                            load_w(e + 1)
                    emit_down(*jobs[-1])
                    for ti in range(8):
                        T = qtr * 8 + ti
                        xt = xt_r.next()
                        k.dma(k.q_sp, xt[:, :], XS[T * 128:(T + 1) * 128, :], reads=[xres[T]], writes=[xt])
                        tt(dve, yacc[:, ti, :], yacc[:, ti, :], gbc2[:, :], ALU.mult, [yacc, gbc2], [yacc])
                        tt(dve, xt[:, :], xt[:, :], yacc[:, ti, :], ALU.add, [xt, yacc], [xt])
                        k.dma(k.q_sp, XS[T * 128:(T + 1) * 128, :], xt[:, :], reads=[xt], writes=[xres[T]])
                k.barrier()

        def emit_slots(layer):
            with ExitStack() as st:
                Aall = k.sb(st, "sl_A", [128, NT, 32], BF16)
                ag = k.sb(st, "sl_ag", [128, NT, 8], F32)
                mltb = k.sb(st, "sl_mlt", [128, 128], BF16)
                pos = k.sb(st, "sl_pos", [128, NT, 32], F32)
                cnt = k.sb(st, "sl_cnt", [128, 32], F32)
                nb_ = k.sb(st, "sl_nb", [128, 32], F32)
                pa = k.sb(st, "sl_pa", [128, 32], F32)
                pb = k.sb(st, "sl_pb", [128, 32], F32)
                smg = k.sb(st, "sl_smg", [128, NT, 8], F32)
                t8 = k.sb(st, "sl_t8", [128, NT, 8], F32)
                sf = k.sb(st, "sl_sf", [128, 2, NT], F32)
                iog = k.sb(st, "sl_iog", [128, 8], F32)
                iob = k.sb(st, "sl_iob", [128, NBLK], F32)
                cmpt = k.sb(st, "sl_cmp", [128, NBLK, 32], F32)
                eb = k.sb(st, "sl_eb", [128, NBLK], F32)
                fg = k.sb(st, "sl_fg", [128, NBLK, 8], F32)
                ppos = k.ps(st, "sl_ppos", [128, NT * 32], F32)
                pcnt = k.ps(st, "sl_pcnt", [128, 512], F32)
                k.dma(k.q_pool, mltb[:, :], I["c_mlt"][:, :], writes=[mltb])
                k.dma(k.q_sp, iog[:, :], I["c_iog"][:, :], writes=[iog])
                k.dma(k.q_sp, iob[:, :], I["c_iob"][:, :], writes=[iob])
                RR = [Aall, ag, pos, cnt, nb_, pa, pb, smg, t8, sf, cmpt, eb, fg, r_ohg, r_oh1, r_oh2, r_v]

                def bc(ap2, n):
                    return ap2.unsqueeze(2).to_broadcast([128, NT, n])
                tt(dve, ag[:, :, :], r_oh1[:, :, :], r_oh2[:, :, :], ALU.add, RR, RR)
                for g in range(4):
                    tt(dve, Aall[:, :, 8 * g:8 * g + 8], ag[:, :, :], bc(r_ohg[:, :, g], 8), ALU.mult, RR, RR)
                for T in range(NT):
                    for T2 in range(T):
                        mm(ppos[:, T * 32:(T + 1) * 32], onesb[:, :], Aall[:, T2, :], T2 == 0, False,
                           [onesb, Aall], [ppos])
                    mm(ppos[:, T * 32:(T + 1) * 32], mltb[:, :], Aall[:, T, :], T == 0, True, [mltb, Aall], [ppos])
                for T in range(NT):
                    mm(pcnt[:, 0:32], onesb[:, :], Aall[:, T, :], T == 0, T == NT - 1, [onesb, Aall], [pcnt])
                cp(dve, pos[:, 0:16, :], ppos[:, 0:512].rearrange("p (t e) -> p t e", e=32), [ppos], RR)
                cp(dve, pos[:, 16:32, :], ppos[:, 512:1024].rearrange("p (t e) -> p t e", e=32), [ppos], RR)
                cp(dve, cnt[:, :], pcnt[:, 0:32], [pcnt], RR)
                ts(dve, nb_[:, :], cnt[:, :], 0.0, None, ALU.is_gt, None, RR, RR)
                for m in range(1, 8):
                    stt(nb_[:, :], cnt[:, :], float(BSZ * m), nb_[:, :], ALU.is_gt, ALU.add, RR, RR)
                cp(dve, pa[:, :], nb_[:, :], RR, RR)
                a_, b_ = pa, pb
                for sh in (1, 2, 4, 8, 16):
                    cp(dve, b_[:, 0:sh], a_[:, 0:sh], RR, RR)
                    tt(dve, b_[:, sh:32], a_[:, sh:32], a_[:, 0:32 - sh], ALU.add, RR, RR)
                    a_, b_ = b_, a_
                incl = a_
                excl = b_
                tt(dve, excl[:, :], incl[:, :], nb_[:, :], ALU.subtract, RR, RR)
                ts(dve, excl[:, :], excl[:, :], float(BSZ), None, ALU.mult, None, RR, RR)
                tt(dve, pos[:, :, :], pos[:, :, :], excl[:, :].unsqueeze(1).to_broadcast([128, NT, 32]), ALU.add,
                   RR, RR)
                for g in range(4):
                    if g == 0:
                        tt(dve, smg[:, :, :], pos[:, :, 0:8], bc(r_ohg[:, :, 0], 8), ALU.mult, RR, RR)
                    else:
                        tt(dve, t8[:, :, :], pos[:, :, 8 * g:8 * g + 8], bc(r_ohg[:, :, g], 8), ALU.mult, RR, RR)
                        tt(dve, smg[:, :, :], smg[:, :, :], t8[:, :, :], ALU.add, RR, RR)
                tt(dve, t8[:, :, :], smg[:, :, :], r_oh1[:, :, :], ALU.mult, RR, RR)
                k.op(dve, lambda: nc.vector.tensor_reduce(out=sf[:, 0, :], in_=t8[:, :, :], axis=AX.X, op=ALU.add),
                     RR, RR)
                tt(dve, t8[:, :, :], smg[:, :, :], r_oh2[:, :, :], ALU.mult, RR, RR)
                k.op(dve, lambda: nc.vector.tensor_reduce(out=sf[:, 1, :], in_=t8[:, :, :], axis=AX.X, op=ALU.add),
                     RR, RR)
                cp(dve, s12u[:, :, :], sf[:, :, :], RR, [s12u])
                tt(dve, cmpt[:, :, :], incl[:, :].unsqueeze(1).to_broadcast([128, NBLK, 32]),
                   iob[:, :].unsqueeze(2).to_broadcast([128, NBLK, 32]), ALU.is_le, RR + [iob], RR)
                k.op(dve, lambda: nc.vector.tensor_reduce(out=eb[:, :], in_=cmpt[:, :, :], axis=AX.X, op=ALU.add),
                     RR, RR)
                ts(dve, eb[:, :], eb[:, :], 31.0, None, ALU.min, None, RR, RR)
                ts(dve, eb[:, :], eb[:, :], 512.0, float(layer * NEXP * DEXP), ALU.mult, ALU.add, RR, RR)
                tt(dve, fg[:, :, 0:4], eb[:, :].unsqueeze(2).to_broadcast([128, NBLK, 4]),
                   iog[:, 0:4].unsqueeze(1).to_broadcast([128, NBLK, 4]), ALU.add, RR + [iog], RR)
                cp(dve, idxd[:, :, :], fg[:, :, 0:4], RR, [idxd])
                ts(dve, eb[:, :], eb[:, :], 2.0, None, ALU.mult, None, RR, RR)
                tt(dve, fg[:, :, :], eb[:, :].unsqueeze(2).to_broadcast([128, NBLK, 8]),
                   iog[:, :].unsqueeze(1).to_broadcast([128, NBLK, 8]), ALU.add, RR + [iog], RR)
                cp(dve, idxg[:, :, :], fg[:, :, :], RR, [idxg])
                io2 = k.sb(st, "sl_io2", [128, 2], F32)
                k.dma(k.q_sp, io2[:, :], I["c_io2"][:, :], writes=[io2])
                ts(dve, eb[:, :], eb[:, :], 0.25, None, ALU.mult, None, RR, RR)
                tt(dve, fg[:, :, 0:2], eb[:, :].unsqueeze(2).to_broadcast([128, NBLK, 2]),
                   io2[:, :].unsqueeze(1).to_broadcast([128, NBLK, 2]), ALU.add, RR + [io2], RR)
                cp(dve, idx2[:, :, :], fg[:, :, 0:2], RR, [idx2])
                k.dma(k.q_sp, MV[0].rearrange("(kc p) -> p kc", p=128), gmod2[:, :], reads=[gmod2], writes=[mvres],
                      allow_slow_non_contiguous=True)
                k.dma(k.q_sp, MV[1].rearrange("(kc p) -> p kc", p=128), modT[:, 24:32], reads=[modT], writes=[mvres],
                      allow_slow_non_contiguous=True)
                k.dma(k.q_sp, gsb[:, 0, :], MV[0].rearrange("(p kc) -> p kc", kc=8), reads=[mvres], writes=[gsb])
                k.dma(k.q_sp, gsb[:, 1, :], MV[1].rearrange("(p kc) -> p kc", kc=8), reads=[mvres], writes=[gsb])
                xr_r = Ring([k.sb(st, "sl_xr%d" % i, [128, D], F32) for i in range(3)])
                for T in range(NT):
                    xr = xr_r.next()
                    k.dma(k.q_sp, xr[:, :], XN[T * 128:(T + 1) * 128, :], reads=[xnres], writes=[xr])
                    for kk in range(2):
                        idma(XG[:, :], bass.IndirectOffsetOnAxis(ap=s12u[:, kk, T:T + 1], axis=0), xr[:, :], None,
                             [xr, s12u], [xgres])
                k.barrier()

        def emit_moe_sparse(layer):
            with ExitStack() as st:
                wg_r = Ring([k.sb(st, "ms_wg%d" % i, [128, 8, DEXP], BF16) for i in range(3)])
                wu_r = Ring([k.sb(st, "ms_wu%d" % i, [128, 8, DEXP], BF16) for i in range(3)])
                wd_r = Ring([k.sb(st, "ms_wd%d" % i, [128, 4, D], BF16) for i in range(3)])
                xg_r = Ring([k.sb(st, "ms_xg%d" % i, [128, D], F32) for i in range(8)])
                hs_r = Ring([k.sb(st, "ms_hs%d" % i, [128, 8, 512], BF16) for i in range(2)])
                aT_r = Ring([k.sb(st, "ms_aT%d" % i, [128, 4, 512], BF16) for i in range(2)])
                sg_r = Ring([k.sb(st, "ms_sg%d" % i, [128, 512], BF16) for i in range(3)])
                yo_r = Ring([k.sb(st, "ms_yo%d" % i, [128, D], F32) for i in range(3)])
                ptr_r = Ring([k.ps(st, "ms_ptr%d" % i, [128, 512], F32) for i in range(2)])
                pg_r = Ring([k.ps(st, "ms_pg%d" % i, [128, 512], F32) for i in range(2)])
                pu_r = Ring([k.ps(st, "ms_pu%d" % i, [128, 512], F32) for i in range(2)])
                py_r = Ring([k.ps(st, "ms_py%d" % i, [128, D], F32) for i in range(1)])
                wgv = I["expert_w_gate"].rearrange("l e (p q) f -> (l e p) (q f)", q=8).rearrange(
                    "r (h c) -> (r h) c", h=2)
                wuv = I["expert_w_up"].rearrange("l e (p q) f -> (l e p) (q f)", q=8).rearrange(
                    "r (h c) -> (r h) c", h=2)
                wdv = I["expert_w_down"].rearrange("l e (p q) n -> (l e p) (q n)", q=4).rearrange(
                    "r (h c) -> (r h) c", h=2)
                wts = {}
                hss = {}
                hss_keep = {}
                acts = {}

                def load_blk(b):
                    if 'ms_nowl' in flags and b >= 3:
                        wts[b] = wts[b - 3]
                        return
                    wg = wg_r.next()
                    wu = wu_r.next()
                    wd = wd_r.next()
                    for h in range(2):
                        off = bass.IndirectOffsetOnAxis(ap=idx2[:, b, h:h + 1], axis=0)
                        idma(wg[:, 4 * h:4 * h + 4, :].rearrange("p a b -> p (a b)"), None, wgv, off, [idx2], [wg])
                        idma(wu[:, 4 * h:4 * h + 4, :].rearrange("p a b -> p (a b)"), None, wuv, off, [idx2], [wu])
                        idma(wd[:, 2 * h:2 * h + 2, :].rearrange("p a b -> p (a b)"), None, wdv, off, [idx2], [wd])
                    wts[b] = (wg, wu, wd)

                xgs = {}

                def load_x(b):
                    tl = []
                    for j in range(4):
                        xg = xg_r.next()
                        r0 = b * BSZ + j * 128
                        k.dma(k.q_sp, xg[:, :], XG[r0:r0 + 128, :], reads=[xgres], writes=[xg])
                        tl.append(xg)
                    xgs[b] = tl

                def prep_h(b):
                    hs = hs_r.next()
                    tl = xgs.pop(b)
                    for j in range(4):
                        xg = tl[j]
                        for hf in range(2):
                            ptr = ptr_r.next()
                            for q in range(4):
                                kc = hf * 4 + q
                                tr(ptr[:, q * 128:(q + 1) * 128], xg[:, kc::8], ident32[:, :], [xg, ident32], [ptr])
                            for q in range(4):
                                kc = hf * 4 + q
                                actf(hs[:, kc, j * 128:(j + 1) * 128], ptr[:, q * 128:(q + 1) * 128], AF.Identity,
                                     [ptr, gsb], [hs], scale=gsb[:, 0, kc:kc + 1], bias=gsb[:, 1, kc:kc + 1])
                    hss[b] = hs

                def emit_gu(b):
                    wg, wu, wd = wts[b]
                    hs = hss.pop(b)
                    aT = aT_r.next()
                    for fc in range(4):
                        pg = pg_r.next()
                        pu = pu_r.next()
                        for kc in range(8):
                            mm(pg[:, :], wg[:, kc, fc::4], hs[:, kc, :], kc == 0, kc == 7,
                               [wg, hs], [pg])
                        for kc in range(8):
                            mm(pu[:, :], wu[:, kc, fc::4], hs[:, kc, :], kc == 0, kc == 7,
                               [wu, hs], [pu])
                        sg = sg_r.next()
                        actf(sg[:, :], pg[:, :], AF.Silu, [pg], [sg])
                        tt(dve, aT[:, fc, :], sg[:, :], pu[:, :], ALU.mult, [sg, pu], [aT])
                    acts[b] = aT

                def emit_down(b):
                    wg, wu, wd = wts[b]
                    aT = acts.pop(b)
                    for j in range(4):
                        py = py_r.next()
                        for nh in range(2):
                            for fc in range(4):
                                mm(py[:, nh * 512:(nh + 1) * 512], aT[:, fc, j * 128:(j + 1) * 128],
                                   wd[:, fc, nh * 512:(nh + 1) * 512], fc == 0, fc == 3, [aT, wd], [py])
                        yo = yo_r.next()
                        cp(dve, yo[:, :], py[:, :], [py], [yo])
                        r0 = b * BSZ + j * 128
                        k.dma(k.q_sp, YS[r0:r0 + 128, :], yo[:, :], reads=[yo], writes=[ysres[b]])

                load_blk(0)
                load_blk(1)
                load_x(0)
                prep_h(0)
                load_x(1)
                for b in range(NBLK):
                    emit_gu(b)
                    if b > 0:
                        emit_down(b - 1)
                    if b + 2 < NBLK:
                        load_blk(b + 2)
                    if b + 1 < NBLK:
                        prep_h(b + 1)
                    if b + 2 < NBLK:
                        load_x(b + 2)
                emit_down(NBLK - 1)
                k.barrier()
            with ExitStack() as st:
                r1_r = Ring([k.sb(st, "ms_r1%d" % i, [128, D], F32) for i in range(2)])
                r2_r = Ring([k.sb(st, "ms_r2%d" % i, [128, D], F32) for i in range(2)])
                xt_r = Ring([k.sb(st, "ms_xt%d" % i, [128, D], F32) for i in range(2)])
                for T in range(NT):
                    r1 = r1_r.next()
                    r2 = r2_r.next()
                    idma(r1[:, :], None, YS[:, :], bass.IndirectOffsetOnAxis(ap=s12u[:, 0, T:T + 1], axis=0),
                         ysres + [s12u], [r1])
                    idma(r2[:, :], None, YS[:, :], bass.IndirectOffsetOnAxis(ap=s12u[:, 1, T:T + 1], axis=0),
                         ysres + [s12u], [r2])
                    xt = xt_r.next()
                    k.dma(k.q_sp, xt[:, :], XS[T * 128:(T + 1) * 128, :], reads=[xres[T]], writes=[xt])
                    ts(dve, r1[:, :], r1[:, :], r_v[:, 9, T:T + 1], None, ALU.mult, None, [r1, r_v], [r1])
                    stt(r1[:, :], r2[:, :], r_v[:, 10, T:T + 1], r1[:, :], ALU.mult, ALU.add, [r2, r_v, r1], [r1])
                    tt(dve, r1[:, :], r1[:, :], gbc2[:, :], ALU.mult, [r1, gbc2], [r1])
                    tt(dve, xt[:, :], xt[:, :], r1[:, :], ALU.add, [xt, r1], [xt])
                    k.dma(k.q_sp, XS[T * 128:(T + 1) * 128, :], xt[:, :], reads=[xt], writes=[xres[T]])
                k.barrier()

        def emit_final(do_norm):
            with ExitStack() as st:
                xt_r = Ring([k.sb(st, "f_xt%d" % i, [128, D], F32) for i in range(3)])
                xo_r = Ring([k.sb(st, "f_xo%d" % i, [128, D], F32) for i in range(2)])
                junk = k.sb(st, "f_junk", [128, D], BF16)
                sm_r = Ring([k.sb(st, "f_sm%d" % i, [128, 4], F32) for i in range(4)])
                gf = k.sb(st, "f_gf", [128, D], F32)
                k.dma(k.q_sp, gf[:, :], I["norm_final"][0].partition_broadcast(128), writes=[gf])
                evs = []
                for t in range(NT):
                    xt = xt_r.next()
                    k.dma(k.q_sp, xt[:, :], XS[t * 128:(t + 1) * 128, :], reads=[xres[t]], writes=[xt])
                    if do_norm:
                        sm = sm_r.next()
                        actf(junk[:, :], xt[:, :], AF.Square, [xt], [junk, sm], accum_out=sm[:, 0:1])
                        ts(dve, sm[:, 1:2], sm[:, 0:1], 1.0 / D, EPS, ALU.mult, ALU.add, [sm], [sm])
                        actf(sm[:, 2:3], sm[:, 1:2], AF.Sqrt, [sm], [sm])
                        recip(sm[:, 3:4], sm[:, 2:3], [sm], [sm])
                        xo = xo_r.next()
                        stt(xo[:, :], xt[:, :], sm[:, 3:4], gf[:, :], ALU.mult, ALU.mult, [xt, sm, gf], [xo])
                        src = xo
                    else:
                        src = xt
                    evs.append(k.dma(k.q_sp, OUT[t * 128:(t + 1) * 128, :], src[:, :], reads=[src]))
                for ev in evs:
                    sp.wait(ev)

        xsrc = I["x"]
        done = False
        if stop == ("evenonly",):
            H["hT"] = k.sb(gst, "hT", [128, 8, S], BF16)
            emit_even_mixer(0, 0)
            emit_final(False)
            done = True
            nlayers = 0
        if stop == ("oddonly",):
            H["hT"] = k.sb(gst, "hT", [128, 8, S], BF16)
            emit_odd_mixer()
            emit_final(False)
            done = True
            nlayers = 0
        for l in range(nlayers):
            emit_mod(l)
            lst = ExitStack()
            H["hT"] = k.sb(lst, "hT", [128, 8, S], BF16)
            emit_norm(xsrc, gmod1, modT[:, 0:8], l, router=False)
            ie = l // 2
            if 'skipmix' in flags:
                pass
            elif l % 2 == 0:
                fj = [(c * 128, 128, c, 0.125) for c in range(4)]
                fj += [(512 + c * 128, 128, 4 + c, 1.0) for c in range(4)]
                fj += [(1536 + c * 128, 128, 12 + c, 0.125) for c in range(2)]
                fj += [(1792 + c * 128, 128, 14 + c, 1.0) for c in range(2)]
                fj += [(2560 + c * 128, 128, 20 + c, 1.0) for c in range(4)]
                fj += [(3072, 16, 24, 1.0)]
                tj = [(1024, 512, 0), (1792, 256, 512), (2048, 512, 768)]
                emit_inproj(I["even_w_in"][ie], EVEN_COLS, fj, tj)
                emit_even_mixer(ie, l)
                emit_outproj(I["even_w_out"][ie], gbc1, xsrc)
            else:
                fj = [(c * 128, 128, c, 0.125) for c in range(8)]
                fj += [(1024 + c * 128, 128, 8 + c, 1.0) for c in range(8)]
                tj = [(2048, 512, 0), (2560, 512, 512)]
                emit_inproj(I["odd_w_in"][ie], 3072, fj, tj)
                emit_odd_mixer()
                emit_outproj(I["odd_w_out"][ie], gbc1, xsrc)
            if 'skipmix' not in flags:
                xsrc = XS
            if SPARSE:
                lst.close()
                H.pop("hT")
            if stop == ("mix", l):
                emit_final(False)
                done = True
                break
            emit_norm(xsrc, gmod2, modT[:, 24:32], l, router=True)
            if 'nomoe' not in flags:
                if SPARSE:
                    emit_slots(layer=l)
                    emit_moe_sparse(l)
                else:
                    emit_moe(l)
            if not SPARSE:
                lst.close()
            if stop == ("ffn", l):
                emit_final(False)
                done = True
                break
        if not done:
            emit_final(True)
        build.stats = (k.n_inst, k.nsem)
    return nc


_CACHE = {}


def _get_nc(nlayers=DEPTH, stop=None):
    key = (nlayers, stop)
    if key not in _CACHE:
        _CACHE[key] = build(nlayers, stop)
    return _CACHE[key]


def make_in_maps(inputs, cores):
    consts = make_consts()
    shared = {}
    for nm in WEIGHT_NAMES:
        a = np.ascontiguousarray(np.asarray(inputs[nm], dtype=np.float32))
        shared[nm] = a.reshape(WEIGHT_SHAPES[nm])
    shared.update(consts)
    x = np.asarray(inputs["x"], dtype=np.float32)
    c = np.asarray(inputs["c"], dtype=np.float32)
    maps = []
    for b in cores:
        m = dict(shared)
        m["x"] = np.ascontiguousarray(x[b])
        m["c"] = np.ascontiguousarray(c[b:b + 1])
        maps.append(m)
    return maps


def kernel(**inputs):
    nc = _get_nc()
    in_maps = make_in_maps(inputs, list(range(8)))
    res = run_bass_kernel_spmd(nc, in_maps, core_ids=list(range(8)))
    return np.stack([np.asarray(r["out"]) for r in res.results], axis=0).astype(np.float32)
```

```python
import math
from contextlib import ExitStack
import numpy as np
import concourse.bass as bass
import concourse.mybir as mybir
from concourse.bass_utils import run_bass_kernel_spmd

F32 = mybir.dt.float32
BF16 = mybir.dt.bfloat16
AF = mybir.ActivationFunctionType
ALU = mybir.AluOpType
AX = mybir.AxisListType

S = 4096
D = 1024
NT = 32
DEPTH = 4
EPS = 1e-6
EVEN_COLS = 3088
NEXP = 32
DEXP = 512
EPOCH = 12000


class Res:
    __slots__ = ("w", "r", "name")

    def __init__(self, name=""):
        self.w = None
        self.r = []
        self.name = name


class Tile:
    def __init__(self, ap, name=""):
        self.t = ap
        self.res = Res(name)

    def __getitem__(self, idx):
        return self.t[idx]


class Eng:
    def __init__(self, k, name, h, is_pe=False):
        self.k = k
        self.name = name
        self.h = h
        self.is_pe = is_pe
        self.sem = k.newsem(name + "_s0")
        self.count = 0
        self.nep = 0
        self.seen = {}
        self.hist = [(self.sem, 0)]

    def tick(self, inst):
        if self.count >= EPOCH:
            self.nep += 1
            self.sem = self.k.newsem("%s_s%d" % (self.name, self.nep))
            self.count = 0
        self.count += 1
        inst.then_inc(self.sem, 1)
        return (self.sem, self.count, self.name)

    def cur(self):
        return (self.sem, self.count, self.name)

    def wait(self, ev):
        sem, val, _ = ev
        if val <= 0:
            return
        key = id(sem)
        if self.seen.get(key, 0) >= val:
            return
        self.h.wait_ge(sem, val)
        self.seen[key] = val


class DmaQ:
    def __init__(self, k, name, eng, nslots=8):
        self.k = k
        self.eng = eng
        self.name = name
        self.sems = [k.newsem("%s_d%d" % (name, i)) for i in range(nslots)]
        self.vals = [0] * nslots
        self.i = 0

    def issue(self, emit, deps):
        s = self.i % len(self.sems)
        self.i += 1
        sem = self.sems[s]
        if self.vals[s] > 0:
            self.eng.wait((sem, self.vals[s], "dma"))
        for ev in deps:
            self.eng.wait(ev)
        if self.vals[s] >= 16 * 1500:
            sem = self.k.newsem("%s_d%d_%d" % (self.name, s, self.i))
            self.sems[s] = sem
            self.vals[s] = 0
        inst = emit()
        self.vals[s] += 16
        inst.then_inc(sem, 16)
        return (sem, self.vals[s], "dma")


class K:
    def __init__(self, nc, stack):
        self.nc = nc
        self.stack = stack
        self.nsem = 0
        self.pe = Eng(self, "pe", nc.tensor, is_pe=True)
        self.act = Eng(self, "act", nc.scalar)
        self.dve = Eng(self, "dve", nc.vector)
        self.pool = Eng(self, "pool", nc.gpsimd)
        self.sp = Eng(self, "sp", nc.sync)
        self.engs = [self.pe, self.act, self.dve, self.pool, self.sp]
        self.q_sp = DmaQ(self, "qsp", self.sp, 12)
        self.q_pool = DmaQ(self, "qpool", self.pool, 8)
        self.qs = [self.q_sp, self.q_pool]
        self.n_inst = 0

    def newsem(self, name):
        self.nsem += 1
        return self.stack.enter_context(self.nc.semaphore(name))

    def sb(self, st, name, shape, dtype):
        self.uid = getattr(self, "uid", 0) + 1
        name = "%s_u%d" % (name, self.uid)
        t = st.enter_context(self.nc.sbuf_tensor(name, list(shape), dtype))
        return Tile(t, name)

    def ps(self, st, name, shape, dtype=F32):
        self.uid = getattr(self, "uid", 0) + 1
        name = "%s_u%d" % (name, self.uid)
        t = st.enter_context(self.nc.psum_tensor(name, list(shape), dtype))
        return Tile(t, name)

    @staticmethod
    def _resof(x):
        return x.res if isinstance(x, Tile) else x

    def _deps(self, reads, writes):
        deps = []
        for r in reads:
            r = self._resof(r)
            if r.w is not None:
                deps.append(r.w)
        for w in writes:
            w = self._resof(w)
            if w.w is not None:
                deps.append(w.w)
            deps.extend(w.r)
        return deps

    def _commit(self, ev, reads, writes):
        for r in reads:
            r = self._resof(r)
            r.r = [e for e in r.r if e[0] is not ev[0]]
            r.r.append(ev)
        for w in writes:
            w = self._resof(w)
            w.w = ev
            w.r = []

    def op(self, eng, emit, reads=(), writes=()):
        for ev in self._deps(reads, writes):
            if eng.is_pe and ev[2] == "pe":
                continue
            eng.wait(ev)
        inst = emit()
        ev = eng.tick(inst)
        self._commit(ev, reads, writes)
        self.n_inst += 1
        return ev

    def dma(self, q, out, in_, reads=(), writes=(), **kw):
        deps = self._deps(reads, writes)
        ev = q.issue(lambda: q.eng.h.dma_start(out=out, in_=in_, **kw), deps)
        self._commit(ev, reads, writes)
        self.n_inst += 1
        return ev

    def barrier(self):
        evs = [e.cur() for e in self.engs]
        for q in self.qs:
            for s, v in zip(q.sems, q.vals):
                evs.append((s, v, "dma"))
        for e in self.engs:
            for ev in evs:
                if ev[0] is e.sem:
                    continue
                e.wait(ev)


class Ring:
    def __init__(self, tiles):
        self.tiles = tiles
        self.i = 0

    def next(self):
        t = self.tiles[self.i % len(self.tiles)]
        self.i += 1
        return t


def _t5_bucket_np(rel):
    nb = 16
    max_exact = 8
    ret = np.where(rel > 0, nb, 0)
    n = np.abs(rel)
    nf = np.maximum(n, 1).astype(np.float32)
    large = max_exact + (np.log(nf / max_exact) / np.float32(math.log(128 / max_exact))
                         * (nb - max_exact)).astype(np.int32)
    large = np.minimum(large, nb - 1)
    return ret + np.where(n < max_exact, n, large)


DELTAS = [-128, 0, 128, 256, 384]
NBLK = 48
BSZ = 512
NSLOT = NBLK * BSZ
R0 = 511
NF = 1280


def make_consts():
    c = {}
    c["c_ident"] = np.eye(128, dtype=np.float32)
    c["c_exch"] = np.ascontiguousarray(np.eye(128, dtype=np.float32)[::-1])
    i = np.arange(128)[:, None]
    j = np.arange(128)[None, :]
    c["c_mlt"] = (i < j).astype(np.float32)
    c["c_uneg"] = -(i >= j).astype(np.float32)
    same = (i // 64) == (j // 64)
    c["c_gtri"] = (-(1.0 / 16.0) * (same & (i <= j))).astype(np.float32)
    c["c_gm2"] = (-(1.0 / 16.0) * (same & (i > j))).astype(np.float32)
    c["c_gmask"] = (same & (i <= j)).astype(np.float32)
    n = np.arange(NF)
    bk = _t5_bucket_np(R0 - n)
    oh = np.zeros((32, NF), np.float32)
    oh[bk, n] = 1.0
    oh[:, 1151:] = 0.0
    c["c_ohb"] = oh
    nm = np.zeros((5, 128, 512), np.float32)
    for di, dl in enumerate(DELTAS):
        kk = dl + np.arange(128)[:, None]
        qq = np.arange(512)[None, :]
        allowed = (kk // 64) <= (qq // 64)
        nm[di] = np.where(allowed, 0.0, -30000.0)
    c["c_negmask"] = nm
    p = np.arange(128, dtype=np.float32)[:, None]
    c["c_iog"] = (p + 128.0 * np.arange(8, dtype=np.float32)[None, :]).astype(np.float32)
    c["c_io2"] = (2.0 * p + np.arange(2, dtype=np.float32)[None, :]).astype(np.float32)
    c["c_iob"] = np.tile(np.arange(NBLK, dtype=np.float32)[None, :], (128, 1))
    return c


WEIGHT_NAMES = ["w_ada", "b_ada", "norm_mix", "norm_ffn", "norm_final", "rel_bias",
                "even_w_in", "even_lambda", "even_subln", "even_w_gk2", "even_b_gk", "even_gla_norm",
                "even_w_out", "odd_w_in", "odd_w_out", "router_group_w", "router_group_b",
                "router_expert_w", "router_expert_b", "expert_w_gate", "expert_w_up", "expert_w_down"]
WEIGHT_SHAPES = {
    "w_ada": [4, 1024, 6144], "b_ada": [4, 6144], "norm_mix": [4, 1024], "norm_ffn": [4, 1024],
    "norm_final": [1, 1024], "rel_bias": [32, 4], "even_w_in": [2, 1024, 3088], "even_lambda": [2, 256],
    "even_subln": [2, 128], "even_w_gk2": [2, 16, 256], "even_b_gk": [2, 256], "even_gla_norm": [2, 128],
    "even_w_out": [2, 1024, 1024], "odd_w_in": [2, 1024, 3072], "odd_w_out": [2, 1024, 1024],
    "router_group_w": [4, 1024, 4], "router_group_b": [4, 4], "router_expert_w": [4, 1024, 32],
    "router_expert_b": [4, 32], "expert_w_gate": [4, 32, 1024, 512], "expert_w_up": [4, 32, 1024, 512],
    "expert_w_down": [4, 32, 512, 1024],
}


def build(nlayers=DEPTH, stop=None, flags=()):
    SPARSE = 'dense' not in flags
    nc = bass.Bass("TRN2", target_bir_lowering=False)
    I = {}
    I["x"] = nc.dram_tensor("x", [S, D], F32, kind="ExternalInput").ap()
    I["c"] = nc.dram_tensor("c", [1, D], F32, kind="ExternalInput").ap()
    for nm in WEIGHT_NAMES:
        I[nm] = nc.dram_tensor(nm, WEIGHT_SHAPES[nm], F32, kind="ExternalInput").ap()
    consts = make_consts()
    for nm, v in consts.items():
        I[nm] = nc.dram_tensor(nm, list(v.shape), F32, kind="ExternalInput").ap()
    OUT = nc.dram_tensor("out", [S, D], F32, kind="ExternalOutput").ap()
    XS = nc.dram_tensor("xs_scr", [S, D], F32, kind="Internal").ap()
    FB = nc.dram_tensor("fb_scr", [25, 128, S], BF16, kind="Internal").ap()
    TB = nc.dram_tensor("tb_scr", [S, 1280], BF16, kind="Internal").ap()
    FS_T = nc.dram_tensor("fs_scr", [4, NF], F32, kind="Internal")
    FS = FS_T.ap()
    XN = nc.dram_tensor("xn_scr", [S, D], F32, kind="Internal").ap()
    MV = nc.dram_tensor("mv_scr", [2, D], F32, kind="Internal").ap()
    XG = nc.dram_tensor("xg_scr", [NSLOT, D], F32, kind="Internal").ap()
    YS = nc.dram_tensor("ys_scr", [NSLOT, D], F32, kind="Internal").ap()

    with ExitStack() as gst:
        k = K(nc, gst)
        pe, act, dve, pool, sp = k.pe, k.act, k.dve, k.pool, k.sp

        def mm(out, lhsT, rhs, start, stop, reads, writes):
            k.op(pe, lambda: nc.tensor.matmul(out, lhsT=lhsT, rhs=rhs, start=start, stop=stop), reads, writes)

        def tr(out, in_, ident, reads, writes):
            k.op(pe, lambda: nc.tensor.transpose(out=out, in_=in_, identity=ident), reads, writes)

        def actf(out, in_, func, reads, writes, **kw):
            k.op(act, lambda: nc.scalar.activation(out=out, in_=in_, func=func, **kw), reads, writes)

        def tt(eng, out, in0, in1, op, reads, writes):
            k.op(eng, lambda: eng.h.tensor_tensor(out=out, in0=in0, in1=in1, op=op), reads, writes)

        def ts(eng, out, in0, s1, s2, op0, op1, reads, writes):
            if op1 is None:
                k.op(eng, lambda: eng.h.tensor_scalar(out=out, in0=in0, scalar1=s1, scalar2=None, op0=op0),
                     reads, writes)
            else:
                k.op(eng, lambda: eng.h.tensor_scalar(out=out, in0=in0, scalar1=s1, scalar2=s2, op0=op0, op1=op1),
                     reads, writes)

        def stt(out, in0, scalar, in1, op0, op1, reads, writes):
            k.op(dve, lambda: nc.vector.scalar_tensor_tensor(out=out, in0=in0, scalar=scalar, in1=in1,
                                                             op0=op0, op1=op1), reads, writes)

        def cp(eng, out, in_, reads, writes):
            k.op(eng, lambda: eng.h.tensor_copy(out=out, in_=in_), reads, writes)

        def recip(out, in_, reads, writes):
            k.op(dve, lambda: nc.vector.reciprocal(out=out, in_=in_), reads, writes)

        def memset(eng, t, val, writes):
            k.op(eng, lambda: eng.h.memset(t, val), (), writes)

        def rmax(out, in_, reads, writes):
            k.op(dve, lambda: nc.vector.tensor_reduce(out=out, in_=in_, axis=AX.X, op=ALU.max), reads, writes)

        xres = [Res("x%d" % i) for i in range(NT)]
        fbres = [Res("fb%d" % i) for i in range(25)]
        tbres = Res("tb")
        fsres = Res("fs")

        ident32 = k.sb(gst, "ident32", [128, 128], F32)
        ones32 = k.sb(gst, "ones32", [128, 128], F32)
        onesb = k.sb(gst, "onesb", [128, 128], BF16)
        negonesb = k.sb(gst, "negonesb", [128, 128], BF16)
        zerob = k.sb(gst, "zerob", [128, 128], BF16)
        identb = k.sb(gst, "identb", [128, 128], BF16)
        k.dma(k.q_sp, ident32[:, :], I["c_ident"][:, :], writes=[ident32])
        memset(dve, ones32[:, :], 1.0, [ones32])
        memset(dve, onesb[:, :], 1.0, [onesb])
        memset(dve, negonesb[:, :], -1.0, [negonesb])
        memset(dve, zerob[:, :], 0.0, [zerob])
        epsc = k.sb(gst, "epsc", [128, 1], F32)
        memset(dve, epsc[:, :], EPS, [epsc])
        cp(dve, identb[:, :], ident32[:, :], [ident32], [identb])

        modT = k.sb(gst, "modT", [128, 64], F32)
        gmod1 = k.sb(gst, "gmod1", [128, 8], F32)
        gmod2 = k.sb(gst, "gmod2", [128, 8], F32)
        gbc1 = k.sb(gst, "gbc1", [128, D], F32)
        gbc2 = k.sb(gst, "gbc2", [128, D], F32)
        scT = k.sb(gst, "scT", [128, 8], F32)
        cmb = k.sb(gst, "cmb", [128, NT, NEXP], F32)
        H = {}
        r_ohg = k.sb(gst, "r_ohg", [128, NT, 4], F32)
        r_oh1 = k.sb(gst, "r_oh1", [128, NT, 8], F32)
        r_oh2 = k.sb(gst, "r_oh2", [128, NT, 8], F32)
        r_v = k.sb(gst, "r_v", [128, 12, NT], F32)
        s12u = k.sb(gst, "s12u", [128, 2, NT], mybir.dt.uint32)
        idxg = k.sb(gst, "idxg", [128, NBLK, 8], mybir.dt.uint32)
        idxd = k.sb(gst, "idxd", [128, NBLK, 4], mybir.dt.uint32)
        idx2 = k.sb(gst, "idx2", [128, NBLK, 2], mybir.dt.uint32)
        gsb = k.sb(gst, "gsb", [128, 2, 8], F32)
        mvres = Res("mv")
        xnres = Res("xn")
        xgres = Res("xg")
        ysres = [Res("ys%d" % i) for i in range(NBLK)]

        def idma(out, out_off, in_, in_off, reads, writes):
            deps = k._deps(reads, writes)
            ev = k.q_pool.issue(lambda: nc.gpsimd.indirect_dma_start(out=out, out_offset=out_off, in_=in_,
                                                                     in_offset=in_off), deps)
            k._commit(ev, reads, writes)
            k.n_inst += 1
            return ev

        def emit_mod(l):
            with ExitStack() as st:
                crow = k.sb(st, "crow", [1, D], F32)
                sc_bc = k.sb(st, "sc_bc", [128, 8, 128], F32)
                wseg = Ring([k.sb(st, "wseg%d" % i, [128, 8, 512], F32) for i in range(2)])
                brow = Ring([k.sb(st, "brow%d" % i, [1, 512], F32) for i in range(2)])
                nrow = k.sb(st, "nrow", [1, 2 * D], F32)
                pm = k.ps(st, "pm", [128, 512], F32)
                pg = Ring([k.ps(st, "pg%d" % i, [128, 512], F32) for i in range(2)])
                k.dma(k.q_sp, crow[:, :], I["c"][:, :], writes=[crow])
                for kc in range(8):
                    mm(pm[:, kc:kc + 1], crow[0:1, kc * 128:(kc + 1) * 128], ones32[0:1, 0:1], True, True,
                       [crow, ones32], [pm])
                actf(scT[:, :], pm[:, 0:8], AF.Silu, [pm], [scT])
                for kc in range(8):
                    actf(sc_bc[:, kc, :], ones32[:, :], AF.Copy, [ones32, scT], [sc_bc], scale=scT[:, kc:kc + 1])
                wv = I["w_ada"][l].rearrange("(kc p) n -> p kc n", p=128)
                for seg in range(6):
                    for half in range(2):
                        n0 = seg * 1024 + half * 512
                        w = wseg.next()
                        b = brow.next()
                        k.dma(k.q_sp, w[:, :, :], wv[:, :, n0:n0 + 512], writes=[w])
                        k.dma(k.q_sp, b[:, :], I["b_ada"][l:l + 1, n0:n0 + 512], writes=[b])
                        if seg in (2, 5):
                            p = pg.next()
                            for kc in range(8):
                                mm(p[:, :], sc_bc[:, kc, :], w[:, kc, :], kc == 0, False, [sc_bc, w], [p])
                            mm(p[:, :], ones32[0:1, :], b[0:1, :], False, True, [ones32, b], [p])
                            g = gbc1 if seg == 2 else gbc2
                            cp(dve, g[:, half * 512:(half + 1) * 512], p[:, :], [p], [g])
                        else:
                            for j in range(4):
                                col = seg * 8 + half * 4 + j
                                for kc in range(8):
                                    mm(pm[:, col:col + 1], w[:, kc, j * 128:(j + 1) * 128], scT[:, kc:kc + 1],
                                       kc == 0, False, [w, scT], [pm])
                                mm(pm[:, col:col + 1], b[0:1, j * 128:(j + 1) * 128], ones32[0:1, 0:1], False, True,
                                   [b, ones32], [pm])
                k.dma(k.q_sp, nrow[:, 0:D], I["norm_mix"][l:l + 1, :], writes=[nrow])
                k.dma(k.q_sp, nrow[:, D:2 * D], I["norm_ffn"][l:l + 1, :], writes=[nrow])
                for j in range(16):
                    mm(pm[:, 48 + j:49 + j], nrow[0:1, j * 128:(j + 1) * 128], ones32[0:1, 0:1], True, True,
                       [nrow, ones32], [pm])
                cp(dve, modT[:, :], pm[:, 0:64], [pm], [modT])
                stt(gmod1[:, :], modT[:, 8:16], 1.0, modT[:, 48:56], ALU.add, ALU.mult, [modT], [gmod1])
                stt(gmod2[:, :], modT[:, 32:40], 1.0, modT[:, 56:64], ALU.add, ALU.mult, [modT], [gmod2])
                k.barrier()

        def emit_norm(xsrc, gmod, shiftc, layer, router):
            with ExitStack() as st:
                xt_r = Ring([k.sb(st, "n_xt%d" % i, [128, D], F32) for i in range(4)])
                xs_r = Ring([k.sb(st, "n_xs%d" % i, [128, D], F32) for i in range(3)])
                junk = k.sb(st, "n_junk", [128, D], BF16)
                sm_r = Ring([k.sb(st, "n_sm%d" % i, [128, 4], F32) for i in range(6)])
                ps_r = Ring([k.ps(st, "n_ps%d" % i, [128, D], F32) for i in range(2)])
                if router:
                    h32_r = Ring([k.sb(st, "n_h32%d" % i, [128, 8, 128], F32) for i in range(2)])
                    wr32 = k.sb(st, "n_wr32", [128, 8, 36], F32)
                    brr = k.sb(st, "n_brr", [1, 36], F32)
                    plg_r = Ring([k.ps(st, "n_plg%d" % i, [128, 512], F32) for i in range(2)])
                    lgall = k.sb(st, "n_lgall", [128, NT, 36], F32)
                    k.dma(k.q_sp, wr32[:, :, 0:4],
                          I["router_group_w"][layer].rearrange("(kc p) n -> p kc n", p=128), writes=[wr32])
                    k.dma(k.q_sp, wr32[:, :, 4:36],
                          I["router_expert_w"][layer].rearrange("(kc p) n -> p kc n", p=128), writes=[wr32])
                    k.dma(k.q_sp, brr[:, 0:4], I["router_group_b"][layer:layer + 1, :], writes=[brr])
                    k.dma(k.q_sp, brr[:, 4:36], I["router_expert_b"][layer:layer + 1, :], writes=[brr])
                state = {}

                def s1(t):
                    xt = xt_r.next()
                    k.dma(k.q_sp, xt[:, :], xsrc[t * 128:(t + 1) * 128, :], reads=[xres[t]], writes=[xt])
                    sm = sm_r.next()
                    actf(junk[:, :], xt[:, :], AF.Square, [xt], [junk, sm], accum_out=sm[:, 0:1])
                    state[t] = (xt, sm)

                def s2(t):
                    xt, sm = state[t]
                    ts(dve, sm[:, 1:2], sm[:, 0:1], 1.0 / D, EPS, ALU.mult, ALU.add, [sm], [sm])
                    actf(sm[:, 2:3], sm[:, 1:2], AF.Sqrt, [sm], [sm])
                    recip(sm[:, 3:4], sm[:, 2:3], [sm], [sm])
                    xs = xs_r.next()
                    ts(dve, xs[:, :], xt[:, :], sm[:, 3:4], None, ALU.mult, None, [xt, sm], [xs])
                    if router and SPARSE:
                        k.dma(k.q_sp, XN[t * 128:(t + 1) * 128, :], xs[:, :], reads=[xs], writes=[xnres])
                    state[t] = xs

                def s3(t):
                    xs = state.pop(t)
                    ps = ps_r.next()
                    for kc in range(8):
                        tr(ps[:, kc * 128:(kc + 1) * 128], xs[:, kc * 128:(kc + 1) * 128], ident32[:, :],
                           [xs, ident32], [ps])
                    for kc in range(8):
                        if router and SPARSE:
                            break
                        actf(H["hT"][:, kc, t * 128:(t + 1) * 128], ps[:, kc * 128:(kc + 1) * 128], AF.Identity,
                             [ps, gmod, modT], [H["hT"]], scale=gmod[:, kc:kc + 1], bias=shiftc[:, kc:kc + 1])
                    if router:
                        h32 = h32_r.next()
                        for kc in range(8):
                            actf(h32[:, kc, :], ps[:, kc * 128:(kc + 1) * 128], AF.Identity,
                                 [ps, gmod, modT], [h32], scale=gmod[:, kc:kc + 1], bias=shiftc[:, kc:kc + 1])
                        plg = plg_r.next()
                        for kc in range(8):
                            mm(plg[:, 0:36], h32[:, kc, :], wr32[:, kc, :], kc == 0, False, [h32, wr32], [plg])
                        mm(plg[:, 0:36], ones32[0:1, :], brr[0:1, :], False, True, [ones32, brr], [plg])
                        cp(dve, lgall[:, t, :], plg[:, 0:36], [plg], [lgall])

                for i in range(NT + 2):
                    if i < NT:
                        s1(i)
                    if 1 <= i <= NT:
                        s2(i - 1)
                    if i >= 2:
                        s3(i - 2)
                if router:
                    def bc(ap2, n):
                        return ap2.unsqueeze(2).to_broadcast([128, NT, n])
                    f4 = k.sb(st, "r_f4", [128, NT, 4], F32)
                    ohg = r_ohg
                    v = r_v
                    el = k.sb(st, "r_el", [128, NT, 8], F32)
                    t8 = k.sb(st, "r_t8", [128, NT, 8], F32)
                    oh1 = r_oh1
                    oh2 = r_oh2
                    elm = k.sb(st, "r_elm", [128, NT, 8], F32)
                    cg = k.sb(st, "r_cg", [128, NT, 8], F32)
                    RR = [f4, ohg, v, el, t8, oh1, oh2, elm, cg, lgall]
                    gm, gs, gval, l1, l2, dd, ee, den, w1, W1, W2 = [v[:, i, :] for i in range(11)]

                    def red(out, in_, op):
                        k.op(dve, lambda: nc.vector.tensor_reduce(out=out, in_=in_, axis=AX.X, op=op), RR, RR)

                    lgg = lgall[:, :, 0:4]
                    red(gm, lgg, ALU.max)
                    tt(dve, ohg[:, :, :], lgg, bc(gm, 4), ALU.is_equal, RR, RR)
                    tt(dve, f4[:, :, :], lgg, bc(gm, 4), ALU.subtract, RR, RR)
                    actf(f4[:, :, :], f4[:, :, :], AF.Exp, RR, RR)
                    red(gs, f4[:, :, :], ALU.add)
                    recip(gval, gs, RR, RR)
                    for g in range(4):
                        src = lgall[:, :, 4 + 8 * g:12 + 8 * g]
                        if g == 0:
                            tt(dve, el[:, :, :], src, bc(ohg[:, :, g], 8), ALU.mult, RR, RR)
                        else:
                            tt(dve, t8[:, :, :], src, bc(ohg[:, :, g], 8), ALU.mult, RR, RR)
                            tt(dve, el[:, :, :], el[:, :, :], t8[:, :, :], ALU.add, RR, RR)
                    red(l1, el[:, :, :], ALU.max)
                    tt(dve, oh1[:, :, :], el[:, :, :], bc(l1, 8), ALU.is_equal, RR, RR)
                    stt(elm[:, :, :], oh1[:, :, :], -1e30, el[:, :, :], ALU.mult, ALU.add, RR, RR)
                    red(l2, elm[:, :, :], ALU.max)
                    tt(dve, oh2[:, :, :], elm[:, :, :], bc(l2, 8), ALU.is_equal, RR, RR)
                    tt(dve, dd, l2, l1, ALU.subtract, RR, RR)
                    actf(ee, dd, AF.Exp, RR, RR)
                    ts(dve, den, ee, 1.0, None, ALU.add, None, RR, RR)
                    recip(w1, den, RR, RR)
                    tt(dve, W1, w1, gval, ALU.mult, RR, RR)
                    tt(dve, W2, W1, ee, ALU.mult, RR, RR)
                    tt(dve, cg[:, :, :], oh1[:, :, :], bc(W1, 8), ALU.mult, RR, RR)
                    tt(dve, t8[:, :, :], oh2[:, :, :], bc(W2, 8), ALU.mult, RR, RR)
                    tt(dve, cg[:, :, :], cg[:, :, :], t8[:, :, :], ALU.add, RR, RR)
                    for g in range(4):
                        tt(dve, cmb[:, :, 8 * g:8 * g + 8], cg[:, :, :], bc(ohg[:, :, g], 8), ALU.mult, RR, [cmb])
                k.barrier()

        def emit_inproj(wsrc, ncols, fjobs, tjobs):
            with ExitStack() as st:
                W = k.sb(st, "ip_W", [128, 8, ncols], BF16)
                wv = wsrc.rearrange("(kc p) n -> p kc n", p=128)
                c0 = 0
                while c0 < ncols:
                    w_ = min(512, ncols - c0)
                    k.dma(k.q_pool, W[:, :, c0:c0 + w_], wv[:, :, c0:c0 + w_], writes=[W])
                    c0 += w_
                stg_r = Ring([k.sb(st, "ip_stg%d" % i, [128, S], BF16) for i in range(2)])
                stt_r = Ring([k.sb(st, "ip_stt%d" % i, [128, 4, 512], BF16) for i in range(2)])
                ps_r = Ring([k.ps(st, "ip_ps%d" % i, [128, 512], F32) for i in range(4)])
                n = 0
                for (col0, nr, fb, scale) in fjobs:
                    stg = stg_r.next()
                    for tg in range(8):
                        ps = ps_r.next()
                        for kc in range(8):
                            mm(ps[0:nr, :], W[:, kc, col0:col0 + nr], H["hT"][:, kc, tg * 512:(tg + 1) * 512],
                               kc == 0, kc == 7, [W, H["hT"]], [ps])
                        if n % 2 == 0:
                            actf(stg[0:nr, tg * 512:(tg + 1) * 512], ps[0:nr, :], AF.Copy, [ps], [stg], scale=scale)
                        else:
                            ts(dve, stg[0:nr, tg * 512:(tg + 1) * 512], ps[0:nr, :], scale, None, ALU.mult, None,
                               [ps], [stg])
                        n += 1
                    k.dma(k.q_sp, FB[fb, 0:nr, :], stg[0:nr, :], reads=[stg], writes=[fbres[fb]])
                for (col0, wd, tcol0) in tjobs:
                    for t4 in range(8):
                        stg = stt_r.next()
                        for j in range(4):
                            t = t4 * 4 + j
                            ps = ps_r.next()
                            for kc in range(8):
                                mm(ps[:, 0:wd], H["hT"][:, kc, t * 128:(t + 1) * 128], W[:, kc, col0:col0 + wd],
                                   kc == 0, kc == 7, [W, H["hT"]], [ps])
                            if n % 2 == 0:
                                actf(stg[:, j, 0:wd], ps[:, 0:wd], AF.Copy, [ps], [stg])
                            else:
                                cp(dve, stg[:, j, 0:wd], ps[:, 0:wd], [ps], [stg])
                            n += 1
                        k.dma(k.q_sp,
                              TB[t4 * 512:(t4 + 1) * 512, tcol0:tcol0 + wd].rearrange("(j p) c -> p j c", p=128),
                              stg[:, :, 0:wd], reads=[stg], writes=[tbres])
                k.barrier()

        def emit_outproj(wsrc, gbc, xsrc):
            with ExitStack() as st:
                W = k.sb(st, "op_W", [128, 8, D], BF16)
                wv = wsrc.rearrange("(kc p) n -> p kc n", p=128)
                for h in range(2):
                    k.dma(k.q_pool, W[:, :, h * 512:(h + 1) * 512], wv[:, :, h * 512:(h + 1) * 512], writes=[W])
                for kc in range(8):
                    tt(dve, W[:, kc, :], W[:, kc, :], gbc[:, :], ALU.mult, [W, gbc], [W])
                xt_r = Ring([k.sb(st, "op_xt%d" % i, [128, D], F32) for i in range(3)])
                xn_r = Ring([k.sb(st, "op_xn%d" % i, [128, D], F32) for i in range(2)])
                ps_r = Ring([k.ps(st, "op_ps%d" % i, [128, D], F32) for i in range(3)])
                for t in range(NT):
                    xt = xt_r.next()
                    k.dma(k.q_sp, xt[:, :], xsrc[t * 128:(t + 1) * 128, :], reads=[xres[t]], writes=[xt])
                    ps = ps_r.next()
                    for h in range(2):
                        for kc in range(8):
                            mm(ps[:, h * 512:(h + 1) * 512], H["hT"][:, kc, t * 128:(t + 1) * 128],
                               W[:, kc, h * 512:(h + 1) * 512], kc == 0, kc == 7, [H["hT"], W], [ps])
                    xn = xn_r.next()
                    tt(dve, xn[:, :], xt[:, :], ps[:, :], ALU.add, [xt, ps], [xn])
                    k.dma(k.q_sp, XS[t * 128:(t + 1) * 128, :], xn[:, :], reads=[xn], writes=[xres[t]])
                k.barrier()

        def emit_odd_mixer():
            with ExitStack() as st:
                mltb = k.sb(st, "sb_mlt", [128, 128], BF16)
                unegb = k.sb(st, "sb_uneg", [128, 128], BF16)
                k.dma(k.q_pool, mltb[:, :], I["c_mlt"][:, :], writes=[mltb])
                k.dma(k.q_pool, unegb[:, :], I["c_uneg"][:, :], writes=[unegb])
                qT_r = Ring([k.sb(st, "sb_qT%d" % i, [128, S], BF16) for i in range(2)])
                kT_r = Ring([k.sb(st, "sb_kT%d" % i, [128, S], BF16) for i in range(2)])
                vt_r = Ring([k.sb(st, "sb_vt%d" % i, [128, NT, 128], BF16) for i in range(2)])
                e32_r = Ring([k.sb(st, "sb_e%d" % i, [128, 512], F32) for i in range(6)])
                sp_r = Ring([k.sb(st, "sb_sp%d" % i, [128, 512], BF16) for i in range(10)])
                rb_r = Ring([k.sb(st, "sb_rb%d" % i, [128, 512], BF16) for i in range(10)])
                ab_r = Ring([k.sb(st, "sb_ab%d" % i, [128, 512], BF16) for i in range(6)])
                R32s = [k.sb(st, "sb_R32_%d" % i, [128, 512], F32) for i in range(2)]
                pz_r = Ring([k.ps(st, "sb_pz%d" % i, [128, 512], F32) for i in range(3)])
                pw_r = Ring([k.ps(st, "sb_pw%d" % i, [128, 512], F32) for i in range(3)])
                pos = [k.ps(st, "sb_po%d" % i, [128, 512], F32) for i in range(2)]
                for hp in range(8):
                    qT = qT_r.next()
                    kT = kT_r.next()
                    vt = vt_r.next()
                    k.dma(k.q_sp, qT[:, :], FB[hp, :, :], reads=[fbres[hp]], writes=[qT])
                    k.dma(k.q_sp, kT[:, :], FB[8 + hp, :, :], reads=[fbres[8 + hp]], writes=[kT])
                    k.dma(k.q_sp, vt[:, :, :],
                          TB[:, hp * 128:(hp + 1) * 128].rearrange("(t p) c -> p t c", p=128),
                          reads=[tbres], writes=[vt])
                    for qg in range(8):
                        kbs = list(range(4 * qg + 3, -1, -1))
                        nb = len(kbs)
                        items = [[None] * nb, [None] * nb]
                        pend = {}
                        pendB = []
                        for j in range(2):
                            mm(pos[j][:, :], zerob[:, :], qT[:, qg * 512:(qg + 1) * 512], True, False,
                               [zerob, qT], [pos[j]])

                        def stageA(j, i):
                            b0 = 64 * j
                            R32 = R32s[j]
                            kb = kbs[i]
                            j0 = max(0, kb - 4 * qg)
                            c0 = 128 * j0
                            wq = 512 - c0
                            q0 = qg * 512 + c0
                            diag = kb >= 4 * qg
                            pz = pz_r.next()
                            mm(pz[:, 0:wq], kT[b0:b0 + 64, kb * 128:(kb + 1) * 128], qT[b0:b0 + 64, q0:q0 + wq],
                               True, True, [kT, qT], [pz])
                            e32 = e32_r.next()
                            actf(e32[:, 0:wq], pz[:, 0:wq], AF.Exp, [pz], [e32])
                            pend[(j, i)] = (R32, kb, c0, wq, q0, diag, e32)

                        def stageA2(j, i):
                            R32, kb, c0, wq, q0, diag, e32 = pend.pop((j, i))
                            spb = sp_r.next()
                            if 'sbx_noln' not in flags:
                                actf(spb[:, 0:wq], e32[:, 0:wq], AF.Ln, [e32], [spb], bias=1.0)
                            if diag:
                                tt(pool, spb[:, 0:128], spb[:, 0:128], mltb[:, :], ALU.mult, [spb, mltb], [spb])
                            rb = None
                            if i > 0 and 'sbx_nor' not in flags:
                                rb = rb_r.next()
                                cp(dve, rb[:, :], R32[:, :], [R32], [rb])
                            if i < nb - 1 and 'sbx_nor' not in flags:
                                if i == 0:
                                    if c0 > 0:
                                        memset(dve, R32[:, 0:c0], 0.0, [R32])
                                    cp(dve, R32[:, c0:512], spb[:, 0:wq], [spb], [R32])
                                else:
                                    tt(dve, R32[:, c0:512], R32[:, c0:512], spb[:, 0:wq], ALU.add, [R32, spb],
                                       [R32])
                            items[j][i] = (kb, c0, wq, q0, diag, spb, rb)

                        def stageB(j, i):
                            if 'sbx_nob' in flags:
                                return
                            b0 = 64 * j
                            po = pos[j]
                            kb, c0, wq, q0, diag, spb, rb = items[j][i]
                            pw = pw_r.next()
                            mm(pw[:, 0:wq], kT[b0:b0 + 64, kb * 128:(kb + 1) * 128], qT[b0:b0 + 64, q0:q0 + wq],
                               True, False, [kT, qT], [pw])
                            mm(pw[:, 0:wq], unegb[:, :], spb[:, 0:wq], False, rb is None, [unegb, spb], [pw])
                            if rb is not None:
                                mm(pw[:, 0:wq], negonesb[:, :], rb[:, c0:512], False, True, [negonesb, rb], [pw])
                            ab = ab_r.next()
                            actf(ab[:, 0:wq], pw[:, 0:wq], AF.Exp, [pw], [ab])
                            if diag:
                                tt(dve, ab[:, 0:128], ab[:, 0:128], mltb[:, :], ALU.mult, [ab, mltb], [ab])
                            pendB.append((po, c0, wq, kb, ab, i))
                            while len(pendB) > 2:
                                stageB2()

                        def stageB2():
                            po, c0, wq, kb, ab, i = pendB.pop(0)
                            mm(po[:, c0:512], vt[:, kb, :], ab[:, 0:wq], False, i == nb - 1, [vt, ab], [po])

                        GW = 2
                        nw = nb // GW
                        for w in range(nw + 1):
                            if w < nw:
                                for i in range(w * GW, (w + 1) * GW):
                                    for j in range(2):
                                        stageA(j, i)
                                for i in range(w * GW, (w + 1) * GW):
                                    for j in range(2):
                                        stageA2(j, i)
                            if w > 0:
                                for i in range((w - 1) * GW, w * GW):
                                    for j in range(2):
                                        stageB(j, i)
                        while pendB:
                            stageB2()
                        for j in range(2):
                            b0 = 64 * j
                            if j == 0:
                                actf(H["hT"][b0:b0 + 64, hp, qg * 512:(qg + 1) * 512], pos[j][b0:b0 + 64, :], AF.Copy,
                                     [pos[j]], [H["hT"]])
                            else:
                                cp(dve, H["hT"][b0:b0 + 64, hp, qg * 512:(qg + 1) * 512], pos[j][b0:b0 + 64, :],
                                   [pos[j]], [H["hT"]])
                k.barrier()

        def emit_even_mixer(i_even, layer):
            lam_init = 0.8 - 0.6 * math.exp(-0.3 * layer)
            with ExitStack() as st:
                BM = [[k.sb(st, "BM%d_%d" % (h, di), [128, 512], BF16) for di in range(5)] for h in range(4)]
                b15 = k.sb(st, "b15", [128, 4], F32)
                neglam = k.sb(st, "neglam", [128, 1], F32)
                gsub = k.sb(st, "gsub", [128, 1], F32)
                with ExitStack() as st2:
                    rb = k.sb(st2, "rb", [32, 4], F32)
                    ohb = k.sb(st2, "ohb", [32, NF], F32)
                    fsb = k.sb(st2, "fsb", [4, NF], F32)
                    exch = k.sb(st2, "exch", [128, 128], F32)
                    nmk = [k.sb(st2, "nmk%d" % di, [128, 512], F32) for di in range(5)]
                    tl_r = Ring([k.sb(st2, "tl%d" % i, [128, 512], F32) for i in range(2)])
                    lrow = k.sb(st2, "lrow", [1, 264], F32)
                    grow = k.sb(st2, "grow", [1, 128], F32)
                    pf = Ring([k.ps(st2, "pf%d" % i, [128, 512], F32) for i in range(2)])
                    k.dma(k.q_sp, rb[:, :], I["rel_bias"][:, :], writes=[rb])
                    k.dma(k.q_sp, ohb[:, :], I["c_ohb"][:, :], writes=[ohb])
                    k.dma(k.q_sp, exch[:, :], I["c_exch"][:, :], writes=[exch])
                    k.dma(k.q_sp, b15[:, :], I["rel_bias"][15].partition_broadcast(128), writes=[b15])
                    for di in range(5):
                        k.dma(k.q_sp, nmk[di][:, :], I["c_negmask"][di], writes=[nmk[di]])
                    for c3 in range(3):
                        c0 = c3 * 512
                        w_ = min(512, NF - c0)
                        p = pf.next()
                        mm(p[0:4, 0:w_], rb[0:32, 0:4], ohb[0:32, c0:c0 + w_], True, True, [rb, ohb], [p])
                        cp(dve, fsb[0:4, c0:c0 + w_], p[0:4, 0:w_], [p], [fsb])
                    k.dma(k.q_sp, FS[:, :], fsb[:, :], reads=[fsb], writes=[fsres])
                    for h in range(4):
                        for di, dl in enumerate(DELTAS):
                            tl = tl_r.next()
                            src = bass.AP(tensor=FS_T, offset=h * NF + (384 - dl), ap=[[1, 128], [1, 512]])
                            k.dma(k.q_sp, tl[:, :], src, reads=[fsres], writes=[tl])
                            p = pf.next()
                            mm(p[:, :], exch[:, :], tl[:, :], True, True, [exch, tl], [p])
                            tt(dve, BM[h][di][:, :], p[:, :], nmk[di][:, :], ALU.add, [p, nmk[di]], [BM[h][di]])
                    k.dma(k.q_sp, lrow[:, 0:256], I["even_lambda"][i_even:i_even + 1, :], writes=[lrow])
                    LR = [lrow]
                    tt(dve, lrow[:, 0:64], lrow[:, 0:64], lrow[:, 64:128], ALU.mult, LR, LR)
                    tt(dve, lrow[:, 128:192], lrow[:, 128:192], lrow[:, 192:256], ALU.mult, LR, LR)
                    k.op(dve, lambda: nc.vector.tensor_reduce(out=lrow[:, 256:257], in_=lrow[:, 0:64], axis=AX.X,
                                                              op=ALU.add), LR, LR)
                    k.op(dve, lambda: nc.vector.tensor_reduce(out=lrow[:, 257:258], in_=lrow[:, 128:192], axis=AX.X,
                                                              op=ALU.add), LR, LR)
                    actf(lrow[:, 258:260], lrow[:, 256:258], AF.Exp, LR, LR)
                    tt(dve, lrow[:, 260:261], lrow[:, 259:260], lrow[:, 258:259], ALU.subtract, LR, LR)
                    ts(dve, lrow[:, 261:262], lrow[:, 260:261], -lam_init, None, ALU.add, None, LR, LR)
                    p = pf.next()
                    mm(p[:, 0:1], ones32[0:1, :], lrow[0:1, 261:262], True, True, [ones32, lrow], [p])
                    cp(dve, neglam[:, :], p[:, 0:1], [p], [neglam])
                    k.dma(k.q_sp, grow[:, :], I["even_subln"][i_even:i_even + 1, :], writes=[grow])
                    p = pf.next()
                    mm(p[:, 0:1], grow[0:1, :], ones32[0:1, 0:1], True, True, [grow, ones32], [p])
                    ts(dve, gsub[:, :], p[:, 0:1], 1.0 - lam_init, None, ALU.mult, None, [p], [gsub])
                    k.barrier()
                qT_r = Ring([k.sb(st, "da_qT%d" % i, [128, S], BF16) for i in range(2)])
                kT_r = Ring([k.sb(st, "da_kT%d" % i, [128, S], BF16) for i in range(2)])
                vt_r = Ring([k.sb(st, "da_vt%d" % i, [128, NT, 128], BF16) for i in range(2)])
                E_r = Ring([k.sb(st, "da_E%d" % i, [128, 512], BF16) for i in range(8)])
                f_r = Ring([k.sb(st, "da_f%d" % i, [128, 512], F32) for i in range(4)])
                o32_r = Ring([k.sb(st, "da_o%d" % i, [128, 512], F32) for i in range(2)])
                sq_r = Ring([k.sb(st, "da_sq%d" % i, [128, 512], BF16) for i in range(2)])
                ps_r = Ring([k.ps(st, "da_ps%d" % i, [128, 512], F32) for i in range(3)])
                pu = [k.ps(st, "da_pu%d" % i, [128, 512], F32) for i in range(2)]
                pd = [k.ps(st, "da_pd%d" % i, [128, 512], F32) for i in range(2)]
                pss = k.ps(st, "da_pss", [128, 512], F32)
                for h in range(0 if 'ev_noA' in flags else 4):
                    qT = qT_r.next()
                    kT = kT_r.next()
                    vt = vt_r.next()
                    k.dma(k.q_sp, qT[:, :], FB[h, :, :], reads=[fbres[h]], writes=[qT])
                    k.dma(k.q_sp, kT[:, :], FB[4 + h, :, :], reads=[fbres[4 + h]], writes=[kT])
                    k.dma(k.q_sp, vt[:, :, :],
                          TB[:, h * 128:(h + 1) * 128].rearrange("(t p) c -> p t c", p=128),
                          reads=[tbres], writes=[vt])
                    for qg in range(8):
                        nkb = 4 * qg + 4
                        its = [(kb, m) for kb in range(nkb) for m in range(2)]
                        held = {}

                        def st1(idx):
                            kb, m = its[idx]
                            j0 = max(0, kb - 4 * qg)
                            c0 = 128 * j0
                            wq = 512 - c0
                            q0 = qg * 512 + c0
                            dl = kb * 128 - qg * 512
                            b0 = 64 * m
                            ps = ps_r.next()
                            near = dl >= -128
                            mm(ps[:, 0:wq], kT[b0:b0 + 64, kb * 128:(kb + 1) * 128], qT[b0:b0 + 64, q0:q0 + wq],
                               True, not near, [kT, qT], [ps])
                            E = E_r.next()
                            if near:
                                bm = BM[h][DELTAS.index(dl)]
                                mm(ps[:, 0:wq], identb[:, :], bm[:, c0:512], False, True, [identb, bm], [ps])
                                actf(E[:, 0:wq], ps[:, 0:wq], AF.Exp, [ps], [E])
                            else:
                                actf(E[:, 0:wq], ps[:, 0:wq], AF.Exp, [ps, b15], [E], bias=b15[:, h:h + 1])
                            held[idx] = (E, c0, wq)

                        def st2(idx):
                            kb, m = its[idx]
                            E, c0, wq = held.pop(idx)
                            mm(pu[m][:, c0:512], vt[:, kb, :], E[:, 0:wq], kb == 0, kb == nkb - 1, [vt, E], [pu[m]])
                            mm(pd[m][:, c0:512], onesb[:, :], E[:, 0:wq], kb == 0, kb == nkb - 1, [onesb, E],
                               [pd[m]])

                        SK = 4 if 'sk4' in flags else 2
                        for idx in range(len(its) + SK):
                            if idx < len(its):
                                st1(idx)
                            if idx >= SK:
                                st2(idx - SK)
                        if 'da_nofin' in flags:
                            continue
                        r0 = f_r.next()
                        actf(r0[:, :], pd[0][:, :], AF.Ln, [pd[0]], [r0])
                        actf(r0[:, :], r0[:, :], AF.Exp, [r0], [r0], scale=-1.0)
                        r1 = f_r.next()
                        actf(r1[:, :], pd[1][:, :], AF.Ln, [pd[1]], [r1])
                        actf(r1[:, :], r1[:, :], AF.Exp, [r1], [r1], scale=-1.0)
                        t0 = f_r.next()
                        tt(dve, t0[:, :], pu[0][:, :], r0[:, :], ALU.mult, [pu[0], r0], [t0])
                        t1 = f_r.next()
                        tt(dve, t1[:, :], pu[1][:, :], r1[:, :], ALU.mult, [pu[1], r1], [t1])
                        o32 = o32_r.next()
                        stt(o32[:, :], t1[:, :], neglam[:, 0:1], t0[:, :], ALU.mult, ALU.add, [t1, t0, neglam], [o32])
                        sq = sq_r.next()
                        actf(sq[:, :], o32[:, :], AF.Square, [o32], [sq])
                        mm(pss[:, :], onesb[:, :], sq[:, :], True, True, [onesb, sq], [pss])
                        rs = f_r.next()
                        actf(rs[:, :], pss[:, :], AF.Ln, [pss, epsc], [rs], scale=1.0 / 128.0, bias=epsc[:, 0:1])
                        rs2 = f_r.next()
                        actf(rs2[:, :], rs[:, :], AF.Exp, [rs], [rs2], scale=-0.5)
                        stt(H["hT"][:, h, qg * 512:(qg + 1) * 512], o32[:, :], gsub[:, 0:1], rs2[:, :], ALU.mult, ALU.mult,
                            [o32, gsub, rs2], [H["hT"]])
                k.barrier()
            with ExitStack() as st:
                gtri = k.sb(st, "g_tri", [128, 128], F32)
                gm2 = k.sb(st, "g_m2", [128, 128], F32)
                gmaskb = k.sb(st, "g_mask", [128, 128], BF16)
                wgk = k.sb(st, "g_wgk", [16, 256], BF16)
                bgk = k.sb(st, "g_bgk", [1, 256], BF16)
                glag = k.sb(st, "g_lag", [128, 1], F32)
                grow = k.sb(st, "g_grow", [1, 128], F32)
                k.dma(k.q_sp, gtri[:, :], I["c_gtri"][:, :], writes=[gtri])
                k.dma(k.q_sp, gm2[:, :], I["c_gm2"][:, :], writes=[gm2])
                k.dma(k.q_pool, gmaskb[:, :], I["c_gmask"][:, :], writes=[gmaskb])
                k.dma(k.q_pool, wgk[:, :], I["even_w_gk2"][i_even], writes=[wgk])
                k.dma(k.q_pool, bgk[:, :], I["even_b_gk"][i_even:i_even + 1, :], writes=[bgk])
                k.dma(k.q_sp, grow[:, :], I["even_gla_norm"][i_even:i_even + 1, :], writes=[grow])
                pA = Ring([k.ps(st, "g_pA%d" % i, [128, 512], F32) for i in range(3)])
                pS = Ring([k.ps(st, "g_pS%d" % i, [128, 512], F32) for i in range(2)])
                pO = Ring([k.ps(st, "g_pO%d" % i, [128, 512], F32) for i in range(2)])
                p = pA.next()
                mm(p[:, 0:1], grow[0:1, :], ones32[0:1, 0:1], True, True, [grow, ones32], [p])
                cp(dve, glag[:, :], p[:, 0:1], [p], [glag])
                qg_r = Ring([k.sb(st, "g_q%d" % i, [128, 2, 512], BF16) for i in range(2)])
                kg_r = Ring([k.sb(st, "g_k%d" % i, [128, 2, 512], BF16) for i in range(2)])
                kt_r = Ring([k.sb(st, "g_kt%d" % i, [128, 4, 256], BF16) for i in range(2)])
                vg_r = Ring([k.sb(st, "g_v%d" % i, [128, 4, 512], BF16) for i in range(2)])
                bg_r = Ring([k.sb(st, "g_bg%d" % i, [16, 512], BF16) for i in range(2)])
                br_r = Ring([k.sb(st, "g_br%d" % i, [128, 4, 512], BF16) for i in range(2)])
                og_r = Ring([k.sb(st, "g_og%d" % i, [128, 4, 512], F32) for i in range(2)])
                e32_r = Ring([k.sb(st, "g_e%d" % i, [128, 256], F32) for i in range(2)])
                sp_r = Ring([k.sb(st, "g_sp%d" % i, [128, 256], F32) for i in range(2)])
                kf_r = Ring([k.sb(st, "g_kf%d" % i, [128, 256], F32) for i in range(2)])
                kd_r = Ring([k.sb(st, "g_kd%d" % i, [128, 256], BF16) for i in range(2)])
                eb_r = Ring([k.sb(st, "g_eb%d" % i, [128, 256], F32) for i in range(2)])
                en_r = Ring([k.sb(st, "g_en%d" % i, [128, 256], F32) for i in range(2)])
                qd_r = Ring([k.sb(st, "g_qd%d" % i, [128, 2, 128], BF16) for i in range(2)])
                ki_r = Ring([k.sb(st, "g_ki%d" % i, [128, 2, 128], BF16) for i in range(2)])
                at_r = Ring([k.sb(st, "g_at%d" % i, [128, 128], BF16) for i in range(3)])
                S32_r = [Ring([k.sb(st, "g_S32_%d_%d" % (h, i), [128, 128], F32) for i in range(2)]) for h in range(4)]
                Sb_r = [Ring([k.sb(st, "g_Sb_%d_%d" % (h, i), [128, 128], BF16) for i in range(3)]) for h in range(4)]
                sq_r = Ring([k.sb(st, "g_sq%d" % i, [128, 512], BF16) for i in range(2)])
                f_r = Ring([k.sb(st, "g_f%d" % i, [128, 512], F32) for i in range(4)])
                S32 = []
                Sb = []
                for h in range(4):
                    s = S32_r[h].next()
                    memset(dve, s[:, :], 0.0, [s])
                    S32.append(s)
                    b = Sb_r[h].next()
                    memset(pool, b[:, :], 0.0, [b])
                    Sb.append(b)
                for g8 in range(0 if 'ev_noB' in flags else 8):
                    t0g = g8 * 512
                    qgt = qg_r.next()
                    kgt = kg_r.next()
                    ktt = kt_r.next()
                    vgt = vg_r.next()
                    bgt = bg_r.next()
                    brt = br_r.next()
                    og = og_r.next()
                    for hh in range(2):
                        k.dma(k.q_sp, qgt[:, hh, :], FB[12 + hh, :, t0g:t0g + 512], reads=[fbres[12 + hh]], writes=[qgt])
                        k.dma(k.q_sp, kgt[:, hh, :], FB[14 + hh, :, t0g:t0g + 512], reads=[fbres[14 + hh]], writes=[kgt])
                    k.dma(k.q_sp, ktt[:, :, :],
                          TB[t0g:t0g + 512, 512:768].rearrange("(j p) c -> p j c", p=128), reads=[tbres], writes=[ktt])
                    k.dma(k.q_sp, vgt[:, :, :],
                          TB[t0g:t0g + 512, 768:1280].rearrange("(j p) c -> p j c", p=128), reads=[tbres], writes=[vgt])
                    k.dma(k.q_sp, bgt[:, :], FB[24, 0:16, t0g:t0g + 512], reads=[fbres[24]], writes=[bgt])
                    for h in range(4):
                        k.dma(k.q_sp, brt[:, h, :], FB[20 + h, :, t0g:t0g + 512], reads=[fbres[20 + h]], writes=[brt])
                    for j4 in range(4):
                        tc0 = j4 * 128
                        ppre = pA.next()
                        mm(ppre[:, 0:256], bgt[0:16, tc0:tc0 + 128], wgk[0:16, :], True, False, [bgt, wgk], [ppre])
                        mm(ppre[:, 0:256], onesb[0:1, :], bgk[0:1, :], False, True, [onesb, bgk], [ppre])
                        e32 = e32_r.next()
                        actf(e32[:, :], ppre[:, 0:256], AF.Exp, [ppre], [e32], scale=-1.0)
                        sp32 = sp_r.next()
                        actf(sp32[:, :], e32[:, :], AF.Ln, [e32], [sp32], bias=1.0)
                        pb = pA.next()
                        for hh in range(2):
                            mm(pb[:, hh * 128:(hh + 1) * 128], sp32[:, hh * 128:(hh + 1) * 128], gtri[:, :], True, True,
                               [sp32, gtri], [pb])
                        pk = pA.next()
                        mm(pk[:, 0:256], gm2[:, :], sp32[:, :], True, True, [gm2, sp32], [pk])
                        kf = kf_r.next()
                        actf(kf[:, :], pk[:, 0:256], AF.Exp, [pk], [kf])
                        kd = kd_r.next()
                        tt(dve, kd[:, :], ktt[:, j4, :], kf[:, :], ALU.mult, [ktt, kf], [kd])
                        eb = eb_r.next()
                        actf(eb[:, :], pb[:, 0:256], AF.Exp, [pb], [eb])
                        en = en_r.next()
                        actf(en[:, :], pb[:, 0:256], AF.Exp, [pb], [en], scale=-1.0)
                        qd = qd_r.next()
                        ki = ki_r.next()
                        for hh in range(2):
                            tt(dve, qd[:, hh, :], qgt[:, hh, tc0:tc0 + 128], eb[:, hh * 128:(hh + 1) * 128], ALU.mult,
                               [qgt, eb], [qd])
                            tt(dve, ki[:, hh, :], kgt[:, hh, tc0:tc0 + 128], en[:, hh * 128:(hh + 1) * 128], ALU.mult,
                               [kgt, en], [ki])
                        for h in range(4):
                            hh, jj = h // 2, h % 2
                            b0 = 64 * jj
                            psc = pS.next()
                            mm(psc[:, 0:128], ki[b0:b0 + 64, hh, :], qd[b0:b0 + 64, hh, :], True, True, [ki, qd], [psc])
                            at = at_r.next()
                            tt(dve, at[:, :], psc[:, 0:128], gmaskb[:, :], ALU.mult, [psc, gmaskb], [at])
                            po = pO.next()
                            mm(po[:, 0:128], vgt[:, j4, h * 128:(h + 1) * 128], at[:, :], True, False, [vgt, at], [po])
                            mm(po[:, 0:64], Sb[h][b0:b0 + 64, :], qd[b0:b0 + 64, hh, 0:64], False, False,
                               [Sb[h], qd], [po])
                            pst = pS.next()
                            mm(pst[:, 0:128], kd[0:64, hh * 128:(hh + 1) * 128], vgt[0:64, j4, h * 128:(h + 1) * 128],
                               True, True, [kd, vgt], [pst])
                            s1 = S32_r[h].next()
                            stt(s1[b0:b0 + 64, :], S32[h][b0:b0 + 64, :], eb[b0:b0 + 64, hh * 128 + 63:hh * 128 + 64],
                                pst[b0:b0 + 64, 0:128], ALU.mult, ALU.add, [S32[h], eb, pst], [s1])
                            sb1 = Sb_r[h].next()
                            actf(sb1[b0:b0 + 64, :], s1[b0:b0 + 64, :], AF.Copy, [s1], [sb1])
                            mm(po[:, 64:128], sb1[b0:b0 + 64, :], qd[b0:b0 + 64, hh, 64:128], False, True,
                               [sb1, qd], [po])
                            pst2 = pS.next()
                            mm(pst2[:, 0:128], kd[64:128, hh * 128:(hh + 1) * 128],
                               vgt[64:128, j4, h * 128:(h + 1) * 128], True, True, [kd, vgt], [pst2])
                            s2 = S32_r[h].next()
                            stt(s2[b0:b0 + 64, :], s1[b0:b0 + 64, :], eb[b0:b0 + 64, hh * 128 + 127:hh * 128 + 128],
                                pst2[b0:b0 + 64, 0:128], ALU.mult, ALU.add, [s1, eb, pst2], [s2])
                            sb2 = Sb_r[h].next()
                            actf(sb2[b0:b0 + 64, :], s2[b0:b0 + 64, :], AF.Copy, [s2], [sb2])
                            S32[h] = s2
                            Sb[h] = sb2
                            actf(og[:, h, tc0:tc0 + 128], po[:, 0:128], AF.Copy, [po], [og])
                    for h in range(4):
                        sq = sq_r.next()
                        actf(sq[:, :], og[:, h, :], AF.Square, [og], [sq])
                        pn = pA.next()
                        mm(pn[:, :], onesb[:, :], sq[:, :], True, True, [onesb, sq], [pn])
                        rs = f_r.next()
                        actf(rs[:, :], pn[:, :], AF.Ln, [pn, epsc], [rs], scale=1.0 / 128.0, bias=epsc[:, 0:1])
                        rs2 = f_r.next()
                        actf(rs2[:, :], rs[:, :], AF.Exp, [rs], [rs2], scale=-0.5)
                        sl = f_r.next()
                        actf(sl[:, :], brt[:, h, :], AF.Silu, [brt], [sl])
                        t1 = f_r.next()
                        stt(t1[:, :], og[:, h, :], glag[:, 0:1], rs2[:, :], ALU.mult, ALU.mult, [og, glag, rs2], [t1])
                        tt(dve, H["hT"][:, 4 + h, t0g:t0g + 512], t1[:, :], sl[:, :], ALU.mult, [t1, sl], [H["hT"]])
                k.barrier()

        def emit_moe(layer):
            with ExitStack() as st:
                wg_r = Ring([k.sb(st, "me_wg%d" % i, [128, 8, DEXP], BF16) for i in range(2)])
                wu_r = Ring([k.sb(st, "me_wu%d" % i, [128, 8, DEXP], BF16) for i in range(2)])
                wd_r = Ring([k.sb(st, "me_wd%d" % i, [128, 4, D], BF16) for i in range(2)])
                yacc = k.sb(st, "me_yacc", [128, 8, D], F32)
                aT_r = Ring([k.sb(st, "me_aT%d" % i, [128, 4, 512], BF16) for i in range(2)])
                sg_r = Ring([k.sb(st, "me_sg%d" % i, [128, 512], BF16) for i in range(3)])
                xt_r = Ring([k.sb(st, "me_xt%d" % i, [128, D], F32) for i in range(2)])
                pg_r = Ring([k.ps(st, "me_pg%d" % i, [128, 512], F32) for i in range(2)])
                pu_r = Ring([k.ps(st, "me_pu%d" % i, [128, 512], F32) for i in range(2)])
                py_r = Ring([k.ps(st, "me_py%d" % i, [128, D], F32) for i in range(2)])
                for qtr in range(4):
                    memset(dve, yacc[:, :, :], 0.0, [yacc])
                    jobs = [(e, tg) for e in range(NEXP) for tg in range(2)]
                    wts = {}

                    def load_w(e):
                        wg = wg_r.next()
                        wu = wu_r.next()
                        wd = wd_r.next()
                        k.dma(k.q_pool, wg[:, :, :],
                              I["expert_w_gate"][layer, e].rearrange("(kc p) f -> p kc f", p=128), writes=[wg])
                        k.dma(k.q_pool, wu[:, :, :],
                              I["expert_w_up"][layer, e].rearrange("(kc p) f -> p kc f", p=128), writes=[wu])
                        k.dma(k.q_pool, wd[:, :, :],
                              I["expert_w_down"][layer, e].rearrange("(fc p) n -> p fc n", p=128), writes=[wd])
                        wts[e] = (wg, wu, wd)

                    acts = {}

                    def emit_gu(e, tg):
                        wg, wu, wd = wts[e]
                        G = qtr * 2 + tg
                        aT = aT_r.next()
                        for fc in range(4):
                            pg = pg_r.next()
                            pu = pu_r.next()
                            for kc in range(8):
                                mm(pg[:, :], wg[:, kc, fc * 128:(fc + 1) * 128], H["hT"][:, kc, G * 512:(G + 1) * 512],
                                   kc == 0, kc == 7, [wg, H["hT"]], [pg])
                            for kc in range(8):
                                mm(pu[:, :], wu[:, kc, fc * 128:(fc + 1) * 128], H["hT"][:, kc, G * 512:(G + 1) * 512],
                                   kc == 0, kc == 7, [wu, H["hT"]], [pu])
                            sg = sg_r.next()
                            actf(sg[:, :], pg[:, :], AF.Silu, [pg], [sg])
                            tt(dve, aT[:, fc, :], sg[:, :], pu[:, :], ALU.mult, [sg, pu], [aT])
                        acts[(e, tg)] = aT

                    def emit_down(e, tg):
                        wg, wu, wd = wts[e]
                        aT = acts.pop((e, tg))
                        for t4 in range(4):
                            ti = tg * 4 + t4
                            T = qtr * 8 + ti
                            py = py_r.next()
                            for nh in range(2):
                                for fc in range(4):
                                    mm(py[:, nh * 512:(nh + 1) * 512], aT[:, fc, t4 * 128:(t4 + 1) * 128],
                                       wd[:, fc, nh * 512:(nh + 1) * 512], fc == 0, fc == 3, [aT, wd], [py])
                            stt(yacc[:, ti, :], py[:, :], cmb[:, T, e:e + 1], yacc[:, ti, :], ALU.mult, ALU.add,
                                [py, cmb, yacc], [yacc])

                    load_w(0)
                    for idx, (e, tg) in enumerate(jobs):
                        emit_gu(e, tg)
                        if idx > 0:
                            emit_down(*jobs[idx - 1])
                        if tg == 0 and e + 1 < NEXP:
                            load_w(e + 1)
                    emit_down(*jobs[-1])
                    for ti in range(8):
                        T = qtr * 8 + ti
                        xt = xt_r.next()
                        k.dma(k.q_sp, xt[:, :], XS[T * 128:(T + 1) * 128, :], reads=[xres[T]], writes=[xt])
                        tt(dve, yacc[:, ti, :], yacc[:, ti, :], gbc2[:, :], ALU.mult, [yacc, gbc2], [yacc])
                        tt(dve, xt[:, :], xt[:, :], yacc[:, ti, :], ALU.add, [xt, yacc], [xt])
                        k.dma(k.q_sp, XS[T * 128:(T + 1) * 128, :], xt[:, :], reads=[xt], writes=[xres[T]])
                k.barrier()

        def emit_slots(layer):
            with ExitStack() as st:
                Aall = k.sb(st, "sl_A", [128, NT, 32], BF16)
                ag = k.sb(st, "sl_ag", [128, NT, 8], F32)
                mltb = k.sb(st, "sl_mlt", [128, 128], BF16)
                pos = k.sb(st, "sl_pos", [128, NT, 32], F32)
                cnt = k.sb(st, "sl_cnt", [128, 32], F32)
                nb_ = k.sb(st, "sl_nb", [128, 32], F32)
                pa = k.sb(st, "sl_pa", [128, 32], F32)
                pb = k.sb(st, "sl_pb", [128, 32], F32)
                smg = k.sb(st, "sl_smg", [128, NT, 8], F32)
                t8 = k.sb(st, "sl_t8", [128, NT, 8], F32)
                sf = k.sb(st, "sl_sf", [128, 2, NT], F32)
                iog = k.sb(st, "sl_iog", [128, 8], F32)
                iob = k.sb(st, "sl_iob", [128, NBLK], F32)
                cmpt = k.sb(st, "sl_cmp", [128, NBLK, 32], F32)
                eb = k.sb(st, "sl_eb", [128, NBLK], F32)
                fg = k.sb(st, "sl_fg", [128, NBLK, 8], F32)
                ppos = k.ps(st, "sl_ppos", [128, NT * 32], F32)
                pcnt = k.ps(st, "sl_pcnt", [128, 512], F32)
                k.dma(k.q_pool, mltb[:, :], I["c_mlt"][:, :], writes=[mltb])
                k.dma(k.q_sp, iog[:, :], I["c_iog"][:, :], writes=[iog])
                k.dma(k.q_sp, iob[:, :], I["c_iob"][:, :], writes=[iob])
                RR = [Aall, ag, pos, cnt, nb_, pa, pb, smg, t8, sf, cmpt, eb, fg, r_ohg, r_oh1, r_oh2, r_v]

                def bc(ap2, n):
                    return ap2.unsqueeze(2).to_broadcast([128, NT, n])
                tt(dve, ag[:, :, :], r_oh1[:, :, :], r_oh2[:, :, :], ALU.add, RR, RR)
                for g in range(4):
                    tt(dve, Aall[:, :, 8 * g:8 * g + 8], ag[:, :, :], bc(r_ohg[:, :, g], 8), ALU.mult, RR, RR)
                for T in range(NT):
                    for T2 in range(T):
                        mm(ppos[:, T * 32:(T + 1) * 32], onesb[:, :], Aall[:, T2, :], T2 == 0, False,
                           [onesb, Aall], [ppos])
                    mm(ppos[:, T * 32:(T + 1) * 32], mltb[:, :], Aall[:, T, :], T == 0, True, [mltb, Aall], [ppos])
                for T in range(NT):
                    mm(pcnt[:, 0:32], onesb[:, :], Aall[:, T, :], T == 0, T == NT - 1, [onesb, Aall], [pcnt])
                cp(dve, pos[:, 0:16, :], ppos[:, 0:512].rearrange("p (t e) -> p t e", e=32), [ppos], RR)
                cp(dve, pos[:, 16:32, :], ppos[:, 512:1024].rearrange("p (t e) -> p t e", e=32), [ppos], RR)
                cp(dve, cnt[:, :], pcnt[:, 0:32], [pcnt], RR)
                ts(dve, nb_[:, :], cnt[:, :], 0.0, None, ALU.is_gt, None, RR, RR)
                for m in range(1, 8):
                    stt(nb_[:, :], cnt[:, :], float(BSZ * m), nb_[:, :], ALU.is_gt, ALU.add, RR, RR)
                cp(dve, pa[:, :], nb_[:, :], RR, RR)
                a_, b_ = pa, pb
                for sh in (1, 2, 4, 8, 16):
                    cp(dve, b_[:, 0:sh], a_[:, 0:sh], RR, RR)
                    tt(dve, b_[:, sh:32], a_[:, sh:32], a_[:, 0:32 - sh], ALU.add, RR, RR)
                    a_, b_ = b_, a_
                incl = a_
                excl = b_
                tt(dve, excl[:, :], incl[:, :], nb_[:, :], ALU.subtract, RR, RR)
                ts(dve, excl[:, :], excl[:, :], float(BSZ), None, ALU.mult, None, RR, RR)
                tt(dve, pos[:, :, :], pos[:, :, :], excl[:, :].unsqueeze(1).to_broadcast([128, NT, 32]), ALU.add,
                   RR, RR)
                for g in range(4):
                    if g == 0:
                        tt(dve, smg[:, :, :], pos[:, :, 0:8], bc(r_ohg[:, :, 0], 8), ALU.mult, RR, RR)
                    else:
                        tt(dve, t8[:, :, :], pos[:, :, 8 * g:8 * g + 8], bc(r_ohg[:, :, g], 8), ALU.mult, RR, RR)
                        tt(dve, smg[:, :, :], smg[:, :, :], t8[:, :, :], ALU.add, RR, RR)
                tt(dve, t8[:, :, :], smg[:, :, :], r_oh1[:, :, :], ALU.mult, RR, RR)
                k.op(dve, lambda: nc.vector.tensor_reduce(out=sf[:, 0, :], in_=t8[:, :, :], axis=AX.X, op=ALU.add),
                     RR, RR)
                tt(dve, t8[:, :, :], smg[:, :, :], r_oh2[:, :, :], ALU.mult, RR, RR)
                k.op(dve, lambda: nc.vector.tensor_reduce(out=sf[:, 1, :], in_=t8[:, :, :], axis=AX.X, op=ALU.add),
                     RR, RR)
                cp(dve, s12u[:, :, :], sf[:, :, :], RR, [s12u])
                tt(dve, cmpt[:, :, :], incl[:, :].unsqueeze(1).to_broadcast([128, NBLK, 32]),
                   iob[:, :].unsqueeze(2).to_broadcast([128, NBLK, 32]), ALU.is_le, RR + [iob], RR)
                k.op(dve, lambda: nc.vector.tensor_reduce(out=eb[:, :], in_=cmpt[:, :, :], axis=AX.X, op=ALU.add),
                     RR, RR)
                ts(dve, eb[:, :], eb[:, :], 31.0, None, ALU.min, None, RR, RR)
                ts(dve, eb[:, :], eb[:, :], 512.0, float(layer * NEXP * DEXP), ALU.mult, ALU.add, RR, RR)
                tt(dve, fg[:, :, 0:4], eb[:, :].unsqueeze(2).to_broadcast([128, NBLK, 4]),
                   iog[:, 0:4].unsqueeze(1).to_broadcast([128, NBLK, 4]), ALU.add, RR + [iog], RR)
                cp(dve, idxd[:, :, :], fg[:, :, 0:4], RR, [idxd])
                ts(dve, eb[:, :], eb[:, :], 2.0, None, ALU.mult, None, RR, RR)
                tt(dve, fg[:, :, :], eb[:, :].unsqueeze(2).to_broadcast([128, NBLK, 8]),
                   iog[:, :].unsqueeze(1).to_broadcast([128, NBLK, 8]), ALU.add, RR + [iog], RR)
                cp(dve, idxg[:, :, :], fg[:, :, :], RR, [idxg])
                io2 = k.sb(st, "sl_io2", [128, 2], F32)
                k.dma(k.q_sp, io2[:, :], I["c_io2"][:, :], writes=[io2])
                ts(dve, eb[:, :], eb[:, :], 0.25, None, ALU.mult, None, RR, RR)
                tt(dve, fg[:, :, 0:2], eb[:, :].unsqueeze(2).to_broadcast([128, NBLK, 2]),
                   io2[:, :].unsqueeze(1).to_broadcast([128, NBLK, 2]), ALU.add, RR + [io2], RR)
                cp(dve, idx2[:, :, :], fg[:, :, 0:2], RR, [idx2])
                k.dma(k.q_sp, MV[0].rearrange("(kc p) -> p kc", p=128), gmod2[:, :], reads=[gmod2], writes=[mvres],
                      allow_slow_non_contiguous=True)
                k.dma(k.q_sp, MV[1].rearrange("(kc p) -> p kc", p=128), modT[:, 24:32], reads=[modT], writes=[mvres],
                      allow_slow_non_contiguous=True)
                k.dma(k.q_sp, gsb[:, 0, :], MV[0].rearrange("(p kc) -> p kc", kc=8), reads=[mvres], writes=[gsb])
                k.dma(k.q_sp, gsb[:, 1, :], MV[1].rearrange("(p kc) -> p kc", kc=8), reads=[mvres], writes=[gsb])
                xr_r = Ring([k.sb(st, "sl_xr%d" % i, [128, D], F32) for i in range(3)])
                for T in range(NT):
                    xr = xr_r.next()
                    k.dma(k.q_sp, xr[:, :], XN[T * 128:(T + 1) * 128, :], reads=[xnres], writes=[xr])
                    for kk in range(2):
                        idma(XG[:, :], bass.IndirectOffsetOnAxis(ap=s12u[:, kk, T:T + 1], axis=0), xr[:, :], None,
                             [xr, s12u], [xgres])
                k.barrier()

        def emit_moe_sparse(layer):
            with ExitStack() as st:
                wg_r = Ring([k.sb(st, "ms_wg%d" % i, [128, 8, DEXP], BF16) for i in range(3)])
                wu_r = Ring([k.sb(st, "ms_wu%d" % i, [128, 8, DEXP], BF16) for i in range(3)])
                wd_r = Ring([k.sb(st, "ms_wd%d" % i, [128, 4, D], BF16) for i in range(3)])
                xg_r = Ring([k.sb(st, "ms_xg%d" % i, [128, D], F32) for i in range(8)])
                hs_r = Ring([k.sb(st, "ms_hs%d" % i, [128, 8, 512], BF16) for i in range(2)])
                aT_r = Ring([k.sb(st, "ms_aT%d" % i, [128, 4, 512], BF16) for i in range(2)])
                sg_r = Ring([k.sb(st, "ms_sg%d" % i, [128, 512], BF16) for i in range(3)])
                yo_r = Ring([k.sb(st, "ms_yo%d" % i, [128, D], F32) for i in range(3)])
                ptr_r = Ring([k.ps(st, "ms_ptr%d" % i, [128, 512], F32) for i in range(2)])
                pg_r = Ring([k.ps(st, "ms_pg%d" % i, [128, 512], F32) for i in range(2)])
                pu_r = Ring([k.ps(st, "ms_pu%d" % i, [128, 512], F32) for i in range(2)])
                py_r = Ring([k.ps(st, "ms_py%d" % i, [128, D], F32) for i in range(1)])
                wgv = I["expert_w_gate"].rearrange("l e (p q) f -> (l e p) (q f)", q=8).rearrange(
                    "r (h c) -> (r h) c", h=2)
                wuv = I["expert_w_up"].rearrange("l e (p q) f -> (l e p) (q f)", q=8).rearrange(
                    "r (h c) -> (r h) c", h=2)
                wdv = I["expert_w_down"].rearrange("l e (p q) n -> (l e p) (q n)", q=4).rearrange(
                    "r (h c) -> (r h) c", h=2)
                wts = {}
                hss = {}
                hss_keep = {}
                acts = {}

                def load_blk(b):
                    if 'ms_nowl' in flags and b >= 3:
                        wts[b] = wts[b - 3]
                        return
                    wg = wg_r.next()
                    wu = wu_r.next()
                    wd = wd_r.next()
                    for h in range(2):
                        off = bass.IndirectOffsetOnAxis(ap=idx2[:, b, h:h + 1], axis=0)
                        idma(wg[:, 4 * h:4 * h + 4, :].rearrange("p a b -> p (a b)"), None, wgv, off, [idx2], [wg])
                        idma(wu[:, 4 * h:4 * h + 4, :].rearrange("p a b -> p (a b)"), None, wuv, off, [idx2], [wu])
                        idma(wd[:, 2 * h:2 * h + 2, :].rearrange("p a b -> p (a b)"), None, wdv, off, [idx2], [wd])
                    wts[b] = (wg, wu, wd)

                xgs = {}

                def load_x(b):
                    tl = []
                    for j in range(4):
                        xg = xg_r.next()
                        r0 = b * BSZ + j * 128
                        k.dma(k.q_sp, xg[:, :], XG[r0:r0 + 128, :], reads=[xgres], writes=[xg])
                        tl.append(xg)
                    xgs[b] = tl

                def prep_h(b):
                    hs = hs_r.next()
                    tl = xgs.pop(b)
                    for j in range(4):
                        xg = tl[j]
                        for hf in range(2):
                            ptr = ptr_r.next()
                            for q in range(4):
                                kc = hf * 4 + q
                                tr(ptr[:, q * 128:(q + 1) * 128], xg[:, kc::8], ident32[:, :], [xg, ident32], [ptr])
                            for q in range(4):
                                kc = hf * 4 + q
                                actf(hs[:, kc, j * 128:(j + 1) * 128], ptr[:, q * 128:(q + 1) * 128], AF.Identity,
                                     [ptr, gsb], [hs], scale=gsb[:, 0, kc:kc + 1], bias=gsb[:, 1, kc:kc + 1])
                    hss[b] = hs

                def emit_gu(b):
                    wg, wu, wd = wts[b]
                    hs = hss.pop(b)
                    aT = aT_r.next()
                    for fc in range(4):
                        pg = pg_r.next()
                        pu = pu_r.next()
                        for kc in range(8):
                            mm(pg[:, :], wg[:, kc, fc::4], hs[:, kc, :], kc == 0, kc == 7,
                               [wg, hs], [pg])
                        for kc in range(8):
                            mm(pu[:, :], wu[:, kc, fc::4], hs[:, kc, :], kc == 0, kc == 7,
                               [wu, hs], [pu])
                        sg = sg_r.next()
                        actf(sg[:, :], pg[:, :], AF.Silu, [pg], [sg])
                        tt(dve, aT[:, fc, :], sg[:, :], pu[:, :], ALU.mult, [sg, pu], [aT])
                    acts[b] = aT

                def emit_down(b):
                    wg, wu, wd = wts[b]
                    aT = acts.pop(b)
                    for j in range(4):
                        py = py_r.next()
                        for nh in range(2):
                            for fc in range(4):
                                mm(py[:, nh * 512:(nh + 1) * 512], aT[:, fc, j * 128:(j + 1) * 128],
                                   wd[:, fc, nh * 512:(nh + 1) * 512], fc == 0, fc == 3, [aT, wd], [py])
                        yo = yo_r.next()
                        cp(dve, yo[:, :], py[:, :], [py], [yo])
                        r0 = b * BSZ + j * 128
                        k.dma(k.q_sp, YS[r0:r0 + 128, :], yo[:, :], reads=[yo], writes=[ysres[b]])

                load_blk(0)
                load_blk(1)
                load_x(0)
                prep_h(0)
                load_x(1)
                for b in range(NBLK):
                    emit_gu(b)
                    if b > 0:
                        emit_down(b - 1)
                    if b + 2 < NBLK:
                        load_blk(b + 2)
                    if b + 1 < NBLK:
                        prep_h(b + 1)
                    if b + 2 < NBLK:
                        load_x(b + 2)
                emit_down(NBLK - 1)
                k.barrier()
            with ExitStack() as st:
                r1_r = Ring([k.sb(st, "ms_r1%d" % i, [128, D], F32) for i in range(2)])
                r2_r = Ring([k.sb(st, "ms_r2%d" % i, [128, D], F32) for i in range(2)])
                xt_r = Ring([k.sb(st, "ms_xt%d" % i, [128, D], F32) for i in range(2)])
                for T in range(NT):
                    r1 = r1_r.next()
                    r2 = r2_r.next()
                    idma(r1[:, :], None, YS[:, :], bass.IndirectOffsetOnAxis(ap=s12u[:, 0, T:T + 1], axis=0),
                         ysres + [s12u], [r1])
                    idma(r2[:, :], None, YS[:, :], bass.IndirectOffsetOnAxis(ap=s12u[:, 1, T:T + 1], axis=0),
                         ysres + [s12u], [r2])
                    xt = xt_r.next()
                    k.dma(k.q_sp, xt[:, :], XS[T * 128:(T + 1) * 128, :], reads=[xres[T]], writes=[xt])
                    ts(dve, r1[:, :], r1[:, :], r_v[:, 9, T:T + 1], None, ALU.mult, None, [r1, r_v], [r1])
                    stt(r1[:, :], r2[:, :], r_v[:, 10, T:T + 1], r1[:, :], ALU.mult, ALU.add, [r2, r_v, r1], [r1])
                    tt(dve, r1[:, :], r1[:, :], gbc2[:, :], ALU.mult, [r1, gbc2], [r1])
                    tt(dve, xt[:, :], xt[:, :], r1[:, :], ALU.add, [xt, r1], [xt])
                    k.dma(k.q_sp, XS[T * 128:(T + 1) * 128, :], xt[:, :], reads=[xt], writes=[xres[T]])
                k.barrier()

        def emit_final(do_norm):
            with ExitStack() as st:
                xt_r = Ring([k.sb(st, "f_xt%d" % i, [128, D], F32) for i in range(3)])
                xo_r = Ring([k.sb(st, "f_xo%d" % i, [128, D], F32) for i in range(2)])
                junk = k.sb(st, "f_junk", [128, D], BF16)
                sm_r = Ring([k.sb(st, "f_sm%d" % i, [128, 4], F32) for i in range(4)])
                gf = k.sb(st, "f_gf", [128, D], F32)
                k.dma(k.q_sp, gf[:, :], I["norm_final"][0].partition_broadcast(128), writes=[gf])
                evs = []
                for t in range(NT):
                    xt = xt_r.next()
                    k.dma(k.q_sp, xt[:, :], XS[t * 128:(t + 1) * 128, :], reads=[xres[t]], writes=[xt])
                    if do_norm:
                        sm = sm_r.next()
                        actf(junk[:, :], xt[:, :], AF.Square, [xt], [junk, sm], accum_out=sm[:, 0:1])
                        ts(dve, sm[:, 1:2], sm[:, 0:1], 1.0 / D, EPS, ALU.mult, ALU.add, [sm], [sm])
                        actf(sm[:, 2:3], sm[:, 1:2], AF.Sqrt, [sm], [sm])
                        recip(sm[:, 3:4], sm[:, 2:3], [sm], [sm])
                        xo = xo_r.next()
                        stt(xo[:, :], xt[:, :], sm[:, 3:4], gf[:, :], ALU.mult, ALU.mult, [xt, sm, gf], [xo])
                        src = xo
                    else:
                        src = xt
                    evs.append(k.dma(k.q_sp, OUT[t * 128:(t + 1) * 128, :], src[:, :], reads=[src]))
                for ev in evs:
                    sp.wait(ev)

        xsrc = I["x"]
        done = False
        if stop == ("evenonly",):
            H["hT"] = k.sb(gst, "hT", [128, 8, S], BF16)
            emit_even_mixer(0, 0)
            emit_final(False)
            done = True
            nlayers = 0
        if stop == ("oddonly",):
            H["hT"] = k.sb(gst, "hT", [128, 8, S], BF16)
            emit_odd_mixer()
            emit_final(False)
            done = True
            nlayers = 0
        for l in range(nlayers):
            emit_mod(l)
            lst = ExitStack()
            H["hT"] = k.sb(lst, "hT", [128, 8, S], BF16)
            emit_norm(xsrc, gmod1, modT[:, 0:8], l, router=False)
            ie = l // 2
            if 'skipmix' in flags:
                pass
            elif l % 2 == 0:
                fj = [(c * 128, 128, c, 0.125) for c in range(4)]
                fj += [(512 + c * 128, 128, 4 + c, 1.0) for c in range(4)]
                fj += [(1536 + c * 128, 128, 12 + c, 0.125) for c in range(2)]
                fj += [(1792 + c * 128, 128, 14 + c, 1.0) for c in range(2)]
                fj += [(2560 + c * 128, 128, 20 + c, 1.0) for c in range(4)]
                fj += [(3072, 16, 24, 1.0)]
                tj = [(1024, 512, 0), (1792, 256, 512), (2048, 512, 768)]
                emit_inproj(I["even_w_in"][ie], EVEN_COLS, fj, tj)
                emit_even_mixer(ie, l)
                emit_outproj(I["even_w_out"][ie], gbc1, xsrc)
            else:
                fj = [(c * 128, 128, c, 0.125) for c in range(8)]
                fj += [(1024 + c * 128, 128, 8 + c, 1.0) for c in range(8)]
                tj = [(2048, 512, 0), (2560, 512, 512)]
                emit_inproj(I["odd_w_in"][ie], 3072, fj, tj)
                emit_odd_mixer()
                emit_outproj(I["odd_w_out"][ie], gbc1, xsrc)
            if 'skipmix' not in flags:
                xsrc = XS
            if SPARSE:
                lst.close()
                H.pop("hT")
            if stop == ("mix", l):
                emit_final(False)
                done = True
                break
            emit_norm(xsrc, gmod2, modT[:, 24:32], l, router=True)
            if 'nomoe' not in flags:
                if SPARSE:
                    emit_slots(layer=l)
                    emit_moe_sparse(l)
                else:
                    emit_moe(l)
            if not SPARSE:
                lst.close()
            if stop == ("ffn", l):
                emit_final(False)
                done = True
                break
        if not done:
            emit_final(True)
        build.stats = (k.n_inst, k.nsem)
    return nc


_CACHE = {}


def _get_nc(nlayers=DEPTH, stop=None):
    key = (nlayers, stop)
    if key not in _CACHE:
        _CACHE[key] = build(nlayers, stop)
    return _CACHE[key]


def make_in_maps(inputs, cores):
    consts = make_consts()
    shared = {}
    for nm in WEIGHT_NAMES:
        a = np.ascontiguousarray(np.asarray(inputs[nm], dtype=np.float32))
        shared[nm] = a.reshape(WEIGHT_SHAPES[nm])
    shared.update(consts)
    x = np.asarray(inputs["x"], dtype=np.float32)
    c = np.asarray(inputs["c"], dtype=np.float32)
    maps = []
    for b in cores:
        m = dict(shared)
        m["x"] = np.ascontiguousarray(x[b])
        m["c"] = np.ascontiguousarray(c[b:b + 1])
        maps.append(m)
    return maps


def kernel(**inputs):
    nc = _get_nc()
    in_maps = make_in_maps(inputs, list(range(8)))
    res = run_bass_kernel_spmd(nc, in_maps, core_ids=list(range(8)))
    return np.stack([np.asarray(r["out"]) for r in res.results], axis=0).astype(np.float32)
```

```python
import math
from contextlib import ExitStack
import numpy as np
import concourse.bass as bass
import concourse.mybir as mybir
from concourse.bass_utils import run_bass_kernel_spmd

F32 = mybir.dt.float32
BF16 = mybir.dt.bfloat16
AF = mybir.ActivationFunctionType
ALU = mybir.AluOpType
AX = mybir.AxisListType

S = 4096
D = 1024
NT = 32
DEPTH = 4
EPS = 1e-6
EVEN_COLS = 3088
NEXP = 32
DEXP = 512
EPOCH = 12000


class Res:
    __slots__ = ("w", "r", "name")

    def __init__(self, name=""):
        self.w = None
        self.r = []
        self.name = name


class Tile:
    def __init__(self, ap, name=""):
        self.t = ap
        self.res = Res(name)

    def __getitem__(self, idx):
        return self.t[idx]


class Eng:
    def __init__(self, k, name, h, is_pe=False):
        self.k = k
        self.name = name
        self.h = h
        self.is_pe = is_pe
        self.sem = k.newsem(name + "_s0")
        self.count = 0
        self.nep = 0
        self.seen = {}
        self.hist = [(self.sem, 0)]

    def tick(self, inst):
        if self.count >= EPOCH:
            self.nep += 1
            self.sem = self.k.newsem("%s_s%d" % (self.name, self.nep))
            self.count = 0
        self.count += 1
        inst.then_inc(self.sem, 1)
        return (self.sem, self.count, self.name)

    def cur(self):
        return (self.sem, self.count, self.name)

    def wait(self, ev):
        sem, val, _ = ev
        if val <= 0:
            return
        key = id(sem)
        if self.seen.get(key, 0) >= val:
            return
        self.h.wait_ge(sem, val)
        self.seen[key] = val


class DmaQ:
    def __init__(self, k, name, eng, nslots=8):
        self.k = k
        self.eng = eng
        self.name = name
        self.sems = [k.newsem("%s_d%d" % (name, i)) for i in range(nslots)]
        self.vals = [0] * nslots
        self.i = 0

    def issue(self, emit, deps):
        s = self.i % len(self.sems)
        self.i += 1
        sem = self.sems[s]
        if self.vals[s] > 0:
            self.eng.wait((sem, self.vals[s], "dma"))
        for ev in deps:
            self.eng.wait(ev)
        if self.vals[s] >= 16 * 1500:
            sem = self.k.newsem("%s_d%d_%d" % (self.name, s, self.i))
            self.sems[s] = sem
            self.vals[s] = 0
        inst = emit()
        self.vals[s] += 16
        inst.then_inc(sem, 16)
        return (sem, self.vals[s], "dma")


class K:
    def __init__(self, nc, stack):
        self.nc = nc
        self.stack = stack
        self.nsem = 0
        self.pe = Eng(self, "pe", nc.tensor, is_pe=True)
        self.act = Eng(self, "act", nc.scalar)
        self.dve = Eng(self, "dve", nc.vector)
        self.pool = Eng(self, "pool", nc.gpsimd)
        self.sp = Eng(self, "sp", nc.sync)
        self.engs = [self.pe, self.act, self.dve, self.pool, self.sp]
        self.q_sp = DmaQ(self, "qsp", self.sp, 12)
        self.q_pool = DmaQ(self, "qpool", self.pool, 8)
        self.qs = [self.q_sp, self.q_pool]
        self.n_inst = 0

    def newsem(self, name):
        self.nsem += 1
        return self.stack.enter_context(self.nc.semaphore(name))

    def sb(self, st, name, shape, dtype):
        self.uid = getattr(self, "uid", 0) + 1
        name = "%s_u%d" % (name, self.uid)
        t = st.enter_context(self.nc.sbuf_tensor(name, list(shape), dtype))
        return Tile(t, name)

    def ps(self, st, name, shape, dtype=F32):
        self.uid = getattr(self, "uid", 0) + 1
        name = "%s_u%d" % (name, self.uid)
        t = st.enter_context(self.nc.psum_tensor(name, list(shape), dtype))
        return Tile(t, name)

    @staticmethod
    def _resof(x):
        return x.res if isinstance(x, Tile) else x

    def _deps(self, reads, writes):
        deps = []
        for r in reads:
            r = self._resof(r)
            if r.w is not None:
                deps.append(r.w)
        for w in writes:
            w = self._resof(w)
            if w.w is not None:
                deps.append(w.w)
            deps.extend(w.r)
        return deps

    def _commit(self, ev, reads, writes):
        for r in reads:
            r = self._resof(r)
            r.r = [e for e in r.r if e[0] is not ev[0]]
            r.r.append(ev)
        for w in writes:
            w = self._resof(w)
            w.w = ev
            w.r = []

    def op(self, eng, emit, reads=(), writes=()):
        for ev in self._deps(reads, writes):
            if eng.is_pe and ev[2] == "pe":
                continue
            eng.wait(ev)
        inst = emit()
        ev = eng.tick(inst)
        self._commit(ev, reads, writes)
        self.n_inst += 1
        return ev

    def dma(self, q, out, in_, reads=(), writes=(), **kw):
        deps = self._deps(reads, writes)
        ev = q.issue(lambda: q.eng.h.dma_start(out=out, in_=in_, **kw), deps)
        self._commit(ev, reads, writes)
        self.n_inst += 1
        return ev

    def barrier(self):
        evs = [e.cur() for e in self.engs]
        for q in self.qs:
            for s, v in zip(q.sems, q.vals):
                evs.append((s, v, "dma"))
        for e in self.engs:
            for ev in evs:
                if ev[0] is e.sem:
                    continue
                e.wait(ev)


class Ring:
    def __init__(self, tiles):
        self.tiles = tiles
        self.i = 0

    def next(self):
        t = self.tiles[self.i % len(self.tiles)]
        self.i += 1
        return t


def _t5_bucket_np(rel):
    nb = 16
    max_exact = 8
    ret = np.where(rel > 0, nb, 0)
    n = np.abs(rel)
    nf = np.maximum(n, 1).astype(np.float32)
    large = max_exact + (np.log(nf / max_exact) / np.float32(math.log(128 / max_exact))
                         * (nb - max_exact)).astype(np.int32)
    large = np.minimum(large, nb - 1)
    return ret + np.where(n < max_exact, n, large)


DELTAS = [-128, 0, 128, 256, 384]
NBLK = 48
BSZ = 512
NSLOT = NBLK * BSZ
R0 = 511
NF = 1280


def make_consts():
    c = {}
    c["c_ident"] = np.eye(128, dtype=np.float32)
    c["c_exch"] = np.ascontiguousarray(np.eye(128, dtype=np.float32)[::-1])
    i = np.arange(128)[:, None]
    j = np.arange(128)[None, :]
    c["c_mlt"] = (i < j).astype(np.float32)
    c["c_uneg"] = -(i >= j).astype(np.float32)
    same = (i // 64) == (j // 64)
    c["c_gtri"] = (-(1.0 / 16.0) * (same & (i <= j))).astype(np.float32)
    c["c_gm2"] = (-(1.0 / 16.0) * (same & (i > j))).astype(np.float32)
    c["c_gmask"] = (same & (i <= j)).astype(np.float32)
    n = np.arange(NF)
    bk = _t5_bucket_np(R0 - n)
    oh = np.zeros((32, NF), np.float32)
    oh[bk, n] = 1.0
    oh[:, 1151:] = 0.0
    c["c_ohb"] = oh
    nm = np.zeros((5, 128, 512), np.float32)
    for di, dl in enumerate(DELTAS):
        kk = dl + np.arange(128)[:, None]
        qq = np.arange(512)[None, :]
        allowed = (kk // 64) <= (qq // 64)
        nm[di] = np.where(allowed, 0.0, -30000.0)
    c["c_negmask"] = nm
    p = np.arange(128, dtype=np.float32)[:, None]
    c["c_iog"] = (p + 128.0 * np.arange(8, dtype=np.float32)[None, :]).astype(np.float32)
    c["c_io2"] = (2.0 * p + np.arange(2, dtype=np.float32)[None, :]).astype(np.float32)
    c["c_iob"] = np.tile(np.arange(NBLK, dtype=np.float32)[None, :], (128, 1))
    return c


WEIGHT_NAMES = ["w_ada", "b_ada", "norm_mix", "norm_ffn", "norm_final", "rel_bias",
                "even_w_in", "even_lambda", "even_subln", "even_w_gk2", "even_b_gk", "even_gla_norm",
                "even_w_out", "odd_w_in", "odd_w_out", "router_group_w", "router_group_b",
                "router_expert_w", "router_expert_b", "expert_w_gate", "expert_w_up", "expert_w_down"]
WEIGHT_SHAPES = {
    "w_ada": [4, 1024, 6144], "b_ada": [4, 6144], "norm_mix": [4, 1024], "norm_ffn": [4, 1024],
    "norm_final": [1, 1024], "rel_bias": [32, 4], "even_w_in": [2, 1024, 3088], "even_lambda": [2, 256],
    "even_subln": [2, 128], "even_w_gk2": [2, 16, 256], "even_b_gk": [2, 256], "even_gla_norm": [2, 128],
    "even_w_out": [2, 1024, 1024], "odd_w_in": [2, 1024, 3072], "odd_w_out": [2, 1024, 1024],
    "router_group_w": [4, 1024, 4], "router_group_b": [4, 4], "router_expert_w": [4, 1024, 32],
    "router_expert_b": [4, 32], "expert_w_gate": [4, 32, 1024, 512], "expert_w_up": [4, 32, 1024, 512],
    "expert_w_down": [4, 32, 512, 1024],
}


def build(nlayers=DEPTH, stop=None, flags=()):
    SPARSE = 'dense' not in flags
    nc = bass.Bass("TRN2", target_bir_lowering=False)
    I = {}
    I["x"] = nc.dram_tensor("x", [S, D], F32, kind="ExternalInput").ap()
    I["c"] = nc.dram_tensor("c", [1, D], F32, kind="ExternalInput").ap()
    for nm in WEIGHT_NAMES:
        I[nm] = nc.dram_tensor(nm, WEIGHT_SHAPES[nm], F32, kind="ExternalInput").ap()
    consts = make_consts()
    for nm, v in consts.items():
        I[nm] = nc.dram_tensor(nm, list(v.shape), F32, kind="ExternalInput").ap()
    OUT = nc.dram_tensor("out", [S, D], F32, kind="ExternalOutput").ap()
    XS = nc.dram_tensor("xs_scr", [S, D], F32, kind="Internal").ap()
    FB = nc.dram_tensor("fb_scr", [25, 128, S], BF16, kind="Internal").ap()
    TB = nc.dram_tensor("tb_scr", [S, 1280], BF16, kind="Internal").ap()
    FS_T = nc.dram_tensor("fs_scr", [4, NF], F32, kind="Internal")
    FS = FS_T.ap()
    XN = nc.dram_tensor("xn_scr", [S, D], F32, kind="Internal").ap()
    MV = nc.dram_tensor("mv_scr", [2, D], F32, kind="Internal").ap()
    XG = nc.dram_tensor("xg_scr", [NSLOT, D], F32, kind="Internal").ap()
    YS = nc.dram_tensor("ys_scr", [NSLOT, D], F32, kind="Internal").ap()

    with ExitStack() as gst:
        k = K(nc, gst)
        pe, act, dve, pool, sp = k.pe, k.act, k.dve, k.pool, k.sp

        def mm(out, lhsT, rhs, start, stop, reads, writes):
            k.op(pe, lambda: nc.tensor.matmul(out, lhsT=lhsT, rhs=rhs, start=start, stop=stop), reads, writes)

        def tr(out, in_, ident, reads, writes):
            k.op(pe, lambda: nc.tensor.transpose(out=out, in_=in_, identity=ident), reads, writes)

        def actf(out, in_, func, reads, writes, **kw):
            k.op(act, lambda: nc.scalar.activation(out=out, in_=in_, func=func, **kw), reads, writes)

        def tt(eng, out, in0, in1, op, reads, writes):
            k.op(eng, lambda: eng.h.tensor_tensor(out=out, in0=in0, in1=in1, op=op), reads, writes)

        def ts(eng, out, in0, s1, s2, op0, op1, reads, writes):
            if op1 is None:
                k.op(eng, lambda: eng.h.tensor_scalar(out=out, in0=in0, scalar1=s1, scalar2=None, op0=op0),
                     reads, writes)
            else:
                k.op(eng, lambda: eng.h.tensor_scalar(out=out, in0=in0, scalar1=s1, scalar2=s2, op0=op0, op1=op1),
                     reads, writes)

        def stt(out, in0, scalar, in1, op0, op1, reads, writes):
            k.op(dve, lambda: nc.vector.scalar_tensor_tensor(out=out, in0=in0, scalar=scalar, in1=in1,
                                                             op0=op0, op1=op1), reads, writes)

        def cp(eng, out, in_, reads, writes):
            k.op(eng, lambda: eng.h.tensor_copy(out=out, in_=in_), reads, writes)

        def recip(out, in_, reads, writes):
            k.op(dve, lambda: nc.vector.reciprocal(out=out, in_=in_), reads, writes)

        def memset(eng, t, val, writes):
            k.op(eng, lambda: eng.h.memset(t, val), (), writes)

        def rmax(out, in_, reads, writes):
            k.op(dve, lambda: nc.vector.tensor_reduce(out=out, in_=in_, axis=AX.X, op=ALU.max), reads, writes)

        xres = [Res("x%d" % i) for i in range(NT)]
        fbres = [Res("fb%d" % i) for i in range(25)]
        tbres = Res("tb")
        fsres = Res("fs")

        ident32 = k.sb(gst, "ident32", [128, 128], F32)
        ones32 = k.sb(gst, "ones32", [128, 128], F32)
        onesb = k.sb(gst, "onesb", [128, 128], BF16)
        negonesb = k.sb(gst, "negonesb", [128, 128], BF16)
        zerob = k.sb(gst, "zerob", [128, 128], BF16)
        identb = k.sb(gst, "identb", [128, 128], BF16)
        k.dma(k.q_sp, ident32[:, :], I["c_ident"][:, :], writes=[ident32])
        memset(dve, ones32[:, :], 1.0, [ones32])
        memset(dve, onesb[:, :], 1.0, [onesb])
        memset(dve, negonesb[:, :], -1.0, [negonesb])
        memset(dve, zerob[:, :], 0.0, [zerob])
        epsc = k.sb(gst, "epsc", [128, 1], F32)
        memset(dve, epsc[:, :], EPS, [epsc])
        cp(dve, identb[:, :], ident32[:, :], [ident32], [identb])

        modT = k.sb(gst, "modT", [128, 64], F32)
        gmod1 = k.sb(gst, "gmod1", [128, 8], F32)
        gmod2 = k.sb(gst, "gmod2", [128, 8], F32)
        gbc1 = k.sb(gst, "gbc1", [128, D], F32)
        gbc2 = k.sb(gst, "gbc2", [128, D], F32)
        scT = k.sb(gst, "scT", [128, 8], F32)
        cmb = k.sb(gst, "cmb", [128, NT, NEXP], F32)
        H = {}
        r_ohg = k.sb(gst, "r_ohg", [128, NT, 4], F32)
        r_oh1 = k.sb(gst, "r_oh1", [128, NT, 8], F32)
        r_oh2 = k.sb(gst, "r_oh2", [128, NT, 8], F32)
        r_v = k.sb(gst, "r_v", [128, 12, NT], F32)
        s12u = k.sb(gst, "s12u", [128, 2, NT], mybir.dt.uint32)
        idxg = k.sb(gst, "idxg", [128, NBLK, 8], mybir.dt.uint32)
        idxd = k.sb(gst, "idxd", [128, NBLK, 4], mybir.dt.uint32)
        idx2 = k.sb(gst, "idx2", [128, NBLK, 2], mybir.dt.uint32)
        gsb = k.sb(gst, "gsb", [128, 2, 8], F32)
        mvres = Res("mv")
        xnres = Res("xn")
        xgres = Res("xg")
        ysres = [Res("ys%d" % i) for i in range(NBLK)]

        def idma(out, out_off, in_, in_off, reads, writes):
            deps = k._deps(reads, writes)
            ev = k.q_pool.issue(lambda: nc.gpsimd.indirect_dma_start(out=out, out_offset=out_off, in_=in_,
                                                                     in_offset=in_off), deps)
            k._commit(ev, reads, writes)
            k.n_inst += 1
            return ev

        def emit_mod(l):
            with ExitStack() as st:
                crow = k.sb(st, "crow", [1, D], F32)
                sc_bc = k.sb(st, "sc_bc", [128, 8, 128], F32)
                wseg = Ring([k.sb(st, "wseg%d" % i, [128, 8, 512], F32) for i in range(2)])
                brow = Ring([k.sb(st, "brow%d" % i, [1, 512], F32) for i in range(2)])
                nrow = k.sb(st, "nrow", [1, 2 * D], F32)
                pm = k.ps(st, "pm", [128, 512], F32)
                pg = Ring([k.ps(st, "pg%d" % i, [128, 512], F32) for i in range(2)])
                k.dma(k.q_sp, crow[:, :], I["c"][:, :], writes=[crow])
                for kc in range(8):
                    mm(pm[:, kc:kc + 1], crow[0:1, kc * 128:(kc + 1) * 128], ones32[0:1, 0:1], True, True,
                       [crow, ones32], [pm])
                actf(scT[:, :], pm[:, 0:8], AF.Silu, [pm], [scT])
                for kc in range(8):
                    actf(sc_bc[:, kc, :], ones32[:, :], AF.Copy, [ones32, scT], [sc_bc], scale=scT[:, kc:kc + 1])
                wv = I["w_ada"][l].rearrange("(kc p) n -> p kc n", p=128)
                for seg in range(6):
                    for half in range(2):
                        n0 = seg * 1024 + half * 512
                        w = wseg.next()
                        b = brow.next()
                        k.dma(k.q_sp, w[:, :, :], wv[:, :, n0:n0 + 512], writes=[w])
                        k.dma(k.q_sp, b[:, :], I["b_ada"][l:l + 1, n0:n0 + 512], writes=[b])
                        if seg in (2, 5):
                            p = pg.next()
                            for kc in range(8):
                                mm(p[:, :], sc_bc[:, kc, :], w[:, kc, :], kc == 0, False, [sc_bc, w], [p])
                            mm(p[:, :], ones32[0:1, :], b[0:1, :], False, True, [ones32, b], [p])
                            g = gbc1 if seg == 2 else gbc2
                            cp(dve, g[:, half * 512:(half + 1) * 512], p[:, :], [p], [g])
                        else:
                            for j in range(4):
                                col = seg * 8 + half * 4 + j
                                for kc in range(8):
                                    mm(pm[:, col:col + 1], w[:, kc, j * 128:(j + 1) * 128], scT[:, kc:kc + 1],
                                       kc == 0, False, [w, scT], [pm])
                                mm(pm[:, col:col + 1], b[0:1, j * 128:(j + 1) * 128], ones32[0:1, 0:1], False, True,
                                   [b, ones32], [pm])
                k.dma(k.q_sp, nrow[:, 0:D], I["norm_mix"][l:l + 1, :], writes=[nrow])
                k.dma(k.q_sp, nrow[:, D:2 * D], I["norm_ffn"][l:l + 1, :], writes=[nrow])
                for j in range(16):
                    mm(pm[:, 48 + j:49 + j], nrow[0:1, j * 128:(j + 1) * 128], ones32[0:1, 0:1], True, True,
                       [nrow, ones32], [pm])
                cp(dve, modT[:, :], pm[:, 0:64], [pm], [modT])
                stt(gmod1[:, :], modT[:, 8:16], 1.0, modT[:, 48:56], ALU.add, ALU.mult, [modT], [gmod1])
                stt(gmod2[:, :], modT[:, 32:40], 1.0, modT[:, 56:64], ALU.add, ALU.mult, [modT], [gmod2])
                k.barrier()

        def emit_norm(xsrc, gmod, shiftc, layer, router):
            with ExitStack() as st:
                xt_r = Ring([k.sb(st, "n_xt%d" % i, [128, D], F32) for i in range(4)])
                xs_r = Ring([k.sb(st, "n_xs%d" % i, [128, D], F32) for i in range(3)])
                junk = k.sb(st, "n_junk", [128, D], BF16)
                sm_r = Ring([k.sb(st, "n_sm%d" % i, [128, 4], F32) for i in range(6)])
                ps_r = Ring([k.ps(st, "n_ps%d" % i, [128, D], F32) for i in range(2)])
                if router:
                    h32_r = Ring([k.sb(st, "n_h32%d" % i, [128, 8, 128], F32) for i in range(2)])
                    wr32 = k.sb(st, "n_wr32", [128, 8, 36], F32)
                    brr = k.sb(st, "n_brr", [1, 36], F32)
                    plg_r = Ring([k.ps(st, "n_plg%d" % i, [128, 512], F32) for i in range(2)])
                    lgall = k.sb(st, "n_lgall", [128, NT, 36], F32)
                    k.dma(k.q_sp, wr32[:, :, 0:4],
                          I["router_group_w"][layer].rearrange("(kc p) n -> p kc n", p=128), writes=[wr32])
                    k.dma(k.q_sp, wr32[:, :, 4:36],
                          I["router_expert_w"][layer].rearrange("(kc p) n -> p kc n", p=128), writes=[wr32])
                    k.dma(k.q_sp, brr[:, 0:4], I["router_group_b"][layer:layer + 1, :], writes=[brr])
                    k.dma(k.q_sp, brr[:, 4:36], I["router_expert_b"][layer:layer + 1, :], writes=[brr])
                state = {}

                def s1(t):
                    xt = xt_r.next()
                    k.dma(k.q_sp, xt[:, :], xsrc[t * 128:(t + 1) * 128, :], reads=[xres[t]], writes=[xt])
                    sm = sm_r.next()
                    actf(junk[:, :], xt[:, :], AF.Square, [xt], [junk, sm], accum_out=sm[:, 0:1])
                    state[t] = (xt, sm)

                def s2(t):
                    xt, sm = state[t]
                    ts(dve, sm[:, 1:2], sm[:, 0:1], 1.0 / D, EPS, ALU.mult, ALU.add, [sm], [sm])
                    actf(sm[:, 2:3], sm[:, 1:2], AF.Sqrt, [sm], [sm])
                    recip(sm[:, 3:4], sm[:, 2:3], [sm], [sm])
                    xs = xs_r.next()
                    ts(dve, xs[:, :], xt[:, :], sm[:, 3:4], None, ALU.mult, None, [xt, sm], [xs])
                    if router and SPARSE:
                        k.dma(k.q_sp, XN[t * 128:(t + 1) * 128, :], xs[:, :], reads=[xs], writes=[xnres])
                    state[t] = xs

                def s3(t):
                    xs = state.pop(t)
                    ps = ps_r.next()
                    for kc in range(8):
                        tr(ps[:, kc * 128:(kc + 1) * 128], xs[:, kc * 128:(kc + 1) * 128], ident32[:, :],
                           [xs, ident32], [ps])
                    for kc in range(8):
                        if router and SPARSE:
                            break
                        actf(H["hT"][:, kc, t * 128:(t + 1) * 128], ps[:, kc * 128:(kc + 1) * 128], AF.Identity,
                             [ps, gmod, modT], [H["hT"]], scale=gmod[:, kc:kc + 1], bias=shiftc[:, kc:kc + 1])
                    if router:
                        h32 = h32_r.next()
                        for kc in range(8):
                            actf(h32[:, kc, :], ps[:, kc * 128:(kc + 1) * 128], AF.Identity,
                                 [ps, gmod, modT], [h32], scale=gmod[:, kc:kc + 1], bias=shiftc[:, kc:kc + 1])
                        plg = plg_r.next()
                        for kc in range(8):
                            mm(plg[:, 0:36], h32[:, kc, :], wr32[:, kc, :], kc == 0, False, [h32, wr32], [plg])
                        mm(plg[:, 0:36], ones32[0:1, :], brr[0:1, :], False, True, [ones32, brr], [plg])
                        cp(dve, lgall[:, t, :], plg[:, 0:36], [plg], [lgall])

                for i in range(NT + 2):
                    if i < NT:
                        s1(i)
                    if 1 <= i <= NT:
                        s2(i - 1)
                    if i >= 2:
                        s3(i - 2)
                if router:
                    def bc(ap2, n):
                        return ap2.unsqueeze(2).to_broadcast([128, NT, n])
                    f4 = k.sb(st, "r_f4", [128, NT, 4], F32)
                    ohg = r_ohg
                    v = r_v
                    el = k.sb(st, "r_el", [128, NT, 8], F32)
                    t8 = k.sb(st, "r_t8", [128, NT, 8], F32)
                    oh1 = r_oh1
                    oh2 = r_oh2
                    elm = k.sb(st, "r_elm", [128, NT, 8], F32)
                    cg = k.sb(st, "r_cg", [128, NT, 8], F32)
                    RR = [f4, ohg, v, el, t8, oh1, oh2, elm, cg, lgall]
                    gm, gs, gval, l1, l2, dd, ee, den, w1, W1, W2 = [v[:, i, :] for i in range(11)]

                    def red(out, in_, op):
                        k.op(dve, lambda: nc.vector.tensor_reduce(out=out, in_=in_, axis=AX.X, op=op), RR, RR)

                    lgg = lgall[:, :, 0:4]
                    red(gm, lgg, ALU.max)
                    tt(dve, ohg[:, :, :], lgg, bc(gm, 4), ALU.is_equal, RR, RR)
                    tt(dve, f4[:, :, :], lgg, bc(gm, 4), ALU.subtract, RR, RR)
                    actf(f4[:, :, :], f4[:, :, :], AF.Exp, RR, RR)
                    red(gs, f4[:, :, :], ALU.add)
                    recip(gval, gs, RR, RR)
                    for g in range(4):
                        src = lgall[:, :, 4 + 8 * g:12 + 8 * g]
                        if g == 0:
                            tt(dve, el[:, :, :], src, bc(ohg[:, :, g], 8), ALU.mult, RR, RR)
                        else:
                            tt(dve, t8[:, :, :], src, bc(ohg[:, :, g], 8), ALU.mult, RR, RR)
                            tt(dve, el[:, :, :], el[:, :, :], t8[:, :, :], ALU.add, RR, RR)
                    red(l1, el[:, :, :], ALU.max)
                    tt(dve, oh1[:, :, :], el[:, :, :], bc(l1, 8), ALU.is_equal, RR, RR)
                    stt(elm[:, :, :], oh1[:, :, :], -1e30, el[:, :, :], ALU.mult, ALU.add, RR, RR)
                    red(l2, elm[:, :, :], ALU.max)
                    tt(dve, oh2[:, :, :], elm[:, :, :], bc(l2, 8), ALU.is_equal, RR, RR)
                    tt(dve, dd, l2, l1, ALU.subtract, RR, RR)
                    actf(ee, dd, AF.Exp, RR, RR)
                    ts(dve, den, ee, 1.0, None, ALU.add, None, RR, RR)
                    recip(w1, den, RR, RR)
                    tt(dve, W1, w1, gval, ALU.mult, RR, RR)
                    tt(dve, W2, W1, ee, ALU.mult, RR, RR)
                    tt(dve, cg[:, :, :], oh1[:, :, :], bc(W1, 8), ALU.mult, RR, RR)
                    tt(dve, t8[:, :, :], oh2[:, :, :], bc(W2, 8), ALU.mult, RR, RR)
                    tt(dve, cg[:, :, :], cg[:, :, :], t8[:, :, :], ALU.add, RR, RR)
                    for g in range(4):
                        tt(dve, cmb[:, :, 8 * g:8 * g + 8], cg[:, :, :], bc(ohg[:, :, g], 8), ALU.mult, RR, [cmb])
                k.barrier()

        def emit_inproj(wsrc, ncols, fjobs, tjobs):
            with ExitStack() as st:
                W = k.sb(st, "ip_W", [128, 8, ncols], BF16)
                wv = wsrc.rearrange("(kc p) n -> p kc n", p=128)
                c0 = 0
                while c0 < ncols:
                    w_ = min(512, ncols - c0)
                    k.dma(k.q_pool, W[:, :, c0:c0 + w_], wv[:, :, c0:c0 + w_], writes=[W])
                    c0 += w_
                stg_r = Ring([k.sb(st, "ip_stg%d" % i, [128, S], BF16) for i in range(2)])
                stt_r = Ring([k.sb(st, "ip_stt%d" % i, [128, 4, 512], BF16) for i in range(2)])
                ps_r = Ring([k.ps(st, "ip_ps%d" % i, [128, 512], F32) for i in range(4)])
                n = 0
                for (col0, nr, fb, scale) in fjobs:
                    stg = stg_r.next()
                    for tg in range(8):
                        ps = ps_r.next()
                        for kc in range(8):
                            mm(ps[0:nr, :], W[:, kc, col0:col0 + nr], H["hT"][:, kc, tg * 512:(tg + 1) * 512],
                               kc == 0, kc == 7, [W, H["hT"]], [ps])
                        if n % 2 == 0:
                            actf(stg[0:nr, tg * 512:(tg + 1) * 512], ps[0:nr, :], AF.Copy, [ps], [stg], scale=scale)
                        else:
                            ts(dve, stg[0:nr, tg * 512:(tg + 1) * 512], ps[0:nr, :], scale, None, ALU.mult, None,
                               [ps], [stg])
                        n += 1
                    k.dma(k.q_sp, FB[fb, 0:nr, :], stg[0:nr, :], reads=[stg], writes=[fbres[fb]])
                for (col0, wd, tcol0) in tjobs:
                    for t4 in range(8):
                        stg = stt_r.next()
                        for j in range(4):
                            t = t4 * 4 + j
                            ps = ps_r.next()
                            for kc in range(8):
                                mm(ps[:, 0:wd], H["hT"][:, kc, t * 128:(t + 1) * 128], W[:, kc, col0:col0 + wd],
                                   kc == 0, kc == 7, [W, H["hT"]], [ps])
                            if n % 2 == 0:
                                actf(stg[:, j, 0:wd], ps[:, 0:wd], AF.Copy, [ps], [stg])
                            else:
                                cp(dve, stg[:, j, 0:wd], ps[:, 0:wd], [ps], [stg])
                            n += 1
                        k.dma(k.q_sp,
                              TB[t4 * 512:(t4 + 1) * 512, tcol0:tcol0 + wd].rearrange("(j p) c -> p j c", p=128),
                              stg[:, :, 0:wd], reads=[stg], writes=[tbres])
                k.barrier()

        def emit_outproj(wsrc, gbc, xsrc):
            with ExitStack() as st:
                W = k.sb(st, "op_W", [128, 8, D], BF16)
                wv = wsrc.rearrange("(kc p) n -> p kc n", p=128)
                for h in range(2):
                    k.dma(k.q_pool, W[:, :, h * 512:(h + 1) * 512], wv[:, :, h * 512:(h + 1) * 512], writes=[W])
                for kc in range(8):
                    tt(dve, W[:, kc, :], W[:, kc, :], gbc[:, :], ALU.mult, [W, gbc], [W])
                xt_r = Ring([k.sb(st, "op_xt%d" % i, [128, D], F32) for i in range(3)])
                xn_r = Ring([k.sb(st, "op_xn%d" % i, [128, D], F32) for i in range(2)])
                ps_r = Ring([k.ps(st, "op_ps%d" % i, [128, D], F32) for i in range(3)])
                for t in range(NT):
                    xt = xt_r.next()
                    k.dma(k.q_sp, xt[:, :], xsrc[t * 128:(t + 1) * 128, :], reads=[xres[t]], writes=[xt])
                    ps = ps_r.next()
                    for h in range(2):
                        for kc in range(8):
                            mm(ps[:, h * 512:(h + 1) * 512], H["hT"][:, kc, t * 128:(t + 1) * 128],
                               W[:, kc, h * 512:(h + 1) * 512], kc == 0, kc == 7, [H["hT"], W], [ps])
                    xn = xn_r.next()
                    tt(dve, xn[:, :], xt[:, :], ps[:, :], ALU.add, [xt, ps], [xn])
                    k.dma(k.q_sp, XS[t * 128:(t + 1) * 128, :], xn[:, :], reads=[xn], writes=[xres[t]])
                k.barrier()

        def emit_odd_mixer():
            with ExitStack() as st:
                mltb = k.sb(st, "sb_mlt", [128, 128], BF16)
                unegb = k.sb(st, "sb_uneg", [128, 128], BF16)
                k.dma(k.q_pool, mltb[:, :], I["c_mlt"][:, :], writes=[mltb])
                k.dma(k.q_pool, unegb[:, :], I["c_uneg"][:, :], writes=[unegb])
                qT_r = Ring([k.sb(st, "sb_qT%d" % i, [128, S], BF16) for i in range(2)])
                kT_r = Ring([k.sb(st, "sb_kT%d" % i, [128, S], BF16) for i in range(2)])
                vt_r = Ring([k.sb(st, "sb_vt%d" % i, [128, NT, 128], BF16) for i in range(2)])
                e32_r = Ring([k.sb(st, "sb_e%d" % i, [128, 512], F32) for i in range(6)])
                sp_r = Ring([k.sb(st, "sb_sp%d" % i, [128, 512], BF16) for i in range(10)])
                rb_r = Ring([k.sb(st, "sb_rb%d" % i, [128, 512], BF16) for i in range(10)])
                ab_r = Ring([k.sb(st, "sb_ab%d" % i, [128, 512], BF16) for i in range(6)])
                R32s = [k.sb(st, "sb_R32_%d" % i, [128, 512], F32) for i in range(2)]
                pz_r = Ring([k.ps(st, "sb_pz%d" % i, [128, 512], F32) for i in range(3)])
                pw_r = Ring([k.ps(st, "sb_pw%d" % i, [128, 512], F32) for i in range(3)])
                pos = [k.ps(st, "sb_po%d" % i, [128, 512], F32) for i in range(2)]
                for hp in range(8):
                    qT = qT_r.next()
                    kT = kT_r.next()
                    vt = vt_r.next()
                    k.dma(k.q_sp, qT[:, :], FB[hp, :, :], reads=[fbres[hp]], writes=[qT])
                    k.dma(k.q_sp, kT[:, :], FB[8 + hp, :, :], reads=[fbres[8 + hp]], writes=[kT])
                    k.dma(k.q_sp, vt[:, :, :],
                          TB[:, hp * 128:(hp + 1) * 128].rearrange("(t p) c -> p t c", p=128),
                          reads=[tbres], writes=[vt])
                    for qg in range(8):
                        kbs = list(range(4 * qg + 3, -1, -1))
                        nb = len(kbs)
                        items = [[None] * nb, [None] * nb]
                        pend = {}
                        pendB = []
                        for j in range(2):
                            mm(pos[j][:, :], zerob[:, :], qT[:, qg * 512:(qg + 1) * 512], True, False,
                               [zerob, qT], [pos[j]])

                        def stageA(j, i):
                            b0 = 64 * j
                            R32 = R32s[j]
                            kb = kbs[i]
                            j0 = max(0, kb - 4 * qg)
                            c0 = 128 * j0
                            wq = 512 - c0
                            q0 = qg * 512 + c0
                            diag = kb >= 4 * qg
                            pz = pz_r.next()
                            mm(pz[:, 0:wq], kT[b0:b0 + 64, kb * 128:(kb + 1) * 128], qT[b0:b0 + 64, q0:q0 + wq],
                               True, True, [kT, qT], [pz])
                            e32 = e32_r.next()
                            actf(e32[:, 0:wq], pz[:, 0:wq], AF.Exp, [pz], [e32])
                            pend[(j, i)] = (R32, kb, c0, wq, q0, diag, e32)

                        def stageA2(j, i):
                            R32, kb, c0, wq, q0, diag, e32 = pend.pop((j, i))
                            spb = sp_r.next()
                            if 'sbx_noln' not in flags:
                                actf(spb[:, 0:wq], e32[:, 0:wq], AF.Ln, [e32], [spb], bias=1.0)
                            if diag:
                                tt(pool, spb[:, 0:128], spb[:, 0:128], mltb[:, :], ALU.mult, [spb, mltb], [spb])
                            rb = None
                            if i > 0 and 'sbx_nor' not in flags:
                                rb = rb_r.next()
                                cp(dve, rb[:, :], R32[:, :], [R32], [rb])
                            if i < nb - 1 and 'sbx_nor' not in flags:
                                if i == 0:
                                    if c0 > 0:
                                        memset(dve, R32[:, 0:c0], 0.0, [R32])
                                    cp(dve, R32[:, c0:512], spb[:, 0:wq], [spb], [R32])
                                else:
                                    tt(dve, R32[:, c0:512], R32[:, c0:512], spb[:, 0:wq], ALU.add, [R32, spb],
                                       [R32])
                            items[j][i] = (kb, c0, wq, q0, diag, spb, rb)

                        def stageB(j, i):
                            if 'sbx_nob' in flags:
                                return
                            b0 = 64 * j
                            po = pos[j]
                            kb, c0, wq, q0, diag, spb, rb = items[j][i]
                            pw = pw_r.next()
                            mm(pw[:, 0:wq], kT[b0:b0 + 64, kb * 128:(kb + 1) * 128], qT[b0:b0 + 64, q0:q0 + wq],
                               True, False, [kT, qT], [pw])
                            mm(pw[:, 0:wq], unegb[:, :], spb[:, 0:wq], False, rb is None, [unegb, spb], [pw])
                            if rb is not None:
                                mm(pw[:, 0:wq], negonesb[:, :], rb[:, c0:512], False, True, [negonesb, rb], [pw])
                            ab = ab_r.next()
                            actf(ab[:, 0:wq], pw[:, 0:wq], AF.Exp, [pw], [ab])
                            if diag:
                                tt(dve, ab[:, 0:128], ab[:, 0:128], mltb[:, :], ALU.mult, [ab, mltb], [ab])
                            pendB.append((po, c0, wq, kb, ab, i))
                            while len(pendB) > 2:
                                stageB2()

                        def stageB2():
                            po, c0, wq, kb, ab, i = pendB.pop(0)
                            mm(po[:, c0:512], vt[:, kb, :], ab[:, 0:wq], False, i == nb - 1, [vt, ab], [po])

                        GW = 2
                        nw = nb // GW
                        for w in range(nw + 1):
                            if w < nw:
                                for i in range(w * GW, (w + 1) * GW):
                                    for j in range(2):
                                        stageA(j, i)
                                for i in range(w * GW, (w + 1) * GW):
                                    for j in range(2):
                                        stageA2(j, i)
                            if w > 0:
                                for i in range((w - 1) * GW, w * GW):
                                    for j in range(2):
                                        stageB(j, i)
                        while pendB:
                            stageB2()
                        for j in range(2):
                            b0 = 64 * j
                            if j == 0:
                                actf(H["hT"][b0:b0 + 64, hp, qg * 512:(qg + 1) * 512], pos[j][b0:b0 + 64, :], AF.Copy,
                                     [pos[j]], [H["hT"]])
                            else:
                                cp(dve, H["hT"][b0:b0 + 64, hp, qg * 512:(qg + 1) * 512], pos[j][b0:b0 + 64, :],
                                   [pos[j]], [H["hT"]])
                k.barrier()

        def emit_even_mixer(i_even, layer):
            lam_init = 0.8 - 0.6 * math.exp(-0.3 * layer)
            with ExitStack() as st:
                BM = [[k.sb(st, "BM%d_%d" % (h, di), [128, 512], BF16) for di in range(5)] for h in range(4)]
                b15 = k.sb(st, "b15", [128, 4], F32)
                neglam = k.sb(st, "neglam", [128, 1], F32)
                gsub = k.sb(st, "gsub", [128, 1], F32)
                with ExitStack() as st2:
                    rb = k.sb(st2, "rb", [32, 4], F32)
                    ohb = k.sb(st2, "ohb", [32, NF], F32)
                    fsb = k.sb(st2, "fsb", [4, NF], F32)
                    exch = k.sb(st2, "exch", [128, 128], F32)
                    nmk = [k.sb(st2, "nmk%d" % di, [128, 512], F32) for di in range(5)]
                    tl_r = Ring([k.sb(st2, "tl%d" % i, [128, 512], F32) for i in range(2)])
                    lrow = k.sb(st2, "lrow", [1, 264], F32)
                    grow = k.sb(st2, "grow", [1, 128], F32)
                    pf = Ring([k.ps(st2, "pf%d" % i, [128, 512], F32) for i in range(2)])
                    k.dma(k.q_sp, rb[:, :], I["rel_bias"][:, :], writes=[rb])
                    k.dma(k.q_sp, ohb[:, :], I["c_ohb"][:, :], writes=[ohb])
                    k.dma(k.q_sp, exch[:, :], I["c_exch"][:, :], writes=[exch])
                    k.dma(k.q_sp, b15[:, :], I["rel_bias"][15].partition_broadcast(128), writes=[b15])
                    for di in range(5):
                        k.dma(k.q_sp, nmk[di][:, :], I["c_negmask"][di], writes=[nmk[di]])
                    for c3 in range(3):
                        c0 = c3 * 512
                        w_ = min(512, NF - c0)
                        p = pf.next()
                        mm(p[0:4, 0:w_], rb[0:32, 0:4], ohb[0:32, c0:c0 + w_], True, True, [rb, ohb], [p])
                        cp(dve, fsb[0:4, c0:c0 + w_], p[0:4, 0:w_], [p], [fsb])
                    k.dma(k.q_sp, FS[:, :], fsb[:, :], reads=[fsb], writes=[fsres])
                    for h in range(4):
                        for di, dl in enumerate(DELTAS):
                            tl = tl_r.next()
                            src = bass.AP(tensor=FS_T, offset=h * NF + (384 - dl), ap=[[1, 128], [1, 512]])
                            k.dma(k.q_sp, tl[:, :], src, reads=[fsres], writes=[tl])
                            p = pf.next()
                            mm(p[:, :], exch[:, :], tl[:, :], True, True, [exch, tl], [p])
                            tt(dve, BM[h][di][:, :], p[:, :], nmk[di][:, :], ALU.add, [p, nmk[di]], [BM[h][di]])
                    k.dma(k.q_sp, lrow[:, 0:256], I["even_lambda"][i_even:i_even + 1, :], writes=[lrow])
                    LR = [lrow]
                    tt(dve, lrow[:, 0:64], lrow[:, 0:64], lrow[:, 64:128], ALU.mult, LR, LR)
                    tt(dve, lrow[:, 128:192], lrow[:, 128:192], lrow[:, 192:256], ALU.mult, LR, LR)
                    k.op(dve, lambda: nc.vector.tensor_reduce(out=lrow[:, 256:257], in_=lrow[:, 0:64], axis=AX.X,
                                                              op=ALU.add), LR, LR)
                    k.op(dve, lambda: nc.vector.tensor_reduce(out=lrow[:, 257:258], in_=lrow[:, 128:192], axis=AX.X,
                                                              op=ALU.add), LR, LR)
                    actf(lrow[:, 258:260], lrow[:, 256:258], AF.Exp, LR, LR)
                    tt(dve, lrow[:, 260:261], lrow[:, 259:260], lrow[:, 258:259], ALU.subtract, LR, LR)
                    ts(dve, lrow[:, 261:262], lrow[:, 260:261], -lam_init, None, ALU.add, None, LR, LR)
                    p = pf.next()
                    mm(p[:, 0:1], ones32[0:1, :], lrow[0:1, 261:262], True, True, [ones32, lrow], [p])
                    cp(dve, neglam[:, :], p[:, 0:1], [p], [neglam])
                    k.dma(k.q_sp, grow[:, :], I["even_subln"][i_even:i_even + 1, :], writes=[grow])
                    p = pf.next()
                    mm(p[:, 0:1], grow[0:1, :], ones32[0:1, 0:1], True, True, [grow, ones32], [p])
                    ts(dve, gsub[:, :], p[:, 0:1], 1.0 - lam_init, None, ALU.mult, None, [p], [gsub])
                    k.barrier()
                qT_r = Ring([k.sb(st, "da_qT%d" % i, [128, S], BF16) for i in range(2)])
                kT_r = Ring([k.sb(st, "da_kT%d" % i, [128, S], BF16) for i in range(2)])
                vt_r = Ring([k.sb(st, "da_vt%d" % i, [128, NT, 128], BF16) for i in range(2)])
                E_r = Ring([k.sb(st, "da_E%d" % i, [128, 512], BF16) for i in range(8)])
                f_r = Ring([k.sb(st, "da_f%d" % i, [128, 512], F32) for i in range(4)])
                o32_r = Ring([k.sb(st, "da_o%d" % i, [128, 512], F32) for i in range(2)])
                sq_r = Ring([k.sb(st, "da_sq%d" % i, [128, 512], BF16) for i in range(2)])
                ps_r = Ring([k.ps(st, "da_ps%d" % i, [128, 512], F32) for i in range(3)])
                pu = [k.ps(st, "da_pu%d" % i, [128, 512], F32) for i in range(2)]
                pd = [k.ps(st, "da_pd%d" % i, [128, 512], F32) for i in range(2)]
                pss = k.ps(st, "da_pss", [128, 512], F32)
                for h in range(0 if 'ev_noA' in flags else 4):
                    qT = qT_r.next()
                    kT = kT_r.next()
                    vt = vt_r.next()
                    k.dma(k.q_sp, qT[:, :], FB[h, :, :], reads=[fbres[h]], writes=[qT])
                    k.dma(k.q_sp, kT[:, :], FB[4 + h, :, :], reads=[fbres[4 + h]], writes=[kT])
                    k.dma(k.q_sp, vt[:, :, :],
                          TB[:, h * 128:(h + 1) * 128].rearrange("(t p) c -> p t c", p=128),
                          reads=[tbres], writes=[vt])
                    for qg in range(8):
                        nkb = 4 * qg + 4
                        its = [(kb, m) for kb in range(nkb) for m in range(2)]
                        held = {}

                        def st1(idx):
                            kb, m = its[idx]
                            j0 = max(0, kb - 4 * qg)
                            c0 = 128 * j0
                            wq = 512 - c0
                            q0 = qg * 512 + c0
                            dl = kb * 128 - qg * 512
                            b0 = 64 * m
                            ps = ps_r.next()
                            near = dl >= -128
                            mm(ps[:, 0:wq], kT[b0:b0 + 64, kb * 128:(kb + 1) * 128], qT[b0:b0 + 64, q0:q0 + wq],
                               True, not near, [kT, qT], [ps])
                            E = E_r.next()
                            if near:
                                bm = BM[h][DELTAS.index(dl)]
                                mm(ps[:, 0:wq], identb[:, :], bm[:, c0:512], False, True, [identb, bm], [ps])
                                actf(E[:, 0:wq], ps[:, 0:wq], AF.Exp, [ps], [E])
                            else:
                                actf(E[:, 0:wq], ps[:, 0:wq], AF.Exp, [ps, b15], [E], bias=b15[:, h:h + 1])
                            held[idx] = (E, c0, wq)

                        def st2(idx):
                            kb, m = its[idx]
                            E, c0, wq = held.pop(idx)
                            mm(pu[m][:, c0:512], vt[:, kb, :], E[:, 0:wq], kb == 0, kb == nkb - 1, [vt, E], [pu[m]])
                            mm(pd[m][:, c0:512], onesb[:, :], E[:, 0:wq], kb == 0, kb == nkb - 1, [onesb, E],
                               [pd[m]])

                        SK = 4 if 'sk4' in flags else 2
                        for idx in range(len(its) + SK):
                            if idx < len(its):
                                st1(idx)
                            if idx >= SK:
                                st2(idx - SK)
                        if 'da_nofin' in flags:
                            continue
                        r0 = f_r.next()
                        actf(r0[:, :], pd[0][:, :], AF.Ln, [pd[0]], [r0])
                        actf(r0[:, :], r0[:, :], AF.Exp, [r0], [r0], scale=-1.0)
                        r1 = f_r.next()
                        actf(r1[:, :], pd[1][:, :], AF.Ln, [pd[1]], [r1])
                        actf(r1[:, :], r1[:, :], AF.Exp, [r1], [r1], scale=-1.0)
                        t0 = f_r.next()
                        tt(dve, t0[:, :], pu[0][:, :], r0[:, :], ALU.mult, [pu[0], r0], [t0])
                        t1 = f_r.next()
                        tt(dve, t1[:, :], pu[1][:, :], r1[:, :], ALU.mult, [pu[1], r1], [t1])
                        o32 = o32_r.next()
                        stt(o32[:, :], t1[:, :], neglam[:, 0:1], t0[:, :], ALU.mult, ALU.add, [t1, t0, neglam], [o32])
                        sq = sq_r.next()
                        actf(sq[:, :], o32[:, :], AF.Square, [o32], [sq])
                        mm(pss[:, :], onesb[:, :], sq[:, :], True, True, [onesb, sq], [pss])
                        rs = f_r.next()
                        actf(rs[:, :], pss[:, :], AF.Ln, [pss, epsc], [rs], scale=1.0 / 128.0, bias=epsc[:, 0:1])
                        rs2 = f_r.next()
                        actf(rs2[:, :], rs[:, :], AF.Exp, [rs], [rs2], scale=-0.5)
                        stt(H["hT"][:, h, qg * 512:(qg + 1) * 512], o32[:, :], gsub[:, 0:1], rs2[:, :], ALU.mult, ALU.mult,
                            [o32, gsub, rs2], [H["hT"]])
                k.barrier()
            with ExitStack() as st:
                gtri = k.sb(st, "g_tri", [128, 128], F32)
                gm2 = k.sb(st, "g_m2", [128, 128], F32)
                gmaskb = k.sb(st, "g_mask", [128, 128], BF16)
                wgk = k.sb(st, "g_wgk", [16, 256], BF16)
                bgk = k.sb(st, "g_bgk", [1, 256], BF16)
                glag = k.sb(st, "g_lag", [128, 1], F32)
                grow = k.sb(st, "g_grow", [1, 128], F32)
                k.dma(k.q_sp, gtri[:, :], I["c_gtri"][:, :], writes=[gtri])
                k.dma(k.q_sp, gm2[:, :], I["c_gm2"][:, :], writes=[gm2])
                k.dma(k.q_pool, gmaskb[:, :], I["c_gmask"][:, :], writes=[gmaskb])
                k.dma(k.q_pool, wgk[:, :], I["even_w_gk2"][i_even], writes=[wgk])
                k.dma(k.q_pool, bgk[:, :], I["even_b_gk"][i_even:i_even + 1, :], writes=[bgk])
                k.dma(k.q_sp, grow[:, :], I["even_gla_norm"][i_even:i_even + 1, :], writes=[grow])
                pA = Ring([k.ps(st, "g_pA%d" % i, [128, 512], F32) for i in range(3)])
                pS = Ring([k.ps(st, "g_pS%d" % i, [128, 512], F32) for i in range(2)])
                pO = Ring([k.ps(st, "g_pO%d" % i, [128, 512], F32) for i in range(2)])
                p = pA.next()
                mm(p[:, 0:1], grow[0:1, :], ones32[0:1, 0:1], True, True, [grow, ones32], [p])
                cp(dve, glag[:, :], p[:, 0:1], [p], [glag])
                qg_r = Ring([k.sb(st, "g_q%d" % i, [128, 2, 512], BF16) for i in range(2)])
                kg_r = Ring([k.sb(st, "g_k%d" % i, [128, 2, 512], BF16) for i in range(2)])
                kt_r = Ring([k.sb(st, "g_kt%d" % i, [128, 4, 256], BF16) for i in range(2)])
                vg_r = Ring([k.sb(st, "g_v%d" % i, [128, 4, 512], BF16) for i in range(2)])
                bg_r = Ring([k.sb(st, "g_bg%d" % i, [16, 512], BF16) for i in range(2)])
                br_r = Ring([k.sb(st, "g_br%d" % i, [128, 4, 512], BF16) for i in range(2)])
                og_r = Ring([k.sb(st, "g_og%d" % i, [128, 4, 512], F32) for i in range(2)])
                e32_r = Ring([k.sb(st, "g_e%d" % i, [128, 256], F32) for i in range(2)])
                sp_r = Ring([k.sb(st, "g_sp%d" % i, [128, 256], F32) for i in range(2)])
                kf_r = Ring([k.sb(st, "g_kf%d" % i, [128, 256], F32) for i in range(2)])
                kd_r = Ring([k.sb(st, "g_kd%d" % i, [128, 256], BF16) for i in range(2)])
                eb_r = Ring([k.sb(st, "g_eb%d" % i, [128, 256], F32) for i in range(2)])
                en_r = Ring([k.sb(st, "g_en%d" % i, [128, 256], F32) for i in range(2)])
                qd_r = Ring([k.sb(st, "g_qd%d" % i, [128, 2, 128], BF16) for i in range(2)])
                ki_r = Ring([k.sb(st, "g_ki%d" % i, [128, 2, 128], BF16) for i in range(2)])
                at_r = Ring([k.sb(st, "g_at%d" % i, [128, 128], BF16) for i in range(3)])
                S32_r = [Ring([k.sb(st, "g_S32_%d_%d" % (h, i), [128, 128], F32) for i in range(2)]) for h in range(4)]
                Sb_r = [Ring([k.sb(st, "g_Sb_%d_%d" % (h, i), [128, 128], BF16) for i in range(3)]) for h in range(4)]
                sq_r = Ring([k.sb(st, "g_sq%d" % i, [128, 512], BF16) for i in range(2)])
                f_r = Ring([k.sb(st, "g_f%d" % i, [128, 512], F32) for i in range(4)])
                S32 = []
                Sb = []
                for h in range(4):
                    s = S32_r[h].next()
                    memset(dve, s[:, :], 0.0, [s])
                    S32.append(s)
                    b = Sb_r[h].next()
                    memset(pool, b[:, :], 0.0, [b])
                    Sb.append(b)
                for g8 in range(0 if 'ev_noB' in flags else 8):
                    t0g = g8 * 512
                    qgt = qg_r.next()
                    kgt = kg_r.next()
                    ktt = kt_r.next()
                    vgt = vg_r.next()
                    bgt = bg_r.next()
                    brt = br_r.next()
                    og = og_r.next()
                    for hh in range(2):
                        k.dma(k.q_sp, qgt[:, hh, :], FB[12 + hh, :, t0g:t0g + 512], reads=[fbres[12 + hh]], writes=[qgt])
                        k.dma(k.q_sp, kgt[:, hh, :], FB[14 + hh, :, t0g:t0g + 512], reads=[fbres[14 + hh]], writes=[kgt])
                    k.dma(k.q_sp, ktt[:, :, :],
                          TB[t0g:t0g + 512, 512:768].rearrange("(j p) c -> p j c", p=128), reads=[tbres], writes=[ktt])
                    k.dma(k.q_sp, vgt[:, :, :],
                          TB[t0g:t0g + 512, 768:1280].rearrange("(j p) c -> p j c", p=128), reads=[tbres], writes=[vgt])
                    k.dma(k.q_sp, bgt[:, :], FB[24, 0:16, t0g:t0g + 512], reads=[fbres[24]], writes=[bgt])
                    for h in range(4):
                        k.dma(k.q_sp, brt[:, h, :], FB[20 + h, :, t0g:t0g + 512], reads=[fbres[20 + h]], writes=[brt])
                    for j4 in range(4):
                        tc0 = j4 * 128
                        ppre = pA.next()
                        mm(ppre[:, 0:256], bgt[0:16, tc0:tc0 + 128], wgk[0:16, :], True, False, [bgt, wgk], [ppre])
                        mm(ppre[:, 0:256], onesb[0:1, :], bgk[0:1, :], False, True, [onesb, bgk], [ppre])
                        e32 = e32_r.next()
                        actf(e32[:, :], ppre[:, 0:256], AF.Exp, [ppre], [e32], scale=-1.0)
                        sp32 = sp_r.next()
                        actf(sp32[:, :], e32[:, :], AF.Ln, [e32], [sp32], bias=1.0)
                        pb = pA.next()
                        for hh in range(2):
                            mm(pb[:, hh * 128:(hh + 1) * 128], sp32[:, hh * 128:(hh + 1) * 128], gtri[:, :], True, True,
                               [sp32, gtri], [pb])
                        pk = pA.next()
                        mm(pk[:, 0:256], gm2[:, :], sp32[:, :], True, True, [gm2, sp32], [pk])
                        kf = kf_r.next()
                        actf(kf[:, :], pk[:, 0:256], AF.Exp, [pk], [kf])
                        kd = kd_r.next()
                        tt(dve, kd[:, :], ktt[:, j4, :], kf[:, :], ALU.mult, [ktt, kf], [kd])
                        eb = eb_r.next()
                        actf(eb[:, :], pb[:, 0:256], AF.Exp, [pb], [eb])
                        en = en_r.next()
                        actf(en[:, :], pb[:, 0:256], AF.Exp, [pb], [en], scale=-1.0)
                        qd = qd_r.next()
                        ki = ki_r.next()
                        for hh in range(2):
                            tt(dve, qd[:, hh, :], qgt[:, hh, tc0:tc0 + 128], eb[:, hh * 128:(hh + 1) * 128], ALU.mult,
                               [qgt, eb], [qd])
                            tt(dve, ki[:, hh, :], kgt[:, hh, tc0:tc0 + 128], en[:, hh * 128:(hh + 1) * 128], ALU.mult,
                               [kgt, en], [ki])
                        for h in range(4):
                            hh, jj = h // 2, h % 2
                            b0 = 64 * jj
                            psc = pS.next()
                            mm(psc[:, 0:128], ki[b0:b0 + 64, hh, :], qd[b0:b0 + 64, hh, :], True, True, [ki, qd], [psc])
                            at = at_r.next()
                            tt(dve, at[:, :], psc[:, 0:128], gmaskb[:, :], ALU.mult, [psc, gmaskb], [at])
                            po = pO.next()
                            mm(po[:, 0:128], vgt[:, j4, h * 128:(h + 1) * 128], at[:, :], True, False, [vgt, at], [po])
                            mm(po[:, 0:64], Sb[h][b0:b0 + 64, :], qd[b0:b0 + 64, hh, 0:64], False, False,
                               [Sb[h], qd], [po])
                            pst = pS.next()
                            mm(pst[:, 0:128], kd[0:64, hh * 128:(hh + 1) * 128], vgt[0:64, j4, h * 128:(h + 1) * 128],
                               True, True, [kd, vgt], [pst])
                            s1 = S32_r[h].next()
                            stt(s1[b0:b0 + 64, :], S32[h][b0:b0 + 64, :], eb[b0:b0 + 64, hh * 128 + 63:hh * 128 + 64],
                                pst[b0:b0 + 64, 0:128], ALU.mult, ALU.add, [S32[h], eb, pst], [s1])
                            sb1 = Sb_r[h].next()
                            actf(sb1[b0:b0 + 64, :], s1[b0:b0 + 64, :], AF.Copy, [s1], [sb1])
                            mm(po[:, 64:128], sb1[b0:b0 + 64, :], qd[b0:b0 + 64, hh, 64:128], False, True,
                               [sb1, qd], [po])
                            pst2 = pS.next()
                            mm(pst2[:, 0:128], kd[64:128, hh * 128:(hh + 1) * 128],
                               vgt[64:128, j4, h * 128:(h + 1) * 128], True, True, [kd, vgt], [pst2])
                            s2 = S32_r[h].next()
                            stt(s2[b0:b0 + 64, :], s1[b0:b0 + 64, :], eb[b0:b0 + 64, hh * 128 + 127:hh * 128 + 128],
                                pst2[b0:b0 + 64, 0:128], ALU.mult, ALU.add, [s1, eb, pst2], [s2])
                            sb2 = Sb_r[h].next()
                            actf(sb2[b0:b0 + 64, :], s2[b0:b0 + 64, :], AF.Copy, [s2], [sb2])
                            S32[h] = s2
                            Sb[h] = sb2
                            actf(og[:, h, tc0:tc0 + 128], po[:, 0:128], AF.Copy, [po], [og])
                    for h in range(4):
                        sq = sq_r.next()
                        actf(sq[:, :], og[:, h, :], AF.Square, [og], [sq])
                        pn = pA.next()
                        mm(pn[:, :], onesb[:, :], sq[:, :], True, True, [onesb, sq], [pn])
                        rs = f_r.next()
                        actf(rs[:, :], pn[:, :], AF.Ln, [pn, epsc], [rs], scale=1.0 / 128.0, bias=epsc[:, 0:1])
                        rs2 = f_r.next()
                        actf(rs2[:, :], rs[:, :], AF.Exp, [rs], [rs2], scale=-0.5)
                        sl = f_r.next()
                        actf(sl[:, :], brt[:, h, :], AF.Silu, [brt], [sl])
                        t1 = f_r.next()
                        stt(t1[:, :], og[:, h, :], glag[:, 0:1], rs2[:, :], ALU.mult, ALU.mult, [og, glag, rs2], [t1])
                        tt(dve, H["hT"][:, 4 + h, t0g:t0g + 512], t1[:, :], sl[:, :], ALU.mult, [t1, sl], [H["hT"]])
                k.barrier()

        def emit_moe(layer):
            with ExitStack() as st:
                wg_r = Ring([k.sb(st, "me_wg%d" % i, [128, 8, DEXP], BF16) for i in range(2)])
                wu_r = Ring([k.sb(st, "me_wu%d" % i, [128, 8, DEXP], BF16) for i in range(2)])
                wd_r = Ring([k.sb(st, "me_wd%d" % i, [128, 4, D], BF16) for i in range(2)])
                yacc = k.sb(st, "me_yacc", [128, 8, D], F32)
                aT_r = Ring([k.sb(st, "me_aT%d" % i, [128, 4, 512], BF16) for i in range(2)])
                sg_r = Ring([k.sb(st, "me_sg%d" % i, [128, 512], BF16) for i in range(3)])
                xt_r = Ring([k.sb(st, "me_xt%d" % i, [128, D], F32) for i in range(2)])
                pg_r = Ring([k.ps(st, "me_pg%d" % i, [128, 512], F32) for i in range(2)])
                pu_r = Ring([k.ps(st, "me_pu%d" % i, [128, 512], F32) for i in range(2)])
                py_r = Ring([k.ps(st, "me_py%d" % i, [128, D], F32) for i in range(2)])
                for qtr in range(4):
                    memset(dve, yacc[:, :, :], 0.0, [yacc])
                    jobs = [(e, tg) for e in range(NEXP) for tg in range(2)]
                    wts = {}

                    def load_w(e):
                        wg = wg_r.next()
                        wu = wu_r.next()
                        wd = wd_r.next()
                        k.dma(k.q_pool, wg[:, :, :],
                              I["expert_w_gate"][layer, e].rearrange("(kc p) f -> p kc f", p=128), writes=[wg])
                        k.dma(k.q_pool, wu[:, :, :],
                              I["expert_w_up"][layer, e].rearrange("(kc p) f -> p kc f", p=128), writes=[wu])
                        k.dma(k.q_pool, wd[:, :, :],
                              I["expert_w_down"][layer, e].rearrange("(fc p) n -> p fc n", p=128), writes=[wd])
                        wts[e] = (wg, wu, wd)

                    acts = {}

                    def emit_gu(e, tg):
                        wg, wu, wd = wts[e]
                        G = qtr * 2 + tg
                        aT = aT_r.next()
                        for fc in range(4):
                            pg = pg_r.next()
                            pu = pu_r.next()
                            for kc in range(8):
                                mm(pg[:, :], wg[:, kc, fc * 128:(fc + 1) * 128], H["hT"][:, kc, G * 512:(G + 1) * 512],
                                   kc == 0, kc == 7, [wg, H["hT"]], [pg])
                            for kc in range(8):
                                mm(pu[:, :], wu[:, kc, fc * 128:(fc + 1) * 128], H["hT"][:, kc, G * 512:(G + 1) * 512],
                                   kc == 0, kc == 7, [wu, H["hT"]], [pu])
                            sg = sg_r.next()
                            actf(sg[:, :], pg[:, :], AF.Silu, [pg], [sg])
                            tt(dve, aT[:, fc, :], sg[:, :], pu[:, :], ALU.mult, [sg, pu], [aT])
                        acts[(e, tg)] = aT

                    def emit_down(e, tg):
                        wg, wu, wd = wts[e]
                        aT = acts.pop((e, tg))
                        for t4 in range(4):
                            ti = tg * 4 + t4
                            T = qtr * 8 + ti
                            py = py_r.next()
                            for nh in range(2):
                                for fc in range(4):
                                    mm(py[:, nh * 512:(nh + 1) * 512], aT[:, fc, t4 * 128:(t4 + 1) * 128],
                                       wd[:, fc, nh * 512:(nh + 1) * 512], fc == 0, fc == 3, [aT, wd], [py])
                            stt(yacc[:, ti, :], py[:, :], cmb[:, T, e:e + 1], yacc[:, ti, :], ALU.mult, ALU.add,
                                [py, cmb, yacc], [yacc])

                    load_w(0)
                    for idx, (e, tg) in enumerate(jobs):
                        emit_gu(e, tg)
                        if idx > 0:
                            emit_down(*jobs[idx - 1])
                        if tg == 0 and e + 1 < NEXP:
                            load_w(e + 1)
                    emit_down(*jobs[-1])
                    for ti in range(8):
                        T = qtr * 8 + ti
                        xt = xt_r.next()
                        k.dma(k.q_sp, xt[:, :], XS[T * 128:(T + 1) * 128, :], reads=[xres[T]], writes=[xt])
                        tt(dve, yacc[:, ti, :], yacc[:, ti, :], gbc2[:, :], ALU.mult, [yacc, gbc2], [yacc])
                        tt(dve, xt[:, :], xt[:, :], yacc[:, ti, :], ALU.add, [xt, yacc], [xt])
                        k.dma(k.q_sp, XS[T * 128:(T + 1) * 128, :], xt[:, :], reads=[xt], writes=[xres[T]])
                k.barrier()

        def emit_slots(layer):
            with ExitStack() as st:
                Aall = k.sb(st, "sl_A", [128, NT, 32], BF16)
                ag = k.sb(st, "sl_ag", [128, NT, 8], F32)
                mltb = k.sb(st, "sl_mlt", [128, 128], BF16)
                pos = k.sb(st, "sl_pos", [128, NT, 32], F32)
                cnt = k.sb(st, "sl_cnt", [128, 32], F32)
                nb_ = k.sb(st, "sl_nb", [128, 32], F32)
                pa = k.sb(st, "sl_pa", [128, 32], F32)
                pb = k.sb(st, "sl_pb", [128, 32], F32)
                smg = k.sb(st, "sl_smg", [128, NT, 8], F32)
                t8 = k.sb(st, "sl_t8", [128, NT, 8], F32)
                sf = k.sb(st, "sl_sf", [128, 2, NT], F32)
                iog = k.sb(st, "sl_iog", [128, 8], F32)
                iob = k.sb(st, "sl_iob", [128, NBLK], F32)
                cmpt = k.sb(st, "sl_cmp", [128, NBLK, 32], F32)
                eb = k.sb(st, "sl_eb", [128, NBLK], F32)
                fg = k.sb(st, "sl_fg", [128, NBLK, 8], F32)
                ppos = k.ps(st, "sl_ppos", [128, NT * 32], F32)
                pcnt = k.ps(st, "sl_pcnt", [128, 512], F32)
                k.dma(k.q_pool, mltb[:, :], I["c_mlt"][:, :], writes=[mltb])
                k.dma(k.q_sp, iog[:, :], I["c_iog"][:, :], writes=[iog])
                k.dma(k.q_sp, iob[:, :], I["c_iob"][:, :], writes=[iob])
                RR = [Aall, ag, pos, cnt, nb_, pa, pb, smg, t8, sf, cmpt, eb, fg, r_ohg, r_oh1, r_oh2, r_v]

                def bc(ap2, n):
                    return ap2.unsqueeze(2).to_broadcast([128, NT, n])
                tt(dve, ag[:, :, :], r_oh1[:, :, :], r_oh2[:, :, :], ALU.add, RR, RR)
                for g in range(4):
                    tt(dve, Aall[:, :, 8 * g:8 * g + 8], ag[:, :, :], bc(r_ohg[:, :, g], 8), ALU.mult, RR, RR)
                for T in range(NT):
                    for T2 in range(T):
                        mm(ppos[:, T * 32:(T + 1) * 32], onesb[:, :], Aall[:, T2, :], T2 == 0, False,
                           [onesb, Aall], [ppos])
                    mm(ppos[:, T * 32:(T + 1) * 32], mltb[:, :], Aall[:, T, :], T == 0, True, [mltb, Aall], [ppos])
                for T in range(NT):
                    mm(pcnt[:, 0:32], onesb[:, :], Aall[:, T, :], T == 0, T == NT - 1, [onesb, Aall], [pcnt])
                cp(dve, pos[:, 0:16, :], ppos[:, 0:512].rearrange("p (t e) -> p t e", e=32), [ppos], RR)
                cp(dve, pos[:, 16:32, :], ppos[:, 512:1024].rearrange("p (t e) -> p t e", e=32), [ppos], RR)
                cp(dve, cnt[:, :], pcnt[:, 0:32], [pcnt], RR)
                ts(dve, nb_[:, :], cnt[:, :], 0.0, None, ALU.is_gt, None, RR, RR)
                for m in range(1, 8):
                    stt(nb_[:, :], cnt[:, :], float(BSZ * m), nb_[:, :], ALU.is_gt, ALU.add, RR, RR)
                cp(dve, pa[:, :], nb_[:, :], RR, RR)
                a_, b_ = pa, pb
                for sh in (1, 2, 4, 8, 16):
                    cp(dve, b_[:, 0:sh], a_[:, 0:sh], RR, RR)
                    tt(dve, b_[:, sh:32], a_[:, sh:32], a_[:, 0:32 - sh], ALU.add, RR, RR)
                    a_, b_ = b_, a_
                incl = a_
                excl = b_
                tt(dve, excl[:, :], incl[:, :], nb_[:, :], ALU.subtract, RR, RR)
                ts(dve, excl[:, :], excl[:, :], float(BSZ), None, ALU.mult, None, RR, RR)
                tt(dve, pos[:, :, :], pos[:, :, :], excl[:, :].unsqueeze(1).to_broadcast([128, NT, 32]), ALU.add,
                   RR, RR)
                for g in range(4):
                    if g == 0:
                        tt(dve, smg[:, :, :], pos[:, :, 0:8], bc(r_ohg[:, :, 0], 8), ALU.mult, RR, RR)
                    else:
                        tt(dve, t8[:, :, :], pos[:, :, 8 * g:8 * g + 8], bc(r_ohg[:, :, g], 8), ALU.mult, RR, RR)
                        tt(dve, smg[:, :, :], smg[:, :, :], t8[:, :, :], ALU.add, RR, RR)
                tt(dve, t8[:, :, :], smg[:, :, :], r_oh1[:, :, :], ALU.mult, RR, RR)
                k.op(dve, lambda: nc.vector.tensor_reduce(out=sf[:, 0, :], in_=t8[:, :, :], axis=AX.X, op=ALU.add),
                     RR, RR)
                tt(dve, t8[:, :, :], smg[:, :, :], r_oh2[:, :, :], ALU.mult, RR, RR)
                k.op(dve, lambda: nc.vector.tensor_reduce(out=sf[:, 1, :], in_=t8[:, :, :], axis=AX.X, op=ALU.add),
                     RR, RR)
                cp(dve, s12u[:, :, :], sf[:, :, :], RR, [s12u])
                tt(dve, cmpt[:, :, :], incl[:, :].unsqueeze(1).to_broadcast([128, NBLK, 32]),
                   iob[:, :].unsqueeze(2).to_broadcast([128, NBLK, 32]), ALU.is_le, RR + [iob], RR)
                k.op(dve, lambda: nc.vector.tensor_reduce(out=eb[:, :], in_=cmpt[:, :, :], axis=AX.X, op=ALU.add),
                     RR, RR)
                ts(dve, eb[:, :], eb[:, :], 31.0, None, ALU.min, None, RR, RR)
                ts(dve, eb[:, :], eb[:, :], 512.0, float(layer * NEXP * DEXP), ALU.mult, ALU.add, RR, RR)
                tt(dve, fg[:, :, 0:4], eb[:, :].unsqueeze(2).to_broadcast([128, NBLK, 4]),
                   iog[:, 0:4].unsqueeze(1).to_broadcast([128, NBLK, 4]), ALU.add, RR + [iog], RR)
                cp(dve, idxd[:, :, :], fg[:, :, 0:4], RR, [idxd])
                ts(dve, eb[:, :], eb[:, :], 2.0, None, ALU.mult, None, RR, RR)
                tt(dve, fg[:, :, :], eb[:, :].unsqueeze(2).to_broadcast([128, NBLK, 8]),
                   iog[:, :].unsqueeze(1).to_broadcast([128, NBLK, 8]), ALU.add, RR + [iog], RR)
                cp(dve, idxg[:, :, :], fg[:, :, :], RR, [idxg])
                io2 = k.sb(st, "sl_io2", [128, 2], F32)
                k.dma(k.q_sp, io2[:, :], I["c_io2"][:, :], writes=[io2])
                ts(dve, eb[:, :], eb[:, :], 0.25, None, ALU.mult, None, RR, RR)
                tt(dve, fg[:, :, 0:2], eb[:, :].unsqueeze(2).to_broadcast([128, NBLK, 2]),
                   io2[:, :].unsqueeze(1).to_broadcast([128, NBLK, 2]), ALU.add, RR + [io2], RR)
                cp(dve, idx2[:, :, :], fg[:, :, 0:2], RR, [idx2])
                k.dma(k.q_sp, MV[0].rearrange("(kc p) -> p kc", p=128), gmod2[:, :], reads=[gmod2], writes=[mvres],
                      allow_slow_non_contiguous=True)
                k.dma(k.q_sp, MV[1].rearrange("(kc p) -> p kc", p=128), modT[:, 24:32], reads=[modT], writes=[mvres],
                      allow_slow_non_contiguous=True)
                k.dma(k.q_sp, gsb[:, 0, :], MV[0].rearrange("(p kc) -> p kc", kc=8), reads=[mvres], writes=[gsb])
                k.dma(k.q_sp, gsb[:, 1, :], MV[1].rearrange("(p kc) -> p kc", kc=8), reads=[mvres], writes=[gsb])
                xr_r = Ring([k.sb(st, "sl_xr%d" % i, [128, D], F32) for i in range(3)])
                for T in range(NT):
                    xr = xr_r.next()
                    k.dma(k.q_sp, xr[:, :], XN[T * 128:(T + 1) * 128, :], reads=[xnres], writes=[xr])
                    for kk in range(2):
                        idma(XG[:, :], bass.IndirectOffsetOnAxis(ap=s12u[:, kk, T:T + 1], axis=0), xr[:, :], None,
                             [xr, s12u], [xgres])
                k.barrier()

        def emit_moe_sparse(layer):
            with ExitStack() as st:
                wg_r = Ring([k.sb(st, "ms_wg%d" % i, [128, 8, DEXP], BF16) for i in range(3)])
                wu_r = Ring([k.sb(st, "ms_wu%d" % i, [128, 8, DEXP], BF16) for i in range(3)])
                wd_r = Ring([k.sb(st, "ms_wd%d" % i, [128, 4, D], BF16) for i in range(3)])
                xg_r = Ring([k.sb(st, "ms_xg%d" % i, [128, D], F32) for i in range(8)])
                hs_r = Ring([k.sb(st, "ms_hs%d" % i, [128, 8, 512], BF16) for i in range(2)])
                aT_r = Ring([k.sb(st, "ms_aT%d" % i, [128, 4, 512], BF16) for i in range(2)])
                sg_r = Ring([k.sb(st, "ms_sg%d" % i, [128, 512], BF16) for i in range(3)])
                yo_r = Ring([k.sb(st, "ms_yo%d" % i, [128, D], F32) for i in range(3)])
                ptr_r = Ring([k.ps(st, "ms_ptr%d" % i, [128, 512], F32) for i in range(2)])
                pg_r = Ring([k.ps(st, "ms_pg%d" % i, [128, 512], F32) for i in range(2)])
                pu_r = Ring([k.ps(st, "ms_pu%d" % i, [128, 512], F32) for i in range(2)])
                py_r = Ring([k.ps(st, "ms_py%d" % i, [128, 512], F32) for i in range(2)])
                wgv = I["expert_w_gate"].rearrange("l e (p q) f -> (l e p) (q f)", q=8).rearrange(
                    "r (h c) -> (r h) c", h=2)
                wuv = I["expert_w_up"].rearrange("l e (p q) f -> (l e p) (q f)", q=8).rearrange(
                    "r (h c) -> (r h) c", h=2)
                wdv = I["expert_w_down"].rearrange("l e (p q) n -> (l e p) (q n)", q=4).rearrange(
                    "r (h c) -> (r h) c", h=2)
                wts = {}
                hss = {}
                hss_keep = {}
                acts = {}

                def load_blk(b):
                    if 'ms_nowl' in flags and b >= 3:
                        wts[b] = wts[b - 3]
                        return
                    wg = wg_r.next()
                    wu = wu_r.next()
                    wd = wd_r.next()
                    for h in range(2):
                        off = bass.IndirectOffsetOnAxis(ap=idx2[:, b, h:h + 1], axis=0)
                        idma(wg[:, 4 * h:4 * h + 4, :].rearrange("p a b -> p (a b)"), None, wgv, off, [idx2], [wg])
                        idma(wu[:, 4 * h:4 * h + 4, :].rearrange("p a b -> p (a b)"), None, wuv, off, [idx2], [wu])
                        idma(wd[:, 2 * h:2 * h + 2, :].rearrange("p a b -> p (a b)"), None, wdv, off, [idx2], [wd])
                    wts[b] = (wg, wu, wd)

                xgs = {}

                def load_x(b):
                    tl = []
                    for j in range(4):
                        xg = xg_r.next()
                        r0 = b * BSZ + j * 128
                        k.dma(k.q_sp, xg[:, :], XG[r0:r0 + 128, :], reads=[xgres], writes=[xg])
                        tl.append(xg)
                    xgs[b] = tl

                def prep_h(b):
                    hs = hs_r.next()
                    tl = xgs.pop(b)
                    for j in range(4):
                        xg = tl[j]
                        for hf in range(2):
                            ptr = ptr_r.next()
                            for q in range(4):
                                kc = hf * 4 + q
                                tr(ptr[:, q * 128:(q + 1) * 128], xg[:, kc::8], ident32[:, :], [xg, ident32], [ptr])
                            for q in range(4):
                                kc = hf * 4 + q
                                actf(hs[:, kc, j * 128:(j + 1) * 128], ptr[:, q * 128:(q + 1) * 128], AF.Identity,
                                     [ptr, gsb], [hs], scale=gsb[:, 0, kc:kc + 1], bias=gsb[:, 1, kc:kc + 1])
                    hss[b] = hs

                def emit_gu(b):
                    wg, wu, wd = wts[b]
                    hs = hss.pop(b)
                    aT = aT_r.next()
                    for fc in range(4):
                        pg = pg_r.next()
                        pu = pu_r.next()
                        for kc in range(8):
                            mm(pg[:, :], wg[:, kc, fc::4], hs[:, kc, :], kc == 0, kc == 7,
                               [wg, hs], [pg])
                        for kc in range(8):
                            mm(pu[:, :], wu[:, kc, fc::4], hs[:, kc, :], kc == 0, kc == 7,
                               [wu, hs], [pu])
                        sg = sg_r.next()
                        actf(sg[:, :], pg[:, :], AF.Silu, [pg], [sg])
                        tt(dve, aT[:, fc, :], sg[:, :], pu[:, :], ALU.mult, [sg, pu], [aT])
                    acts[b] = aT

                def emit_down(b):
                    wg, wu, wd = wts[b]
                    aT = acts.pop(b)
                    for j in range(4):
                        yo = yo_r.next()
                        for nh in range(2):
                            py = py_r.next()
                            for fc in range(4):
                                mm(py[:, :], aT[:, fc, j * 128:(j + 1) * 128],
                                   wd[:, fc, nh * 512:(nh + 1) * 512], fc == 0, fc == 3, [aT, wd], [py])
                            cp(dve, yo[:, nh * 512:(nh + 1) * 512], py[:, :], [py], [yo])
                        r0 = b * BSZ + j * 128
                        k.dma(k.q_sp, YS[r0:r0 + 128, :], yo[:, :], reads=[yo], writes=[ysres[b]])

                load_blk(0)
                load_blk(1)
                load_x(0)
                prep_h(0)
                load_x(1)
                for b in range(NBLK):
                    emit_gu(b)
                    if b > 0:
                        emit_down(b - 1)
                    if b + 2 < NBLK:
                        load_blk(b + 2)
                    if b + 1 < NBLK:
                        prep_h(b + 1)
                    if b + 2 < NBLK:
                        load_x(b + 2)
                emit_down(NBLK - 1)
                k.barrier()
            with ExitStack() as st:
                r1_r = Ring([k.sb(st, "ms_r1%d" % i, [128, D], F32) for i in range(2)])
                r2_r = Ring([k.sb(st, "ms_r2%d" % i, [128, D], F32) for i in range(2)])
                xt_r = Ring([k.sb(st, "ms_xt%d" % i, [128, D], F32) for i in range(2)])
                for T in range(NT):
                    r1 = r1_r.next()
                    r2 = r2_r.next()
                    idma(r1[:, :], None, YS[:, :], bass.IndirectOffsetOnAxis(ap=s12u[:, 0, T:T + 1], axis=0),
                         ysres + [s12u], [r1])
                    idma(r2[:, :], None, YS[:, :], bass.IndirectOffsetOnAxis(ap=s12u[:, 1, T:T + 1], axis=0),
                         ysres + [s12u], [r2])
                    xt = xt_r.next()
                    k.dma(k.q_sp, xt[:, :], XS[T * 128:(T + 1) * 128, :], reads=[xres[T]], writes=[xt])
                    ts(dve, r1[:, :], r1[:, :], r_v[:, 9, T:T + 1], None, ALU.mult, None, [r1, r_v], [r1])
                    stt(r1[:, :], r2[:, :], r_v[:, 10, T:T + 1], r1[:, :], ALU.mult, ALU.add, [r2, r_v, r1], [r1])
                    tt(dve, r1[:, :], r1[:, :], gbc2[:, :], ALU.mult, [r1, gbc2], [r1])
                    tt(dve, xt[:, :], xt[:, :], r1[:, :], ALU.add, [xt, r1], [xt])
                    k.dma(k.q_sp, XS[T * 128:(T + 1) * 128, :], xt[:, :], reads=[xt], writes=[xres[T]])
                k.barrier()

        def emit_final(do_norm):
            with ExitStack() as st:
                xt_r = Ring([k.sb(st, "f_xt%d" % i, [128, D], F32) for i in range(3)])
                xo_r = Ring([k.sb(st, "f_xo%d" % i, [128, D], F32) for i in range(2)])
                junk = k.sb(st, "f_junk", [128, D], BF16)
                sm_r = Ring([k.sb(st, "f_sm%d" % i, [128, 4], F32) for i in range(4)])
                gf = k.sb(st, "f_gf", [128, D], F32)
                k.dma(k.q_sp, gf[:, :], I["norm_final"][0].partition_broadcast(128), writes=[gf])
                evs = []
                for t in range(NT):
                    xt = xt_r.next()
                    k.dma(k.q_sp, xt[:, :], XS[t * 128:(t + 1) * 128, :], reads=[xres[t]], writes=[xt])
                    if do_norm:
                        sm = sm_r.next()
                        actf(junk[:, :], xt[:, :], AF.Square, [xt], [junk, sm], accum_out=sm[:, 0:1])
                        ts(dve, sm[:, 1:2], sm[:, 0:1], 1.0 / D, EPS, ALU.mult, ALU.add, [sm], [sm])
                        actf(sm[:, 2:3], sm[:, 1:2], AF.Sqrt, [sm], [sm])
                        recip(sm[:, 3:4], sm[:, 2:3], [sm], [sm])
                        xo = xo_r.next()
                        stt(xo[:, :], xt[:, :], sm[:, 3:4], gf[:, :], ALU.mult, ALU.mult, [xt, sm, gf], [xo])
                        src = xo
                    else:
                        src = xt
                    evs.append(k.dma(k.q_sp, OUT[t * 128:(t + 1) * 128, :], src[:, :], reads=[src]))
                for ev in evs:
                    sp.wait(ev)

        xsrc = I["x"]
        done = False
        if stop == ("evenonly",):
            H["hT"] = k.sb(gst, "hT", [128, 8, S], BF16)
            emit_even_mixer(0, 0)
            emit_final(False)
            done = True
            nlayers = 0
        if stop == ("oddonly",):
            H["hT"] = k.sb(gst, "hT", [128, 8, S], BF16)
            emit_odd_mixer()
            emit_final(False)
            done = True
            nlayers = 0
        for l in range(nlayers):
            emit_mod(l)
            lst = ExitStack()
            H["hT"] = k.sb(lst, "hT", [128, 8, S], BF16)
            emit_norm(xsrc, gmod1, modT[:, 0:8], l, router=False)
            ie = l // 2
            if 'skipmix' in flags:
                pass
            elif l % 2 == 0:
                fj = [(c * 128, 128, c, 0.125) for c in range(4)]
                fj += [(512 + c * 128, 128, 4 + c, 1.0) for c in range(4)]
                fj += [(1536 + c * 128, 128, 12 + c, 0.125) for c in range(2)]
                fj += [(1792 + c * 128, 128, 14 + c, 1.0) for c in range(2)]
                fj += [(2560 + c * 128, 128, 20 + c, 1.0) for c in range(4)]
                fj += [(3072, 16, 24, 1.0)]
                tj = [(1024, 512, 0), (1792, 256, 512), (2048, 512, 768)]
                emit_inproj(I["even_w_in"][ie], EVEN_COLS, fj, tj)
                emit_even_mixer(ie, l)
                emit_outproj(I["even_w_out"][ie], gbc1, xsrc)
            else:
                fj = [(c * 128, 128, c, 0.125) for c in range(8)]
                fj += [(1024 + c * 128, 128, 8 + c, 1.0) for c in range(8)]
                tj = [(2048, 512, 0), (2560, 512, 512)]
                emit_inproj(I["odd_w_in"][ie], 3072, fj, tj)
                emit_odd_mixer()
                emit_outproj(I["odd_w_out"][ie], gbc1, xsrc)
            if 'skipmix' not in flags:
                xsrc = XS
            if SPARSE:
                lst.close()
                H.pop("hT")
            if stop == ("mix", l):
                emit_final(False)
                done = True
                break
            emit_norm(xsrc, gmod2, modT[:, 24:32], l, router=True)
            if 'nomoe' not in flags:
                if SPARSE:
                    emit_slots(layer=l)
                    emit_moe_sparse(l)
                else:
                    emit_moe(l)
            if not SPARSE:
                lst.close()
            if stop == ("ffn", l):
                emit_final(False)
                done = True
                break
        if not done:
            emit_final(True)
        build.stats = (k.n_inst, k.nsem)
    return nc


_CACHE = {}


def _get_nc(nlayers=DEPTH, stop=None):
    key = (nlayers, stop)
    if key not in _CACHE:
        _CACHE[key] = build(nlayers, stop)
    return _CACHE[key]


def make_in_maps(inputs, cores):
    consts = make_consts()
    shared = {}
    for nm in WEIGHT_NAMES:
        a = np.ascontiguousarray(np.asarray(inputs[nm], dtype=np.float32))
        shared[nm] = a.reshape(WEIGHT_SHAPES[nm])
    shared.update(consts)
    x = np.asarray(inputs["x"], dtype=np.float32)
    c = np.asarray(inputs["c"], dtype=np.float32)
    maps = []
    for b in cores:
        m = dict(shared)
        m["x"] = np.ascontiguousarray(x[b])
        m["c"] = np.ascontiguousarray(c[b:b + 1])
        maps.append(m)
    return maps


def kernel(**inputs):
    nc = _get_nc()
    in_maps = make_in_maps(inputs, list(range(8)))
    res = run_bass_kernel_spmd(nc, in_maps, core_ids=list(range(8)))
    return np.stack([np.asarray(r["out"]) for r in res.results], axis=0).astype(np.float32)
```

```python
import math
from contextlib import ExitStack
import numpy as np
import concourse.bass as bass
import concourse.mybir as mybir
from concourse.bass_utils import run_bass_kernel_spmd

F32 = mybir.dt.float32
BF16 = mybir.dt.bfloat16
AF = mybir.ActivationFunctionType
ALU = mybir.AluOpType
AX = mybir.AxisListType

S = 4096
D = 1024
NT = 32
DEPTH = 4
EPS = 1e-6
EVEN_COLS = 3088
NEXP = 32
DEXP = 512
EPOCH = 12000


class Res:
    __slots__ = ("w", "r", "name")

    def __init__(self, name=""):
        self.w = None
        self.r = []
        self.name = name


class Tile:
    def __init__(self, ap, name=""):
        self.t = ap
        self.res = Res(name)

    def __getitem__(self, idx):
        return self.t[idx]


class Eng:
    def __init__(self, k, name, h, is_pe=False):
        self.k = k
        self.name = name
        self.h = h
        self.is_pe = is_pe
        self.sem = k.newsem(name + "_s0")
        self.count = 0
        self.nep = 0
        self.seen = {}
        self.hist = [(self.sem, 0)]

    def tick(self, inst):
        if self.count >= EPOCH:
            self.nep += 1
            self.sem = self.k.newsem("%s_s%d" % (self.name, self.nep))
            self.count = 0
        self.count += 1
        inst.then_inc(self.sem, 1)
        return (self.sem, self.count, self.name)

    def cur(self):
        return (self.sem, self.count, self.name)

    def wait(self, ev):
        sem, val, _ = ev
        if val <= 0:
            return
        key = id(sem)
        if self.seen.get(key, 0) >= val:
            return
        self.h.wait_ge(sem, val)
        self.seen[key] = val


class DmaQ:
    def __init__(self, k, name, eng, nslots=8):
        self.k = k
        self.eng = eng
        self.name = name
        self.sems = [k.newsem("%s_d%d" % (name, i)) for i in range(nslots)]
        self.vals = [0] * nslots
        self.i = 0

    def issue(self, emit, deps):
        s = self.i % len(self.sems)
        self.i += 1
        sem = self.sems[s]
        if self.vals[s] > 0:
            self.eng.wait((sem, self.vals[s], "dma"))
        for ev in deps:
            self.eng.wait(ev)
        if self.vals[s] >= 16 * 1500:
            sem = self.k.newsem("%s_d%d_%d" % (self.name, s, self.i))
            self.sems[s] = sem
            self.vals[s] = 0
        inst = emit()
        self.vals[s] += 16
        inst.then_inc(sem, 16)
        return (sem, self.vals[s], "dma")


class K:
    def __init__(self, nc, stack):
        self.nc = nc
        self.stack = stack
        self.nsem = 0
        self.pe = Eng(self, "pe", nc.tensor, is_pe=True)
        self.act = Eng(self, "act", nc.scalar)
        self.dve = Eng(self, "dve", nc.vector)
        self.pool = Eng(self, "pool", nc.gpsimd)
        self.sp = Eng(self, "sp", nc.sync)
        self.engs = [self.pe, self.act, self.dve, self.pool, self.sp]
        self.q_sp = DmaQ(self, "qsp", self.sp, 12)
        self.q_pool = DmaQ(self, "qpool", self.pool, 8)
        self.qs = [self.q_sp, self.q_pool]
        self.n_inst = 0

    def newsem(self, name):
        self.nsem += 1
        return self.stack.enter_context(self.nc.semaphore(name))

    def sb(self, st, name, shape, dtype):
        self.uid = getattr(self, "uid", 0) + 1
        name = "%s_u%d" % (name, self.uid)
        t = st.enter_context(self.nc.sbuf_tensor(name, list(shape), dtype))
        return Tile(t, name)

    def ps(self, st, name, shape, dtype=F32):
        self.uid = getattr(self, "uid", 0) + 1
        name = "%s_u%d" % (name, self.uid)
        t = st.enter_context(self.nc.psum_tensor(name, list(shape), dtype))
        return Tile(t, name)

    @staticmethod
    def _resof(x):
        return x.res if isinstance(x, Tile) else x

    def _deps(self, reads, writes):
        deps = []
        for r in reads:
            r = self._resof(r)
            if r.w is not None:
                deps.append(r.w)
        for w in writes:
            w = self._resof(w)
            if w.w is not None:
                deps.append(w.w)
            deps.extend(w.r)
        return deps

    def _commit(self, ev, reads, writes):
        for r in reads:
            r = self._resof(r)
            r.r = [e for e in r.r if e[0] is not ev[0]]
            r.r.append(ev)
        for w in writes:
            w = self._resof(w)
            w.w = ev
            w.r = []

    def op(self, eng, emit, reads=(), writes=()):
        for ev in self._deps(reads, writes):
            if eng.is_pe and ev[2] == "pe":
                continue
            eng.wait(ev)
        inst = emit()
        ev = eng.tick(inst)
        self._commit(ev, reads, writes)
        self.n_inst += 1
        return ev

    def dma(self, q, out, in_, reads=(), writes=(), **kw):
        deps = self._deps(reads, writes)
        ev = q.issue(lambda: q.eng.h.dma_start(out=out, in_=in_, **kw), deps)
        self._commit(ev, reads, writes)
        self.n_inst += 1
        return ev

    def barrier(self):
        evs = [e.cur() for e in self.engs]
        for q in self.qs:
            for s, v in zip(q.sems, q.vals):
                evs.append((s, v, "dma"))
        for e in self.engs:
            for ev in evs:
                if ev[0] is e.sem:
                    continue
                e.wait(ev)


class Ring:
    def __init__(self, tiles):
        self.tiles = tiles
        self.i = 0

    def next(self):
        t = self.tiles[self.i % len(self.tiles)]
        self.i += 1
        return t


def _t5_bucket_np(rel):
    nb = 16
    max_exact = 8
    ret = np.where(rel > 0, nb, 0)
    n = np.abs(rel)
    nf = np.maximum(n, 1).astype(np.float32)
    large = max_exact + (np.log(nf / max_exact) / np.float32(math.log(128 / max_exact))
                         * (nb - max_exact)).astype(np.int32)
    large = np.minimum(large, nb - 1)
    return ret + np.where(n < max_exact, n, large)


DELTAS = [-128, 0, 128, 256, 384]
NBLK = 48
BSZ = 512
NSLOT = NBLK * BSZ
R0 = 511
NF = 1280


def make_consts():
    c = {}
    c["c_ident"] = np.eye(128, dtype=np.float32)
    c["c_exch"] = np.ascontiguousarray(np.eye(128, dtype=np.float32)[::-1])
    i = np.arange(128)[:, None]
    j = np.arange(128)[None, :]
    c["c_mlt"] = (i < j).astype(np.float32)
    c["c_uneg"] = -(i >= j).astype(np.float32)
    same = (i // 64) == (j // 64)
    c["c_gtri"] = (-(1.0 / 16.0) * (same & (i <= j))).astype(np.float32)
    c["c_gm2"] = (-(1.0 / 16.0) * (same & (i > j))).astype(np.float32)
    c["c_gmask"] = (same & (i <= j)).astype(np.float32)
    n = np.arange(NF)
    bk = _t5_bucket_np(R0 - n)
    oh = np.zeros((32, NF), np.float32)
    oh[bk, n] = 1.0
    oh[:, 1151:] = 0.0
    c["c_ohb"] = oh
    nm = np.zeros((5, 128, 512), np.float32)
    for di, dl in enumerate(DELTAS):
        kk = dl + np.arange(128)[:, None]
        qq = np.arange(512)[None, :]
        allowed = (kk // 64) <= (qq // 64)
        nm[di] = np.where(allowed, 0.0, -30000.0)
    c["c_negmask"] = nm
    p = np.arange(128, dtype=np.float32)[:, None]
    c["c_iog"] = (p + 128.0 * np.arange(8, dtype=np.float32)[None, :]).astype(np.float32)
    c["c_io2"] = (2.0 * p + np.arange(2, dtype=np.float32)[None, :]).astype(np.float32)
    c["c_iob"] = np.tile(np.arange(NBLK, dtype=np.float32)[None, :], (128, 1))
    return c


WEIGHT_NAMES = ["w_ada", "b_ada", "norm_mix", "norm_ffn", "norm_final", "rel_bias",
                "even_w_in", "even_lambda", "even_subln", "even_w_gk2", "even_b_gk", "even_gla_norm",
                "even_w_out", "odd_w_in", "odd_w_out", "router_group_w", "router_group_b",
                "router_expert_w", "router_expert_b", "expert_w_gate", "expert_w_up", "expert_w_down"]
WEIGHT_SHAPES = {
    "w_ada": [4, 1024, 6144], "b_ada": [4, 6144], "norm_mix": [4, 1024], "norm_ffn": [4, 1024],
    "norm_final": [1, 1024], "rel_bias": [32, 4], "even_w_in": [2, 1024, 3088], "even_lambda": [2, 256],
    "even_subln": [2, 128], "even_w_gk2": [2, 16, 256], "even_b_gk": [2, 256], "even_gla_norm": [2, 128],
    "even_w_out": [2, 1024, 1024], "odd_w_in": [2, 1024, 3072], "odd_w_out": [2, 1024, 1024],
    "router_group_w": [4, 1024, 4], "router_group_b": [4, 4], "router_expert_w": [4, 1024, 32],
    "router_expert_b": [4, 32], "expert_w_gate": [4, 32, 1024, 512], "expert_w_up": [4, 32, 1024, 512],
    "expert_w_down": [4, 32, 512, 1024],
}


def build(nlayers=DEPTH, stop=None, flags=()):
    SPARSE = 'dense' not in flags
    nc = bass.Bass("TRN2", target_bir_lowering=False)
    I = {}
    I["x"] = nc.dram_tensor("x", [S, D], F32, kind="ExternalInput").ap()
    I["c"] = nc.dram_tensor("c", [1, D], F32, kind="ExternalInput").ap()
    for nm in WEIGHT_NAMES:
        I[nm] = nc.dram_tensor(nm, WEIGHT_SHAPES[nm], F32, kind="ExternalInput").ap()
    consts = make_consts()
    for nm, v in consts.items():
        I[nm] = nc.dram_tensor(nm, list(v.shape), F32, kind="ExternalInput").ap()
    OUT = nc.dram_tensor("out", [S, D], F32, kind="ExternalOutput").ap()
    XS = nc.dram_tensor("xs_scr", [S, D], F32, kind="Internal").ap()
    FB = nc.dram_tensor("fb_scr", [25, 128, S], BF16, kind="Internal").ap()
    TB = nc.dram_tensor("tb_scr", [S, 1280], BF16, kind="Internal").ap()
    FS_T = nc.dram_tensor("fs_scr", [4, NF], F32, kind="Internal")
    FS = FS_T.ap()
    XN = nc.dram_tensor("xn_scr", [S, D], F32, kind="Internal").ap()
    MV = nc.dram_tensor("mv_scr", [2, D], F32, kind="Internal").ap()
    XG = nc.dram_tensor("xg_scr", [NSLOT, D], F32, kind="Internal").ap()
    YS = nc.dram_tensor("ys_scr", [NSLOT, D], F32, kind="Internal").ap()

    with ExitStack() as gst:
        k = K(nc, gst)
        pe, act, dve, pool, sp = k.pe, k.act, k.dve, k.pool, k.sp

        def mm(out, lhsT, rhs, start, stop, reads, writes):
            k.op(pe, lambda: nc.tensor.matmul(out, lhsT=lhsT, rhs=rhs, start=start, stop=stop), reads, writes)

        def tr(out, in_, ident, reads, writes):
            k.op(pe, lambda: nc.tensor.transpose(out=out, in_=in_, identity=ident), reads, writes)

        def actf(out, in_, func, reads, writes, **kw):
            k.op(act, lambda: nc.scalar.activation(out=out, in_=in_, func=func, **kw), reads, writes)

        def tt(eng, out, in0, in1, op, reads, writes):
            k.op(eng, lambda: eng.h.tensor_tensor(out=out, in0=in0, in1=in1, op=op), reads, writes)

        def ts(eng, out, in0, s1, s2, op0, op1, reads, writes):
            if op1 is None:
                k.op(eng, lambda: eng.h.tensor_scalar(out=out, in0=in0, scalar1=s1, scalar2=None, op0=op0),
                     reads, writes)
            else:
                k.op(eng, lambda: eng.h.tensor_scalar(out=out, in0=in0, scalar1=s1, scalar2=s2, op0=op0, op1=op1),
                     reads, writes)

        def stt(out, in0, scalar, in1, op0, op1, reads, writes):
            k.op(dve, lambda: nc.vector.scalar_tensor_tensor(out=out, in0=in0, scalar=scalar, in1=in1,
                                                             op0=op0, op1=op1), reads, writes)

        def cp(eng, out, in_, reads, writes):
            k.op(eng, lambda: eng.h.tensor_copy(out=out, in_=in_), reads, writes)

        def recip(out, in_, reads, writes):
            k.op(dve, lambda: nc.vector.reciprocal(out=out, in_=in_), reads, writes)

        def memset(eng, t, val, writes):
            k.op(eng, lambda: eng.h.memset(t, val), (), writes)

        def rmax(out, in_, reads, writes):
            k.op(dve, lambda: nc.vector.tensor_reduce(out=out, in_=in_, axis=AX.X, op=ALU.max), reads, writes)

        xres = [Res("x%d" % i) for i in range(NT)]
        fbres = [Res("fb%d" % i) for i in range(25)]
        tbres = Res("tb")
        fsres = Res("fs")

        ident32 = k.sb(gst, "ident32", [128, 128], F32)
        ones32 = k.sb(gst, "ones32", [128, 128], F32)
        onesb = k.sb(gst, "onesb", [128, 128], BF16)
        negonesb = k.sb(gst, "negonesb", [128, 128], BF16)
        zerob = k.sb(gst, "zerob", [128, 128], BF16)
        identb = k.sb(gst, "identb", [128, 128], BF16)
        k.dma(k.q_sp, ident32[:, :], I["c_ident"][:, :], writes=[ident32])
        memset(dve, ones32[:, :], 1.0, [ones32])
        memset(dve, onesb[:, :], 1.0, [onesb])
        memset(dve, negonesb[:, :], -1.0, [negonesb])
        memset(dve, zerob[:, :], 0.0, [zerob])
        epsc = k.sb(gst, "epsc", [128, 1], F32)
        memset(dve, epsc[:, :], EPS, [epsc])
        cp(dve, identb[:, :], ident32[:, :], [ident32], [identb])

        modT = k.sb(gst, "modT", [128, 64], F32)
        gmod1 = k.sb(gst, "gmod1", [128, 8], F32)
        gmod2 = k.sb(gst, "gmod2", [128, 8], F32)
        gbc1 = k.sb(gst, "gbc1", [128, D], F32)
        gbc2 = k.sb(gst, "gbc2", [128, D], F32)
        scT = k.sb(gst, "scT", [128, 8], F32)
        cmb = k.sb(gst, "cmb", [128, NT, NEXP], F32)
        H = {}
        r_ohg = k.sb(gst, "r_ohg", [128, NT, 4], F32)
        r_oh1 = k.sb(gst, "r_oh1", [128, NT, 8], F32)
        r_oh2 = k.sb(gst, "r_oh2", [128, NT, 8], F32)
        r_v = k.sb(gst, "r_v", [128, 12, NT], F32)
        s12u = k.sb(gst, "s12u", [128, 2, NT], mybir.dt.uint32)
        idxg = k.sb(gst, "idxg", [128, NBLK, 8], mybir.dt.uint32)
        idxd = k.sb(gst, "idxd", [128, NBLK, 4], mybir.dt.uint32)
        idx2 = k.sb(gst, "idx2", [128, NBLK, 2], mybir.dt.uint32)
        gsb = k.sb(gst, "gsb", [128, 2, 8], F32)
        mvres = Res("mv")
        xnres = Res("xn")
        xgres = Res("xg")
        ysres = [Res("ys%d" % i) for i in range(NBLK)]

        def idma(out, out_off, in_, in_off, reads, writes):
            deps = k._deps(reads, writes)
            ev = k.q_pool.issue(lambda: nc.gpsimd.indirect_dma_start(out=out, out_offset=out_off, in_=in_,
                                                                     in_offset=in_off), deps)
            k._commit(ev, reads, writes)
            k.n_inst += 1
            return ev

        def emit_mod(l):
            with ExitStack() as st:
                crow = k.sb(st, "crow", [1, D], F32)
                sc_bc = k.sb(st, "sc_bc", [128, 8, 128], F32)
                wseg = Ring([k.sb(st, "wseg%d" % i, [128, 8, 512], F32) for i in range(2)])
                brow = Ring([k.sb(st, "brow%d" % i, [1, 512], F32) for i in range(2)])
                nrow = k.sb(st, "nrow", [1, 2 * D], F32)
                pm = k.ps(st, "pm", [128, 512], F32)
                pg = Ring([k.ps(st, "pg%d" % i, [128, 512], F32) for i in range(2)])
                k.dma(k.q_sp, crow[:, :], I["c"][:, :], writes=[crow])
                for kc in range(8):
                    mm(pm[:, kc:kc + 1], crow[0:1, kc * 128:(kc + 1) * 128], ones32[0:1, 0:1], True, True,
                       [crow, ones32], [pm])
                actf(scT[:, :], pm[:, 0:8], AF.Silu, [pm], [scT])
                for kc in range(8):
                    actf(sc_bc[:, kc, :], ones32[:, :], AF.Copy, [ones32, scT], [sc_bc], scale=scT[:, kc:kc + 1])
                wv = I["w_ada"][l].rearrange("(kc p) n -> p kc n", p=128)
                for seg in range(6):
                    for half in range(2):
                        n0 = seg * 1024 + half * 512
                        w = wseg.next()
                        b = brow.next()
                        k.dma(k.q_sp, w[:, :, :], wv[:, :, n0:n0 + 512], writes=[w])
                        k.dma(k.q_sp, b[:, :], I["b_ada"][l:l + 1, n0:n0 + 512], writes=[b])
                        if seg in (2, 5):
                            p = pg.next()
                            for kc in range(8):
                                mm(p[:, :], sc_bc[:, kc, :], w[:, kc, :], kc == 0, False, [sc_bc, w], [p])
                            mm(p[:, :], ones32[0:1, :], b[0:1, :], False, True, [ones32, b], [p])
                            g = gbc1 if seg == 2 else gbc2
                            cp(dve, g[:, half * 512:(half + 1) * 512], p[:, :], [p], [g])
                        else:
                            for j in range(4):
                                col = seg * 8 + half * 4 + j
                                for kc in range(8):
                                    mm(pm[:, col:col + 1], w[:, kc, j * 128:(j + 1) * 128], scT[:, kc:kc + 1],
                                       kc == 0, False, [w, scT], [pm])
                                mm(pm[:, col:col + 1], b[0:1, j * 128:(j + 1) * 128], ones32[0:1, 0:1], False, True,
                                   [b, ones32], [pm])
                k.dma(k.q_sp, nrow[:, 0:D], I["norm_mix"][l:l + 1, :], writes=[nrow])
                k.dma(k.q_sp, nrow[:, D:2 * D], I["norm_ffn"][l:l + 1, :], writes=[nrow])
                for j in range(16):
                    mm(pm[:, 48 + j:49 + j], nrow[0:1, j * 128:(j + 1) * 128], ones32[0:1, 0:1], True, True,
                       [nrow, ones32], [pm])
                cp(dve, modT[:, :], pm[:, 0:64], [pm], [modT])
                stt(gmod1[:, :], modT[:, 8:16], 1.0, modT[:, 48:56], ALU.add, ALU.mult, [modT], [gmod1])
                stt(gmod2[:, :], modT[:, 32:40], 1.0, modT[:, 56:64], ALU.add, ALU.mult, [modT], [gmod2])
                k.barrier()

        def emit_norm(xsrc, gmod, shiftc, layer, router):
            with ExitStack() as st:
                xt_r = Ring([k.sb(st, "n_xt%d" % i, [128, D], F32) for i in range(4)])
                xs_r = Ring([k.sb(st, "n_xs%d" % i, [128, D], F32) for i in range(3)])
                junk = k.sb(st, "n_junk", [128, D], BF16)
                sm_r = Ring([k.sb(st, "n_sm%d" % i, [128, 4], F32) for i in range(6)])
                ps_r = Ring([k.ps(st, "n_ps%d" % i, [128, D], F32) for i in range(2)])
                if router:
                    h32_r = Ring([k.sb(st, "n_h32%d" % i, [128, 8, 128], F32) for i in range(2)])
                    wr32 = k.sb(st, "n_wr32", [128, 8, 36], F32)
                    brr = k.sb(st, "n_brr", [1, 36], F32)
                    plg_r = Ring([k.ps(st, "n_plg%d" % i, [128, 512], F32) for i in range(2)])
                    lgall = k.sb(st, "n_lgall", [128, NT, 36], F32)
                    k.dma(k.q_sp, wr32[:, :, 0:4],
                          I["router_group_w"][layer].rearrange("(kc p) n -> p kc n", p=128), writes=[wr32])
                    k.dma(k.q_sp, wr32[:, :, 4:36],
                          I["router_expert_w"][layer].rearrange("(kc p) n -> p kc n", p=128), writes=[wr32])
                    k.dma(k.q_sp, brr[:, 0:4], I["router_group_b"][layer:layer + 1, :], writes=[brr])
                    k.dma(k.q_sp, brr[:, 4:36], I["router_expert_b"][layer:layer + 1, :], writes=[brr])
                state = {}

                def s1(t):
                    xt = xt_r.next()
                    k.dma(k.q_sp, xt[:, :], xsrc[t * 128:(t + 1) * 128, :], reads=[xres[t]], writes=[xt])
                    sm = sm_r.next()
                    actf(junk[:, :], xt[:, :], AF.Square, [xt], [junk, sm], accum_out=sm[:, 0:1])
                    state[t] = (xt, sm)

                def s2(t):
                    xt, sm = state[t]
                    ts(dve, sm[:, 1:2], sm[:, 0:1], 1.0 / D, EPS, ALU.mult, ALU.add, [sm], [sm])
                    actf(sm[:, 2:3], sm[:, 1:2], AF.Sqrt, [sm], [sm])
                    recip(sm[:, 3:4], sm[:, 2:3], [sm], [sm])
                    xs = xs_r.next()
                    ts(dve, xs[:, :], xt[:, :], sm[:, 3:4], None, ALU.mult, None, [xt, sm], [xs])
                    if router and SPARSE:
                        k.dma(k.q_sp, XN[t * 128:(t + 1) * 128, :], xs[:, :], reads=[xs], writes=[xnres])
                    state[t] = xs

                def s3(t):
                    xs = state.pop(t)
                    ps = ps_r.next()
                    for kc in range(8):
                        tr(ps[:, kc * 128:(kc + 1) * 128], xs[:, kc * 128:(kc + 1) * 128], ident32[:, :],
                           [xs, ident32], [ps])
                    for kc in range(8):
                        if router and SPARSE:
                            break
                        actf(H["hT"][:, kc, t * 128:(t + 1) * 128], ps[:, kc * 128:(kc + 1) * 128], AF.Identity,
                             [ps, gmod, modT], [H["hT"]], scale=gmod[:, kc:kc + 1], bias=shiftc[:, kc:kc + 1])
                    if router:
                        h32 = h32_r.next()
                        for kc in range(8):
                            actf(h32[:, kc, :], ps[:, kc * 128:(kc + 1) * 128], AF.Identity,
                                 [ps, gmod, modT], [h32], scale=gmod[:, kc:kc + 1], bias=shiftc[:, kc:kc + 1])
                        plg = plg_r.next()
                        for kc in range(8):
                            mm(plg[:, 0:36], h32[:, kc, :], wr32[:, kc, :], kc == 0, False, [h32, wr32], [plg])
                        mm(plg[:, 0:36], ones32[0:1, :], brr[0:1, :], False, True, [ones32, brr], [plg])
                        cp(dve, lgall[:, t, :], plg[:, 0:36], [plg], [lgall])

                for i in range(NT + 2):
                    if i < NT:
                        s1(i)
                    if 1 <= i <= NT:
                        s2(i - 1)
                    if i >= 2:
                        s3(i - 2)
                if router:
                    def bc(ap2, n):
                        return ap2.unsqueeze(2).to_broadcast([128, NT, n])
                    f4 = k.sb(st, "r_f4", [128, NT, 4], F32)
                    ohg = r_ohg
                    v = r_v
                    el = k.sb(st, "r_el", [128, NT, 8], F32)
                    t8 = k.sb(st, "r_t8", [128, NT, 8], F32)
                    oh1 = r_oh1
                    oh2 = r_oh2
                    elm = k.sb(st, "r_elm", [128, NT, 8], F32)
                    cg = k.sb(st, "r_cg", [128, NT, 8], F32)
                    RR = [f4, ohg, v, el, t8, oh1, oh2, elm, cg, lgall]
                    gm, gs, gval, l1, l2, dd, ee, den, w1, W1, W2 = [v[:, i, :] for i in range(11)]

                    def red(out, in_, op):
                        k.op(dve, lambda: nc.vector.tensor_reduce(out=out, in_=in_, axis=AX.X, op=op), RR, RR)

                    lgg = lgall[:, :, 0:4]
                    red(gm, lgg, ALU.max)
                    tt(dve, ohg[:, :, :], lgg, bc(gm, 4), ALU.is_equal, RR, RR)
                    tt(dve, f4[:, :, :], lgg, bc(gm, 4), ALU.subtract, RR, RR)
                    actf(f4[:, :, :], f4[:, :, :], AF.Exp, RR, RR)
                    red(gs, f4[:, :, :], ALU.add)
                    recip(gval, gs, RR, RR)
                    for g in range(4):
                        src = lgall[:, :, 4 + 8 * g:12 + 8 * g]
                        if g == 0:
                            tt(dve, el[:, :, :], src, bc(ohg[:, :, g], 8), ALU.mult, RR, RR)
                        else:
                            tt(dve, t8[:, :, :], src, bc(ohg[:, :, g], 8), ALU.mult, RR, RR)
                            tt(dve, el[:, :, :], el[:, :, :], t8[:, :, :], ALU.add, RR, RR)
                    red(l1, el[:, :, :], ALU.max)
                    tt(dve, oh1[:, :, :], el[:, :, :], bc(l1, 8), ALU.is_equal, RR, RR)
                    stt(elm[:, :, :], oh1[:, :, :], -1e30, el[:, :, :], ALU.mult, ALU.add, RR, RR)
                    red(l2, elm[:, :, :], ALU.max)
                    tt(dve, oh2[:, :, :], elm[:, :, :], bc(l2, 8), ALU.is_equal, RR, RR)
                    tt(dve, dd, l2, l1, ALU.subtract, RR, RR)
                    actf(ee, dd, AF.Exp, RR, RR)
                    ts(dve, den, ee, 1.0, None, ALU.add, None, RR, RR)
                    recip(w1, den, RR, RR)
                    tt(dve, W1, w1, gval, ALU.mult, RR, RR)
                    tt(dve, W2, W1, ee, ALU.mult, RR, RR)
                    tt(dve, cg[:, :, :], oh1[:, :, :], bc(W1, 8), ALU.mult, RR, RR)
                    tt(dve, t8[:, :, :], oh2[:, :, :], bc(W2, 8), ALU.mult, RR, RR)
                    tt(dve, cg[:, :, :], cg[:, :, :], t8[:, :, :], ALU.add, RR, RR)
                    for g in range(4):
                        tt(dve, cmb[:, :, 8 * g:8 * g + 8], cg[:, :, :], bc(ohg[:, :, g], 8), ALU.mult, RR, [cmb])
                k.barrier()

        def emit_inproj(wsrc, ncols, fjobs, tjobs):
            with ExitStack() as st:
                W = k.sb(st, "ip_W", [128, 8, ncols], BF16)
                wv = wsrc.rearrange("(kc p) n -> p kc n", p=128)
                c0 = 0
                while c0 < ncols:
                    w_ = min(512, ncols - c0)
                    k.dma(k.q_pool, W[:, :, c0:c0 + w_], wv[:, :, c0:c0 + w_], writes=[W])
                    c0 += w_
                stg_r = Ring([k.sb(st, "ip_stg%d" % i, [128, S], BF16) for i in range(2)])
                stt_r = Ring([k.sb(st, "ip_stt%d" % i, [128, 4, 512], BF16) for i in range(2)])
                ps_r = Ring([k.ps(st, "ip_ps%d" % i, [128, 512], F32) for i in range(4)])
                n = 0
                for (col0, nr, fb, scale) in fjobs:
                    stg = stg_r.next()
                    for tg in range(8):
                        ps = ps_r.next()
                        for kc in range(8):
                            mm(ps[0:nr, :], W[:, kc, col0:col0 + nr], H["hT"][:, kc, tg * 512:(tg + 1) * 512],
                               kc == 0, kc == 7, [W, H["hT"]], [ps])
                        if n % 2 == 0:
                            actf(stg[0:nr, tg * 512:(tg + 1) * 512], ps[0:nr, :], AF.Copy, [ps], [stg], scale=scale)
                        else:
                            ts(dve, stg[0:nr, tg * 512:(tg + 1) * 512], ps[0:nr, :], scale, None, ALU.mult, None,
                               [ps], [stg])
                        n += 1
                    k.dma(k.q_sp, FB[fb, 0:nr, :], stg[0:nr, :], reads=[stg], writes=[fbres[fb]])
                for (col0, wd, tcol0) in tjobs:
                    for t4 in range(8):
                        stg = stt_r.next()
                        for j in range(4):
                            t = t4 * 4 + j
                            ps = ps_r.next()
                            for kc in range(8):
                                mm(ps[:, 0:wd], H["hT"][:, kc, t * 128:(t + 1) * 128], W[:, kc, col0:col0 + wd],
                                   kc == 0, kc == 7, [W, H["hT"]], [ps])
                            if n % 2 == 0:
                                actf(stg[:, j, 0:wd], ps[:, 0:wd], AF.Copy, [ps], [stg])
                            else:
                                cp(dve, stg[:, j, 0:wd], ps[:, 0:wd], [ps], [stg])
                            n += 1
                        k.dma(k.q_sp,
                              TB[t4 * 512:(t4 + 1) * 512, tcol0:tcol0 + wd].rearrange("(j p) c -> p j c", p=128),
                              stg[:, :, 0:wd], reads=[stg], writes=[tbres])
                k.barrier()

        def emit_outproj(wsrc, gbc, xsrc):
            with ExitStack() as st:
                W = k.sb(st, "op_W", [128, 8, D], BF16)
                wv = wsrc.rearrange("(kc p) n -> p kc n", p=128)
                for h in range(2):
                    k.dma(k.q_pool, W[:, :, h * 512:(h + 1) * 512], wv[:, :, h * 512:(h + 1) * 512], writes=[W])
                for kc in range(8):
                    tt(dve, W[:, kc, :], W[:, kc, :], gbc[:, :], ALU.mult, [W, gbc], [W])
                xt_r = Ring([k.sb(st, "op_xt%d" % i, [128, D], F32) for i in range(3)])
                xn_r = Ring([k.sb(st, "op_xn%d" % i, [128, D], F32) for i in range(2)])
                ps_r = Ring([k.ps(st, "op_ps%d" % i, [128, D], F32) for i in range(3)])
                for t in range(NT):
                    xt = xt_r.next()
                    k.dma(k.q_sp, xt[:, :], xsrc[t * 128:(t + 1) * 128, :], reads=[xres[t]], writes=[xt])
                    ps = ps_r.next()
                    for h in range(2):
                        for kc in range(8):
                            mm(ps[:, h * 512:(h + 1) * 512], H["hT"][:, kc, t * 128:(t + 1) * 128],
                               W[:, kc, h * 512:(h + 1) * 512], kc == 0, kc == 7, [H["hT"], W], [ps])
                    xn = xn_r.next()
                    tt(dve, xn[:, :], xt[:, :], ps[:, :], ALU.add, [xt, ps], [xn])
                    k.dma(k.q_sp, XS[t * 128:(t + 1) * 128, :], xn[:, :], reads=[xn], writes=[xres[t]])
                k.barrier()

        def emit_odd_mixer():
            with ExitStack() as st:
                mltb = k.sb(st, "sb_mlt", [128, 128], BF16)
                unegb = k.sb(st, "sb_uneg", [128, 128], BF16)
                k.dma(k.q_pool, mltb[:, :], I["c_mlt"][:, :], writes=[mltb])
                k.dma(k.q_pool, unegb[:, :], I["c_uneg"][:, :], writes=[unegb])
                qT_r = Ring([k.sb(st, "sb_qT%d" % i, [128, S], BF16) for i in range(2)])
                kT_r = Ring([k.sb(st, "sb_kT%d" % i, [128, S], BF16) for i in range(2)])
                vt_r = Ring([k.sb(st, "sb_vt%d" % i, [128, NT, 128], BF16) for i in range(2)])
                e32_r = Ring([k.sb(st, "sb_e%d" % i, [128, 512], F32) for i in range(6)])
                sp_r = Ring([k.sb(st, "sb_sp%d" % i, [128, 512], BF16) for i in range(10)])
                rb_r = Ring([k.sb(st, "sb_rb%d" % i, [128, 512], BF16) for i in range(10)])
                ab_r = Ring([k.sb(st, "sb_ab%d" % i, [128, 512], BF16) for i in range(6)])
                R32s = [k.sb(st, "sb_R32_%d" % i, [128, 512], F32) for i in range(2)]
                pz_r = Ring([k.ps(st, "sb_pz%d" % i, [128, 512], F32) for i in range(3)])
                pw_r = Ring([k.ps(st, "sb_pw%d" % i, [128, 512], F32) for i in range(3)])
                pos = [k.ps(st, "sb_po%d" % i, [128, 512], F32) for i in range(2)]
                for hp in range(8):
                    qT = qT_r.next()
                    kT = kT_r.next()
                    vt = vt_r.next()
                    k.dma(k.q_sp, qT[:, :], FB[hp, :, :], reads=[fbres[hp]], writes=[qT])
                    k.dma(k.q_sp, kT[:, :], FB[8 + hp, :, :], reads=[fbres[8 + hp]], writes=[kT])
                    k.dma(k.q_sp, vt[:, :, :],
                          TB[:, hp * 128:(hp + 1) * 128].rearrange("(t p) c -> p t c", p=128),
                          reads=[tbres], writes=[vt])
                    for qg in range(8):
                        kbs = list(range(4 * qg + 3, -1, -1))
                        nb = len(kbs)
                        items = [[None] * nb, [None] * nb]
                        pend = {}
                        pendB = []
                        for j in range(2):
                            mm(pos[j][:, :], zerob[:, :], qT[:, qg * 512:(qg + 1) * 512], True, False,
                               [zerob, qT], [pos[j]])

                        def stageA(j, i):
                            b0 = 64 * j
                            R32 = R32s[j]
                            kb = kbs[i]
                            j0 = max(0, kb - 4 * qg)
                            c0 = 128 * j0
                            wq = 512 - c0
                            q0 = qg * 512 + c0
                            diag = kb >= 4 * qg
                            pz = pz_r.next()
                            mm(pz[:, 0:wq], kT[b0:b0 + 64, kb * 128:(kb + 1) * 128], qT[b0:b0 + 64, q0:q0 + wq],
                               True, True, [kT, qT], [pz])
                            e32 = e32_r.next()
                            actf(e32[:, 0:wq], pz[:, 0:wq], AF.Exp, [pz], [e32])
                            pend[(j, i)] = (R32, kb, c0, wq, q0, diag, e32)

                        def stageA2(j, i):
                            R32, kb, c0, wq, q0, diag, e32 = pend.pop((j, i))
                            spb = sp_r.next()
                            if 'sbx_noln' not in flags:
                                actf(spb[:, 0:wq], e32[:, 0:wq], AF.Ln, [e32], [spb], bias=1.0)
                            if diag:
                                tt(pool, spb[:, 0:128], spb[:, 0:128], mltb[:, :], ALU.mult, [spb, mltb], [spb])
                            rb = None
                            if i > 0 and 'sbx_nor' not in flags:
                                rb = rb_r.next()
                                cp(dve, rb[:, :], R32[:, :], [R32], [rb])
                            if i < nb - 1 and 'sbx_nor' not in flags:
                                if i == 0:
                                    if c0 > 0:
                                        memset(dve, R32[:, 0:c0], 0.0, [R32])
                                    cp(dve, R32[:, c0:512], spb[:, 0:wq], [spb], [R32])
                                else:
                                    tt(dve, R32[:, c0:512], R32[:, c0:512], spb[:, 0:wq], ALU.add, [R32, spb],
                                       [R32])
                            items[j][i] = (kb, c0, wq, q0, diag, spb, rb)

                        def stageB(j, i):
                            if 'sbx_nob' in flags:
                                return
                            b0 = 64 * j
                            po = pos[j]
                            kb, c0, wq, q0, diag, spb, rb = items[j][i]
                            pw = pw_r.next()
                            mm(pw[:, 0:wq], kT[b0:b0 + 64, kb * 128:(kb + 1) * 128], qT[b0:b0 + 64, q0:q0 + wq],
                               True, False, [kT, qT], [pw])
                            mm(pw[:, 0:wq], unegb[:, :], spb[:, 0:wq], False, rb is None, [unegb, spb], [pw])
                            if rb is not None:
                                mm(pw[:, 0:wq], negonesb[:, :], rb[:, c0:512], False, True, [negonesb, rb], [pw])
                            ab = ab_r.next()
                            actf(ab[:, 0:wq], pw[:, 0:wq], AF.Exp, [pw], [ab])
                            if diag:
                                tt(dve, ab[:, 0:128], ab[:, 0:128], mltb[:, :], ALU.mult, [ab, mltb], [ab])
                            pendB.append((po, c0, wq, kb, ab, i))
                            while len(pendB) > 2:
                                stageB2()

                        def stageB2():
                            po, c0, wq, kb, ab, i = pendB.pop(0)
                            mm(po[:, c0:512], vt[:, kb, :], ab[:, 0:wq], False, i == nb - 1, [vt, ab], [po])

                        GW = 2
                        nw = nb // GW
                        for w in range(nw + 1):
                            if w < nw:
                                for i in range(w * GW, (w + 1) * GW):
                                    for j in range(2):
                                        stageA(j, i)
                                for i in range(w * GW, (w + 1) * GW):
                                    for j in range(2):
                                        stageA2(j, i)
                            if w > 0:
                                for i in range((w - 1) * GW, w * GW):
                                    for j in range(2):
                                        stageB(j, i)
                        while pendB:
                            stageB2()
                        for j in range(2):
                            b0 = 64 * j
                            if j == 0:
                                actf(H["hT"][b0:b0 + 64, hp, qg * 512:(qg + 1) * 512], pos[j][b0:b0 + 64, :], AF.Copy,
                                     [pos[j]], [H["hT"]])
                            else:
                                cp(dve, H["hT"][b0:b0 + 64, hp, qg * 512:(qg + 1) * 512], pos[j][b0:b0 + 64, :],
                                   [pos[j]], [H["hT"]])
                k.barrier()

        def emit_even_mixer(i_even, layer):
            lam_init = 0.8 - 0.6 * math.exp(-0.3 * layer)
            with ExitStack() as st:
                BM = [[k.sb(st, "BM%d_%d" % (h, di), [128, 512], BF16) for di in range(5)] for h in range(4)]
                b15 = k.sb(st, "b15", [128, 4], F32)
                neglam = k.sb(st, "neglam", [128, 1], F32)
                gsub = k.sb(st, "gsub", [128, 1], F32)
                with ExitStack() as st2:
                    rb = k.sb(st2, "rb", [32, 4], F32)
                    ohb = k.sb(st2, "ohb", [32, NF], F32)
                    fsb = k.sb(st2, "fsb", [4, NF], F32)
                    exch = k.sb(st2, "exch", [128, 128], F32)
                    nmk = [k.sb(st2, "nmk%d" % di, [128, 512], F32) for di in range(5)]
                    tl_r = Ring([k.sb(st2, "tl%d" % i, [128, 512], F32) for i in range(2)])
                    lrow = k.sb(st2, "lrow", [1, 264], F32)
                    grow = k.sb(st2, "grow", [1, 128], F32)
                    pf = Ring([k.ps(st2, "pf%d" % i, [128, 512], F32) for i in range(2)])
                    k.dma(k.q_sp, rb[:, :], I["rel_bias"][:, :], writes=[rb])
                    k.dma(k.q_sp, ohb[:, :], I["c_ohb"][:, :], writes=[ohb])
                    k.dma(k.q_sp, exch[:, :], I["c_exch"][:, :], writes=[exch])
                    k.dma(k.q_sp, b15[:, :], I["rel_bias"][15].partition_broadcast(128), writes=[b15])
                    for di in range(5):
                        k.dma(k.q_sp, nmk[di][:, :], I["c_negmask"][di], writes=[nmk[di]])
                    for c3 in range(3):
                        c0 = c3 * 512
                        w_ = min(512, NF - c0)
                        p = pf.next()
                        mm(p[0:4, 0:w_], rb[0:32, 0:4], ohb[0:32, c0:c0 + w_], True, True, [rb, ohb], [p])
                        cp(dve, fsb[0:4, c0:c0 + w_], p[0:4, 0:w_], [p], [fsb])
                    k.dma(k.q_sp, FS[:, :], fsb[:, :], reads=[fsb], writes=[fsres])
                    for h in range(4):
                        for di, dl in enumerate(DELTAS):
                            tl = tl_r.next()
                            src = bass.AP(tensor=FS_T, offset=h * NF + (384 - dl), ap=[[1, 128], [1, 512]])
                            k.dma(k.q_sp, tl[:, :], src, reads=[fsres], writes=[tl])
                            p = pf.next()
                            mm(p[:, :], exch[:, :], tl[:, :], True, True, [exch, tl], [p])
                            tt(dve, BM[h][di][:, :], p[:, :], nmk[di][:, :], ALU.add, [p, nmk[di]], [BM[h][di]])
                    k.dma(k.q_sp, lrow[:, 0:256], I["even_lambda"][i_even:i_even + 1, :], writes=[lrow])
                    LR = [lrow]
                    tt(dve, lrow[:, 0:64], lrow[:, 0:64], lrow[:, 64:128], ALU.mult, LR, LR)
                    tt(dve, lrow[:, 128:192], lrow[:, 128:192], lrow[:, 192:256], ALU.mult, LR, LR)
                    k.op(dve, lambda: nc.vector.tensor_reduce(out=lrow[:, 256:257], in_=lrow[:, 0:64], axis=AX.X,
                                                              op=ALU.add), LR, LR)
                    k.op(dve, lambda: nc.vector.tensor_reduce(out=lrow[:, 257:258], in_=lrow[:, 128:192], axis=AX.X,
                                                              op=ALU.add), LR, LR)
                    actf(lrow[:, 258:260], lrow[:, 256:258], AF.Exp, LR, LR)
                    tt(dve, lrow[:, 260:261], lrow[:, 259:260], lrow[:, 258:259], ALU.subtract, LR, LR)
                    ts(dve, lrow[:, 261:262], lrow[:, 260:261], -lam_init, None, ALU.add, None, LR, LR)
                    p = pf.next()
                    mm(p[:, 0:1], ones32[0:1, :], lrow[0:1, 261:262], True, True, [ones32, lrow], [p])
                    cp(dve, neglam[:, :], p[:, 0:1], [p], [neglam])
                    k.dma(k.q_sp, grow[:, :], I["even_subln"][i_even:i_even + 1, :], writes=[grow])
                    p = pf.next()
                    mm(p[:, 0:1], grow[0:1, :], ones32[0:1, 0:1], True, True, [grow, ones32], [p])
                    ts(dve, gsub[:, :], p[:, 0:1], 1.0 - lam_init, None, ALU.mult, None, [p], [gsub])
                    k.barrier()
                qT_r = Ring([k.sb(st, "da_qT%d" % i, [128, S], BF16) for i in range(2)])
                kT_r = Ring([k.sb(st, "da_kT%d" % i, [128, S], BF16) for i in range(2)])
                vt_r = Ring([k.sb(st, "da_vt%d" % i, [128, NT, 128], BF16) for i in range(2)])
                E_r = Ring([k.sb(st, "da_E%d" % i, [128, 512], BF16) for i in range(8)])
                f_r = Ring([k.sb(st, "da_f%d" % i, [128, 512], F32) for i in range(6)])
                o32_r = Ring([k.sb(st, "da_o%d" % i, [128, 512], F32) for i in range(2)])
                sq_r = Ring([k.sb(st, "da_sq%d" % i, [128, 512], BF16) for i in range(2)])
                ps_r = Ring([k.ps(st, "da_ps%d" % i, [128, 512], F32) for i in range(3)])
                pu = [k.ps(st, "da_pu%d" % i, [128, 512], F32) for i in range(2)]
                pd = [k.ps(st, "da_pd%d" % i, [128, 512], F32) for i in range(2)]
                pss = k.ps(st, "da_pss", [128, 512], F32)
                us_r = Ring([k.sb(st, "da_us%d" % i, [128, 512], F32) for i in range(4)])
                pend_fin = []
                for h in range(0 if 'ev_noA' in flags else 4):
                    qT = qT_r.next()
                    kT = kT_r.next()
                    vt = vt_r.next()
                    k.dma(k.q_sp, qT[:, :], FB[h, :, :], reads=[fbres[h]], writes=[qT])
                    k.dma(k.q_sp, kT[:, :], FB[4 + h, :, :], reads=[fbres[4 + h]], writes=[kT])
                    k.dma(k.q_sp, vt[:, :, :],
                          TB[:, h * 128:(h + 1) * 128].rearrange("(t p) c -> p t c", p=128),
                          reads=[tbres], writes=[vt])
                    for qg in range(8):
                        nkb = 4 * qg + 4
                        its = [(kb, m) for kb in range(nkb) for m in range(2)]
                        held = {}

                        def st1(idx):
                            kb, m = its[idx]
                            j0 = max(0, kb - 4 * qg)
                            c0 = 128 * j0
                            wq = 512 - c0
                            q0 = qg * 512 + c0
                            dl = kb * 128 - qg * 512
                            b0 = 64 * m
                            ps = ps_r.next()
                            near = dl >= -128
                            mm(ps[:, 0:wq], kT[b0:b0 + 64, kb * 128:(kb + 1) * 128], qT[b0:b0 + 64, q0:q0 + wq],
                               True, not near, [kT, qT], [ps])
                            E = E_r.next()
                            if near:
                                bm = BM[h][DELTAS.index(dl)]
                                mm(ps[:, 0:wq], identb[:, :], bm[:, c0:512], False, True, [identb, bm], [ps])
                                actf(E[:, 0:wq], ps[:, 0:wq], AF.Exp, [ps], [E])
                            else:
                                actf(E[:, 0:wq], ps[:, 0:wq], AF.Exp, [ps, b15], [E], bias=b15[:, h:h + 1])
                            held[idx] = (E, c0, wq)

                        def st2(idx):
                            kb, m = its[idx]
                            E, c0, wq = held.pop(idx)
                            mm(pu[m][:, c0:512], vt[:, kb, :], E[:, 0:wq], kb == 0, kb == nkb - 1, [vt, E], [pu[m]])
                            mm(pd[m][:, c0:512], onesb[:, :], E[:, 0:wq], kb == 0, kb == nkb - 1, [onesb, E],
                               [pd[m]])

                        SK = 4 if 'sk4' in flags else 2
                        for idx in range(len(its) + SK):
                            if idx < len(its):
                                st1(idx)
                            if idx >= SK:
                                st2(idx - SK)
                            if pend_fin and idx >= 2 and idx % 2 == 0:
                                pend_fin.pop(0)()
                        if 'da_nofin' in flags:
                            continue
                        while pend_fin:
                            pend_fin.pop(0)()
                        u0 = us_r.next()
                        cp(dve, u0[:, :], pu[0][:, :], [pu[0]], [u0])
                        u1 = us_r.next()
                        cp(dve, u1[:, :], pu[1][:, :], [pu[1]], [u1])
                        r0 = f_r.next()
                        actf(r0[:, :], pd[0][:, :], AF.Ln, [pd[0]], [r0])
                        r1 = f_r.next()
                        actf(r1[:, :], pd[1][:, :], AF.Ln, [pd[1]], [r1])

                        def mk(h=h, qg=qg, u0=u0, u1=u1, r0=r0, r1=r1):
                            box = {}

                            def s_a():
                                actf(r0[:, :], r0[:, :], AF.Exp, [r0], [r0], scale=-1.0)
                                actf(r1[:, :], r1[:, :], AF.Exp, [r1], [r1], scale=-1.0)

                            def s_b():
                                tt(dve, u0[:, :], u0[:, :], r0[:, :], ALU.mult, [u0, r0], [u0])
                                tt(dve, u1[:, :], u1[:, :], r1[:, :], ALU.mult, [u1, r1], [u1])
                                o32 = o32_r.next()
                                stt(o32[:, :], u1[:, :], neglam[:, 0:1], u0[:, :], ALU.mult, ALU.add,
                                    [u1, u0, neglam], [o32])
                                box["o"] = o32

                            def s_c():
                                sq = sq_r.next()
                                actf(sq[:, :], box["o"][:, :], AF.Square, [box["o"]], [sq])
                                mm(pss[:, :], onesb[:, :], sq[:, :], True, True, [onesb, sq], [pss])

                            def s_d():
                                rs = f_r.next()
                                actf(rs[:, :], pss[:, :], AF.Ln, [pss, epsc], [rs], scale=1.0 / 128.0,
                                     bias=epsc[:, 0:1])
                                box["rs"] = rs

                            def s_e():
                                rs = box["rs"]
                                actf(rs[:, :], rs[:, :], AF.Exp, [rs], [rs], scale=-0.5)
                                stt(H["hT"][:, h, qg * 512:(qg + 1) * 512], box["o"][:, :], gsub[:, 0:1], rs[:, :],
                                    ALU.mult, ALU.mult, [box["o"], gsub, rs], [H["hT"]])
                            return [s_a, s_b, s_c, s_d, s_e]
                        pend_fin.extend(mk())
                while pend_fin:
                    pend_fin.pop(0)()
                k.barrier()
            with ExitStack() as st:
                gtri = k.sb(st, "g_tri", [128, 128], F32)
                gm2 = k.sb(st, "g_m2", [128, 128], F32)
                gmaskb = k.sb(st, "g_mask", [128, 128], BF16)
                wgk = k.sb(st, "g_wgk", [16, 256], BF16)
                bgk = k.sb(st, "g_bgk", [1, 256], BF16)
                glag = k.sb(st, "g_lag", [128, 1], F32)
                grow = k.sb(st, "g_grow", [1, 128], F32)
                k.dma(k.q_sp, gtri[:, :], I["c_gtri"][:, :], writes=[gtri])
                k.dma(k.q_sp, gm2[:, :], I["c_gm2"][:, :], writes=[gm2])
                k.dma(k.q_pool, gmaskb[:, :], I["c_gmask"][:, :], writes=[gmaskb])
                k.dma(k.q_pool, wgk[:, :], I["even_w_gk2"][i_even], writes=[wgk])
                k.dma(k.q_pool, bgk[:, :], I["even_b_gk"][i_even:i_even + 1, :], writes=[bgk])
                k.dma(k.q_sp, grow[:, :], I["even_gla_norm"][i_even:i_even + 1, :], writes=[grow])
                pA = Ring([k.ps(st, "g_pA%d" % i, [128, 512], F32) for i in range(3)])
                pS = Ring([k.ps(st, "g_pS%d" % i, [128, 512], F32) for i in range(2)])
                pO = Ring([k.ps(st, "g_pO%d" % i, [128, 512], F32) for i in range(2)])
                p = pA.next()
                mm(p[:, 0:1], grow[0:1, :], ones32[0:1, 0:1], True, True, [grow, ones32], [p])
                cp(dve, glag[:, :], p[:, 0:1], [p], [glag])
                qg_r = Ring([k.sb(st, "g_q%d" % i, [128, 2, 512], BF16) for i in range(2)])
                kg_r = Ring([k.sb(st, "g_k%d" % i, [128, 2, 512], BF16) for i in range(2)])
                kt_r = Ring([k.sb(st, "g_kt%d" % i, [128, 4, 256], BF16) for i in range(2)])
                vg_r = Ring([k.sb(st, "g_v%d" % i, [128, 4, 512], BF16) for i in range(2)])
                bg_r = Ring([k.sb(st, "g_bg%d" % i, [16, 512], BF16) for i in range(2)])
                br_r = Ring([k.sb(st, "g_br%d" % i, [128, 4, 512], BF16) for i in range(2)])
                og_r = Ring([k.sb(st, "g_og%d" % i, [128, 4, 512], F32) for i in range(2)])
                e32_r = Ring([k.sb(st, "g_e%d" % i, [128, 256], F32) for i in range(2)])
                sp_r = Ring([k.sb(st, "g_sp%d" % i, [128, 256], F32) for i in range(2)])
                kf_r = Ring([k.sb(st, "g_kf%d" % i, [128, 256], F32) for i in range(2)])
                kd_r = Ring([k.sb(st, "g_kd%d" % i, [128, 256], BF16) for i in range(2)])
                eb_r = Ring([k.sb(st, "g_eb%d" % i, [128, 256], F32) for i in range(2)])
                en_r = Ring([k.sb(st, "g_en%d" % i, [128, 256], F32) for i in range(2)])
                qd_r = Ring([k.sb(st, "g_qd%d" % i, [128, 2, 128], BF16) for i in range(2)])
                ki_r = Ring([k.sb(st, "g_ki%d" % i, [128, 2, 128], BF16) for i in range(2)])
                at_r = Ring([k.sb(st, "g_at%d" % i, [128, 128], BF16) for i in range(3)])
                S32_r = [Ring([k.sb(st, "g_S32_%d_%d" % (h, i), [128, 128], F32) for i in range(2)]) for h in range(4)]
                Sb_r = [Ring([k.sb(st, "g_Sb_%d_%d" % (h, i), [128, 128], BF16) for i in range(3)]) for h in range(4)]
                sq_r = Ring([k.sb(st, "g_sq%d" % i, [128, 512], BF16) for i in range(2)])
                f_r = Ring([k.sb(st, "g_f%d" % i, [128, 512], F32) for i in range(4)])
                S32 = []
                Sb = []
                for h in range(4):
                    s = S32_r[h].next()
                    memset(dve, s[:, :], 0.0, [s])
                    S32.append(s)
                    b = Sb_r[h].next()
                    memset(pool, b[:, :], 0.0, [b])
                    Sb.append(b)
                for g8 in range(0 if 'ev_noB' in flags else 8):
                    t0g = g8 * 512
                    qgt = qg_r.next()
                    kgt = kg_r.next()
                    ktt = kt_r.next()
                    vgt = vg_r.next()
                    bgt = bg_r.next()
                    brt = br_r.next()
                    og = og_r.next()
                    for hh in range(2):
                        k.dma(k.q_sp, qgt[:, hh, :], FB[12 + hh, :, t0g:t0g + 512], reads=[fbres[12 + hh]], writes=[qgt])
                        k.dma(k.q_sp, kgt[:, hh, :], FB[14 + hh, :, t0g:t0g + 512], reads=[fbres[14 + hh]], writes=[kgt])
                    k.dma(k.q_sp, ktt[:, :, :],
                          TB[t0g:t0g + 512, 512:768].rearrange("(j p) c -> p j c", p=128), reads=[tbres], writes=[ktt])
                    k.dma(k.q_sp, vgt[:, :, :],
                          TB[t0g:t0g + 512, 768:1280].rearrange("(j p) c -> p j c", p=128), reads=[tbres], writes=[vgt])
                    k.dma(k.q_sp, bgt[:, :], FB[24, 0:16, t0g:t0g + 512], reads=[fbres[24]], writes=[bgt])
                    for h in range(4):
                        k.dma(k.q_sp, brt[:, h, :], FB[20 + h, :, t0g:t0g + 512], reads=[fbres[20 + h]], writes=[brt])
                    for j4 in range(4):
                        tc0 = j4 * 128
                        ppre = pA.next()
                        mm(ppre[:, 0:256], bgt[0:16, tc0:tc0 + 128], wgk[0:16, :], True, False, [bgt, wgk], [ppre])
                        mm(ppre[:, 0:256], onesb[0:1, :], bgk[0:1, :], False, True, [onesb, bgk], [ppre])
                        e32 = e32_r.next()
                        actf(e32[:, :], ppre[:, 0:256], AF.Exp, [ppre], [e32], scale=-1.0)
                        sp32 = sp_r.next()
                        actf(sp32[:, :], e32[:, :], AF.Ln, [e32], [sp32], bias=1.0)
                        pb = pA.next()
                        for hh in range(2):
                            mm(pb[:, hh * 128:(hh + 1) * 128], sp32[:, hh * 128:(hh + 1) * 128], gtri[:, :], True, True,
                               [sp32, gtri], [pb])
                        pk = pA.next()
                        mm(pk[:, 0:256], gm2[:, :], sp32[:, :], True, True, [gm2, sp32], [pk])
                        kf = kf_r.next()
                        actf(kf[:, :], pk[:, 0:256], AF.Exp, [pk], [kf])
                        kd = kd_r.next()
                        tt(dve, kd[:, :], ktt[:, j4, :], kf[:, :], ALU.mult, [ktt, kf], [kd])
                        eb = eb_r.next()
                        actf(eb[:, :], pb[:, 0:256], AF.Exp, [pb], [eb])
                        en = en_r.next()
                        actf(en[:, :], pb[:, 0:256], AF.Exp, [pb], [en], scale=-1.0)
                        qd = qd_r.next()
                        ki = ki_r.next()
                        for hh in range(2):
                            tt(dve, qd[:, hh, :], qgt[:, hh, tc0:tc0 + 128], eb[:, hh * 128:(hh + 1) * 128], ALU.mult,
                               [qgt, eb], [qd])
                            tt(dve, ki[:, hh, :], kgt[:, hh, tc0:tc0 + 128], en[:, hh * 128:(hh + 1) * 128], ALU.mult,
                               [kgt, en], [ki])
                        for h in range(4):
                            hh, jj = h // 2, h % 2
                            b0 = 64 * jj
                            psc = pS.next()
                            mm(psc[:, 0:128], ki[b0:b0 + 64, hh, :], qd[b0:b0 + 64, hh, :], True, True, [ki, qd], [psc])
                            at = at_r.next()
                            tt(dve, at[:, :], psc[:, 0:128], gmaskb[:, :], ALU.mult, [psc, gmaskb], [at])
                            po = pO.next()
                            mm(po[:, 0:128], vgt[:, j4, h * 128:(h + 1) * 128], at[:, :], True, False, [vgt, at], [po])
                            mm(po[:, 0:64], Sb[h][b0:b0 + 64, :], qd[b0:b0 + 64, hh, 0:64], False, False,
                               [Sb[h], qd], [po])
                            pst = pS.next()
                            mm(pst[:, 0:128], kd[0:64, hh * 128:(hh + 1) * 128], vgt[0:64, j4, h * 128:(h + 1) * 128],
                               True, True, [kd, vgt], [pst])
                            s1 = S32_r[h].next()
                            stt(s1[b0:b0 + 64, :], S32[h][b0:b0 + 64, :], eb[b0:b0 + 64, hh * 128 + 63:hh * 128 + 64],
                                pst[b0:b0 + 64, 0:128], ALU.mult, ALU.add, [S32[h], eb, pst], [s1])
                            sb1 = Sb_r[h].next()
                            actf(sb1[b0:b0 + 64, :], s1[b0:b0 + 64, :], AF.Copy, [s1], [sb1])
                            mm(po[:, 64:128], sb1[b0:b0 + 64, :], qd[b0:b0 + 64, hh, 64:128], False, True,
                               [sb1, qd], [po])
                            pst2 = pS.next()
                            mm(pst2[:, 0:128], kd[64:128, hh * 128:(hh + 1) * 128],
                               vgt[64:128, j4, h * 128:(h + 1) * 128], True, True, [kd, vgt], [pst2])
                            s2 = S32_r[h].next()
                            stt(s2[b0:b0 + 64, :], s1[b0:b0 + 64, :], eb[b0:b0 + 64, hh * 128 + 127:hh * 128 + 128],
                                pst2[b0:b0 + 64, 0:128], ALU.mult, ALU.add, [s1, eb, pst2], [s2])
                            sb2 = Sb_r[h].next()
                            actf(sb2[b0:b0 + 64, :], s2[b0:b0 + 64, :], AF.Copy, [s2], [sb2])
                            S32[h] = s2
                            Sb[h] = sb2
                            actf(og[:, h, tc0:tc0 + 128], po[:, 0:128], AF.Copy, [po], [og])
                    for h in range(4):
                        sq = sq_r.next()
                        actf(sq[:, :], og[:, h, :], AF.Square, [og], [sq])
                        pn = pA.next()
                        mm(pn[:, :], onesb[:, :], sq[:, :], True, True, [onesb, sq], [pn])
                        rs = f_r.next()
                        actf(rs[:, :], pn[:, :], AF.Ln, [pn, epsc], [rs], scale=1.0 / 128.0, bias=epsc[:, 0:1])
                        rs2 = f_r.next()
                        actf(rs2[:, :], rs[:, :], AF.Exp, [rs], [rs2], scale=-0.5)
                        sl = f_r.next()
                        actf(sl[:, :], brt[:, h, :], AF.Silu, [brt], [sl])
                        t1 = f_r.next()
                        stt(t1[:, :], og[:, h, :], glag[:, 0:1], rs2[:, :], ALU.mult, ALU.mult, [og, glag, rs2], [t1])
                        tt(dve, H["hT"][:, 4 + h, t0g:t0g + 512], t1[:, :], sl[:, :], ALU.mult, [t1, sl], [H["hT"]])
                k.barrier()

        def emit_moe(layer):
            with ExitStack() as st:
                wg_r = Ring([k.sb(st, "me_wg%d" % i, [128, 8, DEXP], BF16) for i in range(2)])
                wu_r = Ring([k.sb(st, "me_wu%d" % i, [128, 8, DEXP], BF16) for i in range(2)])
                wd_r = Ring([k.sb(st, "me_wd%d" % i, [128, 4, D], BF16) for i in range(2)])
                yacc = k.sb(st, "me_yacc", [128, 8, D], F32)
                aT_r = Ring([k.sb(st, "me_aT%d" % i, [128, 4, 512], BF16) for i in range(2)])
                sg_r = Ring([k.sb(st, "me_sg%d" % i, [128, 512], BF16) for i in range(3)])
                xt_r = Ring([k.sb(st, "me_xt%d" % i, [128, D], F32) for i in range(2)])
                pg_r = Ring([k.ps(st, "me_pg%d" % i, [128, 512], F32) for i in range(2)])
                pu_r = Ring([k.ps(st, "me_pu%d" % i, [128, 512], F32) for i in range(2)])
                py_r = Ring([k.ps(st, "me_py%d" % i, [128, D], F32) for i in range(2)])
                for qtr in range(4):
                    memset(dve, yacc[:, :, :], 0.0, [yacc])
                    jobs = [(e, tg) for e in range(NEXP) for tg in range(2)]
                    wts = {}

                    def load_w(e):
                        wg = wg_r.next()
                        wu = wu_r.next()
                        wd = wd_r.next()
                        k.dma(k.q_pool, wg[:, :, :],
                              I["expert_w_gate"][layer, e].rearrange("(kc p) f -> p kc f", p=128), writes=[wg])
                        k.dma(k.q_pool, wu[:, :, :],
                              I["expert_w_up"][layer, e].rearrange("(kc p) f -> p kc f", p=128), writes=[wu])
                        k.dma(k.q_pool, wd[:, :, :],
                              I["expert_w_down"][layer, e].rearrange("(fc p) n -> p fc n", p=128), writes=[wd])
                        wts[e] = (wg, wu, wd)

                    acts = {}

                    def emit_gu(e, tg):
                        wg, wu, wd = wts[e]
                        G = qtr * 2 + tg
                        aT = aT_r.next()
                        for fc in range(4):
                            pg = pg_r.next()
                            pu = pu_r.next()
                            for kc in range(8):
                                mm(pg[:, :], wg[:, kc, fc * 128:(fc + 1) * 128], H["hT"][:, kc, G * 512:(G + 1) * 512],
                                   kc == 0, kc == 7, [wg, H["hT"]], [pg])
                            for kc in range(8):
                                mm(pu[:, :], wu[:, kc, fc * 128:(fc + 1) * 128], H["hT"][:, kc, G * 512:(G + 1) * 512],
                                   kc == 0, kc == 7, [wu, H["hT"]], [pu])
                            sg = sg_r.next()
                            actf(sg[:, :], pg[:, :], AF.Silu, [pg], [sg])
                            tt(dve, aT[:, fc, :], sg[:, :], pu[:, :], ALU.mult, [sg, pu], [aT])
                        acts[(e, tg)] = aT

                    def emit_down(e, tg):
                        wg, wu, wd = wts[e]
                        aT = acts.pop((e, tg))
                        for t4 in range(4):
                            ti = tg * 4 + t4
                            T = qtr * 8 + ti
                            py = py_r.next()
                            for nh in range(2):
                                for fc in range(4):
                                    mm(py[:, nh * 512:(nh + 1) * 512], aT[:, fc, t4 * 128:(t4 + 1) * 128],
                                       wd[:, fc, nh * 512:(nh + 1) * 512], fc == 0, fc == 3, [aT, wd], [py])
                            stt(yacc[:, ti, :], py[:, :], cmb[:, T, e:e + 1], yacc[:, ti, :], ALU.mult, ALU.add,
                                [py, cmb, yacc], [yacc])

                    load_w(0)
                    for idx, (e, tg) in enumerate(jobs):
                        emit_gu(e, tg)
                        if idx > 0:
                            emit_down(*jobs[idx - 1])
                        if tg == 0 and e + 1 < NEXP:
                            load_w(e + 1)
                    emit_down(*jobs[-1])
                    for ti in range(8):
                        T = qtr * 8 + ti
                        xt = xt_r.next()
                        k.dma(k.q_sp, xt[:, :], XS[T * 128:(T + 1) * 128, :], reads=[xres[T]], writes=[xt])
                        tt(dve, yacc[:, ti, :], yacc[:, ti, :], gbc2[:, :], ALU.mult, [yacc, gbc2], [yacc])
                        tt(dve, xt[:, :], xt[:, :], yacc[:, ti, :], ALU.add, [xt, yacc], [xt])
                        k.dma(k.q_sp, XS[T * 128:(T + 1) * 128, :], xt[:, :], reads=[xt], writes=[xres[T]])
                k.barrier()

        def emit_slots(layer):
            with ExitStack() as st:
                Aall = k.sb(st, "sl_A", [128, NT, 32], BF16)
                ag = k.sb(st, "sl_ag", [128, NT, 8], F32)
                mltb = k.sb(st, "sl_mlt", [128, 128], BF16)
                pos = k.sb(st, "sl_pos", [128, NT, 32], F32)
                cnt = k.sb(st, "sl_cnt", [128, 32], F32)
                nb_ = k.sb(st, "sl_nb", [128, 32], F32)
                pa = k.sb(st, "sl_pa", [128, 32], F32)
                pb = k.sb(st, "sl_pb", [128, 32], F32)
                smg = k.sb(st, "sl_smg", [128, NT, 8], F32)
                t8 = k.sb(st, "sl_t8", [128, NT, 8], F32)
                sf = k.sb(st, "sl_sf", [128, 2, NT], F32)
                iog = k.sb(st, "sl_iog", [128, 8], F32)
                iob = k.sb(st, "sl_iob", [128, NBLK], F32)
                cmpt = k.sb(st, "sl_cmp", [128, NBLK, 32], F32)
                eb = k.sb(st, "sl_eb", [128, NBLK], F32)
                fg = k.sb(st, "sl_fg", [128, NBLK, 8], F32)
                ppos = k.ps(st, "sl_ppos", [128, NT * 32], F32)
                pcnt = k.ps(st, "sl_pcnt", [128, 512], F32)
                k.dma(k.q_pool, mltb[:, :], I["c_mlt"][:, :], writes=[mltb])
                k.dma(k.q_sp, iog[:, :], I["c_iog"][:, :], writes=[iog])
                k.dma(k.q_sp, iob[:, :], I["c_iob"][:, :], writes=[iob])
                RR = [Aall, ag, pos, cnt, nb_, pa, pb, smg, t8, sf, cmpt, eb, fg, r_ohg, r_oh1, r_oh2, r_v]

                def bc(ap2, n):
                    return ap2.unsqueeze(2).to_broadcast([128, NT, n])
                tt(dve, ag[:, :, :], r_oh1[:, :, :], r_oh2[:, :, :], ALU.add, RR, RR)
                for g in range(4):
                    tt(dve, Aall[:, :, 8 * g:8 * g + 8], ag[:, :, :], bc(r_ohg[:, :, g], 8), ALU.mult, RR, RR)
                for T in range(NT):
                    for T2 in range(T):
                        mm(ppos[:, T * 32:(T + 1) * 32], onesb[:, :], Aall[:, T2, :], T2 == 0, False,
                           [onesb, Aall], [ppos])
                    mm(ppos[:, T * 32:(T + 1) * 32], mltb[:, :], Aall[:, T, :], T == 0, True, [mltb, Aall], [ppos])
                for T in range(NT):
                    mm(pcnt[:, 0:32], onesb[:, :], Aall[:, T, :], T == 0, T == NT - 1, [onesb, Aall], [pcnt])
                cp(dve, pos[:, 0:16, :], ppos[:, 0:512].rearrange("p (t e) -> p t e", e=32), [ppos], RR)
                cp(dve, pos[:, 16:32, :], ppos[:, 512:1024].rearrange("p (t e) -> p t e", e=32), [ppos], RR)
                cp(dve, cnt[:, :], pcnt[:, 0:32], [pcnt], RR)
                ts(dve, nb_[:, :], cnt[:, :], 0.0, None, ALU.is_gt, None, RR, RR)
                for m in range(1, 8):
                    stt(nb_[:, :], cnt[:, :], float(BSZ * m), nb_[:, :], ALU.is_gt, ALU.add, RR, RR)
                cp(dve, pa[:, :], nb_[:, :], RR, RR)
                a_, b_ = pa, pb
                for sh in (1, 2, 4, 8, 16):
                    cp(dve, b_[:, 0:sh], a_[:, 0:sh], RR, RR)
                    tt(dve, b_[:, sh:32], a_[:, sh:32], a_[:, 0:32 - sh], ALU.add, RR, RR)
                    a_, b_ = b_, a_
                incl = a_
                excl = b_
                tt(dve, excl[:, :], incl[:, :], nb_[:, :], ALU.subtract, RR, RR)
                ts(dve, excl[:, :], excl[:, :], float(BSZ), None, ALU.mult, None, RR, RR)
                tt(dve, pos[:, :, :], pos[:, :, :], excl[:, :].unsqueeze(1).to_broadcast([128, NT, 32]), ALU.add,
                   RR, RR)
                for g in range(4):
                    if g == 0:
                        tt(dve, smg[:, :, :], pos[:, :, 0:8], bc(r_ohg[:, :, 0], 8), ALU.mult, RR, RR)
                    else:
                        tt(dve, t8[:, :, :], pos[:, :, 8 * g:8 * g + 8], bc(r_ohg[:, :, g], 8), ALU.mult, RR, RR)
                        tt(dve, smg[:, :, :], smg[:, :, :], t8[:, :, :], ALU.add, RR, RR)
                tt(dve, t8[:, :, :], smg[:, :, :], r_oh1[:, :, :], ALU.mult, RR, RR)
                k.op(dve, lambda: nc.vector.tensor_reduce(out=sf[:, 0, :], in_=t8[:, :, :], axis=AX.X, op=ALU.add),
                     RR, RR)
                tt(dve, t8[:, :, :], smg[:, :, :], r_oh2[:, :, :], ALU.mult, RR, RR)
                k.op(dve, lambda: nc.vector.tensor_reduce(out=sf[:, 1, :], in_=t8[:, :, :], axis=AX.X, op=ALU.add),
                     RR, RR)
                cp(dve, s12u[:, :, :], sf[:, :, :], RR, [s12u])
                tt(dve, cmpt[:, :, :], incl[:, :].unsqueeze(1).to_broadcast([128, NBLK, 32]),
                   iob[:, :].unsqueeze(2).to_broadcast([128, NBLK, 32]), ALU.is_le, RR + [iob], RR)
                k.op(dve, lambda: nc.vector.tensor_reduce(out=eb[:, :], in_=cmpt[:, :, :], axis=AX.X, op=ALU.add),
                     RR, RR)
                ts(dve, eb[:, :], eb[:, :], 31.0, None, ALU.min, None, RR, RR)
                ts(dve, eb[:, :], eb[:, :], 512.0, float(layer * NEXP * DEXP), ALU.mult, ALU.add, RR, RR)
                tt(dve, fg[:, :, 0:4], eb[:, :].unsqueeze(2).to_broadcast([128, NBLK, 4]),
                   iog[:, 0:4].unsqueeze(1).to_broadcast([128, NBLK, 4]), ALU.add, RR + [iog], RR)
                cp(dve, idxd[:, :, :], fg[:, :, 0:4], RR, [idxd])
                ts(dve, eb[:, :], eb[:, :], 2.0, None, ALU.mult, None, RR, RR)
                tt(dve, fg[:, :, :], eb[:, :].unsqueeze(2).to_broadcast([128, NBLK, 8]),
                   iog[:, :].unsqueeze(1).to_broadcast([128, NBLK, 8]), ALU.add, RR + [iog], RR)
                cp(dve, idxg[:, :, :], fg[:, :, :], RR, [idxg])
                io2 = k.sb(st, "sl_io2", [128, 2], F32)
                k.dma(k.q_sp, io2[:, :], I["c_io2"][:, :], writes=[io2])
                ts(dve, eb[:, :], eb[:, :], 0.25, None, ALU.mult, None, RR, RR)
                tt(dve, fg[:, :, 0:2], eb[:, :].unsqueeze(2).to_broadcast([128, NBLK, 2]),
                   io2[:, :].unsqueeze(1).to_broadcast([128, NBLK, 2]), ALU.add, RR + [io2], RR)
                cp(dve, idx2[:, :, :], fg[:, :, 0:2], RR, [idx2])
                k.dma(k.q_sp, MV[0].rearrange("(kc p) -> p kc", p=128), gmod2[:, :], reads=[gmod2], writes=[mvres],
                      allow_slow_non_contiguous=True)
                k.dma(k.q_sp, MV[1].rearrange("(kc p) -> p kc", p=128), modT[:, 24:32], reads=[modT], writes=[mvres],
                      allow_slow_non_contiguous=True)
                k.dma(k.q_sp, gsb[:, 0, :], MV[0].rearrange("(p kc) -> p kc", kc=8), reads=[mvres], writes=[gsb])
                k.dma(k.q_sp, gsb[:, 1, :], MV[1].rearrange("(p kc) -> p kc", kc=8), reads=[mvres], writes=[gsb])
                xr_r = Ring([k.sb(st, "sl_xr%d" % i, [128, D], F32) for i in range(3)])
                for T in range(NT):
                    xr = xr_r.next()
                    k.dma(k.q_sp, xr[:, :], XN[T * 128:(T + 1) * 128, :], reads=[xnres], writes=[xr])
                    for kk in range(2):
                        idma(XG[:, :], bass.IndirectOffsetOnAxis(ap=s12u[:, kk, T:T + 1], axis=0), xr[:, :], None,
                             [xr, s12u], [xgres])
                k.barrier()

        def emit_moe_sparse(layer):
            with ExitStack() as st:
                wg_r = Ring([k.sb(st, "ms_wg%d" % i, [128, 8, DEXP], BF16) for i in range(3)])
                wu_r = Ring([k.sb(st, "ms_wu%d" % i, [128, 8, DEXP], BF16) for i in range(3)])
                wd_r = Ring([k.sb(st, "ms_wd%d" % i, [128, 4, D], BF16) for i in range(3)])
                xg_r = Ring([k.sb(st, "ms_xg%d" % i, [128, D], F32) for i in range(8)])
                hs_r = Ring([k.sb(st, "ms_hs%d" % i, [128, 8, 512], BF16) for i in range(2)])
                aT_r = Ring([k.sb(st, "ms_aT%d" % i, [128, 4, 512], BF16) for i in range(2)])
                sg_r = Ring([k.sb(st, "ms_sg%d" % i, [128, 512], BF16) for i in range(3)])
                yo_r = Ring([k.sb(st, "ms_yo%d" % i, [128, D], F32) for i in range(3)])
                ptr_r = Ring([k.ps(st, "ms_ptr%d" % i, [128, 512], F32) for i in range(2)])
                pg_r = Ring([k.ps(st, "ms_pg%d" % i, [128, 512], F32) for i in range(2)])
                pu_r = Ring([k.ps(st, "ms_pu%d" % i, [128, 512], F32) for i in range(2)])
                py_r = Ring([k.ps(st, "ms_py%d" % i, [128, 512], F32) for i in range(2)])
                wgv = I["expert_w_gate"].rearrange("l e (p q) f -> (l e p) (q f)", q=8).rearrange(
                    "r (h c) -> (r h) c", h=2)
                wuv = I["expert_w_up"].rearrange("l e (p q) f -> (l e p) (q f)", q=8).rearrange(
                    "r (h c) -> (r h) c", h=2)
                wdv = I["expert_w_down"].rearrange("l e (p q) n -> (l e p) (q n)", q=4).rearrange(
                    "r (h c) -> (r h) c", h=2)
                wts = {}
                hss = {}
                hss_keep = {}
                acts = {}

                def load_blk(b):
                    if 'ms_nowl' in flags and b >= 3:
                        wts[b] = wts[b - 3]
                        return
                    wg = wg_r.next()
                    wu = wu_r.next()
                    wd = wd_r.next()
                    for h in range(2):
                        off = bass.IndirectOffsetOnAxis(ap=idx2[:, b, h:h + 1], axis=0)
                        idma(wg[:, 4 * h:4 * h + 4, :].rearrange("p a b -> p (a b)"), None, wgv, off, [idx2], [wg])
                        idma(wu[:, 4 * h:4 * h + 4, :].rearrange("p a b -> p (a b)"), None, wuv, off, [idx2], [wu])
                        idma(wd[:, 2 * h:2 * h + 2, :].rearrange("p a b -> p (a b)"), None, wdv, off, [idx2], [wd])
                    wts[b] = (wg, wu, wd)

                xgs = {}

                def load_x(b):
                    tl = []
                    for j in range(4):
                        xg = xg_r.next()
                        r0 = b * BSZ + j * 128
                        k.dma(k.q_sp, xg[:, :], XG[r0:r0 + 128, :], reads=[xgres], writes=[xg])
                        tl.append(xg)
                    xgs[b] = tl

                def prep_h(b):
                    hs = hs_r.next()
                    tl = xgs.pop(b)
                    for j in range(4):
                        xg = tl[j]
                        for hf in range(2):
                            ptr = ptr_r.next()
                            for q in range(4):
                                kc = hf * 4 + q
                                tr(ptr[:, q * 128:(q + 1) * 128], xg[:, kc::8], ident32[:, :], [xg, ident32], [ptr])
                            for q in range(4):
                                kc = hf * 4 + q
                                actf(hs[:, kc, j * 128:(j + 1) * 128], ptr[:, q * 128:(q + 1) * 128], AF.Identity,
                                     [ptr, gsb], [hs], scale=gsb[:, 0, kc:kc + 1], bias=gsb[:, 1, kc:kc + 1])
                    hss[b] = hs

                def emit_gu(b):
                    wg, wu, wd = wts[b]
                    hs = hss.pop(b)
                    aT = aT_r.next()
                    for fc in range(4):
                        pg = pg_r.next()
                        pu = pu_r.next()
                        for kc in range(8):
                            mm(pg[:, :], wg[:, kc, fc::4], hs[:, kc, :], kc == 0, kc == 7,
                               [wg, hs], [pg])
                        for kc in range(8):
                            mm(pu[:, :], wu[:, kc, fc::4], hs[:, kc, :], kc == 0, kc == 7,
                               [wu, hs], [pu])
                        sg = sg_r.next()
                        actf(sg[:, :], pg[:, :], AF.Silu, [pg], [sg])
                        tt(dve, aT[:, fc, :], sg[:, :], pu[:, :], ALU.mult, [sg, pu], [aT])
                    acts[b] = aT

                def emit_down(b):
                    wg, wu, wd = wts[b]
                    aT = acts.pop(b)
                    for j in range(4):
                        yo = yo_r.next()
                        for nh in range(2):
                            py = py_r.next()
                            for fc in range(4):
                                mm(py[:, :], aT[:, fc, j * 128:(j + 1) * 128],
                                   wd[:, fc, nh * 512:(nh + 1) * 512], fc == 0, fc == 3, [aT, wd], [py])
                            cp(dve, yo[:, nh * 512:(nh + 1) * 512], py[:, :], [py], [yo])
                        r0 = b * BSZ + j * 128
                        k.dma(k.q_sp, YS[r0:r0 + 128, :], yo[:, :], reads=[yo], writes=[ysres[b]])

                load_blk(0)
                load_blk(1)
                load_x(0)
                prep_h(0)
                load_x(1)
                for b in range(NBLK):
                    emit_gu(b)
                    if b > 0:
                        emit_down(b - 1)
                    if b + 2 < NBLK:
                        load_blk(b + 2)
                    if b + 1 < NBLK:
                        prep_h(b + 1)
                    if b + 2 < NBLK:
                        load_x(b + 2)
                emit_down(NBLK - 1)
                k.barrier()
            with ExitStack() as st:
                r1_r = Ring([k.sb(st, "ms_r1%d" % i, [128, D], F32) for i in range(2)])
                r2_r = Ring([k.sb(st, "ms_r2%d" % i, [128, D], F32) for i in range(2)])
                xt_r = Ring([k.sb(st, "ms_xt%d" % i, [128, D], F32) for i in range(2)])
                for T in range(NT):
                    r1 = r1_r.next()
                    r2 = r2_r.next()
                    idma(r1[:, :], None, YS[:, :], bass.IndirectOffsetOnAxis(ap=s12u[:, 0, T:T + 1], axis=0),
                         ysres + [s12u], [r1])
                    idma(r2[:, :], None, YS[:, :], bass.IndirectOffsetOnAxis(ap=s12u[:, 1, T:T + 1], axis=0),
                         ysres + [s12u], [r2])
                    xt = xt_r.next()
                    k.dma(k.q_sp, xt[:, :], XS[T * 128:(T + 1) * 128, :], reads=[xres[T]], writes=[xt])
                    ts(dve, r1[:, :], r1[:, :], r_v[:, 9, T:T + 1], None, ALU.mult, None, [r1, r_v], [r1])
                    stt(r1[:, :], r2[:, :], r_v[:, 10, T:T + 1], r1[:, :], ALU.mult, ALU.add, [r2, r_v, r1], [r1])
                    tt(dve, r1[:, :], r1[:, :], gbc2[:, :], ALU.mult, [r1, gbc2], [r1])
                    tt(dve, xt[:, :], xt[:, :], r1[:, :], ALU.add, [xt, r1], [xt])
                    k.dma(k.q_sp, XS[T * 128:(T + 1) * 128, :], xt[:, :], reads=[xt], writes=[xres[T]])
                k.barrier()

        def emit_final(do_norm):
            with ExitStack() as st:
                xt_r = Ring([k.sb(st, "f_xt%d" % i, [128, D], F32) for i in range(3)])
                xo_r = Ring([k.sb(st, "f_xo%d" % i, [128, D], F32) for i in range(2)])
                junk = k.sb(st, "f_junk", [128, D], BF16)
                sm_r = Ring([k.sb(st, "f_sm%d" % i, [128, 4], F32) for i in range(4)])
                gf = k.sb(st, "f_gf", [128, D], F32)
                k.dma(k.q_sp, gf[:, :], I["norm_final"][0].partition_broadcast(128), writes=[gf])
                evs = []
                for t in range(NT):
                    xt = xt_r.next()
                    k.dma(k.q_sp, xt[:, :], XS[t * 128:(t + 1) * 128, :], reads=[xres[t]], writes=[xt])
                    if do_norm:
                        sm = sm_r.next()
                        actf(junk[:, :], xt[:, :], AF.Square, [xt], [junk, sm], accum_out=sm[:, 0:1])
                        ts(dve, sm[:, 1:2], sm[:, 0:1], 1.0 / D, EPS, ALU.mult, ALU.add, [sm], [sm])
                        actf(sm[:, 2:3], sm[:, 1:2], AF.Sqrt, [sm], [sm])
                        recip(sm[:, 3:4], sm[:, 2:3], [sm], [sm])
                        xo = xo_r.next()
                        stt(xo[:, :], xt[:, :], sm[:, 3:4], gf[:, :], ALU.mult, ALU.mult, [xt, sm, gf], [xo])
                        src = xo
                    else:
                        src = xt
                    evs.append(k.dma(k.q_sp, OUT[t * 128:(t + 1) * 128, :], src[:, :], reads=[src]))
                for ev in evs:
                    sp.wait(ev)

        xsrc = I["x"]
        done = False
        if stop == ("evenonly",):
            H["hT"] = k.sb(gst, "hT", [128, 8, S], BF16)
            emit_even_mixer(0, 0)
            emit_final(False)
            done = True
            nlayers = 0
        if stop == ("oddonly",):
            H["hT"] = k.sb(gst, "hT", [128, 8, S], BF16)
            emit_odd_mixer()
            emit_final(False)
            done = True
            nlayers = 0
        for l in range(nlayers):
            emit_mod(l)
            lst = ExitStack()
            H["hT"] = k.sb(lst, "hT", [128, 8, S], BF16)
            emit_norm(xsrc, gmod1, modT[:, 0:8], l, router=False)
            ie = l // 2
            if 'skipmix' in flags:
                pass
            elif l % 2 == 0:
                fj = [(c * 128, 128, c, 0.125) for c in range(4)]
                fj += [(512 + c * 128, 128, 4 + c, 1.0) for c in range(4)]
                fj += [(1536 + c * 128, 128, 12 + c, 0.125) for c in range(2)]
                fj += [(1792 + c * 128, 128, 14 + c, 1.0) for c in range(2)]
                fj += [(2560 + c * 128, 128, 20 + c, 1.0) for c in range(4)]
                fj += [(3072, 16, 24, 1.0)]
                tj = [(1024, 512, 0), (1792, 256, 512), (2048, 512, 768)]
                emit_inproj(I["even_w_in"][ie], EVEN_COLS, fj, tj)
                emit_even_mixer(ie, l)
                emit_outproj(I["even_w_out"][ie], gbc1, xsrc)
            else:
                fj = [(c * 128, 128, c, 0.125) for c in range(8)]
                fj += [(1024 + c * 128, 128, 8 + c, 1.0) for c in range(8)]
                tj = [(2048, 512, 0), (2560, 512, 512)]
                emit_inproj(I["odd_w_in"][ie], 3072, fj, tj)
                emit_odd_mixer()
                emit_outproj(I["odd_w_out"][ie], gbc1, xsrc)
            if 'skipmix' not in flags:
                xsrc = XS
            if SPARSE:
                lst.close()
                H.pop("hT")
            if stop == ("mix", l):
                emit_final(False)
                done = True
                break
            emit_norm(xsrc, gmod2, modT[:, 24:32], l, router=True)
            if 'nomoe' not in flags:
                if SPARSE:
                    emit_slots(layer=l)
                    emit_moe_sparse(l)
                else:
                    emit_moe(l)
            if not SPARSE:
                lst.close()
            if stop == ("ffn", l):
                emit_final(False)
                done = True
                break
        if not done:
            emit_final(True)
        build.stats = (k.n_inst, k.nsem)
    return nc


_CACHE = {}


def _get_nc(nlayers=DEPTH, stop=None):
    key = (nlayers, stop)
    if key not in _CACHE:
        _CACHE[key] = build(nlayers, stop)
    return _CACHE[key]


def make_in_maps(inputs, cores):
    consts = make_consts()
    shared = {}
    for nm in WEIGHT_NAMES:
        a = np.ascontiguousarray(np.asarray(inputs[nm], dtype=np.float32))
        shared[nm] = a.reshape(WEIGHT_SHAPES[nm])
    shared.update(consts)
    x = np.asarray(inputs["x"], dtype=np.float32)
    c = np.asarray(inputs["c"], dtype=np.float32)
    maps = []
    for b in cores:
        m = dict(shared)
        m["x"] = np.ascontiguousarray(x[b])
        m["c"] = np.ascontiguousarray(c[b:b + 1])
        maps.append(m)
    return maps


def kernel(**inputs):
    nc = _get_nc()
    in_maps = make_in_maps(inputs, list(range(8)))
    res = run_bass_kernel_spmd(nc, in_maps, core_ids=list(range(8)))
    return np.stack([np.asarray(r["out"]) for r in res.results], axis=0).astype(np.float32)
```

```python
import math
from contextlib import ExitStack
import numpy as np
import concourse.bass as bass
import concourse.mybir as mybir
from concourse.bass_utils import run_bass_kernel_spmd

F32 = mybir.dt.float32
BF16 = mybir.dt.bfloat16
AF = mybir.ActivationFunctionType
ALU = mybir.AluOpType
AX = mybir.AxisListType

S = 4096
D = 1024
NT = 32
DEPTH = 4
EPS = 1e-6
EVEN_COLS = 3088
NEXP = 32
DEXP = 512
EPOCH = 12000


class Res:
    __slots__ = ("w", "r", "name")

    def __init__(self, name=""):
        self.w = None
        self.r = []
        self.name = name


class Tile:
    def __init__(self, ap, name=""):
        self.t = ap
        self.res = Res(name)

    def __getitem__(self, idx):
        return self.t[idx]


class Eng:
    def __init__(self, k, name, h, is_pe=False):
        self.k = k
        self.name = name
        self.h = h
        self.is_pe = is_pe
        self.sem = k.newsem(name + "_s0")
        self.count = 0
        self.nep = 0
        self.seen = {}
        self.hist = [(self.sem, 0)]

    def tick(self, inst):
        if self.count >= EPOCH:
            self.nep += 1
            self.sem = self.k.newsem("%s_s%d" % (self.name, self.nep))
            self.count = 0
        self.count += 1
        inst.then_inc(self.sem, 1)
        return (self.sem, self.count, self.name)

    def cur(self):
        return (self.sem, self.count, self.name)

    def wait(self, ev):
        sem, val, _ = ev
        if val <= 0:
            return
        key = id(sem)
        if self.seen.get(key, 0) >= val:
            return
        self.h.wait_ge(sem, val)
        self.seen[key] = val


class DmaQ:
    def __init__(self, k, name, eng, nslots=8):
        self.k = k
        self.eng = eng
        self.name = name
        self.sems = [k.newsem("%s_d%d" % (name, i)) for i in range(nslots)]
        self.vals = [0] * nslots
        self.i = 0

    def issue(self, emit, deps):
        s = self.i % len(self.sems)
        self.i += 1
        sem = self.sems[s]
        if self.vals[s] > 0:
            self.eng.wait((sem, self.vals[s], "dma"))
        for ev in deps:
            self.eng.wait(ev)
        if self.vals[s] >= 16 * 1500:
            sem = self.k.newsem("%s_d%d_%d" % (self.name, s, self.i))
            self.sems[s] = sem
            self.vals[s] = 0
        inst = emit()
        self.vals[s] += 16
        inst.then_inc(sem, 16)
        return (sem, self.vals[s], "dma")


class K:
    def __init__(self, nc, stack):
        self.nc = nc
        self.stack = stack
        self.nsem = 0
        self.pe = Eng(self, "pe", nc.tensor, is_pe=True)
        self.act = Eng(self, "act", nc.scalar)
        self.dve = Eng(self, "dve", nc.vector)
        self.pool = Eng(self, "pool", nc.gpsimd)
        self.sp = Eng(self, "sp", nc.sync)
        self.engs = [self.pe, self.act, self.dve, self.pool, self.sp]
        self.q_sp = DmaQ(self, "qsp", self.sp, 12)
        self.q_pool = DmaQ(self, "qpool", self.pool, 8)
        self.qs = [self.q_sp, self.q_pool]
        self.n_inst = 0

    def newsem(self, name):
        self.nsem += 1
        return self.stack.enter_context(self.nc.semaphore(name))

    def sb(self, st, name, shape, dtype):
        self.uid = getattr(self, "uid", 0) + 1
        name = "%s_u%d" % (name, self.uid)
        t = st.enter_context(self.nc.sbuf_tensor(name, list(shape), dtype))
        return Tile(t, name)

    def ps(self, st, name, shape, dtype=F32):
        self.uid = getattr(self, "uid", 0) + 1
        name = "%s_u%d" % (name, self.uid)
        t = st.enter_context(self.nc.psum_tensor(name, list(shape), dtype))
        return Tile(t, name)

    @staticmethod
    def _resof(x):
        return x.res if isinstance(x, Tile) else x

    def _deps(self, reads, writes):
        deps = []
        for r in reads:
            r = self._resof(r)
            if r.w is not None:
                deps.append(r.w)
        for w in writes:
            w = self._resof(w)
            if w.w is not None:
                deps.append(w.w)
            deps.extend(w.r)
        return deps

    def _commit(self, ev, reads, writes):
        for r in reads:
            r = self._resof(r)
            r.r = [e for e in r.r if e[0] is not ev[0]]
            r.r.append(ev)
        for w in writes:
            w = self._resof(w)
            w.w = ev
            w.r = []

    def op(self, eng, emit, reads=(), writes=()):
        for ev in self._deps(reads, writes):
            if eng.is_pe and ev[2] == "pe":
                continue
            eng.wait(ev)
        inst = emit()
        ev = eng.tick(inst)
        self._commit(ev, reads, writes)
        self.n_inst += 1
        return ev

    def dma(self, q, out, in_, reads=(), writes=(), **kw):
        deps = self._deps(reads, writes)
        ev = q.issue(lambda: q.eng.h.dma_start(out=out, in_=in_, **kw), deps)
        self._commit(ev, reads, writes)
        self.n_inst += 1
        return ev

    def barrier(self):
        evs = [e.cur() for e in self.engs]
        for q in self.qs:
            for s, v in zip(q.sems, q.vals):
                evs.append((s, v, "dma"))
        for e in self.engs:
            for ev in evs:
                if ev[0] is e.sem:
                    continue
                e.wait(ev)


class Ring:
    def __init__(self, tiles):
        self.tiles = tiles
        self.i = 0

    def next(self):
        t = self.tiles[self.i % len(self.tiles)]
        self.i += 1
        return t


def _t5_bucket_np(rel):
    nb = 16
    max_exact = 8
    ret = np.where(rel > 0, nb, 0)
    n = np.abs(rel)
    nf = np.maximum(n, 1).astype(np.float32)
    large = max_exact + (np.log(nf / max_exact) / np.float32(math.log(128 / max_exact))
                         * (nb - max_exact)).astype(np.int32)
    large = np.minimum(large, nb - 1)
    return ret + np.where(n < max_exact, n, large)


DELTAS = [-128, 0, 128, 256, 384]
NBLK = 48
BSZ = 512
NSLOT = NBLK * BSZ
R0 = 511
NF = 1280


def make_consts():
    c = {}
    c["c_ident"] = np.eye(128, dtype=np.float32)
    c["c_exch"] = np.ascontiguousarray(np.eye(128, dtype=np.float32)[::-1])
    i = np.arange(128)[:, None]
    j = np.arange(128)[None, :]
    c["c_mlt"] = (i < j).astype(np.float32)
    c["c_uneg"] = -(i >= j).astype(np.float32)
    same = (i // 64) == (j // 64)
    c["c_gtri"] = (-(1.0 / 16.0) * (same & (i <= j))).astype(np.float32)
    c["c_gm2"] = (-(1.0 / 16.0) * (same & (i > j))).astype(np.float32)
    c["c_gmask"] = (same & (i <= j)).astype(np.float32)
    n = np.arange(NF)
    bk = _t5_bucket_np(R0 - n)
    oh = np.zeros((32, NF), np.float32)
    oh[bk, n] = 1.0
    oh[:, 1151:] = 0.0
    c["c_ohb"] = oh
    nm = np.zeros((5, 128, 512), np.float32)
    for di, dl in enumerate(DELTAS):
        kk = dl + np.arange(128)[:, None]
        qq = np.arange(512)[None, :]
        allowed = (kk // 64) <= (qq // 64)
        nm[di] = np.where(allowed, 0.0, -30000.0)
    c["c_negmask"] = nm
    p = np.arange(128, dtype=np.float32)[:, None]
    c["c_iog"] = (p + 128.0 * np.arange(8, dtype=np.float32)[None, :]).astype(np.float32)
    c["c_io2"] = (2.0 * p + np.arange(2, dtype=np.float32)[None, :]).astype(np.float32)
    c["c_iob"] = np.tile(np.arange(NBLK, dtype=np.float32)[None, :], (128, 1))
    return c


WEIGHT_NAMES = ["w_ada", "b_ada", "norm_mix", "norm_ffn", "norm_final", "rel_bias",
                "even_w_in", "even_lambda", "even_subln", "even_w_gk2", "even_b_gk", "even_gla_norm",
                "even_w_out", "odd_w_in", "odd_w_out", "router_group_w", "router_group_b",
                "router_expert_w", "router_expert_b", "expert_w_gate", "expert_w_up", "expert_w_down"]
WEIGHT_SHAPES = {
    "w_ada": [4, 1024, 6144], "b_ada": [4, 6144], "norm_mix": [4, 1024], "norm_ffn": [4, 1024],
    "norm_final": [1, 1024], "rel_bias": [32, 4], "even_w_in": [2, 1024, 3088], "even_lambda": [2, 256],
    "even_subln": [2, 128], "even_w_gk2": [2, 16, 256], "even_b_gk": [2, 256], "even_gla_norm": [2, 128],
    "even_w_out": [2, 1024, 1024], "odd_w_in": [2, 1024, 3072], "odd_w_out": [2, 1024, 1024],
    "router_group_w": [4, 1024, 4], "router_group_b": [4, 4], "router_expert_w": [4, 1024, 32],
    "router_expert_b": [4, 32], "expert_w_gate": [4, 32, 1024, 512], "expert_w_up": [4, 32, 1024, 512],
    "expert_w_down": [4, 32, 512, 1024],
}


def build(nlayers=DEPTH, stop=None, flags=()):
    SPARSE = 'dense' not in flags
    nc = bass.Bass("TRN2", target_bir_lowering=False)
    I = {}
    I["x"] = nc.dram_tensor("x", [S, D], F32, kind="ExternalInput").ap()
    I["c"] = nc.dram_tensor("c", [1, D], F32, kind="ExternalInput").ap()
    for nm in WEIGHT_NAMES:
        I[nm] = nc.dram_tensor(nm, WEIGHT_SHAPES[nm], F32, kind="ExternalInput").ap()
    consts = make_consts()
    for nm, v in consts.items():
        I[nm] = nc.dram_tensor(nm, list(v.shape), F32, kind="ExternalInput").ap()
    OUT = nc.dram_tensor("out", [S, D], F32, kind="ExternalOutput").ap()
    XS = nc.dram_tensor("xs_scr", [S, D], F32, kind="Internal").ap()
    FB = nc.dram_tensor("fb_scr", [25, 128, S], BF16, kind="Internal").ap()
    TB = nc.dram_tensor("tb_scr", [S, 1280], BF16, kind="Internal").ap()
    FS_T = nc.dram_tensor("fs_scr", [4, NF], F32, kind="Internal")
    FS = FS_T.ap()
    XN = nc.dram_tensor("xn_scr", [S, D], F32, kind="Internal").ap()
    MV = nc.dram_tensor("mv_scr", [2, D], F32, kind="Internal").ap()
    XG = nc.dram_tensor("xg_scr", [NSLOT, D], F32, kind="Internal").ap()
    YS = nc.dram_tensor("ys_scr", [NSLOT, D], F32, kind="Internal").ap()

    with ExitStack() as gst:
        k = K(nc, gst)
        pe, act, dve, pool, sp = k.pe, k.act, k.dve, k.pool, k.sp

        def mm(out, lhsT, rhs, start, stop, reads, writes):
            k.op(pe, lambda: nc.tensor.matmul(out, lhsT=lhsT, rhs=rhs, start=start, stop=stop), reads, writes)

        def tr(out, in_, ident, reads, writes):
            k.op(pe, lambda: nc.tensor.transpose(out=out, in_=in_, identity=ident), reads, writes)

        def actf(out, in_, func, reads, writes, **kw):
            k.op(act, lambda: nc.scalar.activation(out=out, in_=in_, func=func, **kw), reads, writes)

        def tt(eng, out, in0, in1, op, reads, writes):
            k.op(eng, lambda: eng.h.tensor_tensor(out=out, in0=in0, in1=in1, op=op), reads, writes)

        def ts(eng, out, in0, s1, s2, op0, op1, reads, writes):
            if op1 is None:
                k.op(eng, lambda: eng.h.tensor_scalar(out=out, in0=in0, scalar1=s1, scalar2=None, op0=op0),
                     reads, writes)
            else:
                k.op(eng, lambda: eng.h.tensor_scalar(out=out, in0=in0, scalar1=s1, scalar2=s2, op0=op0, op1=op1),
                     reads, writes)

        def stt(out, in0, scalar, in1, op0, op1, reads, writes):
            k.op(dve, lambda: nc.vector.scalar_tensor_tensor(out=out, in0=in0, scalar=scalar, in1=in1,
                                                             op0=op0, op1=op1), reads, writes)

        def cp(eng, out, in_, reads, writes):
            k.op(eng, lambda: eng.h.tensor_copy(out=out, in_=in_), reads, writes)

        def recip(out, in_, reads, writes):
            k.op(dve, lambda: nc.vector.reciprocal(out=out, in_=in_), reads, writes)

        def memset(eng, t, val, writes):
            k.op(eng, lambda: eng.h.memset(t, val), (), writes)

        def rmax(out, in_, reads, writes):
            k.op(dve, lambda: nc.vector.tensor_reduce(out=out, in_=in_, axis=AX.X, op=ALU.max), reads, writes)

        xres = [Res("x%d" % i) for i in range(NT)]
        fbres = [Res("fb%d" % i) for i in range(25)]
        tbres = Res("tb")
        fsres = Res("fs")

        ident32 = k.sb(gst, "ident32", [128, 128], F32)
        ones32 = k.sb(gst, "ones32", [128, 128], F32)
        onesb = k.sb(gst, "onesb", [128, 128], BF16)
        negonesb = k.sb(gst, "negonesb", [128, 128], BF16)
        zerob = k.sb(gst, "zerob", [128, 128], BF16)
        identb = k.sb(gst, "identb", [128, 128], BF16)
        k.dma(k.q_sp, ident32[:, :], I["c_ident"][:, :], writes=[ident32])
        memset(dve, ones32[:, :], 1.0, [ones32])
        memset(dve, onesb[:, :], 1.0, [onesb])
        memset(dve, negonesb[:, :], -1.0, [negonesb])
        memset(dve, zerob[:, :], 0.0, [zerob])
        epsc = k.sb(gst, "epsc", [128, 1], F32)
        memset(dve, epsc[:, :], EPS, [epsc])
        cp(dve, identb[:, :], ident32[:, :], [ident32], [identb])

        modT = k.sb(gst, "modT", [128, 64], F32)
        gmod1 = k.sb(gst, "gmod1", [128, 8], F32)
        gmod2 = k.sb(gst, "gmod2", [128, 8], F32)
        gbc1 = k.sb(gst, "gbc1", [128, D], F32)
        gbc2 = k.sb(gst, "gbc2", [128, D], F32)
        scT = k.sb(gst, "scT", [128, 8], F32)
        cmb = k.sb(gst, "cmb", [128, NT, NEXP], F32)
        H = {}
        r_ohg = k.sb(gst, "r_ohg", [128, NT, 4], F32)
        r_oh1 = k.sb(gst, "r_oh1", [128, NT, 8], F32)
        r_oh2 = k.sb(gst, "r_oh2", [128, NT, 8], F32)
        r_v = k.sb(gst, "r_v", [128, 12, NT], F32)
        s12u = k.sb(gst, "s12u", [128, 2, NT], mybir.dt.uint32)
        idxg = k.sb(gst, "idxg", [128, NBLK, 8], mybir.dt.uint32)
        idxd = k.sb(gst, "idxd", [128, NBLK, 4], mybir.dt.uint32)
        idx2 = k.sb(gst, "idx2", [128, NBLK, 2], mybir.dt.uint32)
        gsb = k.sb(gst, "gsb", [128, 2, 8], F32)
        mvres = Res("mv")
        xnres = Res("xn")
        xgres = Res("xg")
        ysres = [Res("ys%d" % i) for i in range(NBLK)]

        def idma(out, out_off, in_, in_off, reads, writes):
            deps = k._deps(reads, writes)
            ev = k.q_pool.issue(lambda: nc.gpsimd.indirect_dma_start(out=out, out_offset=out_off, in_=in_,
                                                                     in_offset=in_off), deps)
            k._commit(ev, reads, writes)
            k.n_inst += 1
            return ev

        def emit_mod(l):
            with ExitStack() as st:
                crow = k.sb(st, "crow", [1, D], F32)
                sc_bc = k.sb(st, "sc_bc", [128, 8, 128], F32)
                wseg = Ring([k.sb(st, "wseg%d" % i, [128, 8, 512], F32) for i in range(2)])
                brow = Ring([k.sb(st, "brow%d" % i, [1, 512], F32) for i in range(2)])
                nrow = k.sb(st, "nrow", [1, 2 * D], F32)
                pm = k.ps(st, "pm", [128, 512], F32)
                pg = Ring([k.ps(st, "pg%d" % i, [128, 512], F32) for i in range(2)])
                k.dma(k.q_sp, crow[:, :], I["c"][:, :], writes=[crow])
                for kc in range(8):
                    mm(pm[:, kc:kc + 1], crow[0:1, kc * 128:(kc + 1) * 128], ones32[0:1, 0:1], True, True,
                       [crow, ones32], [pm])
                actf(scT[:, :], pm[:, 0:8], AF.Silu, [pm], [scT])
                for kc in range(8):
                    actf(sc_bc[:, kc, :], ones32[:, :], AF.Copy, [ones32, scT], [sc_bc], scale=scT[:, kc:kc + 1])
                wv = I["w_ada"][l].rearrange("(kc p) n -> p kc n", p=128)
                for seg in range(6):
                    for half in range(2):
                        n0 = seg * 1024 + half * 512
                        w = wseg.next()
                        b = brow.next()
                        k.dma(k.q_sp, w[:, :, :], wv[:, :, n0:n0 + 512], writes=[w])
                        k.dma(k.q_sp, b[:, :], I["b_ada"][l:l + 1, n0:n0 + 512], writes=[b])
                        if seg in (2, 5):
                            p = pg.next()
                            for kc in range(8):
                                mm(p[:, :], sc_bc[:, kc, :], w[:, kc, :], kc == 0, False, [sc_bc, w], [p])
                            mm(p[:, :], ones32[0:1, :], b[0:1, :], False, True, [ones32, b], [p])
                            g = gbc1 if seg == 2 else gbc2
                            cp(dve, g[:, half * 512:(half + 1) * 512], p[:, :], [p], [g])
                        else:
                            for j in range(4):
                                col = seg * 8 + half * 4 + j
                                for kc in range(8):
                                    mm(pm[:, col:col + 1], w[:, kc, j * 128:(j + 1) * 128], scT[:, kc:kc + 1],
                                       kc == 0, False, [w, scT], [pm])
                                mm(pm[:, col:col + 1], b[0:1, j * 128:(j + 1) * 128], ones32[0:1, 0:1], False, True,
                                   [b, ones32], [pm])
                k.dma(k.q_sp, nrow[:, 0:D], I["norm_mix"][l:l + 1, :], writes=[nrow])
                k.dma(k.q_sp, nrow[:, D:2 * D], I["norm_ffn"][l:l + 1, :], writes=[nrow])
                for j in range(16):
                    mm(pm[:, 48 + j:49 + j], nrow[0:1, j * 128:(j + 1) * 128], ones32[0:1, 0:1], True, True,
                       [nrow, ones32], [pm])
                cp(dve, modT[:, :], pm[:, 0:64], [pm], [modT])
                stt(gmod1[:, :], modT[:, 8:16], 1.0, modT[:, 48:56], ALU.add, ALU.mult, [modT], [gmod1])
                stt(gmod2[:, :], modT[:, 32:40], 1.0, modT[:, 56:64], ALU.add, ALU.mult, [modT], [gmod2])
                k.barrier()

        def emit_norm(xsrc, gmod, shiftc, layer, router):
            with ExitStack() as st:
                xt_r = Ring([k.sb(st, "n_xt%d" % i, [128, D], F32) for i in range(4)])
                xs_r = Ring([k.sb(st, "n_xs%d" % i, [128, D], F32) for i in range(3)])
                junk = k.sb(st, "n_junk", [128, D], BF16)
                sm_r = Ring([k.sb(st, "n_sm%d" % i, [128, 4], F32) for i in range(6)])
                ps_r = Ring([k.ps(st, "n_ps%d" % i, [128, D], F32) for i in range(2)])
                if router:
                    h32_r = Ring([k.sb(st, "n_h32%d" % i, [128, 8, 128], F32) for i in range(2)])
                    wr32 = k.sb(st, "n_wr32", [128, 8, 36], F32)
                    brr = k.sb(st, "n_brr", [1, 36], F32)
                    plg_r = Ring([k.ps(st, "n_plg%d" % i, [128, 512], F32) for i in range(2)])
                    lgall = k.sb(st, "n_lgall", [128, NT, 36], F32)
                    k.dma(k.q_sp, wr32[:, :, 0:4],
                          I["router_group_w"][layer].rearrange("(kc p) n -> p kc n", p=128), writes=[wr32])
                    k.dma(k.q_sp, wr32[:, :, 4:36],
                          I["router_expert_w"][layer].rearrange("(kc p) n -> p kc n", p=128), writes=[wr32])
                    k.dma(k.q_sp, brr[:, 0:4], I["router_group_b"][layer:layer + 1, :], writes=[brr])
                    k.dma(k.q_sp, brr[:, 4:36], I["router_expert_b"][layer:layer + 1, :], writes=[brr])
                state = {}

                def s1(t):
                    xt = xt_r.next()
                    k.dma(k.q_sp, xt[:, :], xsrc[t * 128:(t + 1) * 128, :], reads=[xres[t]], writes=[xt])
                    sm = sm_r.next()
                    actf(junk[:, :], xt[:, :], AF.Square, [xt], [junk, sm], accum_out=sm[:, 0:1])
                    state[t] = (xt, sm)

                def s2(t):
                    xt, sm = state[t]
                    ts(dve, sm[:, 1:2], sm[:, 0:1], 1.0 / D, EPS, ALU.mult, ALU.add, [sm], [sm])
                    actf(sm[:, 2:3], sm[:, 1:2], AF.Sqrt, [sm], [sm])
                    recip(sm[:, 3:4], sm[:, 2:3], [sm], [sm])
                    xs = xs_r.next()
                    ts(dve, xs[:, :], xt[:, :], sm[:, 3:4], None, ALU.mult, None, [xt, sm], [xs])
                    if router and SPARSE:
                        k.dma(k.q_sp, XN[t * 128:(t + 1) * 128, :], xs[:, :], reads=[xs], writes=[xnres])
                    state[t] = xs

                def s3(t):
                    xs = state.pop(t)
                    ps = ps_r.next()
                    for kc in range(8):
                        tr(ps[:, kc * 128:(kc + 1) * 128], xs[:, kc * 128:(kc + 1) * 128], ident32[:, :],
                           [xs, ident32], [ps])
                    for kc in range(8):
                        if router and SPARSE:
                            break
                        actf(H["hT"][:, kc, t * 128:(t + 1) * 128], ps[:, kc * 128:(kc + 1) * 128], AF.Identity,
                             [ps, gmod, modT], [H["hT"]], scale=gmod[:, kc:kc + 1], bias=shiftc[:, kc:kc + 1])
                    if router:
                        h32 = h32_r.next()
                        for kc in range(8):
                            actf(h32[:, kc, :], ps[:, kc * 128:(kc + 1) * 128], AF.Identity,
                                 [ps, gmod, modT], [h32], scale=gmod[:, kc:kc + 1], bias=shiftc[:, kc:kc + 1])
                        plg = plg_r.next()
                        for kc in range(8):
                            mm(plg[:, 0:36], h32[:, kc, :], wr32[:, kc, :], kc == 0, False, [h32, wr32], [plg])
                        mm(plg[:, 0:36], ones32[0:1, :], brr[0:1, :], False, True, [ones32, brr], [plg])
                        cp(dve, lgall[:, t, :], plg[:, 0:36], [plg], [lgall])

                for i in range(NT + 2):
                    if i < NT:
                        s1(i)
                    if 1 <= i <= NT:
                        s2(i - 1)
                    if i >= 2:
                        s3(i - 2)
                if router:
                    def bc(ap2, n):
                        return ap2.unsqueeze(2).to_broadcast([128, NT, n])
                    f4 = k.sb(st, "r_f4", [128, NT, 4], F32)
                    ohg = r_ohg
                    v = r_v
                    el = k.sb(st, "r_el", [128, NT, 8], F32)
                    t8 = k.sb(st, "r_t8", [128, NT, 8], F32)
                    oh1 = r_oh1
                    oh2 = r_oh2
                    elm = k.sb(st, "r_elm", [128, NT, 8], F32)
                    cg = k.sb(st, "r_cg", [128, NT, 8], F32)
                    RR = [f4, ohg, v, el, t8, oh1, oh2, elm, cg, lgall]
                    gm, gs, gval, l1, l2, dd, ee, den, w1, W1, W2 = [v[:, i, :] for i in range(11)]

                    def red(out, in_, op):
                        k.op(dve, lambda: nc.vector.tensor_reduce(out=out, in_=in_, axis=AX.X, op=op), RR, RR)

                    lgg = lgall[:, :, 0:4]
                    red(gm, lgg, ALU.max)
                    tt(dve, ohg[:, :, :], lgg, bc(gm, 4), ALU.is_equal, RR, RR)
                    tt(dve, f4[:, :, :], lgg, bc(gm, 4), ALU.subtract, RR, RR)
                    actf(f4[:, :, :], f4[:, :, :], AF.Exp, RR, RR)
                    red(gs, f4[:, :, :], ALU.add)
                    recip(gval, gs, RR, RR)
                    for g in range(4):
                        src = lgall[:, :, 4 + 8 * g:12 + 8 * g]
                        if g == 0:
                            tt(dve, el[:, :, :], src, bc(ohg[:, :, g], 8), ALU.mult, RR, RR)
                        else:
                            tt(dve, t8[:, :, :], src, bc(ohg[:, :, g], 8), ALU.mult, RR, RR)
                            tt(dve, el[:, :, :], el[:, :, :], t8[:, :, :], ALU.add, RR, RR)
                    red(l1, el[:, :, :], ALU.max)
                    tt(dve, oh1[:, :, :], el[:, :, :], bc(l1, 8), ALU.is_equal, RR, RR)
                    stt(elm[:, :, :], oh1[:, :, :], -1e30, el[:, :, :], ALU.mult, ALU.add, RR, RR)
                    red(l2, elm[:, :, :], ALU.max)
                    tt(dve, oh2[:, :, :], elm[:, :, :], bc(l2, 8), ALU.is_equal, RR, RR)
                    tt(dve, dd, l2, l1, ALU.subtract, RR, RR)
                    actf(ee, dd, AF.Exp, RR, RR)
                    ts(dve, den, ee, 1.0, None, ALU.add, None, RR, RR)
                    recip(w1, den, RR, RR)
                    tt(dve, W1, w1, gval, ALU.mult, RR, RR)
                    tt(dve, W2, W1, ee, ALU.mult, RR, RR)
                    tt(dve, cg[:, :, :], oh1[:, :, :], bc(W1, 8), ALU.mult, RR, RR)
                    tt(dve, t8[:, :, :], oh2[:, :, :], bc(W2, 8), ALU.mult, RR, RR)
                    tt(dve, cg[:, :, :], cg[:, :, :], t8[:, :, :], ALU.add, RR, RR)
                    for g in range(4):
                        tt(dve, cmb[:, :, 8 * g:8 * g + 8], cg[:, :, :], bc(ohg[:, :, g], 8), ALU.mult, RR, [cmb])
                k.barrier()

        def emit_inproj(wsrc, ncols, fjobs, tjobs):
            with ExitStack() as st:
                W = k.sb(st, "ip_W", [128, 8, ncols], BF16)
                wv = wsrc.rearrange("(kc p) n -> p kc n", p=128)
                c0 = 0
                while c0 < ncols:
                    w_ = min(512, ncols - c0)
                    k.dma(k.q_pool, W[:, :, c0:c0 + w_], wv[:, :, c0:c0 + w_], writes=[W])
                    c0 += w_
                stg_r = Ring([k.sb(st, "ip_stg%d" % i, [128, S], BF16) for i in range(2)])
                stt_r = Ring([k.sb(st, "ip_stt%d" % i, [128, 4, 512], BF16) for i in range(2)])
                ps_r = Ring([k.ps(st, "ip_ps%d" % i, [128, 512], F32) for i in range(4)])
                n = 0
                for (col0, nr, fb, scale) in fjobs:
                    stg = stg_r.next()
                    for tg in range(8):
                        ps = ps_r.next()
                        for kc in range(8):
                            mm(ps[0:nr, :], W[:, kc, col0:col0 + nr], H["hT"][:, kc, tg * 512:(tg + 1) * 512],
                               kc == 0, kc == 7, [W, H["hT"]], [ps])
                        if n % 2 == 0:
                            actf(stg[0:nr, tg * 512:(tg + 1) * 512], ps[0:nr, :], AF.Copy, [ps], [stg], scale=scale)
                        else:
                            ts(dve, stg[0:nr, tg * 512:(tg + 1) * 512], ps[0:nr, :], scale, None, ALU.mult, None,
                               [ps], [stg])
                        n += 1
                    k.dma(k.q_sp, FB[fb, 0:nr, :], stg[0:nr, :], reads=[stg], writes=[fbres[fb]])
                for (col0, wd, tcol0) in tjobs:
                    for t4 in range(8):
                        stg = stt_r.next()
                        for j in range(4):
                            t = t4 * 4 + j
                            ps = ps_r.next()
                            for kc in range(8):
                                mm(ps[:, 0:wd], H["hT"][:, kc, t * 128:(t + 1) * 128], W[:, kc, col0:col0 + wd],
                                   kc == 0, kc == 7, [W, H["hT"]], [ps])
                            if n % 2 == 0:
                                actf(stg[:, j, 0:wd], ps[:, 0:wd], AF.Copy, [ps], [stg])
                            else:
                                cp(dve, stg[:, j, 0:wd], ps[:, 0:wd], [ps], [stg])
                            n += 1
                        k.dma(k.q_sp,
                              TB[t4 * 512:(t4 + 1) * 512, tcol0:tcol0 + wd].rearrange("(j p) c -> p j c", p=128),
                              stg[:, :, 0:wd], reads=[stg], writes=[tbres])
                k.barrier()

        def emit_outproj(wsrc, gbc, xsrc):
            with ExitStack() as st:
                W = k.sb(st, "op_W", [128, 8, D], BF16)
                wv = wsrc.rearrange("(kc p) n -> p kc n", p=128)
                for h in range(2):
                    k.dma(k.q_pool, W[:, :, h * 512:(h + 1) * 512], wv[:, :, h * 512:(h + 1) * 512], writes=[W])
                for kc in range(8):
                    tt(dve, W[:, kc, :], W[:, kc, :], gbc[:, :], ALU.mult, [W, gbc], [W])
                xt_r = Ring([k.sb(st, "op_xt%d" % i, [128, D], F32) for i in range(3)])
                xn_r = Ring([k.sb(st, "op_xn%d" % i, [128, D], F32) for i in range(2)])
                ps_r = Ring([k.ps(st, "op_ps%d" % i, [128, D], F32) for i in range(3)])
                for t in range(NT):
                    xt = xt_r.next()
                    k.dma(k.q_sp, xt[:, :], xsrc[t * 128:(t + 1) * 128, :], reads=[xres[t]], writes=[xt])
                    ps = ps_r.next()
                    for h in range(2):
                        for kc in range(8):
                            mm(ps[:, h * 512:(h + 1) * 512], H["hT"][:, kc, t * 128:(t + 1) * 128],
                               W[:, kc, h * 512:(h + 1) * 512], kc == 0, kc == 7, [H["hT"], W], [ps])
                    xn = xn_r.next()
                    tt(dve, xn[:, :], xt[:, :], ps[:, :], ALU.add, [xt, ps], [xn])
                    k.dma(k.q_sp, XS[t * 128:(t + 1) * 128, :], xn[:, :], reads=[xn], writes=[xres[t]])
                k.barrier()

        def emit_odd_mixer():
            with ExitStack() as st:
                mltb = k.sb(st, "sb_mlt", [128, 128], BF16)
                unegb = k.sb(st, "sb_uneg", [128, 128], BF16)
                k.dma(k.q_pool, mltb[:, :], I["c_mlt"][:, :], writes=[mltb])
                k.dma(k.q_pool, unegb[:, :], I["c_uneg"][:, :], writes=[unegb])
                qT_r = Ring([k.sb(st, "sb_qT%d" % i, [128, S], BF16) for i in range(2)])
                kT_r = Ring([k.sb(st, "sb_kT%d" % i, [128, S], BF16) for i in range(2)])
                vt_r = Ring([k.sb(st, "sb_vt%d" % i, [128, NT, 128], BF16) for i in range(2)])
                e32_r = Ring([k.sb(st, "sb_e%d" % i, [128, 512], F32) for i in range(6)])
                sp_r = Ring([k.sb(st, "sb_sp%d" % i, [128, 512], BF16) for i in range(10)])
                rb_r = Ring([k.sb(st, "sb_rb%d" % i, [128, 512], BF16) for i in range(10)])
                ab_r = Ring([k.sb(st, "sb_ab%d" % i, [128, 512], BF16) for i in range(8)])
                R32s = [k.sb(st, "sb_R32_%d" % i, [128, 512], F32) for i in range(2)]
                pz_r = Ring([k.ps(st, "sb_pz%d" % i, [128, 512], F32) for i in range(3)])
                pw_r = Ring([k.ps(st, "sb_pw%d" % i, [128, 512], F32) for i in range(3)])
                pos = [k.ps(st, "sb_po%d" % i, [128, 512], F32) for i in range(2)]
                for hp in range(8):
                    qT = qT_r.next()
                    kT = kT_r.next()
                    vt = vt_r.next()
                    k.dma(k.q_sp, qT[:, :], FB[hp, :, :], reads=[fbres[hp]], writes=[qT])
                    k.dma(k.q_sp, kT[:, :], FB[8 + hp, :, :], reads=[fbres[8 + hp]], writes=[kT])
                    k.dma(k.q_sp, vt[:, :, :],
                          TB[:, hp * 128:(hp + 1) * 128].rearrange("(t p) c -> p t c", p=128),
                          reads=[tbres], writes=[vt])
                    for qg in range(8):
                        kbs = list(range(4 * qg + 3, -1, -1))
                        nb = len(kbs)
                        items = [[None] * nb, [None] * nb]
                        pend = {}
                        pendB = []
                        for j in range(2):
                            mm(pos[j][:, :], zerob[:, :], qT[:, qg * 512:(qg + 1) * 512], True, False,
                               [zerob, qT], [pos[j]])

                        def stageA(j, i):
                            b0 = 64 * j
                            R32 = R32s[j]
                            kb = kbs[i]
                            j0 = max(0, kb - 4 * qg)
                            c0 = 128 * j0
                            wq = 512 - c0
                            q0 = qg * 512 + c0
                            diag = kb >= 4 * qg
                            pz = pz_r.next()
                            mm(pz[:, 0:wq], kT[b0:b0 + 64, kb * 128:(kb + 1) * 128], qT[b0:b0 + 64, q0:q0 + wq],
                               True, True, [kT, qT], [pz])
                            e32 = e32_r.next()
                            actf(e32[:, 0:wq], pz[:, 0:wq], AF.Exp, [pz], [e32])
                            pend[(j, i)] = (R32, kb, c0, wq, q0, diag, e32)

                        def stageA2(j, i):
                            R32, kb, c0, wq, q0, diag, e32 = pend.pop((j, i))
                            spb = sp_r.next()
                            if 'sbx_noln' not in flags:
                                actf(spb[:, 0:wq], e32[:, 0:wq], AF.Ln, [e32], [spb], bias=1.0)
                            if diag:
                                tt(pool, spb[:, 0:128], spb[:, 0:128], mltb[:, :], ALU.mult, [spb, mltb], [spb])
                            rb = None
                            if i > 0 and 'sbx_nor' not in flags:
                                rb = rb_r.next()
                                cp(dve, rb[:, :], R32[:, :], [R32], [rb])
                            if i < nb - 1 and 'sbx_nor' not in flags:
                                if i == 0:
                                    if c0 > 0:
                                        memset(dve, R32[:, 0:c0], 0.0, [R32])
                                    cp(dve, R32[:, c0:512], spb[:, 0:wq], [spb], [R32])
                                else:
                                    tt(dve, R32[:, c0:512], R32[:, c0:512], spb[:, 0:wq], ALU.add, [R32, spb],
                                       [R32])
                            items[j][i] = (kb, c0, wq, q0, diag, spb, rb)

                        def stageB(j, i):
                            if 'sbx_nob' in flags:
                                return
                            b0 = 64 * j
                            po = pos[j]
                            kb, c0, wq, q0, diag, spb, rb = items[j][i]
                            pw = pw_r.next()
                            mm(pw[:, 0:wq], kT[b0:b0 + 64, kb * 128:(kb + 1) * 128], qT[b0:b0 + 64, q0:q0 + wq],
                               True, False, [kT, qT], [pw])
                            mm(pw[:, 0:wq], unegb[:, :], spb[:, 0:wq], False, rb is None, [unegb, spb], [pw])
                            if rb is not None:
                                mm(pw[:, 0:wq], negonesb[:, :], rb[:, c0:512], False, True, [negonesb, rb], [pw])
                            ab = ab_r.next()
                            actf(ab[:, 0:wq], pw[:, 0:wq], AF.Exp, [pw], [ab])
                            if diag:
                                tt(dve, ab[:, 0:128], ab[:, 0:128], mltb[:, :], ALU.mult, [ab, mltb], [ab])
                            pendB.append((po, c0, wq, kb, ab, i))
                            while len(pendB) > 3:
                                stageB2()

                        def stageB2():
                            po, c0, wq, kb, ab, i = pendB.pop(0)
                            mm(po[:, c0:512], vt[:, kb, :], ab[:, 0:wq], False, i == nb - 1, [vt, ab], [po])

                        GW = 2
                        nw = nb // GW
                        for w in range(nw + 1):
                            if w < nw:
                                for i in range(w * GW, (w + 1) * GW):
                                    for j in range(2):
                                        stageA(j, i)
                                for i in range(w * GW, (w + 1) * GW):
                                    for j in range(2):
                                        stageA2(j, i)
                            if w > 0:
                                for i in range((w - 1) * GW, w * GW):
                                    for j in range(2):
                                        stageB(j, i)
                        while pendB:
                            stageB2()
                        for j in range(2):
                            b0 = 64 * j
                            if False:
                                pass
                            else:
                                cp(dve, H["hT"][b0:b0 + 64, hp, qg * 512:(qg + 1) * 512], pos[j][b0:b0 + 64, :],
                                   [pos[j]], [H["hT"]])
                k.barrier()

        def emit_even_mixer(i_even, layer):
            lam_init = 0.8 - 0.6 * math.exp(-0.3 * layer)
            with ExitStack() as st:
                BM = [[k.sb(st, "BM%d_%d" % (h, di), [128, 512], BF16) for di in range(5)] for h in range(4)]
                b15 = k.sb(st, "b15", [128, 4], F32)
                neglam = k.sb(st, "neglam", [128, 1], F32)
                gsub = k.sb(st, "gsub", [128, 1], F32)
                with ExitStack() as st2:
                    rb = k.sb(st2, "rb", [32, 4], F32)
                    ohb = k.sb(st2, "ohb", [32, NF], F32)
                    fsb = k.sb(st2, "fsb", [4, NF], F32)
                    exch = k.sb(st2, "exch", [128, 128], F32)
                    nmk = [k.sb(st2, "nmk%d" % di, [128, 512], F32) for di in range(5)]
                    tl_r = Ring([k.sb(st2, "tl%d" % i, [128, 512], F32) for i in range(2)])
                    lrow = k.sb(st2, "lrow", [1, 264], F32)
                    grow = k.sb(st2, "grow", [1, 128], F32)
                    pf = Ring([k.ps(st2, "pf%d" % i, [128, 512], F32) for i in range(2)])
                    k.dma(k.q_sp, rb[:, :], I["rel_bias"][:, :], writes=[rb])
                    k.dma(k.q_sp, ohb[:, :], I["c_ohb"][:, :], writes=[ohb])
                    k.dma(k.q_sp, exch[:, :], I["c_exch"][:, :], writes=[exch])
                    k.dma(k.q_sp, b15[:, :], I["rel_bias"][15].partition_broadcast(128), writes=[b15])
                    for di in range(5):
                        k.dma(k.q_sp, nmk[di][:, :], I["c_negmask"][di], writes=[nmk[di]])
                    for c3 in range(3):
                        c0 = c3 * 512
                        w_ = min(512, NF - c0)
                        p = pf.next()
                        mm(p[0:4, 0:w_], rb[0:32, 0:4], ohb[0:32, c0:c0 + w_], True, True, [rb, ohb], [p])
                        cp(dve, fsb[0:4, c0:c0 + w_], p[0:4, 0:w_], [p], [fsb])
                    k.dma(k.q_sp, FS[:, :], fsb[:, :], reads=[fsb], writes=[fsres])
                    for h in range(4):
                        for di, dl in enumerate(DELTAS):
                            tl = tl_r.next()
                            src = bass.AP(tensor=FS_T, offset=h * NF + (384 - dl), ap=[[1, 128], [1, 512]])
                            k.dma(k.q_sp, tl[:, :], src, reads=[fsres], writes=[tl])
                            p = pf.next()
                            mm(p[:, :], exch[:, :], tl[:, :], True, True, [exch, tl], [p])
                            tt(dve, BM[h][di][:, :], p[:, :], nmk[di][:, :], ALU.add, [p, nmk[di]], [BM[h][di]])
                    k.dma(k.q_sp, lrow[:, 0:256], I["even_lambda"][i_even:i_even + 1, :], writes=[lrow])
                    LR = [lrow]
                    tt(dve, lrow[:, 0:64], lrow[:, 0:64], lrow[:, 64:128], ALU.mult, LR, LR)
                    tt(dve, lrow[:, 128:192], lrow[:, 128:192], lrow[:, 192:256], ALU.mult, LR, LR)
                    k.op(dve, lambda: nc.vector.tensor_reduce(out=lrow[:, 256:257], in_=lrow[:, 0:64], axis=AX.X,
                                                              op=ALU.add), LR, LR)
                    k.op(dve, lambda: nc.vector.tensor_reduce(out=lrow[:, 257:258], in_=lrow[:, 128:192], axis=AX.X,
                                                              op=ALU.add), LR, LR)
                    actf(lrow[:, 258:260], lrow[:, 256:258], AF.Exp, LR, LR)
                    tt(dve, lrow[:, 260:261], lrow[:, 259:260], lrow[:, 258:259], ALU.subtract, LR, LR)
                    ts(dve, lrow[:, 261:262], lrow[:, 260:261], -lam_init, None, ALU.add, None, LR, LR)
                    p = pf.next()
                    mm(p[:, 0:1], ones32[0:1, :], lrow[0:1, 261:262], True, True, [ones32, lrow], [p])
                    cp(dve, neglam[:, :], p[:, 0:1], [p], [neglam])
                    k.dma(k.q_sp, grow[:, :], I["even_subln"][i_even:i_even + 1, :], writes=[grow])
                    p = pf.next()
                    mm(p[:, 0:1], grow[0:1, :], ones32[0:1, 0:1], True, True, [grow, ones32], [p])
                    ts(dve, gsub[:, :], p[:, 0:1], 1.0 - lam_init, None, ALU.mult, None, [p], [gsub])
                    k.barrier()
                qT_r = Ring([k.sb(st, "da_qT%d" % i, [128, S], BF16) for i in range(2)])
                kT_r = Ring([k.sb(st, "da_kT%d" % i, [128, S], BF16) for i in range(2)])
                vt_r = Ring([k.sb(st, "da_vt%d" % i, [128, NT, 128], BF16) for i in range(2)])
                E_r = Ring([k.sb(st, "da_E%d" % i, [128, 512], BF16) for i in range(8)])
                f_r = Ring([k.sb(st, "da_f%d" % i, [128, 512], F32) for i in range(6)])
                o32_r = Ring([k.sb(st, "da_o%d" % i, [128, 512], F32) for i in range(2)])
                sq_r = Ring([k.sb(st, "da_sq%d" % i, [128, 512], BF16) for i in range(2)])
                ps_r = Ring([k.ps(st, "da_ps%d" % i, [128, 512], F32) for i in range(3)])
                pu = [k.ps(st, "da_pu%d" % i, [128, 512], F32) for i in range(2)]
                pd = [k.ps(st, "da_pd%d" % i, [128, 512], F32) for i in range(2)]
                pss = k.ps(st, "da_pss", [128, 512], F32)
                us_r = Ring([k.sb(st, "da_us%d" % i, [128, 512], F32) for i in range(4)])
                pend_fin = []
                for h in range(0 if 'ev_noA' in flags else 4):
                    qT = qT_r.next()
                    kT = kT_r.next()
                    vt = vt_r.next()
                    k.dma(k.q_sp, qT[:, :], FB[h, :, :], reads=[fbres[h]], writes=[qT])
                    k.dma(k.q_sp, kT[:, :], FB[4 + h, :, :], reads=[fbres[4 + h]], writes=[kT])
                    k.dma(k.q_sp, vt[:, :, :],
                          TB[:, h * 128:(h + 1) * 128].rearrange("(t p) c -> p t c", p=128),
                          reads=[tbres], writes=[vt])
                    for qg in range(8):
                        nkb = 4 * qg + 4
                        its = [(kb, m) for kb in range(nkb) for m in range(2)]
                        held = {}

                        def st1(idx):
                            kb, m = its[idx]
                            j0 = max(0, kb - 4 * qg)
                            c0 = 128 * j0
                            wq = 512 - c0
                            q0 = qg * 512 + c0
                            dl = kb * 128 - qg * 512
                            b0 = 64 * m
                            ps = ps_r.next()
                            near = dl >= -128
                            mm(ps[:, 0:wq], kT[b0:b0 + 64, kb * 128:(kb + 1) * 128], qT[b0:b0 + 64, q0:q0 + wq],
                               True, not near, [kT, qT], [ps])
                            E = E_r.next()
                            if near:
                                bm = BM[h][DELTAS.index(dl)]
                                mm(ps[:, 0:wq], identb[:, :], bm[:, c0:512], False, True, [identb, bm], [ps])
                                actf(E[:, 0:wq], ps[:, 0:wq], AF.Exp, [ps], [E])
                            else:
                                actf(E[:, 0:wq], ps[:, 0:wq], AF.Exp, [ps, b15], [E], bias=b15[:, h:h + 1])
                            held[idx] = (E, c0, wq)

                        def st2(idx):
                            kb, m = its[idx]
                            E, c0, wq = held.pop(idx)
                            mm(pu[m][:, c0:512], vt[:, kb, :], E[:, 0:wq], kb == 0, kb == nkb - 1, [vt, E], [pu[m]])
                            mm(pd[m][:, c0:512], onesb[:, :], E[:, 0:wq], kb == 0, kb == nkb - 1, [onesb, E],
                               [pd[m]])

                        SK = 4 if 'sk4' in flags else 2
                        for idx in range(len(its) + SK):
                            if idx < len(its):
                                st1(idx)
                            if idx >= SK:
                                st2(idx - SK)
                            if pend_fin and idx >= 2 and idx % 2 == 0:
                                pend_fin.pop(0)()
                        if 'da_nofin' in flags:
                            continue
                        while pend_fin:
                            pend_fin.pop(0)()
                        u0 = us_r.next()
                        cp(dve, u0[:, :], pu[0][:, :], [pu[0]], [u0])
                        u1 = us_r.next()
                        cp(dve, u1[:, :], pu[1][:, :], [pu[1]], [u1])
                        r0 = f_r.next()
                        actf(r0[:, :], pd[0][:, :], AF.Ln, [pd[0]], [r0])
                        r1 = f_r.next()
                        actf(r1[:, :], pd[1][:, :], AF.Ln, [pd[1]], [r1])

                        def mk(h=h, qg=qg, u0=u0, u1=u1, r0=r0, r1=r1):
                            box = {}

                            def s_a():
                                actf(r0[:, :], r0[:, :], AF.Exp, [r0], [r0], scale=-1.0)
                                actf(r1[:, :], r1[:, :], AF.Exp, [r1], [r1], scale=-1.0)

                            def s_b():
                                tt(dve, u0[:, :], u0[:, :], r0[:, :], ALU.mult, [u0, r0], [u0])
                                tt(dve, u1[:, :], u1[:, :], r1[:, :], ALU.mult, [u1, r1], [u1])
                                o32 = o32_r.next()
                                stt(o32[:, :], u1[:, :], neglam[:, 0:1], u0[:, :], ALU.mult, ALU.add,
                                    [u1, u0, neglam], [o32])
                                box["o"] = o32

                            def s_c():
                                sq = sq_r.next()
                                actf(sq[:, :], box["o"][:, :], AF.Square, [box["o"]], [sq])
                                mm(pss[:, :], onesb[:, :], sq[:, :], True, True, [onesb, sq], [pss])

                            def s_d():
                                rs = f_r.next()
                                actf(rs[:, :], pss[:, :], AF.Ln, [pss, epsc], [rs], scale=1.0 / 128.0,
                                     bias=epsc[:, 0:1])
                                box["rs"] = rs

                            def s_e():
                                rs = box["rs"]
                                actf(rs[:, :], rs[:, :], AF.Exp, [rs], [rs], scale=-0.5)
                                stt(H["hT"][:, h, qg * 512:(qg + 1) * 512], box["o"][:, :], gsub[:, 0:1], rs[:, :],
                                    ALU.mult, ALU.mult, [box["o"], gsub, rs], [H["hT"]])
                            return [s_a, s_b, s_c, s_d, s_e]
                        pend_fin.extend(mk())
                while pend_fin:
                    pend_fin.pop(0)()
                k.barrier()
            with ExitStack() as st:
                gtri = k.sb(st, "g_tri", [128, 128], F32)
                gm2 = k.sb(st, "g_m2", [128, 128], F32)
                gmaskb = k.sb(st, "g_mask", [128, 128], BF16)
                wgk = k.sb(st, "g_wgk", [16, 256], BF16)
                bgk = k.sb(st, "g_bgk", [1, 256], BF16)
                glag = k.sb(st, "g_lag", [128, 1], F32)
                grow = k.sb(st, "g_grow", [1, 128], F32)
                k.dma(k.q_sp, gtri[:, :], I["c_gtri"][:, :], writes=[gtri])
                k.dma(k.q_sp, gm2[:, :], I["c_gm2"][:, :], writes=[gm2])
                k.dma(k.q_pool, gmaskb[:, :], I["c_gmask"][:, :], writes=[gmaskb])
                k.dma(k.q_pool, wgk[:, :], I["even_w_gk2"][i_even], writes=[wgk])
                k.dma(k.q_pool, bgk[:, :], I["even_b_gk"][i_even:i_even + 1, :], writes=[bgk])
                k.dma(k.q_sp, grow[:, :], I["even_gla_norm"][i_even:i_even + 1, :], writes=[grow])
                pA = Ring([k.ps(st, "g_pA%d" % i, [128, 512], F32) for i in range(3)])
                pS = Ring([k.ps(st, "g_pS%d" % i, [128, 512], F32) for i in range(2)])
                pO = Ring([k.ps(st, "g_pO%d" % i, [128, 512], F32) for i in range(2)])
                p = pA.next()
                mm(p[:, 0:1], grow[0:1, :], ones32[0:1, 0:1], True, True, [grow, ones32], [p])
                cp(dve, glag[:, :], p[:, 0:1], [p], [glag])
                qg_r = Ring([k.sb(st, "g_q%d" % i, [128, 2, 512], BF16) for i in range(2)])
                kg_r = Ring([k.sb(st, "g_k%d" % i, [128, 2, 512], BF16) for i in range(2)])
                kt_r = Ring([k.sb(st, "g_kt%d" % i, [128, 4, 256], BF16) for i in range(2)])
                vg_r = Ring([k.sb(st, "g_v%d" % i, [128, 4, 512], BF16) for i in range(2)])
                bg_r = Ring([k.sb(st, "g_bg%d" % i, [16, 512], BF16) for i in range(2)])
                br_r = Ring([k.sb(st, "g_br%d" % i, [128, 4, 512], BF16) for i in range(2)])
                og_r = Ring([k.sb(st, "g_og%d" % i, [128, 4, 512], F32) for i in range(2)])
                e32_r = Ring([k.sb(st, "g_e%d" % i, [128, 256], F32) for i in range(2)])
                sp_r = Ring([k.sb(st, "g_sp%d" % i, [128, 256], F32) for i in range(2)])
                kf_r = Ring([k.sb(st, "g_kf%d" % i, [128, 256], F32) for i in range(2)])
                kd_r = Ring([k.sb(st, "g_kd%d" % i, [128, 256], BF16) for i in range(2)])
                eb_r = Ring([k.sb(st, "g_eb%d" % i, [128, 256], F32) for i in range(2)])
                en_r = Ring([k.sb(st, "g_en%d" % i, [128, 256], F32) for i in range(2)])
                qd_r = Ring([k.sb(st, "g_qd%d" % i, [128, 2, 128], BF16) for i in range(2)])
                ki_r = Ring([k.sb(st, "g_ki%d" % i, [128, 2, 128], BF16) for i in range(2)])
                at_r = Ring([k.sb(st, "g_at%d" % i, [128, 128], BF16) for i in range(3)])
                S32_r = [Ring([k.sb(st, "g_S32_%d_%d" % (h, i), [128, 128], F32) for i in range(2)]) for h in range(4)]
                Sb_r = [Ring([k.sb(st, "g_Sb_%d_%d" % (h, i), [128, 128], BF16) for i in range(3)]) for h in range(4)]
                sq_r = Ring([k.sb(st, "g_sq%d" % i, [128, 512], BF16) for i in range(2)])
                f_r = Ring([k.sb(st, "g_f%d" % i, [128, 512], F32) for i in range(4)])
                S32 = []
                Sb = []
                for h in range(4):
                    s = S32_r[h].next()
                    memset(dve, s[:, :], 0.0, [s])
                    S32.append(s)
                    b = Sb_r[h].next()
                    memset(pool, b[:, :], 0.0, [b])
                    Sb.append(b)
                for g8 in range(0 if 'ev_noB' in flags else 8):
                    t0g = g8 * 512
                    qgt = qg_r.next()
                    kgt = kg_r.next()
                    ktt = kt_r.next()
                    vgt = vg_r.next()
                    bgt = bg_r.next()
                    brt = br_r.next()
                    og = og_r.next()
                    for hh in range(2):
                        k.dma(k.q_sp, qgt[:, hh, :], FB[12 + hh, :, t0g:t0g + 512], reads=[fbres[12 + hh]], writes=[qgt])
                        k.dma(k.q_sp, kgt[:, hh, :], FB[14 + hh, :, t0g:t0g + 512], reads=[fbres[14 + hh]], writes=[kgt])
                    k.dma(k.q_sp, ktt[:, :, :],
                          TB[t0g:t0g + 512, 512:768].rearrange("(j p) c -> p j c", p=128), reads=[tbres], writes=[ktt])
                    k.dma(k.q_sp, vgt[:, :, :],
                          TB[t0g:t0g + 512, 768:1280].rearrange("(j p) c -> p j c", p=128), reads=[tbres], writes=[vgt])
                    k.dma(k.q_sp, bgt[:, :], FB[24, 0:16, t0g:t0g + 512], reads=[fbres[24]], writes=[bgt])
                    for h in range(4):
                        k.dma(k.q_sp, brt[:, h, :], FB[20 + h, :, t0g:t0g + 512], reads=[fbres[20 + h]], writes=[brt])
                    for j4 in range(4):
                        tc0 = j4 * 128
                        ppre = pA.next()
                        mm(ppre[:, 0:256], bgt[0:16, tc0:tc0 + 128], wgk[0:16, :], True, False, [bgt, wgk], [ppre])
                        mm(ppre[:, 0:256], onesb[0:1, :], bgk[0:1, :], False, True, [onesb, bgk], [ppre])
                        e32 = e32_r.next()
                        actf(e32[:, :], ppre[:, 0:256], AF.Exp, [ppre], [e32], scale=-1.0)
                        sp32 = sp_r.next()
                        actf(sp32[:, :], e32[:, :], AF.Ln, [e32], [sp32], bias=1.0)
                        pb = pA.next()
                        for hh in range(2):
                            mm(pb[:, hh * 128:(hh + 1) * 128], sp32[:, hh * 128:(hh + 1) * 128], gtri[:, :], True, True,
                               [sp32, gtri], [pb])
                        pk = pA.next()
                        mm(pk[:, 0:256], gm2[:, :], sp32[:, :], True, True, [gm2, sp32], [pk])
                        kf = kf_r.next()
                        actf(kf[:, :], pk[:, 0:256], AF.Exp, [pk], [kf])
                        kd = kd_r.next()
                        tt(dve, kd[:, :], ktt[:, j4, :], kf[:, :], ALU.mult, [ktt, kf], [kd])
                        eb = eb_r.next()
                        actf(eb[:, :], pb[:, 0:256], AF.Exp, [pb], [eb])
                        en = en_r.next()
                        actf(en[:, :], pb[:, 0:256], AF.Exp, [pb], [en], scale=-1.0)
                        qd = qd_r.next()
                        ki = ki_r.next()
                        for hh in range(2):
                            tt(dve, qd[:, hh, :], qgt[:, hh, tc0:tc0 + 128], eb[:, hh * 128:(hh + 1) * 128], ALU.mult,
                               [qgt, eb], [qd])
                            tt(dve, ki[:, hh, :], kgt[:, hh, tc0:tc0 + 128], en[:, hh * 128:(hh + 1) * 128], ALU.mult,
                               [kgt, en], [ki])
                        for h in range(4):
                            hh, jj = h // 2, h % 2
                            b0 = 64 * jj
                            psc = pS.next()
                            mm(psc[:, 0:128], ki[b0:b0 + 64, hh, :], qd[b0:b0 + 64, hh, :], True, True, [ki, qd], [psc])
                            at = at_r.next()
                            tt(dve, at[:, :], psc[:, 0:128], gmaskb[:, :], ALU.mult, [psc, gmaskb], [at])
                            po = pO.next()
                            mm(po[:, 0:128], vgt[:, j4, h * 128:(h + 1) * 128], at[:, :], True, False, [vgt, at], [po])
                            mm(po[:, 0:64], Sb[h][b0:b0 + 64, :], qd[b0:b0 + 64, hh, 0:64], False, False,
                               [Sb[h], qd], [po])
                            pst = pS.next()
                            mm(pst[:, 0:128], kd[0:64, hh * 128:(hh + 1) * 128], vgt[0:64, j4, h * 128:(h + 1) * 128],
                               True, True, [kd, vgt], [pst])
                            s1 = S32_r[h].next()
                            stt(s1[b0:b0 + 64, :], S32[h][b0:b0 + 64, :], eb[b0:b0 + 64, hh * 128 + 63:hh * 128 + 64],
                                pst[b0:b0 + 64, 0:128], ALU.mult, ALU.add, [S32[h], eb, pst], [s1])
                            sb1 = Sb_r[h].next()
                            actf(sb1[b0:b0 + 64, :], s1[b0:b0 + 64, :], AF.Copy, [s1], [sb1])
                            mm(po[:, 64:128], sb1[b0:b0 + 64, :], qd[b0:b0 + 64, hh, 64:128], False, True,
                               [sb1, qd], [po])
                            pst2 = pS.next()
                            mm(pst2[:, 0:128], kd[64:128, hh * 128:(hh + 1) * 128],
                               vgt[64:128, j4, h * 128:(h + 1) * 128], True, True, [kd, vgt], [pst2])
                            s2 = S32_r[h].next()
                            stt(s2[b0:b0 + 64, :], s1[b0:b0 + 64, :], eb[b0:b0 + 64, hh * 128 + 127:hh * 128 + 128],
                                pst2[b0:b0 + 64, 0:128], ALU.mult, ALU.add, [s1, eb, pst2], [s2])
                            sb2 = Sb_r[h].next()
                            actf(sb2[b0:b0 + 64, :], s2[b0:b0 + 64, :], AF.Copy, [s2], [sb2])
                            S32[h] = s2
                            Sb[h] = sb2
                            actf(og[:, h, tc0:tc0 + 128], po[:, 0:128], AF.Copy, [po], [og])
                    for h in range(4):
                        sq = sq_r.next()
                        actf(sq[:, :], og[:, h, :], AF.Square, [og], [sq])
                        pn = pA.next()
                        mm(pn[:, :], onesb[:, :], sq[:, :], True, True, [onesb, sq], [pn])
                        rs = f_r.next()
                        actf(rs[:, :], pn[:, :], AF.Ln, [pn, epsc], [rs], scale=1.0 / 128.0, bias=epsc[:, 0:1])
                        rs2 = f_r.next()
                        actf(rs2[:, :], rs[:, :], AF.Exp, [rs], [rs2], scale=-0.5)
                        sl = f_r.next()
                        actf(sl[:, :], brt[:, h, :], AF.Silu, [brt], [sl])
                        t1 = f_r.next()
                        stt(t1[:, :], og[:, h, :], glag[:, 0:1], rs2[:, :], ALU.mult, ALU.mult, [og, glag, rs2], [t1])
                        tt(dve, H["hT"][:, 4 + h, t0g:t0g + 512], t1[:, :], sl[:, :], ALU.mult, [t1, sl], [H["hT"]])
                k.barrier()

        def emit_moe(layer):
            with ExitStack() as st:
                wg_r = Ring([k.sb(st, "me_wg%d" % i, [128, 8, DEXP], BF16) for i in range(2)])
                wu_r = Ring([k.sb(st, "me_wu%d" % i, [128, 8, DEXP], BF16) for i in range(2)])
                wd_r = Ring([k.sb(st, "me_wd%d" % i, [128, 4, D], BF16) for i in range(2)])
                yacc = k.sb(st, "me_yacc", [128, 8, D], F32)
                aT_r = Ring([k.sb(st, "me_aT%d" % i, [128, 4, 512], BF16) for i in range(2)])
                sg_r = Ring([k.sb(st, "me_sg%d" % i, [128, 512], BF16) for i in range(3)])
                xt_r = Ring([k.sb(st, "me_xt%d" % i, [128, D], F32) for i in range(2)])
                pg_r = Ring([k.ps(st, "me_pg%d" % i, [128, 512], F32) for i in range(2)])
                pu_r = Ring([k.ps(st, "me_pu%d" % i, [128, 512], F32) for i in range(2)])
                py_r = Ring([k.ps(st, "me_py%d" % i, [128, D], F32) for i in range(2)])
                for qtr in range(4):
                    memset(dve, yacc[:, :, :], 0.0, [yacc])
                    jobs = [(e, tg) for e in range(NEXP) for tg in range(2)]
                    wts = {}

                    def load_w(e):
                        wg = wg_r.next()
                        wu = wu_r.next()
                        wd = wd_r.next()
                        k.dma(k.q_pool, wg[:, :, :],
                              I["expert_w_gate"][layer, e].rearrange("(kc p) f -> p kc f", p=128), writes=[wg])
                        k.dma(k.q_pool, wu[:, :, :],
                              I["expert_w_up"][layer, e].rearrange("(kc p) f -> p kc f", p=128), writes=[wu])
                        k.dma(k.q_pool, wd[:, :, :],
                              I["expert_w_down"][layer, e].rearrange("(fc p) n -> p fc n", p=128), writes=[wd])
                        wts[e] = (wg, wu, wd)

                    acts = {}

                    def emit_gu(e, tg):
                        wg, wu, wd = wts[e]
                        G = qtr * 2 + tg
                        aT = aT_r.next()
                        for fc in range(4):
                            pg = pg_r.next()
                            pu = pu_r.next()
                            for kc in range(8):
                                mm(pg[:, :], wg[:, kc, fc * 128:(fc + 1) * 128], H["hT"][:, kc, G * 512:(G + 1) * 512],
                                   kc == 0, kc == 7, [wg, H["hT"]], [pg])
                            for kc in range(8):
                                mm(pu[:, :], wu[:, kc, fc * 128:(fc + 1) * 128], H["hT"][:, kc, G * 512:(G + 1) * 512],
                                   kc == 0, kc == 7, [wu, H["hT"]], [pu])
                            sg = sg_r.next()
                            actf(sg[:, :], pg[:, :], AF.Silu, [pg], [sg])
                            tt(dve, aT[:, fc, :], sg[:, :], pu[:, :], ALU.mult, [sg, pu], [aT])
                        acts[(e, tg)] = aT

                    def emit_down(e, tg):
                        wg, wu, wd = wts[e]
                        aT = acts.pop((e, tg))
                        for t4 in range(4):
                            ti = tg * 4 + t4
                            T = qtr * 8 + ti
                            py = py_r.next()
                            for nh in range(2):
                                for fc in range(4):
                                    mm(py[:, nh * 512:(nh + 1) * 512], aT[:, fc, t4 * 128:(t4 + 1) * 128],
                                       wd[:, fc, nh * 512:(nh + 1) * 512], fc == 0, fc == 3, [aT, wd], [py])
                            stt(yacc[:, ti, :], py[:, :], cmb[:, T, e:e + 1], yacc[:, ti, :], ALU.mult, ALU.add,
                                [py, cmb, yacc], [yacc])

                    load_w(0)
                    for idx, (e, tg) in enumerate(jobs):
                        emit_gu(e, tg)
                        if idx > 0:
                            emit_down(*jobs[idx - 1])
                        if tg == 0 and e + 1 < NEXP:
                            load_w(e + 1)
                    emit_down(*jobs[-1])
                    for ti in range(8):
                        T = qtr * 8 + ti
                        xt = xt_r.next()
                        k.dma(k.q_sp, xt[:, :], XS[T * 128:(T + 1) * 128, :], reads=[xres[T]], writes=[xt])
                        tt(dve, yacc[:, ti, :], yacc[:, ti, :], gbc2[:, :], ALU.mult, [yacc, gbc2], [yacc])
                        tt(dve, xt[:, :], xt[:, :], yacc[:, ti, :], ALU.add, [xt, yacc], [xt])
                        k.dma(k.q_sp, XS[T * 128:(T + 1) * 128, :], xt[:, :], reads=[xt], writes=[xres[T]])
                k.barrier()

        def emit_slots(layer):
            with ExitStack() as st:
                Aall = k.sb(st, "sl_A", [128, NT, 32], BF16)
                ag = k.sb(st, "sl_ag", [128, NT, 8], F32)
                mltb = k.sb(st, "sl_mlt", [128, 128], BF16)
                pos = k.sb(st, "sl_pos", [128, NT, 32], F32)
                cnt = k.sb(st, "sl_cnt", [128, 32], F32)
                nb_ = k.sb(st, "sl_nb", [128, 32], F32)
                pa = k.sb(st, "sl_pa", [128, 32], F32)
                pb = k.sb(st, "sl_pb", [128, 32], F32)
                smg = k.sb(st, "sl_smg", [128, NT, 8], F32)
                t8 = k.sb(st, "sl_t8", [128, NT, 8], F32)
                sf = k.sb(st, "sl_sf", [128, 2, NT], F32)
                iog = k.sb(st, "sl_iog", [128, 8], F32)
                iob = k.sb(st, "sl_iob", [128, NBLK], F32)
                cmpt = k.sb(st, "sl_cmp", [128, NBLK, 32], F32)
                eb = k.sb(st, "sl_eb", [128, NBLK], F32)
                fg = k.sb(st, "sl_fg", [128, NBLK, 8], F32)
                ppos = k.ps(st, "sl_ppos", [128, NT * 32], F32)
                pcnt = k.ps(st, "sl_pcnt", [128, 512], F32)
                k.dma(k.q_pool, mltb[:, :], I["c_mlt"][:, :], writes=[mltb])
                k.dma(k.q_sp, iog[:, :], I["c_iog"][:, :], writes=[iog])
                k.dma(k.q_sp, iob[:, :], I["c_iob"][:, :], writes=[iob])
                RR = [Aall, ag, pos, cnt, nb_, pa, pb, smg, t8, sf, cmpt, eb, fg, r_ohg, r_oh1, r_oh2, r_v]

                def bc(ap2, n):
                    return ap2.unsqueeze(2).to_broadcast([128, NT, n])
                tt(dve, ag[:, :, :], r_oh1[:, :, :], r_oh2[:, :, :], ALU.add, RR, RR)
                for g in range(4):
                    tt(dve, Aall[:, :, 8 * g:8 * g + 8], ag[:, :, :], bc(r_ohg[:, :, g], 8), ALU.mult, RR, RR)
                for T in range(NT):
                    for T2 in range(T):
                        mm(ppos[:, T * 32:(T + 1) * 32], onesb[:, :], Aall[:, T2, :], T2 == 0, False,
                           [onesb, Aall], [ppos])
                    mm(ppos[:, T * 32:(T + 1) * 32], mltb[:, :], Aall[:, T, :], T == 0, True, [mltb, Aall], [ppos])
                for T in range(NT):
                    mm(pcnt[:, 0:32], onesb[:, :], Aall[:, T, :], T == 0, T == NT - 1, [onesb, Aall], [pcnt])
                cp(dve, pos[:, 0:16, :], ppos[:, 0:512].rearrange("p (t e) -> p t e", e=32), [ppos], RR)
                cp(dve, pos[:, 16:32, :], ppos[:, 512:1024].rearrange("p (t e) -> p t e", e=32), [ppos], RR)
                cp(dve, cnt[:, :], pcnt[:, 0:32], [pcnt], RR)
                ts(dve, nb_[:, :], cnt[:, :], 0.0, None, ALU.is_gt, None, RR, RR)
                for m in range(1, 8):
                    stt(nb_[:, :], cnt[:, :], float(BSZ * m), nb_[:, :], ALU.is_gt, ALU.add, RR, RR)
                cp(dve, pa[:, :], nb_[:, :], RR, RR)
                a_, b_ = pa, pb
                for sh in (1, 2, 4, 8, 16):
                    cp(dve, b_[:, 0:sh], a_[:, 0:sh], RR, RR)
                    tt(dve, b_[:, sh:32], a_[:, sh:32], a_[:, 0:32 - sh], ALU.add, RR, RR)
                    a_, b_ = b_, a_
                incl = a_
                excl = b_
                tt(dve, excl[:, :], incl[:, :], nb_[:, :], ALU.subtract, RR, RR)
                ts(dve, excl[:, :], excl[:, :], float(BSZ), None, ALU.mult, None, RR, RR)
                tt(dve, pos[:, :, :], pos[:, :, :], excl[:, :].unsqueeze(1).to_broadcast([128, NT, 32]), ALU.add,
                   RR, RR)
                for g in range(4):
                    if g == 0:
                        tt(dve, smg[:, :, :], pos[:, :, 0:8], bc(r_ohg[:, :, 0], 8), ALU.mult, RR, RR)
                    else:
                        tt(dve, t8[:, :, :], pos[:, :, 8 * g:8 * g + 8], bc(r_ohg[:, :, g], 8), ALU.mult, RR, RR)
                        tt(dve, smg[:, :, :], smg[:, :, :], t8[:, :, :], ALU.add, RR, RR)
                tt(dve, t8[:, :, :], smg[:, :, :], r_oh1[:, :, :], ALU.mult, RR, RR)
                k.op(dve, lambda: nc.vector.tensor_reduce(out=sf[:, 0, :], in_=t8[:, :, :], axis=AX.X, op=ALU.add),
                     RR, RR)
                tt(dve, t8[:, :, :], smg[:, :, :], r_oh2[:, :, :], ALU.mult, RR, RR)
                k.op(dve, lambda: nc.vector.tensor_reduce(out=sf[:, 1, :], in_=t8[:, :, :], axis=AX.X, op=ALU.add),
                     RR, RR)
                cp(dve, s12u[:, :, :], sf[:, :, :], RR, [s12u])
                tt(dve, cmpt[:, :, :], incl[:, :].unsqueeze(1).to_broadcast([128, NBLK, 32]),
                   iob[:, :].unsqueeze(2).to_broadcast([128, NBLK, 32]), ALU.is_le, RR + [iob], RR)
                k.op(dve, lambda: nc.vector.tensor_reduce(out=eb[:, :], in_=cmpt[:, :, :], axis=AX.X, op=ALU.add),
                     RR, RR)
                ts(dve, eb[:, :], eb[:, :], 31.0, None, ALU.min, None, RR, RR)
                ts(dve, eb[:, :], eb[:, :], 512.0, float(layer * NEXP * DEXP), ALU.mult, ALU.add, RR, RR)
                tt(dve, fg[:, :, 0:4], eb[:, :].unsqueeze(2).to_broadcast([128, NBLK, 4]),
                   iog[:, 0:4].unsqueeze(1).to_broadcast([128, NBLK, 4]), ALU.add, RR + [iog], RR)
                cp(dve, idxd[:, :, :], fg[:, :, 0:4], RR, [idxd])
                ts(dve, eb[:, :], eb[:, :], 2.0, None, ALU.mult, None, RR, RR)
                tt(dve, fg[:, :, :], eb[:, :].unsqueeze(2).to_broadcast([128, NBLK, 8]),
                   iog[:, :].unsqueeze(1).to_broadcast([128, NBLK, 8]), ALU.add, RR + [iog], RR)
                cp(dve, idxg[:, :, :], fg[:, :, :], RR, [idxg])
                io2 = k.sb(st, "sl_io2", [128, 2], F32)
                k.dma(k.q_sp, io2[:, :], I["c_io2"][:, :], writes=[io2])
                ts(dve, eb[:, :], eb[:, :], 0.25, None, ALU.mult, None, RR, RR)
                tt(dve, fg[:, :, 0:2], eb[:, :].unsqueeze(2).to_broadcast([128, NBLK, 2]),
                   io2[:, :].unsqueeze(1).to_broadcast([128, NBLK, 2]), ALU.add, RR + [io2], RR)
                cp(dve, idx2[:, :, :], fg[:, :, 0:2], RR, [idx2])
                k.dma(k.q_sp, MV[0].rearrange("(kc p) -> p kc", p=128), gmod2[:, :], reads=[gmod2], writes=[mvres],
                      allow_slow_non_contiguous=True)
                k.dma(k.q_sp, MV[1].rearrange("(kc p) -> p kc", p=128), modT[:, 24:32], reads=[modT], writes=[mvres],
                      allow_slow_non_contiguous=True)
                k.dma(k.q_sp, gsb[:, 0, :], MV[0].rearrange("(p kc) -> p kc", kc=8), reads=[mvres], writes=[gsb])
                k.dma(k.q_sp, gsb[:, 1, :], MV[1].rearrange("(p kc) -> p kc", kc=8), reads=[mvres], writes=[gsb])
                xr_r = Ring([k.sb(st, "sl_xr%d" % i, [128, D], F32) for i in range(3)])
                for T in range(NT):
                    xr = xr_r.next()
                    k.dma(k.q_sp, xr[:, :], XN[T * 128:(T + 1) * 128, :], reads=[xnres], writes=[xr])
                    for kk in range(2):
                        idma(XG[:, :], bass.IndirectOffsetOnAxis(ap=s12u[:, kk, T:T + 1], axis=0), xr[:, :], None,
                             [xr, s12u], [xgres])
                k.barrier()

        def emit_moe_sparse(layer):
            with ExitStack() as st:
                wg_r = Ring([k.sb(st, "ms_wg%d" % i, [128, 8, DEXP], BF16) for i in range(3)])
                wu_r = Ring([k.sb(st, "ms_wu%d" % i, [128, 8, DEXP], BF16) for i in range(3)])
                wd_r = Ring([k.sb(st, "ms_wd%d" % i, [128, 4, D], BF16) for i in range(3)])
                xg_r = Ring([k.sb(st, "ms_xg%d" % i, [128, D], F32) for i in range(8)])
                hs_r = Ring([k.sb(st, "ms_hs%d" % i, [128, 8, 512], BF16) for i in range(2)])
                aT_r = Ring([k.sb(st, "ms_aT%d" % i, [128, 4, 512], BF16) for i in range(2)])
                sg_r = Ring([k.sb(st, "ms_sg%d" % i, [128, 512], BF16) for i in range(3)])
                yo_r = Ring([k.sb(st, "ms_yo%d" % i, [128, D], F32) for i in range(3)])
                ptr_r = Ring([k.ps(st, "ms_ptr%d" % i, [128, 512], F32) for i in range(2)])
                pg_r = Ring([k.ps(st, "ms_pg%d" % i, [128, 512], F32) for i in range(2)])
                pu_r = Ring([k.ps(st, "ms_pu%d" % i, [128, 512], F32) for i in range(2)])
                py_r = Ring([k.ps(st, "ms_py%d" % i, [128, 512], F32) for i in range(2)])
                wgv = I["expert_w_gate"].rearrange("l e (p q) f -> (l e p) (q f)", q=8).rearrange(
                    "r (h c) -> (r h) c", h=2)
                wuv = I["expert_w_up"].rearrange("l e (p q) f -> (l e p) (q f)", q=8).rearrange(
                    "r (h c) -> (r h) c", h=2)
                wdv = I["expert_w_down"].rearrange("l e (p q) n -> (l e p) (q n)", q=4).rearrange(
                    "r (h c) -> (r h) c", h=2)
                wts = {}
                hss = {}
                hss_keep = {}
                acts = {}

                def load_blk(b):
                    if 'ms_nowl' in flags and b >= 3:
                        wts[b] = wts[b - 3]
                        return
                    wg = wg_r.next()
                    wu = wu_r.next()
                    wd = wd_r.next()
                    for h in range(2):
                        off = bass.IndirectOffsetOnAxis(ap=idx2[:, b, h:h + 1], axis=0)
                        idma(wg[:, 4 * h:4 * h + 4, :].rearrange("p a b -> p (a b)"), None, wgv, off, [idx2], [wg])
                        idma(wu[:, 4 * h:4 * h + 4, :].rearrange("p a b -> p (a b)"), None, wuv, off, [idx2], [wu])
                        idma(wd[:, 2 * h:2 * h + 2, :].rearrange("p a b -> p (a b)"), None, wdv, off, [idx2], [wd])
                    wts[b] = (wg, wu, wd)

                xgs = {}

                def load_x(b):
                    tl = []
                    for j in range(4):
                        xg = xg_r.next()
                        r0 = b * BSZ + j * 128
                        k.dma(k.q_sp, xg[:, :], XG[r0:r0 + 128, :], reads=[xgres], writes=[xg])
                        tl.append(xg)
                    xgs[b] = tl

                def prep_h(b):
                    hs = hs_r.next()
                    tl = xgs.pop(b)
                    for j in range(4):
                        xg = tl[j]
                        for hf in range(2):
                            ptr = ptr_r.next()
                            for q in range(4):
                                kc = hf * 4 + q
                                tr(ptr[:, q * 128:(q + 1) * 128], xg[:, kc::8], ident32[:, :], [xg, ident32], [ptr])
                            for q in range(4):
                                kc = hf * 4 + q
                                actf(hs[:, kc, j * 128:(j + 1) * 128], ptr[:, q * 128:(q + 1) * 128], AF.Identity,
                                     [ptr, gsb], [hs], scale=gsb[:, 0, kc:kc + 1], bias=gsb[:, 1, kc:kc + 1])
                    hss[b] = hs

                def emit_gu(b):
                    wg, wu, wd = wts[b]
                    hs = hss.pop(b)
                    aT = aT_r.next()
                    for fc in range(4):
                        pg = pg_r.next()
                        pu = pu_r.next()
                        for kc in range(8):
                            mm(pg[:, :], wg[:, kc, fc::4], hs[:, kc, :], kc == 0, kc == 7,
                               [wg, hs], [pg])
                        for kc in range(8):
                            mm(pu[:, :], wu[:, kc, fc::4], hs[:, kc, :], kc == 0, kc == 7,
                               [wu, hs], [pu])
                        sg = sg_r.next()
                        actf(sg[:, :], pg[:, :], AF.Silu, [pg], [sg])
                        tt(dve, aT[:, fc, :], sg[:, :], pu[:, :], ALU.mult, [sg, pu], [aT])
                    acts[b] = aT

                def emit_down(b):
                    wg, wu, wd = wts[b]
                    aT = acts.pop(b)
                    for j in range(4):
                        yo = yo_r.next()
                        for nh in range(2):
                            py = py_r.next()
                            for fc in range(4):
                                mm(py[:, :], aT[:, fc, j * 128:(j + 1) * 128],
                                   wd[:, fc, nh * 512:(nh + 1) * 512], fc == 0, fc == 3, [aT, wd], [py])
                            cp(dve, yo[:, nh * 512:(nh + 1) * 512], py[:, :], [py], [yo])
                        r0 = b * BSZ + j * 128
                        k.dma(k.q_sp, YS[r0:r0 + 128, :], yo[:, :], reads=[yo], writes=[ysres[b]])

                load_blk(0)
                load_blk(1)
                load_x(0)
                prep_h(0)
                load_x(1)
                for b in range(NBLK):
                    emit_gu(b)
                    if b > 0:
                        emit_down(b - 1)
                    if b + 2 < NBLK:
                        load_blk(b + 2)
                    if b + 1 < NBLK:
                        prep_h(b + 1)
                    if b + 2 < NBLK:
                        load_x(b + 2)
                emit_down(NBLK - 1)
                k.barrier()
            with ExitStack() as st:
                r1_r = Ring([k.sb(st, "ms_r1%d" % i, [128, D], F32) for i in range(2)])
                r2_r = Ring([k.sb(st, "ms_r2%d" % i, [128, D], F32) for i in range(2)])
                xt_r = Ring([k.sb(st, "ms_xt%d" % i, [128, D], F32) for i in range(2)])
                for T in range(NT):
                    r1 = r1_r.next()
                    r2 = r2_r.next()
                    idma(r1[:, :], None, YS[:, :], bass.IndirectOffsetOnAxis(ap=s12u[:, 0, T:T + 1], axis=0),
                         ysres + [s12u], [r1])
                    idma(r2[:, :], None, YS[:, :], bass.IndirectOffsetOnAxis(ap=s12u[:, 1, T:T + 1], axis=0),
                         ysres + [s12u], [r2])
                    xt = xt_r.next()
                    k.dma(k.q_sp, xt[:, :], XS[T * 128:(T + 1) * 128, :], reads=[xres[T]], writes=[xt])
                    ts(dve, r1[:, :], r1[:, :], r_v[:, 9, T:T + 1], None, ALU.mult, None, [r1, r_v], [r1])
                    stt(r1[:, :], r2[:, :], r_v[:, 10, T:T + 1], r1[:, :], ALU.mult, ALU.add, [r2, r_v, r1], [r1])
                    tt(dve, r1[:, :], r1[:, :], gbc2[:, :], ALU.mult, [r1, gbc2], [r1])
                    tt(dve, xt[:, :], xt[:, :], r1[:, :], ALU.add, [xt, r1], [xt])
                    k.dma(k.q_sp, XS[T * 128:(T + 1) * 128, :], xt[:, :], reads=[xt], writes=[xres[T]])
                k.barrier()

        def emit_final(do_norm):
            with ExitStack() as st:
                xt_r = Ring([k.sb(st, "f_xt%d" % i, [128, D], F32) for i in range(3)])
                xo_r = Ring([k.sb(st, "f_xo%d" % i, [128, D], F32) for i in range(2)])
                junk = k.sb(st, "f_junk", [128, D], BF16)
                sm_r = Ring([k.sb(st, "f_sm%d" % i, [128, 4], F32) for i in range(4)])
                gf = k.sb(st, "f_gf", [128, D], F32)
                k.dma(k.q_sp, gf[:, :], I["norm_final"][0].partition_broadcast(128), writes=[gf])
                evs = []
                for t in range(NT):
                    xt = xt_r.next()
                    k.dma(k.q_sp, xt[:, :], XS[t * 128:(t + 1) * 128, :], reads=[xres[t]], writes=[xt])
                    if do_norm:
                        sm = sm_r.next()
                        actf(junk[:, :], xt[:, :], AF.Square, [xt], [junk, sm], accum_out=sm[:, 0:1])
                        ts(dve, sm[:, 1:2], sm[:, 0:1], 1.0 / D, EPS, ALU.mult, ALU.add, [sm], [sm])
                        actf(sm[:, 2:3], sm[:, 1:2], AF.Sqrt, [sm], [sm])
                        recip(sm[:, 3:4], sm[:, 2:3], [sm], [sm])
                        xo = xo_r.next()
                        stt(xo[:, :], xt[:, :], sm[:, 3:4], gf[:, :], ALU.mult, ALU.mult, [xt, sm, gf], [xo])
                        src = xo
                    else:
                        src = xt
                    evs.append(k.dma(k.q_sp, OUT[t * 128:(t + 1) * 128, :], src[:, :], reads=[src]))
                for ev in evs:
                    sp.wait(ev)

        xsrc = I["x"]
        done = False
        if stop == ("evenonly",):
            H["hT"] = k.sb(gst, "hT", [128, 8, S], BF16)
            emit_even_mixer(0, 0)
            emit_final(False)
            done = True
            nlayers = 0
        if stop == ("oddonly",):
            H["hT"] = k.sb(gst, "hT", [128, 8, S], BF16)
            emit_odd_mixer()
            emit_final(False)
            done = True
            nlayers = 0
        for l in range(nlayers):
            emit_mod(l)
            lst = ExitStack()
            H["hT"] = k.sb(lst, "hT", [128, 8, S], BF16)
            emit_norm(xsrc, gmod1, modT[:, 0:8], l, router=False)
            ie = l // 2
            if 'skipmix' in flags:
                pass
            elif l % 2 == 0:
                fj = [(c * 128, 128, c, 0.125) for c in range(4)]
                fj += [(512 + c * 128, 128, 4 + c, 1.0) for c in range(4)]
                fj += [(1536 + c * 128, 128, 12 + c, 0.125) for c in range(2)]
                fj += [(1792 + c * 128, 128, 14 + c, 1.0) for c in range(2)]
                fj += [(2560 + c * 128, 128, 20 + c, 1.0) for c in range(4)]
                fj += [(3072, 16, 24, 1.0)]
                tj = [(1024, 512, 0), (1792, 256, 512), (2048, 512, 768)]
                emit_inproj(I["even_w_in"][ie], EVEN_COLS, fj, tj)
                emit_even_mixer(ie, l)
                emit_outproj(I["even_w_out"][ie], gbc1, xsrc)
            else:
                fj = [(c * 128, 128, c, 0.125) for c in range(8)]
                fj += [(1024 + c * 128, 128, 8 + c, 1.0) for c in range(8)]
                tj = [(2048, 512, 0), (2560, 512, 512)]
                emit_inproj(I["odd_w_in"][ie], 3072, fj, tj)
                emit_odd_mixer()
                emit_outproj(I["odd_w_out"][ie], gbc1, xsrc)
            if 'skipmix' not in flags:
                xsrc = XS
            if SPARSE:
                lst.close()
                H.pop("hT")
            if stop == ("mix", l):
                emit_final(False)
                done = True
                break
            emit_norm(xsrc, gmod2, modT[:, 24:32], l, router=True)
            if 'nomoe' not in flags:
                if SPARSE:
                    emit_slots(layer=l)
                    emit_moe_sparse(l)
                else:
                    emit_moe(l)
            if not SPARSE:
                lst.close()
            if stop == ("ffn", l):
                emit_final(False)
                done = True
                break
        if not done:
            emit_final(True)
        build.stats = (k.n_inst, k.nsem)
    return nc


_CACHE = {}


def _get_nc(nlayers=DEPTH, stop=None):
    key = (nlayers, stop)
    if key not in _CACHE:
        _CACHE[key] = build(nlayers, stop)
    return _CACHE[key]


def make_in_maps(inputs, cores):
    consts = make_consts()
    shared = {}
    for nm in WEIGHT_NAMES:
        a = np.ascontiguousarray(np.asarray(inputs[nm], dtype=np.float32))
        shared[nm] = a.reshape(WEIGHT_SHAPES[nm])
    shared.update(consts)
    x = np.asarray(inputs["x"], dtype=np.float32)
    c = np.asarray(inputs["c"], dtype=np.float32)
    maps = []
    for b in cores:
        m = dict(shared)
        m["x"] = np.ascontiguousarray(x[b])
        m["c"] = np.ascontiguousarray(c[b:b + 1])
        maps.append(m)
    return maps


def kernel(**inputs):
    nc = _get_nc()
    in_maps = make_in_maps(inputs, list(range(8)))
    res = run_bass_kernel_spmd(nc, in_maps, core_ids=list(range(8)))
    return np.stack([np.asarray(r["out"]) for r in res.results], axis=0).astype(np.float32)
```

```python
import math
from contextlib import ExitStack
import numpy as np
import concourse.bass as bass
import concourse.mybir as mybir
from concourse.bass_utils import run_bass_kernel_spmd

F32 = mybir.dt.float32
BF16 = mybir.dt.bfloat16
AF = mybir.ActivationFunctionType
ALU = mybir.AluOpType
AX = mybir.AxisListType

S = 4096
D = 1024
NT = 32
DEPTH = 4
EPS = 1e-6
EVEN_COLS = 3088
NEXP = 32
DEXP = 512
EPOCH = 12000


class Res:
    __slots__ = ("w", "r", "name")

    def __init__(self, name=""):
        self.w = None
        self.r = []
        self.name = name


class Tile:
    def __init__(self, ap, name=""):
        self.t = ap
        self.res = Res(name)

    def __getitem__(self, idx):
        return self.t[idx]


class Eng:
    def __init__(self, k, name, h, is_pe=False):
        self.k = k
        self.name = name
        self.h = h
        self.is_pe = is_pe
        self.sem = k.newsem(name + "_s0")
        self.count = 0
        self.nep = 0
        self.seen = {}
        self.hist = [(self.sem, 0)]

    def tick(self, inst):
        if self.count >= EPOCH:
            self.nep += 1
            self.sem = self.k.newsem("%s_s%d" % (self.name, self.nep))
            self.count = 0
        self.count += 1
        inst.then_inc(self.sem, 1)
        return (self.sem, self.count, self.name)

    def cur(self):
        return (self.sem, self.count, self.name)

    def wait(self, ev):
        sem, val, _ = ev
        if val <= 0:
            return
        key = id(sem)
        if self.seen.get(key, 0) >= val:
            return
        self.h.wait_ge(sem, val)
        self.seen[key] = val


class DmaQ:
    def __init__(self, k, name, eng, nslots=8):
        self.k = k
        self.eng = eng
        self.name = name
        self.sems = [k.newsem("%s_d%d" % (name, i)) for i in range(nslots)]
        self.vals = [0] * nslots
        self.i = 0

    def issue(self, emit, deps):
        s = self.i % len(self.sems)
        self.i += 1
        sem = self.sems[s]
        if self.vals[s] > 0:
            self.eng.wait((sem, self.vals[s], "dma"))
        for ev in deps:
            self.eng.wait(ev)
        if self.vals[s] >= 16 * 1500:
            sem = self.k.newsem("%s_d%d_%d" % (self.name, s, self.i))
            self.sems[s] = sem
            self.vals[s] = 0
        inst = emit()
        self.vals[s] += 16
        inst.then_inc(sem, 16)
        return (sem, self.vals[s], "dma")


class K:
    def __init__(self, nc, stack):
        self.nc = nc
        self.stack = stack
        self.nsem = 0
        self.pe = Eng(self, "pe", nc.tensor, is_pe=True)
        self.act = Eng(self, "act", nc.scalar)
        self.dve = Eng(self, "dve", nc.vector)
        self.pool = Eng(self, "pool", nc.gpsimd)
        self.sp = Eng(self, "sp", nc.sync)
        self.engs = [self.pe, self.act, self.dve, self.pool, self.sp]
        self.q_sp = DmaQ(self, "qsp", self.sp, 12)
        self.q_pool = DmaQ(self, "qpool", self.pool, 8)
        self.qs = [self.q_sp, self.q_pool]
        self.n_inst = 0

    def newsem(self, name):
        self.nsem += 1
        return self.stack.enter_context(self.nc.semaphore(name))

    def sb(self, st, name, shape, dtype):
        self.uid = getattr(self, "uid", 0) + 1
        name = "%s_u%d" % (name, self.uid)
        t = st.enter_context(self.nc.sbuf_tensor(name, list(shape), dtype))
        return Tile(t, name)

    def ps(self, st, name, shape, dtype=F32):
        self.uid = getattr(self, "uid", 0) + 1
        name = "%s_u%d" % (name, self.uid)
        t = st.enter_context(self.nc.psum_tensor(name, list(shape), dtype))
        return Tile(t, name)

    @staticmethod
    def _resof(x):
        return x.res if isinstance(x, Tile) else x

    def _deps(self, reads, writes):
        deps = []
        for r in reads:
            r = self._resof(r)
            if r.w is not None:
                deps.append(r.w)
        for w in writes:
            w = self._resof(w)
            if w.w is not None:
                deps.append(w.w)
            deps.extend(w.r)
        return deps

    def _commit(self, ev, reads, writes):
        for r in reads:
            r = self._resof(r)
            r.r = [e for e in r.r if e[0] is not ev[0]]
            r.r.append(ev)
        for w in writes:
            w = self._resof(w)
            w.w = ev
            w.r = []

    def op(self, eng, emit, reads=(), writes=()):
        for ev in self._deps(reads, writes):
            if eng.is_pe and ev[2] == "pe":
                continue
            eng.wait(ev)
        inst = emit()
        ev = eng.tick(inst)
        self._commit(ev, reads, writes)
        self.n_inst += 1
        return ev

    def dma(self, q, out, in_, reads=(), writes=(), **kw):
        deps = self._deps(reads, writes)
        ev = q.issue(lambda: q.eng.h.dma_start(out=out, in_=in_, **kw), deps)
        self._commit(ev, reads, writes)
        self.n_inst += 1
        return ev

    def barrier(self):
        evs = [e.cur() for e in self.engs]
        for q in self.qs:
            for s, v in zip(q.sems, q.vals):
                evs.append((s, v, "dma"))
        for e in self.engs:
            for ev in evs:
                if ev[0] is e.sem:
                    continue
                e.wait(ev)


class Ring:
    def __init__(self, tiles):
        self.tiles = tiles
        self.i = 0

    def next(self):
        t = self.tiles[self.i % len(self.tiles)]
        self.i += 1
        return t


def _t5_bucket_np(rel):
    nb = 16
    max_exact = 8
    ret = np.where(rel > 0, nb, 0)
    n = np.abs(rel)
    nf = np.maximum(n, 1).astype(np.float32)
    large = max_exact + (np.log(nf / max_exact) / np.float32(math.log(128 / max_exact))
                         * (nb - max_exact)).astype(np.int32)
    large = np.minimum(large, nb - 1)
    return ret + np.where(n < max_exact, n, large)


DELTAS = [-128, 0, 128, 256, 384]
NBLK = 48
BSZ = 512
NSLOT = NBLK * BSZ
R0 = 511
NF = 1280


def make_consts():
    c = {}
    c["c_ident"] = np.eye(128, dtype=np.float32)
    c["c_exch"] = np.ascontiguousarray(np.eye(128, dtype=np.float32)[::-1])
    i = np.arange(128)[:, None]
    j = np.arange(128)[None, :]
    c["c_mlt"] = (i < j).astype(np.float32)
    c["c_uneg"] = -(i >= j).astype(np.float32)
    same = (i // 64) == (j // 64)
    c["c_gtri"] = (-(1.0 / 16.0) * (same & (i <= j))).astype(np.float32)
    c["c_gm2"] = (-(1.0 / 16.0) * (same & (i > j))).astype(np.float32)
    c["c_gmask"] = (same & (i <= j)).astype(np.float32)
    n = np.arange(NF)
    bk = _t5_bucket_np(R0 - n)
    oh = np.zeros((32, NF), np.float32)
    oh[bk, n] = 1.0
    oh[:, 1151:] = 0.0
    c["c_ohb"] = oh
    nm = np.zeros((5, 128, 512), np.float32)
    for di, dl in enumerate(DELTAS):
        kk = dl + np.arange(128)[:, None]
        qq = np.arange(512)[None, :]
        allowed = (kk // 64) <= (qq // 64)
        nm[di] = np.where(allowed, 0.0, -30000.0)
    c["c_negmask"] = nm
    p = np.arange(128, dtype=np.float32)[:, None]
    c["c_iog"] = (p + 128.0 * np.arange(8, dtype=np.float32)[None, :]).astype(np.float32)
    c["c_io2"] = (2.0 * p + np.arange(2, dtype=np.float32)[None, :]).astype(np.float32)
    c["c_iob"] = np.tile(np.arange(NBLK, dtype=np.float32)[None, :], (128, 1))
    return c


WEIGHT_NAMES = ["w_ada", "b_ada", "norm_mix", "norm_ffn", "norm_final", "rel_bias",
                "even_w_in", "even_lambda", "even_subln", "even_w_gk2", "even_b_gk", "even_gla_norm",
                "even_w_out", "odd_w_in", "odd_w_out", "router_group_w", "router_group_b",
                "router_expert_w", "router_expert_b", "expert_w_gate", "expert_w_up", "expert_w_down"]
WEIGHT_SHAPES = {
    "w_ada": [4, 1024, 6144], "b_ada": [4, 6144], "norm_mix": [4, 1024], "norm_ffn": [4, 1024],
    "norm_final": [1, 1024], "rel_bias": [32, 4], "even_w_in": [2, 1024, 3088], "even_lambda": [2, 256],
    "even_subln": [2, 128], "even_w_gk2": [2, 16, 256], "even_b_gk": [2, 256], "even_gla_norm": [2, 128],
    "even_w_out": [2, 1024, 1024], "odd_w_in": [2, 1024, 3072], "odd_w_out": [2, 1024, 1024],
    "router_group_w": [4, 1024, 4], "router_group_b": [4, 4], "router_expert_w": [4, 1024, 32],
    "router_expert_b": [4, 32], "expert_w_gate": [4, 32, 1024, 512], "expert_w_up": [4, 32, 1024, 512],
    "expert_w_down": [4, 32, 512, 1024],
}


def build(nlayers=DEPTH, stop=None, flags=()):
    SPARSE = 'dense' not in flags
    nc = bass.Bass("TRN2", target_bir_lowering=False)
    I = {}
    I["x"] = nc.dram_tensor("x", [S, D], F32, kind="ExternalInput").ap()
    I["c"] = nc.dram_tensor("c", [1, D], F32, kind="ExternalInput").ap()
    for nm in WEIGHT_NAMES:
        I[nm] = nc.dram_tensor(nm, WEIGHT_SHAPES[nm], F32, kind="ExternalInput").ap()
    consts = make_consts()
    for nm, v in consts.items():
        I[nm] = nc.dram_tensor(nm, list(v.shape), F32, kind="ExternalInput").ap()
    OUT = nc.dram_tensor("out", [S, D], F32, kind="ExternalOutput").ap()
    XS = nc.dram_tensor("xs_scr", [S, D], F32, kind="Internal").ap()
    FB = nc.dram_tensor("fb_scr", [25, 128, S], BF16, kind="Internal").ap()
    TB = nc.dram_tensor("tb_scr", [S, 1280], BF16, kind="Internal").ap()
    FS_T = nc.dram_tensor("fs_scr", [4, NF], F32, kind="Internal")
    FS = FS_T.ap()
    XN = nc.dram_tensor("xn_scr", [S, D], F32, kind="Internal").ap()
    MV = nc.dram_tensor("mv_scr", [2, D], F32, kind="Internal").ap()
    XG = nc.dram_tensor("xg_scr", [NSLOT, D], F32, kind="Internal").ap()
    YS = nc.dram_tensor("ys_scr", [NSLOT, D], F32, kind="Internal").ap()

    with ExitStack() as gst:
        k = K(nc, gst)
        pe, act, dve, pool, sp = k.pe, k.act, k.dve, k.pool, k.sp

        def mm(out, lhsT, rhs, start, stop, reads, writes):
            k.op(pe, lambda: nc.tensor.matmul(out, lhsT=lhsT, rhs=rhs, start=start, stop=stop), reads, writes)

        def tr(out, in_, ident, reads, writes):
            k.op(pe, lambda: nc.tensor.transpose(out=out, in_=in_, identity=ident), reads, writes)

        def actf(out, in_, func, reads, writes, **kw):
            k.op(act, lambda: nc.scalar.activation(out=out, in_=in_, func=func, **kw), reads, writes)

        def tt(eng, out, in0, in1, op, reads, writes):
            k.op(eng, lambda: eng.h.tensor_tensor(out=out, in0=in0, in1=in1, op=op), reads, writes)

        def ts(eng, out, in0, s1, s2, op0, op1, reads, writes):
            if op1 is None:
                k.op(eng, lambda: eng.h.tensor_scalar(out=out, in0=in0, scalar1=s1, scalar2=None, op0=op0),
                     reads, writes)
            else:
                k.op(eng, lambda: eng.h.tensor_scalar(out=out, in0=in0, scalar1=s1, scalar2=s2, op0=op0, op1=op1),
                     reads, writes)

        def stt(out, in0, scalar, in1, op0, op1, reads, writes):
            k.op(dve, lambda: nc.vector.scalar_tensor_tensor(out=out, in0=in0, scalar=scalar, in1=in1,
                                                             op0=op0, op1=op1), reads, writes)

        def cp(eng, out, in_, reads, writes):
            k.op(eng, lambda: eng.h.tensor_copy(out=out, in_=in_), reads, writes)

        def recip(out, in_, reads, writes):
            k.op(dve, lambda: nc.vector.reciprocal(out=out, in_=in_), reads, writes)

        def memset(eng, t, val, writes):
            k.op(eng, lambda: eng.h.memset(t, val), (), writes)

        def rmax(out, in_, reads, writes):
            k.op(dve, lambda: nc.vector.tensor_reduce(out=out, in_=in_, axis=AX.X, op=ALU.max), reads, writes)

        xres = [Res("x%d" % i) for i in range(NT)]
        fbres = [Res("fb%d" % i) for i in range(25)]
        tbres = Res("tb")
        fsres = Res("fs")

        ident32 = k.sb(gst, "ident32", [128, 128], F32)
        ones32 = k.sb(gst, "ones32", [128, 128], F32)
        onesb = k.sb(gst, "onesb", [128, 128], BF16)
        negonesb = k.sb(gst, "negonesb", [128, 128], BF16)
        zerob = k.sb(gst, "zerob", [128, 128], BF16)
        identb = k.sb(gst, "identb", [128, 128], BF16)
        k.dma(k.q_sp, ident32[:, :], I["c_ident"][:, :], writes=[ident32])
        memset(dve, ones32[:, :], 1.0, [ones32])
        memset(dve, onesb[:, :], 1.0, [onesb])
        memset(dve, negonesb[:, :], -1.0, [negonesb])
        memset(dve, zerob[:, :], 0.0, [zerob])
        epsc = k.sb(gst, "epsc", [128, 1], F32)
        memset(dve, epsc[:, :], EPS, [epsc])
        cp(dve, identb[:, :], ident32[:, :], [ident32], [identb])

        modT = k.sb(gst, "modT", [128, 64], F32)
        gmod1 = k.sb(gst, "gmod1", [128, 8], F32)
        gmod2 = k.sb(gst, "gmod2", [128, 8], F32)
        gbc1 = k.sb(gst, "gbc1", [128, D], F32)
        gbc2 = k.sb(gst, "gbc2", [128, D], F32)
        scT = k.sb(gst, "scT", [128, 8], F32)
        cmb = k.sb(gst, "cmb", [128, NT, NEXP], F32)
        H = {}
        r_ohg = k.sb(gst, "r_ohg", [128, NT, 4], F32)
        r_oh1 = k.sb(gst, "r_oh1", [128, NT, 8], F32)
        r_oh2 = k.sb(gst, "r_oh2", [128, NT, 8], F32)
        r_v = k.sb(gst, "r_v", [128, 12, NT], F32)
        s12u = k.sb(gst, "s12u", [128, 2, NT], mybir.dt.uint32)
        idxg = k.sb(gst, "idxg", [128, NBLK, 8], mybir.dt.uint32)
        idxd = k.sb(gst, "idxd", [128, NBLK, 4], mybir.dt.uint32)
        idx2 = k.sb(gst, "idx2", [128, NBLK, 2], mybir.dt.uint32)
        gsb = k.sb(gst, "gsb", [128, 2, 8], F32)
        mvres = Res("mv")
        xnres = Res("xn")
        xgres = Res("xg")
        ysres = [Res("ys%d" % i) for i in range(NBLK)]

        def idma(out, out_off, in_, in_off, reads, writes):
            deps = k._deps(reads, writes)
            ev = k.q_pool.issue(lambda: nc.gpsimd.indirect_dma_start(out=out, out_offset=out_off, in_=in_,
                                                                     in_offset=in_off), deps)
            k._commit(ev, reads, writes)
            k.n_inst += 1
            return ev

        def emit_mod(l):
            with ExitStack() as st:
                crow = k.sb(st, "crow", [1, D], F32)
                sc_bc = k.sb(st, "sc_bc", [128, 8, 128], F32)
                wseg = Ring([k.sb(st, "wseg%d" % i, [128, 8, 512], F32) for i in range(2)])
                brow = Ring([k.sb(st, "brow%d" % i, [1, 512], F32) for i in range(2)])
                nrow = k.sb(st, "nrow", [1, 2 * D], F32)
                pm = k.ps(st, "pm", [128, 512], F32)
                pg = Ring([k.ps(st, "pg%d" % i, [128, 512], F32) for i in range(2)])
                k.dma(k.q_sp, crow[:, :], I["c"][:, :], writes=[crow])
                for kc in range(8):
                    mm(pm[:, kc:kc + 1], crow[0:1, kc * 128:(kc + 1) * 128], ones32[0:1, 0:1], True, True,
                       [crow, ones32], [pm])
                actf(scT[:, :], pm[:, 0:8], AF.Silu, [pm], [scT])
                for kc in range(8):
                    actf(sc_bc[:, kc, :], ones32[:, :], AF.Copy, [ones32, scT], [sc_bc], scale=scT[:, kc:kc + 1])
                wv = I["w_ada"][l].rearrange("(kc p) n -> p kc n", p=128)
                for seg in range(6):
                    for half in range(2):
                        n0 = seg * 1024 + half * 512
                        w = wseg.next()
                        b = brow.next()
                        k.dma(k.q_sp, w[:, :, :], wv[:, :, n0:n0 + 512], writes=[w])
                        k.dma(k.q_sp, b[:, :], I["b_ada"][l:l + 1, n0:n0 + 512], writes=[b])
                        if seg in (2, 5):
                            p = pg.next()
                            for kc in range(8):
                                mm(p[:, :], sc_bc[:, kc, :], w[:, kc, :], kc == 0, False, [sc_bc, w], [p])
                            mm(p[:, :], ones32[0:1, :], b[0:1, :], False, True, [ones32, b], [p])
                            g = gbc1 if seg == 2 else gbc2
                            cp(dve, g[:, half * 512:(half + 1) * 512], p[:, :], [p], [g])
                        else:
                            for j in range(4):
                                col = seg * 8 + half * 4 + j
                                for kc in range(8):
                                    mm(pm[:, col:col + 1], w[:, kc, j * 128:(j + 1) * 128], scT[:, kc:kc + 1],
                                       kc == 0, False, [w, scT], [pm])
                                mm(pm[:, col:col + 1], b[0:1, j * 128:(j + 1) * 128], ones32[0:1, 0:1], False, True,
                                   [b, ones32], [pm])
                k.dma(k.q_sp, nrow[:, 0:D], I["norm_mix"][l:l + 1, :], writes=[nrow])
                k.dma(k.q_sp, nrow[:, D:2 * D], I["norm_ffn"][l:l + 1, :], writes=[nrow])
                for j in range(16):
                    mm(pm[:, 48 + j:49 + j], nrow[0:1, j * 128:(j + 1) * 128], ones32[0:1, 0:1], True, True,
                       [nrow, ones32], [pm])
                cp(dve, modT[:, :], pm[:, 0:64], [pm], [modT])
                stt(gmod1[:, :], modT[:, 8:16], 1.0, modT[:, 48:56], ALU.add, ALU.mult, [modT], [gmod1])
                stt(gmod2[:, :], modT[:, 32:40], 1.0, modT[:, 56:64], ALU.add, ALU.mult, [modT], [gmod2])
                k.barrier()

        def emit_norm(xsrc, gmod, shiftc, layer, router):
            with ExitStack() as st:
                xt_r = Ring([k.sb(st, "n_xt%d" % i, [128, D], F32) for i in range(4)])
                xs_r = Ring([k.sb(st, "n_xs%d" % i, [128, D], F32) for i in range(3)])
                junk = k.sb(st, "n_junk", [128, D], BF16)
                sm_r = Ring([k.sb(st, "n_sm%d" % i, [128, 4], F32) for i in range(6)])
                ps_r = Ring([k.ps(st, "n_ps%d" % i, [128, D], F32) for i in range(2)])
                if router:
                    h32_r = Ring([k.sb(st, "n_h32%d" % i, [128, 8, 128], F32) for i in range(2)])
                    wr32 = k.sb(st, "n_wr32", [128, 8, 36], F32)
                    brr = k.sb(st, "n_brr", [1, 36], F32)
                    plg_r = Ring([k.ps(st, "n_plg%d" % i, [128, 512], F32) for i in range(2)])
                    lgall = k.sb(st, "n_lgall", [128, NT, 36], F32)
                    k.dma(k.q_sp, wr32[:, :, 0:4],
                          I["router_group_w"][layer].rearrange("(kc p) n -> p kc n", p=128), writes=[wr32])
                    k.dma(k.q_sp, wr32[:, :, 4:36],
                          I["router_expert_w"][layer].rearrange("(kc p) n -> p kc n", p=128), writes=[wr32])
                    k.dma(k.q_sp, brr[:, 0:4], I["router_group_b"][layer:layer + 1, :], writes=[brr])
                    k.dma(k.q_sp, brr[:, 4:36], I["router_expert_b"][layer:layer + 1, :], writes=[brr])
                state = {}

                def s1(t):
                    xt = xt_r.next()
                    k.dma(k.q_sp, xt[:, :], xsrc[t * 128:(t + 1) * 128, :], reads=[xres[t]], writes=[xt])
                    sm = sm_r.next()
                    actf(junk[:, :], xt[:, :], AF.Square, [xt], [junk, sm], accum_out=sm[:, 0:1])
                    state[t] = (xt, sm)

                def s2(t):
                    xt, sm = state[t]
                    ts(dve, sm[:, 1:2], sm[:, 0:1], 1.0 / D, EPS, ALU.mult, ALU.add, [sm], [sm])
                    actf(sm[:, 2:3], sm[:, 1:2], AF.Sqrt, [sm], [sm])
                    recip(sm[:, 3:4], sm[:, 2:3], [sm], [sm])
                    xs = xs_r.next()
                    ts(dve, xs[:, :], xt[:, :], sm[:, 3:4], None, ALU.mult, None, [xt, sm], [xs])
                    if router and SPARSE:
                        k.dma(k.q_sp, XN[t * 128:(t + 1) * 128, :], xs[:, :], reads=[xs], writes=[xnres])
                    state[t] = xs

                def s3(t):
                    xs = state.pop(t)
                    ps = ps_r.next()
                    for kc in range(8):
                        tr(ps[:, kc * 128:(kc + 1) * 128], xs[:, kc * 128:(kc + 1) * 128], ident32[:, :],
                           [xs, ident32], [ps])
                    for kc in range(8):
                        if router and SPARSE:
                            break
                        actf(H["hT"][:, kc, t * 128:(t + 1) * 128], ps[:, kc * 128:(kc + 1) * 128], AF.Identity,
                             [ps, gmod, modT], [H["hT"]], scale=gmod[:, kc:kc + 1], bias=shiftc[:, kc:kc + 1])
                    if router:
                        h32 = h32_r.next()
                        for kc in range(8):
                            actf(h32[:, kc, :], ps[:, kc * 128:(kc + 1) * 128], AF.Identity,
                                 [ps, gmod, modT], [h32], scale=gmod[:, kc:kc + 1], bias=shiftc[:, kc:kc + 1])
                        plg = plg_r.next()
                        for kc in range(8):
                            mm(plg[:, 0:36], h32[:, kc, :], wr32[:, kc, :], kc == 0, False, [h32, wr32], [plg])
                        mm(plg[:, 0:36], ones32[0:1, :], brr[0:1, :], False, True, [ones32, brr], [plg])
                        cp(dve, lgall[:, t, :], plg[:, 0:36], [plg], [lgall])

                for i in range(NT + 2):
                    if i < NT:
                        s1(i)
                    if 1 <= i <= NT:
                        s2(i - 1)
                    if i >= 2:
                        s3(i - 2)
                if router:
                    def bc(ap2, n):
                        return ap2.unsqueeze(2).to_broadcast([128, NT, n])
                    f4 = k.sb(st, "r_f4", [128, NT, 4], F32)
                    ohg = r_ohg
                    v = r_v
                    el = k.sb(st, "r_el", [128, NT, 8], F32)
                    t8 = k.sb(st, "r_t8", [128, NT, 8], F32)
                    oh1 = r_oh1
                    oh2 = r_oh2
                    elm = k.sb(st, "r_elm", [128, NT, 8], F32)
                    cg = k.sb(st, "r_cg", [128, NT, 8], F32)
                    RR = [f4, ohg, v, el, t8, oh1, oh2, elm, cg, lgall]
                    gm, gs, gval, l1, l2, dd, ee, den, w1, W1, W2 = [v[:, i, :] for i in range(11)]

                    def red(out, in_, op):
                        k.op(dve, lambda: nc.vector.tensor_reduce(out=out, in_=in_, axis=AX.X, op=op), RR, RR)

                    lgg = lgall[:, :, 0:4]
                    red(gm, lgg, ALU.max)
                    tt(dve, ohg[:, :, :], lgg, bc(gm, 4), ALU.is_equal, RR, RR)
                    tt(dve, f4[:, :, :], lgg, bc(gm, 4), ALU.subtract, RR, RR)
                    actf(f4[:, :, :], f4[:, :, :], AF.Exp, RR, RR)
                    red(gs, f4[:, :, :], ALU.add)
                    recip(gval, gs, RR, RR)
                    for g in range(4):
                        src = lgall[:, :, 4 + 8 * g:12 + 8 * g]
                        if g == 0:
                            tt(dve, el[:, :, :], src, bc(ohg[:, :, g], 8), ALU.mult, RR, RR)
                        else:
                            tt(dve, t8[:, :, :], src, bc(ohg[:, :, g], 8), ALU.mult, RR, RR)
                            tt(dve, el[:, :, :], el[:, :, :], t8[:, :, :], ALU.add, RR, RR)
                    red(l1, el[:, :, :], ALU.max)
                    tt(dve, oh1[:, :, :], el[:, :, :], bc(l1, 8), ALU.is_equal, RR, RR)
                    stt(elm[:, :, :], oh1[:, :, :], -1e30, el[:, :, :], ALU.mult, ALU.add, RR, RR)
                    red(l2, elm[:, :, :], ALU.max)
                    tt(dve, oh2[:, :, :], elm[:, :, :], bc(l2, 8), ALU.is_equal, RR, RR)
                    tt(dve, dd, l2, l1, ALU.subtract, RR, RR)
                    actf(ee, dd, AF.Exp, RR, RR)
                    ts(dve, den, ee, 1.0, None, ALU.add, None, RR, RR)
                    recip(w1, den, RR, RR)
                    tt(dve, W1, w1, gval, ALU.mult, RR, RR)
                    tt(dve, W2, W1, ee, ALU.mult, RR, RR)
                    tt(dve, cg[:, :, :], oh1[:, :, :], bc(W1, 8), ALU.mult, RR, RR)
                    tt(dve, t8[:, :, :], oh2[:, :, :], bc(W2, 8), ALU.mult, RR, RR)
                    tt(dve, cg[:, :, :], cg[:, :, :], t8[:, :, :], ALU.add, RR, RR)
                    for g in range(4):
                        tt(dve, cmb[:, :, 8 * g:8 * g + 8], cg[:, :, :], bc(ohg[:, :, g], 8), ALU.mult, RR, [cmb])
                k.barrier()

        def emit_inproj(wsrc, ncols, fjobs, tjobs):
            with ExitStack() as st:
                W = k.sb(st, "ip_W", [128, 8, ncols], BF16)
                wv = wsrc.rearrange("(kc p) n -> p kc n", p=128)
                c0 = 0
                while c0 < ncols:
                    w_ = min(512, ncols - c0)
                    k.dma(k.q_pool, W[:, :, c0:c0 + w_], wv[:, :, c0:c0 + w_], writes=[W])
                    c0 += w_
                stg_r = Ring([k.sb(st, "ip_stg%d" % i, [128, S], BF16) for i in range(2)])
                stt_r = Ring([k.sb(st, "ip_stt%d" % i, [128, 4, 512], BF16) for i in range(2)])
                ps_r = Ring([k.ps(st, "ip_ps%d" % i, [128, 512], F32) for i in range(4)])
                n = 0
                for (col0, nr, fb, scale) in fjobs:
                    stg = stg_r.next()
                    for tg in range(8):
                        ps = ps_r.next()
                        for kc in range(8):
                            mm(ps[0:nr, :], W[:, kc, col0:col0 + nr], H["hT"][:, kc, tg * 512:(tg + 1) * 512],
                               kc == 0, kc == 7, [W, H["hT"]], [ps])
                        if n % 2 == 0:
                            actf(stg[0:nr, tg * 512:(tg + 1) * 512], ps[0:nr, :], AF.Copy, [ps], [stg], scale=scale)
                        else:
                            ts(dve, stg[0:nr, tg * 512:(tg + 1) * 512], ps[0:nr, :], scale, None, ALU.mult, None,
                               [ps], [stg])
                        n += 1
                    k.dma(k.q_sp, FB[fb, 0:nr, :], stg[0:nr, :], reads=[stg], writes=[fbres[fb]])
                for (col0, wd, tcol0) in tjobs:
                    for t4 in range(8):
                        stg = stt_r.next()
                        for j in range(4):
                            t = t4 * 4 + j
                            ps = ps_r.next()
                            for kc in range(8):
                                mm(ps[:, 0:wd], H["hT"][:, kc, t * 128:(t + 1) * 128], W[:, kc, col0:col0 + wd],
                                   kc == 0, kc == 7, [W, H["hT"]], [ps])
                            if n % 2 == 0:
                                actf(stg[:, j, 0:wd], ps[:, 0:wd], AF.Copy, [ps], [stg])
                            else:
                                cp(dve, stg[:, j, 0:wd], ps[:, 0:wd], [ps], [stg])
                            n += 1
                        k.dma(k.q_sp,
                              TB[t4 * 512:(t4 + 1) * 512, tcol0:tcol0 + wd].rearrange("(j p) c -> p j c", p=128),
                              stg[:, :, 0:wd], reads=[stg], writes=[tbres])
                k.barrier()

        def emit_outproj(wsrc, gbc, xsrc):
            with ExitStack() as st:
                W = k.sb(st, "op_W", [128, 8, D], BF16)
                wv = wsrc.rearrange("(kc p) n -> p kc n", p=128)
                for h in range(2):
                    k.dma(k.q_pool, W[:, :, h * 512:(h + 1) * 512], wv[:, :, h * 512:(h + 1) * 512], writes=[W])
                for kc in range(8):
                    tt(dve, W[:, kc, :], W[:, kc, :], gbc[:, :], ALU.mult, [W, gbc], [W])
                xt_r = Ring([k.sb(st, "op_xt%d" % i, [128, D], F32) for i in range(3)])
                xn_r = Ring([k.sb(st, "op_xn%d" % i, [128, D], F32) for i in range(2)])
                ps_r = Ring([k.ps(st, "op_ps%d" % i, [128, D], F32) for i in range(3)])
                for t in range(NT):
                    xt = xt_r.next()
                    k.dma(k.q_sp, xt[:, :], xsrc[t * 128:(t + 1) * 128, :], reads=[xres[t]], writes=[xt])
                    ps = ps_r.next()
                    for h in range(2):
                        for kc in range(8):
                            mm(ps[:, h * 512:(h + 1) * 512], H["hT"][:, kc, t * 128:(t + 1) * 128],
                               W[:, kc, h * 512:(h + 1) * 512], kc == 0, kc == 7, [H["hT"], W], [ps])
                    xn = xn_r.next()
                    tt(dve, xn[:, :], xt[:, :], ps[:, :], ALU.add, [xt, ps], [xn])
                    k.dma(k.q_sp, XS[t * 128:(t + 1) * 128, :], xn[:, :], reads=[xn], writes=[xres[t]])
                k.barrier()

        def emit_odd_mixer():
            with ExitStack() as st:
                mltb = k.sb(st, "sb_mlt", [128, 128], BF16)
                unegb = k.sb(st, "sb_uneg", [128, 128], BF16)
                k.dma(k.q_pool, mltb[:, :], I["c_mlt"][:, :], writes=[mltb])
                k.dma(k.q_pool, unegb[:, :], I["c_uneg"][:, :], writes=[unegb])
                qT_r = Ring([k.sb(st, "sb_qT%d" % i, [128, S], BF16) for i in range(2)])
                kT_r = Ring([k.sb(st, "sb_kT%d" % i, [128, S], BF16) for i in range(2)])
                vt_r = Ring([k.sb(st, "sb_vt%d" % i, [128, NT, 128], BF16) for i in range(2)])
                e32_r = Ring([k.sb(st, "sb_e%d" % i, [128, 512], F32) for i in range(6)])
                sp_r = Ring([k.sb(st, "sb_sp%d" % i, [128, 512], BF16) for i in range(10)])
                rb_r = Ring([k.sb(st, "sb_rb%d" % i, [128, 512], BF16) for i in range(10)])
                ab_r = Ring([k.sb(st, "sb_ab%d" % i, [128, 512], BF16) for i in range(10)])
                R32s = [k.sb(st, "sb_R32_%d" % i, [128, 512], F32) for i in range(2)]
                pz_r = Ring([k.ps(st, "sb_pz%d" % i, [128, 512], F32) for i in range(3)])
                pw_r = Ring([k.ps(st, "sb_pw%d" % i, [128, 512], F32) for i in range(3)])
                pos = [k.ps(st, "sb_po%d" % i, [128, 512], F32) for i in range(2)]
                for hp in range(8):
                    qT = qT_r.next()
                    kT = kT_r.next()
                    vt = vt_r.next()
                    k.dma(k.q_sp, qT[:, :], FB[hp, :, :], reads=[fbres[hp]], writes=[qT])
                    k.dma(k.q_sp, kT[:, :], FB[8 + hp, :, :], reads=[fbres[8 + hp]], writes=[kT])
                    k.dma(k.q_sp, vt[:, :, :],
                          TB[:, hp * 128:(hp + 1) * 128].rearrange("(t p) c -> p t c", p=128),
                          reads=[tbres], writes=[vt])
                    for qg in range(8):
                        kbs = list(range(4 * qg + 3, -1, -1))
                        nb = len(kbs)
                        items = [[None] * nb, [None] * nb]
                        pend = {}
                        pendB = []
                        for j in range(2):
                            mm(pos[j][:, :], zerob[:, :], qT[:, qg * 512:(qg + 1) * 512], True, False,
                               [zerob, qT], [pos[j]])

                        def stageA(j, i):
                            b0 = 64 * j
                            R32 = R32s[j]
                            kb = kbs[i]
                            j0 = max(0, kb - 4 * qg)
                            c0 = 128 * j0
                            wq = 512 - c0
                            q0 = qg * 512 + c0
                            diag = kb >= 4 * qg
                            pz = pz_r.next()
                            mm(pz[:, 0:wq], kT[b0:b0 + 64, kb * 128:(kb + 1) * 128], qT[b0:b0 + 64, q0:q0 + wq],
                               True, True, [kT, qT], [pz])
                            e32 = e32_r.next()
                            actf(e32[:, 0:wq], pz[:, 0:wq], AF.Exp, [pz], [e32])
                            pend[(j, i)] = (R32, kb, c0, wq, q0, diag, e32)

                        def stageA2(j, i):
                            R32, kb, c0, wq, q0, diag, e32 = pend.pop((j, i))
                            spb = sp_r.next()
                            if 'sbx_noln' not in flags:
                                actf(spb[:, 0:wq], e32[:, 0:wq], AF.Ln, [e32], [spb], bias=1.0)
                            if diag:
                                tt(pool, spb[:, 0:128], spb[:, 0:128], mltb[:, :], ALU.mult, [spb, mltb], [spb])
                            rb = None
                            if i > 0 and 'sbx_nor' not in flags:
                                rb = rb_r.next()
                                cp(dve, rb[:, :], R32[:, :], [R32], [rb])
                            if i < nb - 1 and 'sbx_nor' not in flags:
                                if i == 0:
                                    if c0 > 0:
                                        memset(dve, R32[:, 0:c0], 0.0, [R32])
                                    cp(dve, R32[:, c0:512], spb[:, 0:wq], [spb], [R32])
                                else:
                                    tt(dve, R32[:, c0:512], R32[:, c0:512], spb[:, 0:wq], ALU.add, [R32, spb],
                                       [R32])
                            items[j][i] = (kb, c0, wq, q0, diag, spb, rb)

                        def stageB(j, i):
                            if 'sbx_nob' in flags:
                                return
                            b0 = 64 * j
                            po = pos[j]
                            kb, c0, wq, q0, diag, spb, rb = items[j][i]
                            pw = pw_r.next()
                            mm(pw[:, 0:wq], kT[b0:b0 + 64, kb * 128:(kb + 1) * 128], qT[b0:b0 + 64, q0:q0 + wq],
                               True, False, [kT, qT], [pw])
                            mm(pw[:, 0:wq], unegb[:, :], spb[:, 0:wq], False, rb is None, [unegb, spb], [pw])
                            if rb is not None:
                                mm(pw[:, 0:wq], negonesb[:, :], rb[:, c0:512], False, True, [negonesb, rb], [pw])
                            ab = ab_r.next()
                            actf(ab[:, 0:wq], pw[:, 0:wq], AF.Exp, [pw], [ab])
                            if diag:
                                tt(dve, ab[:, 0:128], ab[:, 0:128], mltb[:, :], ALU.mult, [ab, mltb], [ab])
                            pendB.append((po, c0, wq, kb, ab, i))
                            while len(pendB) > 5:
                                stageB2()

                        def stageB2():
                            po, c0, wq, kb, ab, i = pendB.pop(0)
                            mm(po[:, c0:512], vt[:, kb, :], ab[:, 0:wq], False, i == nb - 1, [vt, ab], [po])

                        GW = 2
                        nw = nb // GW
                        for w in range(nw + 1):
                            if w < nw:
                                for i in range(w * GW, (w + 1) * GW):
                                    for j in range(2):
                                        stageA(j, i)
                                for i in range(w * GW, (w + 1) * GW):
                                    for j in range(2):
                                        stageA2(j, i)
                            if w > 0:
                                for i in range((w - 1) * GW, w * GW):
                                    for j in range(2):
                                        stageB(j, i)
                        while pendB:
                            stageB2()
                        for j in range(2):
                            b0 = 64 * j
                            if False:
                                pass
                            else:
                                cp(dve, H["hT"][b0:b0 + 64, hp, qg * 512:(qg + 1) * 512], pos[j][b0:b0 + 64, :],
                                   [pos[j]], [H["hT"]])
                k.barrier()

        def emit_even_mixer(i_even, layer):
            lam_init = 0.8 - 0.6 * math.exp(-0.3 * layer)
            with ExitStack() as st:
                BM = [[k.sb(st, "BM%d_%d" % (h, di), [128, 512], BF16) for di in range(5)] for h in range(4)]
                b15 = k.sb(st, "b15", [128, 4], F32)
                neglam = k.sb(st, "neglam", [128, 1], F32)
                gsub = k.sb(st, "gsub", [128, 1], F32)
                with ExitStack() as st2:
                    rb = k.sb(st2, "rb", [32, 4], F32)
                    ohb = k.sb(st2, "ohb", [32, NF], F32)
                    fsb = k.sb(st2, "fsb", [4, NF], F32)
                    exch = k.sb(st2, "exch", [128, 128], F32)
                    nmk = [k.sb(st2, "nmk%d" % di, [128, 512], F32) for di in range(5)]
                    tl_r = Ring([k.sb(st2, "tl%d" % i, [128, 512], F32) for i in range(2)])
                    lrow = k.sb(st2, "lrow", [1, 264], F32)
                    grow = k.sb(st2, "grow", [1, 128], F32)
                    pf = Ring([k.ps(st2, "pf%d" % i, [128, 512], F32) for i in range(2)])
                    k.dma(k.q_sp, rb[:, :], I["rel_bias"][:, :], writes=[rb])
                    k.dma(k.q_sp, ohb[:, :], I["c_ohb"][:, :], writes=[ohb])
                    k.dma(k.q_sp, exch[:, :], I["c_exch"][:, :], writes=[exch])
                    k.dma(k.q_sp, b15[:, :], I["rel_bias"][15].partition_broadcast(128), writes=[b15])
                    for di in range(5):
                        k.dma(k.q_sp, nmk[di][:, :], I["c_negmask"][di], writes=[nmk[di]])
                    for c3 in range(3):
                        c0 = c3 * 512
                        w_ = min(512, NF - c0)
                        p = pf.next()
                        mm(p[0:4, 0:w_], rb[0:32, 0:4], ohb[0:32, c0:c0 + w_], True, True, [rb, ohb], [p])
                        cp(dve, fsb[0:4, c0:c0 + w_], p[0:4, 0:w_], [p], [fsb])
                    k.dma(k.q_sp, FS[:, :], fsb[:, :], reads=[fsb], writes=[fsres])
                    for h in range(4):
                        for di, dl in enumerate(DELTAS):
                            tl = tl_r.next()
                            src = bass.AP(tensor=FS_T, offset=h * NF + (384 - dl), ap=[[1, 128], [1, 512]])
                            k.dma(k.q_sp, tl[:, :], src, reads=[fsres], writes=[tl])
                            p = pf.next()
                            mm(p[:, :], exch[:, :], tl[:, :], True, True, [exch, tl], [p])
                            tt(dve, BM[h][di][:, :], p[:, :], nmk[di][:, :], ALU.add, [p, nmk[di]], [BM[h][di]])
                    k.dma(k.q_sp, lrow[:, 0:256], I["even_lambda"][i_even:i_even + 1, :], writes=[lrow])
                    LR = [lrow]
                    tt(dve, lrow[:, 0:64], lrow[:, 0:64], lrow[:, 64:128], ALU.mult, LR, LR)
                    tt(dve, lrow[:, 128:192], lrow[:, 128:192], lrow[:, 192:256], ALU.mult, LR, LR)
                    k.op(dve, lambda: nc.vector.tensor_reduce(out=lrow[:, 256:257], in_=lrow[:, 0:64], axis=AX.X,
                                                              op=ALU.add), LR, LR)
                    k.op(dve, lambda: nc.vector.tensor_reduce(out=lrow[:, 257:258], in_=lrow[:, 128:192], axis=AX.X,
                                                              op=ALU.add), LR, LR)
                    actf(lrow[:, 258:260], lrow[:, 256:258], AF.Exp, LR, LR)
                    tt(dve, lrow[:, 260:261], lrow[:, 259:260], lrow[:, 258:259], ALU.subtract, LR, LR)
                    ts(dve, lrow[:, 261:262], lrow[:, 260:261], -lam_init, None, ALU.add, None, LR, LR)
                    p = pf.next()
                    mm(p[:, 0:1], ones32[0:1, :], lrow[0:1, 261:262], True, True, [ones32, lrow], [p])
                    cp(dve, neglam[:, :], p[:, 0:1], [p], [neglam])
                    k.dma(k.q_sp, grow[:, :], I["even_subln"][i_even:i_even + 1, :], writes=[grow])
                    p = pf.next()
                    mm(p[:, 0:1], grow[0:1, :], ones32[0:1, 0:1], True, True, [grow, ones32], [p])
                    ts(dve, gsub[:, :], p[:, 0:1], 1.0 - lam_init, None, ALU.mult, None, [p], [gsub])
                    k.barrier()
                qT_r = Ring([k.sb(st, "da_qT%d" % i, [128, S], BF16) for i in range(2)])
                kT_r = Ring([k.sb(st, "da_kT%d" % i, [128, S], BF16) for i in range(2)])
                vt_r = Ring([k.sb(st, "da_vt%d" % i, [128, NT, 128], BF16) for i in range(2)])
                E_r = Ring([k.sb(st, "da_E%d" % i, [128, 512], BF16) for i in range(8)])
                f_r = Ring([k.sb(st, "da_f%d" % i, [128, 512], F32) for i in range(6)])
                o32_r = Ring([k.sb(st, "da_o%d" % i, [128, 512], F32) for i in range(2)])
                sq_r = Ring([k.sb(st, "da_sq%d" % i, [128, 512], BF16) for i in range(2)])
                ps_r = Ring([k.ps(st, "da_ps%d" % i, [128, 512], F32) for i in range(3)])
                pu = [k.ps(st, "da_pu%d" % i, [128, 512], F32) for i in range(2)]
                pd = [k.ps(st, "da_pd%d" % i, [128, 512], F32) for i in range(2)]
                pss = k.ps(st, "da_pss", [128, 512], F32)
                us_r = Ring([k.sb(st, "da_us%d" % i, [128, 512], F32) for i in range(4)])
                pend_fin = []
                for h in range(0 if 'ev_noA' in flags else 4):
                    qT = qT_r.next()
                    kT = kT_r.next()
                    vt = vt_r.next()
                    k.dma(k.q_sp, qT[:, :], FB[h, :, :], reads=[fbres[h]], writes=[qT])
                    k.dma(k.q_sp, kT[:, :], FB[4 + h, :, :], reads=[fbres[4 + h]], writes=[kT])
                    k.dma(k.q_sp, vt[:, :, :],
                          TB[:, h * 128:(h + 1) * 128].rearrange("(t p) c -> p t c", p=128),
                          reads=[tbres], writes=[vt])
                    for qg in range(8):
                        nkb = 4 * qg + 4
                        its = [(kb, m) for kb in range(nkb) for m in range(2)]
                        held = {}

                        def st1(idx):
                            kb, m = its[idx]
                            j0 = max(0, kb - 4 * qg)
                            c0 = 128 * j0
                            wq = 512 - c0
                            q0 = qg * 512 + c0
                            dl = kb * 128 - qg * 512
                            b0 = 64 * m
                            ps = ps_r.next()
                            near = dl >= -128
                            mm(ps[:, 0:wq], kT[b0:b0 + 64, kb * 128:(kb + 1) * 128], qT[b0:b0 + 64, q0:q0 + wq],
                               True, not near, [kT, qT], [ps])
                            E = E_r.next()
                            if near:
                                bm = BM[h][DELTAS.index(dl)]
                                mm(ps[:, 0:wq], identb[:, :], bm[:, c0:512], False, True, [identb, bm], [ps])
                                actf(E[:, 0:wq], ps[:, 0:wq], AF.Exp, [ps], [E])
                            else:
                                actf(E[:, 0:wq], ps[:, 0:wq], AF.Exp, [ps, b15], [E], bias=b15[:, h:h + 1])
                            held[idx] = (E, c0, wq)

                        def st2(idx):
                            kb, m = its[idx]
                            E, c0, wq = held.pop(idx)
                            mm(pu[m][:, c0:512], vt[:, kb, :], E[:, 0:wq], kb == 0, kb == nkb - 1, [vt, E], [pu[m]])
                            mm(pd[m][:, c0:512], onesb[:, :], E[:, 0:wq], kb == 0, kb == nkb - 1, [onesb, E],
                               [pd[m]])

                        SK = 4 if 'sk4' in flags else 2
                        for idx in range(len(its) + SK):
                            if idx < len(its):
                                st1(idx)
                            if idx >= SK:
                                st2(idx - SK)
                            if pend_fin and idx >= 2 and idx % 2 == 0:
                                pend_fin.pop(0)()
                        if 'da_nofin' in flags:
                            continue
                        while pend_fin:
                            pend_fin.pop(0)()
                        u0 = us_r.next()
                        cp(dve, u0[:, :], pu[0][:, :], [pu[0]], [u0])
                        u1 = us_r.next()
                        cp(dve, u1[:, :], pu[1][:, :], [pu[1]], [u1])
                        r0 = f_r.next()
                        actf(r0[:, :], pd[0][:, :], AF.Ln, [pd[0]], [r0])
                        r1 = f_r.next()
                        actf(r1[:, :], pd[1][:, :], AF.Ln, [pd[1]], [r1])

                        def mk(h=h, qg=qg, u0=u0, u1=u1, r0=r0, r1=r1):
                            box = {}

                            def s_a():
                                actf(r0[:, :], r0[:, :], AF.Exp, [r0], [r0], scale=-1.0)
                                actf(r1[:, :], r1[:, :], AF.Exp, [r1], [r1], scale=-1.0)

                            def s_b():
                                tt(dve, u0[:, :], u0[:, :], r0[:, :], ALU.mult, [u0, r0], [u0])
                                tt(dve, u1[:, :], u1[:, :], r1[:, :], ALU.mult, [u1, r1], [u1])
                                o32 = o32_r.next()
                                stt(o32[:, :], u1[:, :], neglam[:, 0:1], u0[:, :], ALU.mult, ALU.add,
                                    [u1, u0, neglam], [o32])
                                box["o"] = o32

                            def s_c():
                                sq = sq_r.next()
                                actf(sq[:, :], box["o"][:, :], AF.Square, [box["o"]], [sq])
                                mm(pss[:, :], onesb[:, :], sq[:, :], True, True, [onesb, sq], [pss])

                            def s_d():
                                rs = f_r.next()
                                actf(rs[:, :], pss[:, :], AF.Ln, [pss, epsc], [rs], scale=1.0 / 128.0,
                                     bias=epsc[:, 0:1])
                                box["rs"] = rs

                            def s_e():
                                rs = box["rs"]
                                actf(rs[:, :], rs[:, :], AF.Exp, [rs], [rs], scale=-0.5)
                                stt(H["hT"][:, h, qg * 512:(qg + 1) * 512], box["o"][:, :], gsub[:, 0:1], rs[:, :],
                                    ALU.mult, ALU.mult, [box["o"], gsub, rs], [H["hT"]])
                            return [s_a, s_b, s_c, s_d, s_e]
                        pend_fin.extend(mk())
                while pend_fin:
                    pend_fin.pop(0)()
                k.barrier()
            with ExitStack() as st:
                gtri = k.sb(st, "g_tri", [128, 128], F32)
                gm2 = k.sb(st, "g_m2", [128, 128], F32)
                gmaskb = k.sb(st, "g_mask", [128, 128], BF16)
                wgk = k.sb(st, "g_wgk", [16, 256], BF16)
                bgk = k.sb(st, "g_bgk", [1, 256], BF16)
                glag = k.sb(st, "g_lag", [128, 1], F32)
                grow = k.sb(st, "g_grow", [1, 128], F32)
                k.dma(k.q_sp, gtri[:, :], I["c_gtri"][:, :], writes=[gtri])
                k.dma(k.q_sp, gm2[:, :], I["c_gm2"][:, :], writes=[gm2])
                k.dma(k.q_pool, gmaskb[:, :], I["c_gmask"][:, :], writes=[gmaskb])
                k.dma(k.q_pool, wgk[:, :], I["even_w_gk2"][i_even], writes=[wgk])
                k.dma(k.q_pool, bgk[:, :], I["even_b_gk"][i_even:i_even + 1, :], writes=[bgk])
                k.dma(k.q_sp, grow[:, :], I["even_gla_norm"][i_even:i_even + 1, :], writes=[grow])
                pA = Ring([k.ps(st, "g_pA%d" % i, [128, 512], F32) for i in range(3)])
                pS = Ring([k.ps(st, "g_pS%d" % i, [128, 512], F32) for i in range(2)])
                pO = Ring([k.ps(st, "g_pO%d" % i, [128, 512], F32) for i in range(2)])
                p = pA.next()
                mm(p[:, 0:1], grow[0:1, :], ones32[0:1, 0:1], True, True, [grow, ones32], [p])
                cp(dve, glag[:, :], p[:, 0:1], [p], [glag])
                qg_r = Ring([k.sb(st, "g_q%d" % i, [128, 2, 512], BF16) for i in range(2)])
                kg_r = Ring([k.sb(st, "g_k%d" % i, [128, 2, 512], BF16) for i in range(2)])
                kt_r = Ring([k.sb(st, "g_kt%d" % i, [128, 4, 256], BF16) for i in range(2)])
                vg_r = Ring([k.sb(st, "g_v%d" % i, [128, 4, 512], BF16) for i in range(2)])
                bg_r = Ring([k.sb(st, "g_bg%d" % i, [16, 512], BF16) for i in range(2)])
                br_r = Ring([k.sb(st, "g_br%d" % i, [128, 4, 512], BF16) for i in range(2)])
                og_r = Ring([k.sb(st, "g_og%d" % i, [128, 4, 512], F32) for i in range(2)])
                e32_r = Ring([k.sb(st, "g_e%d" % i, [128, 256], F32) for i in range(2)])
                sp_r = Ring([k.sb(st, "g_sp%d" % i, [128, 256], F32) for i in range(2)])
                kf_r = Ring([k.sb(st, "g_kf%d" % i, [128, 256], F32) for i in range(2)])
                kd_r = Ring([k.sb(st, "g_kd%d" % i, [128, 256], BF16) for i in range(2)])
                eb_r = Ring([k.sb(st, "g_eb%d" % i, [128, 256], F32) for i in range(2)])
                en_r = Ring([k.sb(st, "g_en%d" % i, [128, 256], F32) for i in range(2)])
                qd_r = Ring([k.sb(st, "g_qd%d" % i, [128, 2, 128], BF16) for i in range(2)])
                ki_r = Ring([k.sb(st, "g_ki%d" % i, [128, 2, 128], BF16) for i in range(2)])
                at_r = Ring([k.sb(st, "g_at%d" % i, [128, 128], BF16) for i in range(3)])
                S32_r = [Ring([k.sb(st, "g_S32_%d_%d" % (h, i), [128, 128], F32) for i in range(2)]) for h in range(4)]
                Sb_r = [Ring([k.sb(st, "g_Sb_%d_%d" % (h, i), [128, 128], BF16) for i in range(3)]) for h in range(4)]
                sq_r = Ring([k.sb(st, "g_sq%d" % i, [128, 512], BF16) for i in range(2)])
                f_r = Ring([k.sb(st, "g_f%d" % i, [128, 512], F32) for i in range(4)])
                S32 = []
                Sb = []
                for h in range(4):
                    s = S32_r[h].next()
                    memset(dve, s[:, :], 0.0, [s])
                    S32.append(s)
                    b = Sb_r[h].next()
                    memset(pool, b[:, :], 0.0, [b])
                    Sb.append(b)
                for g8 in range(0 if 'ev_noB' in flags else 8):
                    t0g = g8 * 512
                    qgt = qg_r.next()
                    kgt = kg_r.next()
                    ktt = kt_r.next()
                    vgt = vg_r.next()
                    bgt = bg_r.next()
                    brt = br_r.next()
                    og = og_r.next()
                    for hh in range(2):
                        k.dma(k.q_sp, qgt[:, hh, :], FB[12 + hh, :, t0g:t0g + 512], reads=[fbres[12 + hh]], writes=[qgt])
                        k.dma(k.q_sp, kgt[:, hh, :], FB[14 + hh, :, t0g:t0g + 512], reads=[fbres[14 + hh]], writes=[kgt])
                    k.dma(k.q_sp, ktt[:, :, :],
                          TB[t0g:t0g + 512, 512:768].rearrange("(j p) c -> p j c", p=128), reads=[tbres], writes=[ktt])
                    k.dma(k.q_sp, vgt[:, :, :],
                          TB[t0g:t0g + 512, 768:1280].rearrange("(j p) c -> p j c", p=128), reads=[tbres], writes=[vgt])
                    k.dma(k.q_sp, bgt[:, :], FB[24, 0:16, t0g:t0g + 512], reads=[fbres[24]], writes=[bgt])
                    for h in range(4):
                        k.dma(k.q_sp, brt[:, h, :], FB[20 + h, :, t0g:t0g + 512], reads=[fbres[20 + h]], writes=[brt])
                    for j4 in range(4):
                        tc0 = j4 * 128
                        ppre = pA.next()
                        mm(ppre[:, 0:256], bgt[0:16, tc0:tc0 + 128], wgk[0:16, :], True, False, [bgt, wgk], [ppre])
                        mm(ppre[:, 0:256], onesb[0:1, :], bgk[0:1, :], False, True, [onesb, bgk], [ppre])
                        e32 = e32_r.next()
                        actf(e32[:, :], ppre[:, 0:256], AF.Exp, [ppre], [e32], scale=-1.0)
                        sp32 = sp_r.next()
                        actf(sp32[:, :], e32[:, :], AF.Ln, [e32], [sp32], bias=1.0)
                        pb = pA.next()
                        for hh in range(2):
                            mm(pb[:, hh * 128:(hh + 1) * 128], sp32[:, hh * 128:(hh + 1) * 128], gtri[:, :], True, True,
                               [sp32, gtri], [pb])
                        pk = pA.next()
                        mm(pk[:, 0:256], gm2[:, :], sp32[:, :], True, True, [gm2, sp32], [pk])
                        kf = kf_r.next()
                        actf(kf[:, :], pk[:, 0:256], AF.Exp, [pk], [kf])
                        kd = kd_r.next()
                        tt(dve, kd[:, :], ktt[:, j4, :], kf[:, :], ALU.mult, [ktt, kf], [kd])
                        eb = eb_r.next()
                        actf(eb[:, :], pb[:, 0:256], AF.Exp, [pb], [eb])
                        en = en_r.next()
                        actf(en[:, :], pb[:, 0:256], AF.Exp, [pb], [en], scale=-1.0)
                        qd = qd_r.next()
                        ki = ki_r.next()
                        for hh in range(2):
                            tt(dve, qd[:, hh, :], qgt[:, hh, tc0:tc0 + 128], eb[:, hh * 128:(hh + 1) * 128], ALU.mult,
                               [qgt, eb], [qd])
                            tt(dve, ki[:, hh, :], kgt[:, hh, tc0:tc0 + 128], en[:, hh * 128:(hh + 1) * 128], ALU.mult,
                               [kgt, en], [ki])
                        for h in range(4):
                            hh, jj = h // 2, h % 2
                            b0 = 64 * jj
                            psc = pS.next()
                            mm(psc[:, 0:128], ki[b0:b0 + 64, hh, :], qd[b0:b0 + 64, hh, :], True, True, [ki, qd], [psc])
                            at = at_r.next()
                            tt(dve, at[:, :], psc[:, 0:128], gmaskb[:, :], ALU.mult, [psc, gmaskb], [at])
                            po = pO.next()
                            mm(po[:, 0:128], vgt[:, j4, h * 128:(h + 1) * 128], at[:, :], True, False, [vgt, at], [po])
                            mm(po[:, 0:64], Sb[h][b0:b0 + 64, :], qd[b0:b0 + 64, hh, 0:64], False, False,
                               [Sb[h], qd], [po])
                            pst = pS.next()
                            mm(pst[:, 0:128], kd[0:64, hh * 128:(hh + 1) * 128], vgt[0:64, j4, h * 128:(h + 1) * 128],
                               True, True, [kd, vgt], [pst])
                            s1 = S32_r[h].next()
                            stt(s1[b0:b0 + 64, :], S32[h][b0:b0 + 64, :], eb[b0:b0 + 64, hh * 128 + 63:hh * 128 + 64],
                                pst[b0:b0 + 64, 0:128], ALU.mult, ALU.add, [S32[h], eb, pst], [s1])
                            sb1 = Sb_r[h].next()
                            actf(sb1[b0:b0 + 64, :], s1[b0:b0 + 64, :], AF.Copy, [s1], [sb1])
                            mm(po[:, 64:128], sb1[b0:b0 + 64, :], qd[b0:b0 + 64, hh, 64:128], False, True,
                               [sb1, qd], [po])
                            pst2 = pS.next()
                            mm(pst2[:, 0:128], kd[64:128, hh * 128:(hh + 1) * 128],
                               vgt[64:128, j4, h * 128:(h + 1) * 128], True, True, [kd, vgt], [pst2])
                            s2 = S32_r[h].next()
                            stt(s2[b0:b0 + 64, :], s1[b0:b0 + 64, :], eb[b0:b0 + 64, hh * 128 + 127:hh * 128 + 128],
                                pst2[b0:b0 + 64, 0:128], ALU.mult, ALU.add, [s1, eb, pst2], [s2])
                            sb2 = Sb_r[h].next()
                            actf(sb2[b0:b0 + 64, :], s2[b0:b0 + 64, :], AF.Copy, [s2], [sb2])
                            S32[h] = s2
                            Sb[h] = sb2
                            actf(og[:, h, tc0:tc0 + 128], po[:, 0:128], AF.Copy, [po], [og])
                    for h in range(4):
                        sq = sq_r.next()
                        actf(sq[:, :], og[:, h, :], AF.Square, [og], [sq])
                        pn = pA.next()
                        mm(pn[:, :], onesb[:, :], sq[:, :], True, True, [onesb, sq], [pn])
                        rs = f_r.next()
                        actf(rs[:, :], pn[:, :], AF.Ln, [pn, epsc], [rs], scale=1.0 / 128.0, bias=epsc[:, 0:1])
                        rs2 = f_r.next()
                        actf(rs2[:, :], rs[:, :], AF.Exp, [rs], [rs2], scale=-0.5)
                        sl = f_r.next()
                        actf(sl[:, :], brt[:, h, :], AF.Silu, [brt], [sl])
                        t1 = f_r.next()
                        stt(t1[:, :], og[:, h, :], glag[:, 0:1], rs2[:, :], ALU.mult, ALU.mult, [og, glag, rs2], [t1])
                        tt(dve, H["hT"][:, 4 + h, t0g:t0g + 512], t1[:, :], sl[:, :], ALU.mult, [t1, sl], [H["hT"]])
                k.barrier()

        def emit_moe(layer):
            with ExitStack() as st:
                wg_r = Ring([k.sb(st, "me_wg%d" % i, [128, 8, DEXP], BF16) for i in range(2)])
                wu_r = Ring([k.sb(st, "me_wu%d" % i, [128, 8, DEXP], BF16) for i in range(2)])
                wd_r = Ring([k.sb(st, "me_wd%d" % i, [128, 4, D], BF16) for i in range(2)])
                yacc = k.sb(st, "me_yacc", [128, 8, D], F32)
                aT_r = Ring([k.sb(st, "me_aT%d" % i, [128, 4, 512], BF16) for i in range(2)])
                sg_r = Ring([k.sb(st, "me_sg%d" % i, [128, 512], BF16) for i in range(3)])
                xt_r = Ring([k.sb(st, "me_xt%d" % i, [128, D], F32) for i in range(2)])
                pg_r = Ring([k.ps(st, "me_pg%d" % i, [128, 512], F32) for i in range(2)])
                pu_r = Ring([k.ps(st, "me_pu%d" % i, [128, 512], F32) for i in range(2)])
                py_r = Ring([k.ps(st, "me_py%d" % i, [128, D], F32) for i in range(2)])
                for qtr in range(4):
                    memset(dve, yacc[:, :, :], 0.0, [yacc])
                    jobs = [(e, tg) for e in range(NEXP) for tg in range(2)]
                    wts = {}

                    def load_w(e):
                        wg = wg_r.next()
                        wu = wu_r.next()
                        wd = wd_r.next()
                        k.dma(k.q_pool, wg[:, :, :],
                              I["expert_w_gate"][layer, e].rearrange("(kc p) f -> p kc f", p=128), writes=[wg])
                        k.dma(k.q_pool, wu[:, :, :],
                              I["expert_w_up"][layer, e].rearrange("(kc p) f -> p kc f", p=128), writes=[wu])
                        k.dma(k.q_pool, wd[:, :, :],
                              I["expert_w_down"][layer, e].rearrange("(fc p) n -> p fc n", p=128), writes=[wd])
                        wts[e] = (wg, wu, wd)

                    acts = {}

                    def emit_gu(e, tg):
                        wg, wu, wd = wts[e]
                        G = qtr * 2 + tg
                        aT = aT_r.next()
                        for fc in range(4):
                            pg = pg_r.next()
                            pu = pu_r.next()
                            for kc in range(8):
                                mm(pg[:, :], wg[:, kc, fc * 128:(fc + 1) * 128], H["hT"][:, kc, G * 512:(G + 1) * 512],
                                   kc == 0, kc == 7, [wg, H["hT"]], [pg])
                            for kc in range(8):
                                mm(pu[:, :], wu[:, kc, fc * 128:(fc + 1) * 128], H["hT"][:, kc, G * 512:(G + 1) * 512],
                                   kc == 0, kc == 7, [wu, H["hT"]], [pu])
                            sg = sg_r.next()
                            actf(sg[:, :], pg[:, :], AF.Silu, [pg], [sg])
                            tt(dve, aT[:, fc, :], sg[:, :], pu[:, :], ALU.mult, [sg, pu], [aT])
                        acts[(e, tg)] = aT

                    def emit_down(e, tg):
                        wg, wu, wd = wts[e]
                        aT = acts.pop((e, tg))
                        for t4 in range(4):
                            ti = tg * 4 + t4
                            T = qtr * 8 + ti
                            py = py_r.next()
                            for nh in range(2):
                                for fc in range(4):
                                    mm(py[:, nh * 512:(nh + 1) * 512], aT[:, fc, t4 * 128:(t4 + 1) * 128],
                                       wd[:, fc, nh * 512:(nh + 1) * 512], fc == 0, fc == 3, [aT, wd], [py])
                            stt(yacc[:, ti, :], py[:, :], cmb[:, T, e:e + 1], yacc[:, ti, :], ALU.mult, ALU.add,
                                [py, cmb, yacc], [yacc])

                    load_w(0)
                    for idx, (e, tg) in enumerate(jobs):
                        emit_gu(e, tg)
                        if idx > 0:
                            emit_down(*jobs[idx - 1])
                        if tg == 0 and e + 1 < NEXP:
                            load_w(e + 1)
                    emit_down(*jobs[-1])
                    for ti in range(8):
                        T = qtr * 8 + ti
                        xt = xt_r.next()
                        k.dma(k.q_sp, xt[:, :], XS[T * 128:(T + 1) * 128, :], reads=[xres[T]], writes=[xt])
                        tt(dve, yacc[:, ti, :], yacc[:, ti, :], gbc2[:, :], ALU.mult, [yacc, gbc2], [yacc])
                        tt(dve, xt[:, :], xt[:, :], yacc[:, ti, :], ALU.add, [xt, yacc], [xt])
                        k.dma(k.q_sp, XS[T * 128:(T + 1) * 128, :], xt[:, :], reads=[xt], writes=[xres[T]])
                k.barrier()

        def emit_slots(layer):
            with ExitStack() as st:
                Aall = k.sb(st, "sl_A", [128, NT, 32], BF16)
                ag = k.sb(st, "sl_ag", [128, NT, 8], F32)
                mltb = k.sb(st, "sl_mlt", [128, 128], BF16)
                pos = k.sb(st, "sl_pos", [128, NT, 32], F32)
                cnt = k.sb(st, "sl_cnt", [128, 32], F32)
                nb_ = k.sb(st, "sl_nb", [128, 32], F32)
                pa = k.sb(st, "sl_pa", [128, 32], F32)
                pb = k.sb(st, "sl_pb", [128, 32], F32)
                smg = k.sb(st, "sl_smg", [128, NT, 8], F32)
                t8 = k.sb(st, "sl_t8", [128, NT, 8], F32)
                sf = k.sb(st, "sl_sf", [128, 2, NT], F32)
                iog = k.sb(st, "sl_iog", [128, 8], F32)
                iob = k.sb(st, "sl_iob", [128, NBLK], F32)
                cmpt = k.sb(st, "sl_cmp", [128, NBLK, 32], F32)
                eb = k.sb(st, "sl_eb", [128, NBLK], F32)
                fg = k.sb(st, "sl_fg", [128, NBLK, 8], F32)
                ppos = k.ps(st, "sl_ppos", [128, NT * 32], F32)
                pcnt = k.ps(st, "sl_pcnt", [128, 512], F32)
                k.dma(k.q_pool, mltb[:, :], I["c_mlt"][:, :], writes=[mltb])
                k.dma(k.q_sp, iog[:, :], I["c_iog"][:, :], writes=[iog])
                k.dma(k.q_sp, iob[:, :], I["c_iob"][:, :], writes=[iob])
                RR = [Aall, ag, pos, cnt, nb_, pa, pb, smg, t8, sf, cmpt, eb, fg, r_ohg, r_oh1, r_oh2, r_v]

                def bc(ap2, n):
                    return ap2.unsqueeze(2).to_broadcast([128, NT, n])
                tt(dve, ag[:, :, :], r_oh1[:, :, :], r_oh2[:, :, :], ALU.add, RR, RR)
                for g in range(4):
                    tt(dve, Aall[:, :, 8 * g:8 * g + 8], ag[:, :, :], bc(r_ohg[:, :, g], 8), ALU.mult, RR, RR)
                for T in range(NT):
                    for T2 in range(T):
                        mm(ppos[:, T * 32:(T + 1) * 32], onesb[:, :], Aall[:, T2, :], T2 == 0, False,
                           [onesb, Aall], [ppos])
                    mm(ppos[:, T * 32:(T + 1) * 32], mltb[:, :], Aall[:, T, :], T == 0, True, [mltb, Aall], [ppos])
                for T in range(NT):
                    mm(pcnt[:, 0:32], onesb[:, :], Aall[:, T, :], T == 0, T == NT - 1, [onesb, Aall], [pcnt])
                cp(dve, pos[:, 0:16, :], ppos[:, 0:512].rearrange("p (t e) -> p t e", e=32), [ppos], RR)
                cp(dve, pos[:, 16:32, :], ppos[:, 512:1024].rearrange("p (t e) -> p t e", e=32), [ppos], RR)
                cp(dve, cnt[:, :], pcnt[:, 0:32], [pcnt], RR)
                ts(dve, nb_[:, :], cnt[:, :], 0.0, None, ALU.is_gt, None, RR, RR)
                for m in range(1, 8):
                    stt(nb_[:, :], cnt[:, :], float(BSZ * m), nb_[:, :], ALU.is_gt, ALU.add, RR, RR)
                cp(dve, pa[:, :], nb_[:, :], RR, RR)
                a_, b_ = pa, pb
                for sh in (1, 2, 4, 8, 16):
                    cp(dve, b_[:, 0:sh], a_[:, 0:sh], RR, RR)
                    tt(dve, b_[:, sh:32], a_[:, sh:32], a_[:, 0:32 - sh], ALU.add, RR, RR)
                    a_, b_ = b_, a_
                incl = a_
                excl = b_
                tt(dve, excl[:, :], incl[:, :], nb_[:, :], ALU.subtract, RR, RR)
                ts(dve, excl[:, :], excl[:, :], float(BSZ), None, ALU.mult, None, RR, RR)
                tt(dve, pos[:, :, :], pos[:, :, :], excl[:, :].unsqueeze(1).to_broadcast([128, NT, 32]), ALU.add,
                   RR, RR)
                for g in range(4):
                    if g == 0:
                        tt(dve, smg[:, :, :], pos[:, :, 0:8], bc(r_ohg[:, :, 0], 8), ALU.mult, RR, RR)
                    else:
                        tt(dve, t8[:, :, :], pos[:, :, 8 * g:8 * g + 8], bc(r_ohg[:, :, g], 8), ALU.mult, RR, RR)
                        tt(dve, smg[:, :, :], smg[:, :, :], t8[:, :, :], ALU.add, RR, RR)
                tt(dve, t8[:, :, :], smg[:, :, :], r_oh1[:, :, :], ALU.mult, RR, RR)
                k.op(dve, lambda: nc.vector.tensor_reduce(out=sf[:, 0, :], in_=t8[:, :, :], axis=AX.X, op=ALU.add),
                     RR, RR)
                tt(dve, t8[:, :, :], smg[:, :, :], r_oh2[:, :, :], ALU.mult, RR, RR)
                k.op(dve, lambda: nc.vector.tensor_reduce(out=sf[:, 1, :], in_=t8[:, :, :], axis=AX.X, op=ALU.add),
                     RR, RR)
                cp(dve, s12u[:, :, :], sf[:, :, :], RR, [s12u])
                tt(dve, cmpt[:, :, :], incl[:, :].unsqueeze(1).to_broadcast([128, NBLK, 32]),
                   iob[:, :].unsqueeze(2).to_broadcast([128, NBLK, 32]), ALU.is_le, RR + [iob], RR)
                k.op(dve, lambda: nc.vector.tensor_reduce(out=eb[:, :], in_=cmpt[:, :, :], axis=AX.X, op=ALU.add),
                     RR, RR)
                ts(dve, eb[:, :], eb[:, :], 31.0, None, ALU.min, None, RR, RR)
                ts(dve, eb[:, :], eb[:, :], 512.0, float(layer * NEXP * DEXP), ALU.mult, ALU.add, RR, RR)
                tt(dve, fg[:, :, 0:4], eb[:, :].unsqueeze(2).to_broadcast([128, NBLK, 4]),
                   iog[:, 0:4].unsqueeze(1).to_broadcast([128, NBLK, 4]), ALU.add, RR + [iog], RR)
                cp(dve, idxd[:, :, :], fg[:, :, 0:4], RR, [idxd])
                ts(dve, eb[:, :], eb[:, :], 2.0, None, ALU.mult, None, RR, RR)
                tt(dve, fg[:, :, :], eb[:, :].unsqueeze(2).to_broadcast([128, NBLK, 8]),
                   iog[:, :].unsqueeze(1).to_broadcast([128, NBLK, 8]), ALU.add, RR + [iog], RR)
                cp(dve, idxg[:, :, :], fg[:, :, :], RR, [idxg])
                io2 = k.sb(st, "sl_io2", [128, 2], F32)
                k.dma(k.q_sp, io2[:, :], I["c_io2"][:, :], writes=[io2])
                ts(dve, eb[:, :], eb[:, :], 0.25, None, ALU.mult, None, RR, RR)
                tt(dve, fg[:, :, 0:2], eb[:, :].unsqueeze(2).to_broadcast([128, NBLK, 2]),
                   io2[:, :].unsqueeze(1).to_broadcast([128, NBLK, 2]), ALU.add, RR + [io2], RR)
                cp(dve, idx2[:, :, :], fg[:, :, 0:2], RR, [idx2])
                k.dma(k.q_sp, MV[0].rearrange("(kc p) -> p kc", p=128), gmod2[:, :], reads=[gmod2], writes=[mvres],
                      allow_slow_non_contiguous=True)
                k.dma(k.q_sp, MV[1].rearrange("(kc p) -> p kc", p=128), modT[:, 24:32], reads=[modT], writes=[mvres],
                      allow_slow_non_contiguous=True)
                k.dma(k.q_sp, gsb[:, 0, :], MV[0].rearrange("(p kc) -> p kc", kc=8), reads=[mvres], writes=[gsb])
                k.dma(k.q_sp, gsb[:, 1, :], MV[1].rearrange("(p kc) -> p kc", kc=8), reads=[mvres], writes=[gsb])
                xr_r = Ring([k.sb(st, "sl_xr%d" % i, [128, D], F32) for i in range(3)])
                for T in range(NT):
                    xr = xr_r.next()
                    k.dma(k.q_sp, xr[:, :], XN[T * 128:(T + 1) * 128, :], reads=[xnres], writes=[xr])
                    for kk in range(2):
                        idma(XG[:, :], bass.IndirectOffsetOnAxis(ap=s12u[:, kk, T:T + 1], axis=0), xr[:, :], None,
                             [xr, s12u], [xgres])
                k.barrier()

        def emit_moe_sparse(layer):
            with ExitStack() as st:
                wg_r = Ring([k.sb(st, "ms_wg%d" % i, [128, 8, DEXP], BF16) for i in range(3)])
                wu_r = Ring([k.sb(st, "ms_wu%d" % i, [128, 8, DEXP], BF16) for i in range(3)])
                wd_r = Ring([k.sb(st, "ms_wd%d" % i, [128, 4, D], BF16) for i in range(3)])
                xg_r = Ring([k.sb(st, "ms_xg%d" % i, [128, D], F32) for i in range(8)])
                hs_r = Ring([k.sb(st, "ms_hs%d" % i, [128, 8, 512], BF16) for i in range(2)])
                aT_r = Ring([k.sb(st, "ms_aT%d" % i, [128, 4, 512], BF16) for i in range(2)])
                sg_r = Ring([k.sb(st, "ms_sg%d" % i, [128, 512], BF16) for i in range(3)])
                yo_r = Ring([k.sb(st, "ms_yo%d" % i, [128, D], F32) for i in range(3)])
                ptr_r = Ring([k.ps(st, "ms_ptr%d" % i, [128, 512], F32) for i in range(2)])
                pg_r = Ring([k.ps(st, "ms_pg%d" % i, [128, 512], F32) for i in range(2)])
                pu_r = Ring([k.ps(st, "ms_pu%d" % i, [128, 512], F32) for i in range(2)])
                py_r = Ring([k.ps(st, "ms_py%d" % i, [128, 512], F32) for i in range(2)])
                wgv = I["expert_w_gate"].rearrange("l e (p q) f -> (l e p) (q f)", q=8).rearrange(
                    "r (h c) -> (r h) c", h=2)
                wuv = I["expert_w_up"].rearrange("l e (p q) f -> (l e p) (q f)", q=8).rearrange(
                    "r (h c) -> (r h) c", h=2)
                wdv = I["expert_w_down"].rearrange("l e (p q) n -> (l e p) (q n)", q=4).rearrange(
                    "r (h c) -> (r h) c", h=2)
                wts = {}
                hss = {}
                hss_keep = {}
                acts = {}

                def load_blk(b):
                    if 'ms_nowl' in flags and b >= 3:
                        wts[b] = wts[b - 3]
                        return
                    wg = wg_r.next()
                    wu = wu_r.next()
                    wd = wd_r.next()
                    for h in range(2):
                        off = bass.IndirectOffsetOnAxis(ap=idx2[:, b, h:h + 1], axis=0)
                        idma(wg[:, 4 * h:4 * h + 4, :].rearrange("p a b -> p (a b)"), None, wgv, off, [idx2], [wg])
                        idma(wu[:, 4 * h:4 * h + 4, :].rearrange("p a b -> p (a b)"), None, wuv, off, [idx2], [wu])
                        idma(wd[:, 2 * h:2 * h + 2, :].rearrange("p a b -> p (a b)"), None, wdv, off, [idx2], [wd])
                    wts[b] = (wg, wu, wd)

                xgs = {}

                def load_x(b):
                    tl = []
                    for j in range(4):
                        xg = xg_r.next()
                        r0 = b * BSZ + j * 128
                        k.dma(k.q_sp, xg[:, :], XG[r0:r0 + 128, :], reads=[xgres], writes=[xg])
                        tl.append(xg)
                    xgs[b] = tl

                def prep_h(b):
                    hs = hs_r.next()
                    tl = xgs.pop(b)
                    for j in range(4):
                        xg = tl[j]
                        for hf in range(2):
                            ptr = ptr_r.next()
                            for q in range(4):
                                kc = hf * 4 + q
                                tr(ptr[:, q * 128:(q + 1) * 128], xg[:, kc::8], ident32[:, :], [xg, ident32], [ptr])
                            for q in range(4):
                                kc = hf * 4 + q
                                actf(hs[:, kc, j * 128:(j + 1) * 128], ptr[:, q * 128:(q + 1) * 128], AF.Identity,
                                     [ptr, gsb], [hs], scale=gsb[:, 0, kc:kc + 1], bias=gsb[:, 1, kc:kc + 1])
                    hss[b] = hs

                def emit_gu(b):
                    wg, wu, wd = wts[b]
                    hs = hss.pop(b)
                    aT = aT_r.next()
                    for fc in range(4):
                        pg = pg_r.next()
                        pu = pu_r.next()
                        for kc in range(8):
                            mm(pg[:, :], wg[:, kc, fc::4], hs[:, kc, :], kc == 0, kc == 7,
                               [wg, hs], [pg])
                        for kc in range(8):
                            mm(pu[:, :], wu[:, kc, fc::4], hs[:, kc, :], kc == 0, kc == 7,
                               [wu, hs], [pu])
                        sg = sg_r.next()
                        actf(sg[:, :], pg[:, :], AF.Silu, [pg], [sg])
                        tt(dve, aT[:, fc, :], sg[:, :], pu[:, :], ALU.mult, [sg, pu], [aT])
                    acts[b] = aT

                def emit_down(b):
                    wg, wu, wd = wts[b]
                    aT = acts.pop(b)
                    for j in range(4):
                        yo = yo_r.next()
                        for nh in range(2):
                            py = py_r.next()
                            for fc in range(4):
                                mm(py[:, :], aT[:, fc, j * 128:(j + 1) * 128],
                                   wd[:, fc, nh * 512:(nh + 1) * 512], fc == 0, fc == 3, [aT, wd], [py])
                            cp(dve, yo[:, nh * 512:(nh + 1) * 512], py[:, :], [py], [yo])
                        r0 = b * BSZ + j * 128
                        k.dma(k.q_sp, YS[r0:r0 + 128, :], yo[:, :], reads=[yo], writes=[ysres[b]])

                load_blk(0)
                load_blk(1)
                load_x(0)
                prep_h(0)
                load_x(1)
                for b in range(NBLK):
                    emit_gu(b)
                    if b > 0:
                        emit_down(b - 1)
                    if b + 2 < NBLK:
                        load_blk(b + 2)
                    if b + 1 < NBLK:
                        prep_h(b + 1)
                    if b + 2 < NBLK:
                        load_x(b + 2)
                emit_down(NBLK - 1)
                k.barrier()
            with ExitStack() as st:
                r1_r = Ring([k.sb(st, "ms_r1%d" % i, [128, D], F32) for i in range(2)])
                r2_r = Ring([k.sb(st, "ms_r2%d" % i, [128, D], F32) for i in range(2)])
                xt_r = Ring([k.sb(st, "ms_xt%d" % i, [128, D], F32) for i in range(2)])
                for T in range(NT):
                    r1 = r1_r.next()
                    r2 = r2_r.next()
                    idma(r1[:, :], None, YS[:, :], bass.IndirectOffsetOnAxis(ap=s12u[:, 0, T:T + 1], axis=0),
                         ysres + [s12u], [r1])
                    idma(r2[:, :], None, YS[:, :], bass.IndirectOffsetOnAxis(ap=s12u[:, 1, T:T + 1], axis=0),
                         ysres + [s12u], [r2])
                    xt = xt_r.next()
                    k.dma(k.q_sp, xt[:, :], XS[T * 128:(T + 1) * 128, :], reads=[xres[T]], writes=[xt])
                    ts(dve, r1[:, :], r1[:, :], r_v[:, 9, T:T + 1], None, ALU.mult, None, [r1, r_v], [r1])
                    stt(r1[:, :], r2[:, :], r_v[:, 10, T:T + 1], r1[:, :], ALU.mult, ALU.add, [r2, r_v, r1], [r1])
                    tt(dve, r1[:, :], r1[:, :], gbc2[:, :], ALU.mult, [r1, gbc2], [r1])
                    tt(dve, xt[:, :], xt[:, :], r1[:, :], ALU.add, [xt, r1], [xt])
                    k.dma(k.q_sp, XS[T * 128:(T + 1) * 128, :], xt[:, :], reads=[xt], writes=[xres[T]])
                k.barrier()

        def emit_final(do_norm):
            with ExitStack() as st:
                xt_r = Ring([k.sb(st, "f_xt%d" % i, [128, D], F32) for i in range(3)])
                xo_r = Ring([k.sb(st, "f_xo%d" % i, [128, D], F32) for i in range(2)])
                junk = k.sb(st, "f_junk", [128, D], BF16)
                sm_r = Ring([k.sb(st, "f_sm%d" % i, [128, 4], F32) for i in range(4)])
                gf = k.sb(st, "f_gf", [128, D], F32)
                k.dma(k.q_sp, gf[:, :], I["norm_final"][0].partition_broadcast(128), writes=[gf])
                evs = []
                for t in range(NT):
                    xt = xt_r.next()
                    k.dma(k.q_sp, xt[:, :], XS[t * 128:(t + 1) * 128, :], reads=[xres[t]], writes=[xt])
                    if do_norm:
                        sm = sm_r.next()
                        actf(junk[:, :], xt[:, :], AF.Square, [xt], [junk, sm], accum_out=sm[:, 0:1])
                        ts(dve, sm[:, 1:2], sm[:, 0:1], 1.0 / D, EPS, ALU.mult, ALU.add, [sm], [sm])
                        actf(sm[:, 2:3], sm[:, 1:2], AF.Sqrt, [sm], [sm])
                        recip(sm[:, 3:4], sm[:, 2:3], [sm], [sm])
                        xo = xo_r.next()
                        stt(xo[:, :], xt[:, :], sm[:, 3:4], gf[:, :], ALU.mult, ALU.mult, [xt, sm, gf], [xo])
                        src = xo
                    else:
                        src = xt
                    evs.append(k.dma(k.q_sp, OUT[t * 128:(t + 1) * 128, :], src[:, :], reads=[src]))
                for ev in evs:
                    sp.wait(ev)

        xsrc = I["x"]
        done = False
        if stop == ("evenonly",):
            H["hT"] = k.sb(gst, "hT", [128, 8, S], BF16)
            emit_even_mixer(0, 0)
            emit_final(False)
            done = True
            nlayers = 0
        if stop == ("oddonly",):
            H["hT"] = k.sb(gst, "hT", [128, 8, S], BF16)
            emit_odd_mixer()
            emit_final(False)
            done = True
            nlayers = 0
        for l in range(nlayers):
            emit_mod(l)
            lst = ExitStack()
            H["hT"] = k.sb(lst, "hT", [128, 8, S], BF16)
            emit_norm(xsrc, gmod1, modT[:, 0:8], l, router=False)
            ie = l // 2
            if 'skipmix' in flags:
                pass
            elif l % 2 == 0:
                fj = [(c * 128, 128, c, 0.125) for c in range(4)]
                fj += [(512 + c * 128, 128, 4 + c, 1.0) for c in range(4)]
                fj += [(1536 + c * 128, 128, 12 + c, 0.125) for c in range(2)]
                fj += [(1792 + c * 128, 128, 14 + c, 1.0) for c in range(2)]
                fj += [(2560 + c * 128, 128, 20 + c, 1.0) for c in range(4)]
                fj += [(3072, 16, 24, 1.0)]
                tj = [(1024, 512, 0), (1792, 256, 512), (2048, 512, 768)]
                emit_inproj(I["even_w_in"][ie], EVEN_COLS, fj, tj)
                emit_even_mixer(ie, l)
                emit_outproj(I["even_w_out"][ie], gbc1, xsrc)
            else:
                fj = [(c * 128, 128, c, 0.125) for c in range(8)]
                fj += [(1024 + c * 128, 128, 8 + c, 1.0) for c in range(8)]
                tj = [(2048, 512, 0), (2560, 512, 512)]
                emit_inproj(I["odd_w_in"][ie], 3072, fj, tj)
                emit_odd_mixer()
                emit_outproj(I["odd_w_out"][ie], gbc1, xsrc)
            if 'skipmix' not in flags:
                xsrc = XS
            if SPARSE:
                lst.close()
                H.pop("hT")
            if stop == ("mix", l):
                emit_final(False)
                done = True
                break
            emit_norm(xsrc, gmod2, modT[:, 24:32], l, router=True)
            if 'nomoe' not in flags:
                if SPARSE:
                    emit_slots(layer=l)
                    emit_moe_sparse(l)
                else:
                    emit_moe(l)
            if not SPARSE:
                lst.close()
            if stop == ("ffn", l):
                emit_final(False)
                done = True
                break
        if not done:
            emit_final(True)
        build.stats = (k.n_inst, k.nsem)
    return nc


_CACHE = {}


def _get_nc(nlayers=DEPTH, stop=None):
    key = (nlayers, stop)
    if key not in _CACHE:
        _CACHE[key] = build(nlayers, stop)
    return _CACHE[key]


def make_in_maps(inputs, cores):
    consts = make_consts()
    shared = {}
    for nm in WEIGHT_NAMES:
        a = np.ascontiguousarray(np.asarray(inputs[nm], dtype=np.float32))
        shared[nm] = a.reshape(WEIGHT_SHAPES[nm])
    shared.update(consts)
    x = np.asarray(inputs["x"], dtype=np.float32)
    c = np.asarray(inputs["c"], dtype=np.float32)
    maps = []
    for b in cores:
        m = dict(shared)
        m["x"] = np.ascontiguousarray(x[b])
        m["c"] = np.ascontiguousarray(c[b:b + 1])
        maps.append(m)
    return maps


def kernel(**inputs):
    nc = _get_nc()
    in_maps = make_in_maps(inputs, list(range(8)))
    res = run_bass_kernel_spmd(nc, in_maps, core_ids=list(range(8)))
    return np.stack([np.asarray(r["out"]) for r in res.results], axis=0).astype(np.float32)
```
